# Optimizing a Trainium2 kernel written in Bass

```python
import math
import jax
import jax.numpy as jnp
from jax import lax
import numpy as np

D_MODEL = 1024
BATCH = 8
SEQ = 4096
DEPTH = 1

GRID_W = 64
CTX_LEN = 256
EPS = 1e-6

S5_WIDTH = D_MODEL
S5_GROUP = 16
S5_GROUPS = S5_WIDTH // S5_GROUP
S5_STATE = 64
S5_DT_MIN = 1e-3
S5_DT_MAX = 1e-1

SSD_WIDTH = 2 * D_MODEL
SSD_HEADDIM = 64
SSD_HEADS = SSD_WIDTH // SSD_HEADDIM
SSD_GROUPS = 8
SSD_HPG = SSD_HEADS // SSD_GROUPS
SSD_STATE = 128
SSD_CONV = 5
SSD_CHUNK = 128
SSD_CONV_DIM = SSD_WIDTH + 2 * SSD_GROUPS * SSD_STATE
SSD_DT_MIN = 1e-3
SSD_DT_MAX = 1e-1

IN_STATE_COLS = S5_WIDTH + SSD_CONV_DIM + 2 * SSD_HEADS
IN_COLS = IN_STATE_COLS + SSD_WIDTH + 2 * D_MODEL
IN_SPLITS = (S5_WIDTH, S5_WIDTH + SSD_CONV_DIM, IN_STATE_COLS, IN_STATE_COLS + SSD_WIDTH, IN_STATE_COLS + SSD_WIDTH + D_MODEL)

N_EXPERTS = 32
TOP_K = 4
D_EXPERT = D_MODEL
SWIGLU_ALPHA = 1.702
SWIGLU_LIMIT = 7.0
MOE_BLOCK = 128

kernel_name = 'hybrid_s5_ssd_moe_dit_block'


def rms_norm(h, w):
    hf = h.astype(jnp.float32)
    hf = hf * lax.rsqrt(jnp.mean(hf * hf, axis=-1, keepdims=True) + EPS)
    return hf.astype(h.dtype) * w


def ada_modulation(cond, w, b):
    m = jax.nn.silu(cond) @ w + b
    return jnp.split(m[:, None, :], 6, axis=-1)


def to_col_major(h):
    b_, l, ch = h.shape
    rows = l // GRID_W
    return h.reshape(b_, rows, GRID_W, ch).transpose(0, 2, 1, 3).reshape(b_, l, ch)


def to_row_major(h):
    b_, l, ch = h.shape
    rows = l // GRID_W
    return h.reshape(b_, GRID_W, rows, ch).transpose(0, 2, 1, 3).reshape(b_, l, ch)


def s5_discretise(lam_re, lam_im, log_dt, b_re, b_im):
    f32 = jnp.float32
    lam = lax.complex(lam_re.astype(f32), lam_im.astype(f32))
    step = jnp.exp(log_dt.astype(f32))[:, None]
    a_bar = jnp.exp(lam * step)
    b_mat = lax.complex(b_re.astype(f32), b_im.astype(f32))
    b_bar = ((a_bar - 1.0) / lam)[:, :, None] * b_mat
    return a_bar, b_bar


def _diag_linear_combine(left, right):
    a_l, b_l = left
    a_r, b_r = right
    return a_l * a_r, a_r * b_l + b_r


def s5_states(u, a_bar, b_bar, init, reverse):
    bu = jnp.einsum('blgh,gph->lbgp', u.astype(jnp.complex64), b_bar)
    if init is not None:
        edge = -1 if reverse else 0
        bu = bu.at[edge].add(a_bar * init)
    a = jnp.broadcast_to(a_bar, (bu.shape[0], 1) + a_bar.shape)
    _, states = lax.associative_scan(_diag_linear_combine, (a, bu), reverse=reverse, axis=0)
    return states


def s5_readout(u, states_f, states_b, p):
    f32 = jnp.float32
    c_f = lax.complex(p['s5_c_re'][0].astype(f32), p['s5_c_im'][0].astype(f32))
    c_b = lax.complex(p['s5_c_re'][1].astype(f32), p['s5_c_im'][1].astype(f32))
    y = (jnp.einsum('lbgp,ghp->blgh', states_f, c_f).real
         + jnp.einsum('lbgp,ghp->blgh', states_b, c_b).real)
    b_, l = u.shape[:2]
    y = y.reshape(b_, l, S5_WIDTH) + p['s5_d'].astype(f32) * u.reshape(b_, l, S5_WIDTH)
    g = jax.nn.gelu(y)
    return g * jax.nn.sigmoid(g @ p['s5_glu_w'].astype(f32) + p['s5_glu_b'].astype(f32))


def s5_mixer(u_ctx, u_lat, p, with_ctx_out):
    dtype = u_lat.dtype
    b_, lc, _ = u_ctx.shape
    l = u_lat.shape[1]
    uc = u_ctx.astype(jnp.float32).reshape(b_, lc, S5_GROUPS, S5_GROUP)
    ul = u_lat.astype(jnp.float32).reshape(b_, l, S5_GROUPS, S5_GROUP)
    a_f, b_f = s5_discretise(p['s5_lam_re'][0], p['s5_lam_im'][0], p['s5_log_dt'][0], p['s5_b_re'][0], p['s5_b_im'][0])
    a_b, b_b = s5_discretise(p['s5_lam_re'][1], p['s5_lam_im'][1], p['s5_log_dt'][1], p['s5_b_re'][1], p['s5_b_im'][1])
    ctx_f = s5_states(uc, a_f, b_f, None, False)
    ctx_b = s5_states(uc, a_b, b_b, None, True)
    lat_f = s5_states(ul, a_f, b_f, ctx_f[-1], False)
    lat_b = s5_states(ul, a_b, b_b, ctx_b[0], True)
    y_lat = s5_readout(ul, lat_f, lat_b, p).astype(dtype)
    y_ctx = s5_readout(uc, ctx_f, ctx_b, p).astype(dtype) if with_ctx_out else None
    return y_lat, y_ctx


def depthwise_conv_centred(h, w, b):
    pad = w.shape[0] // 2
    out = lax.conv_general_dilated(h, w[:, None, :], window_strides=(1,), padding=((pad, pad),),
                                   dimension_numbers=('NWC', 'WIO', 'NWC'), feature_group_count=h.shape[-1])
    return out + b


def ssd_prep(xbc, dt_raw, p):
    f32 = jnp.float32
    xbc = jax.nn.silu(depthwise_conv_centred(xbc, p['ssd_conv_w'], p['ssd_conv_b'])).astype(f32)
    b_, l, _ = xbc.shape
    xs, bm, cm = jnp.split(xbc, [SSD_WIDTH, SSD_WIDTH + SSD_GROUPS * SSD_STATE], axis=-1)
    xs = xs.reshape(b_, l, SSD_HEADS, SSD_HEADDIM)
    bm = bm.reshape(b_, l, SSD_GROUPS, SSD_STATE)
    cm = cm.reshape(b_, l, SSD_GROUPS, SSD_STATE)
    dt = jax.nn.softplus(dt_raw.astype(f32).reshape(b_, l, 2, SSD_HEADS) + p['ssd_dt_bias'].astype(f32))
    return xs, bm, cm, dt[:, :, 0], dt[:, :, 1]


def decay_matrix(a_cum):
    q = a_cum.shape[-1]
    mask = jnp.tril(jnp.ones((q, q), dtype=bool))
    diff = a_cum[..., :, None] - a_cum[..., None, :]
    return jnp.where(mask, jnp.exp(jnp.where(mask, diff, 0.0)), 0.0)


def ssd_scan(xs, dt, a, bm, cm, init, with_output):
    b_, l = xs.shape[:2]
    nc = l // SSD_CHUNK
    shp = (b_, nc, SSD_CHUNK, SSD_GROUPS)
    x = xs.reshape(shp + (SSD_HPG, SSD_HEADDIM))
    dtc = dt.reshape(shp + (SSD_HPG,))
    bc = bm.reshape(shp + (SSD_STATE,))
    cc = cm.reshape(shp + (SSD_STATE,))
    a_cum = jnp.moveaxis(jnp.cumsum(dtc * a.reshape(SSD_GROUPS, SSD_HPG), axis=2), 2, -1)
    dt_t = jnp.moveaxis(dtc, 2, -1)
    w_end = jnp.exp(a_cum[..., -1:] - a_cum) * dt_t
    chunk_states = jnp.einsum('bcsgn,bcgrs,bcsgrp->bcgrpn', bc, w_end, x)
    chunk_decay = jnp.exp(a_cum[..., -1])

    def carry_step(state, inp):
        decay, new = inp
        return decay[..., None, None] * state + new, state

    final, prev = lax.scan(carry_step, init, (jnp.moveaxis(chunk_decay, 1, 0), jnp.moveaxis(chunk_states, 1, 0)))
    if not with_output:
        return None, final
    prev = jnp.moveaxis(prev, 0, 1)
    cb = jnp.einsum('bclgn,bcsgn->bcgls', cc, bc)
    lmat = decay_matrix(a_cum) * dt_t[..., None, :]
    y_diag = jnp.einsum('bcgls,bcgrls,bcsgrp->bclgrp', cb, lmat, x)
    y_off = jnp.einsum('bclgn,bcgrpn,bcgrl->bclgrp', cc, prev, jnp.exp(a_cum))
    return (y_diag + y_off).reshape(b_, l, SSD_HEADS, SSD_HEADDIM), final


def ssd_bidirectional(xs, bm, cm, dt_f, dt_b, a, init_f, init_b, with_output):
    y_f, fin_f = ssd_scan(xs, dt_f, a[0], bm, cm, init_f, with_output)
    flip = lambda t: jnp.flip(t, axis=1)
    y_b, fin_b = ssd_scan(flip(xs), flip(dt_b), a[1], flip(bm), flip(cm), init_b, with_output)
    y = y_f + flip(y_b) if with_output else None
    return y, fin_f, fin_b


def ssd_output(y, xs, z, p):
    f32 = jnp.float32
    b_, l = xs.shape[:2]
    y = (y + p['ssd_d'].astype(f32)[:, None] * xs).reshape(b_, l, SSD_WIDTH)
    y = y * jax.nn.silu(z.astype(f32))
    yg = y.reshape(b_, l, SSD_GROUPS, SSD_WIDTH // SSD_GROUPS)
    yg = yg * lax.rsqrt(jnp.mean(yg * yg, axis=-1, keepdims=True) + EPS)
    return yg.reshape(b_, l, SSD_WIDTH).astype(z.dtype) * p['ssd_norm_w']


def ssd_mixer(xbc_ctx, dt_ctx, z_ctx, xbc_lat, dt_lat, z_lat, p, with_ctx_out):
    a = -jnp.exp(p['ssd_a_log'].astype(jnp.float32))
    b_ = xbc_lat.shape[0]
    zero_state = jnp.zeros((b_, SSD_GROUPS, SSD_HPG, SSD_HEADDIM, SSD_STATE), jnp.float32)
    xc, bc, cc, dfc, dbc = ssd_prep(xbc_ctx, dt_ctx, p)
    y_ctx, s_f, s_b = ssd_bidirectional(xc, bc, cc, dfc, dbc, a, zero_state, zero_state, with_ctx_out)
    xl, bl, cl, dfl, dbl = ssd_prep(to_col_major(xbc_lat), to_col_major(dt_lat), p)
    y_lat, _, _ = ssd_bidirectional(xl, bl, cl, dfl, dbl, a, s_f, s_b, True)
    y_lat = to_row_major(ssd_output(y_lat, xl, to_col_major(z_lat), p))
    y_ctx = ssd_output(y_ctx, xc, z_ctx, p) if with_ctx_out else None
    return y_lat, y_ctx


def moe_ffn(h, p):
    b_, l, d = h.shape
    xf = h.reshape(b_ * l, d)
    n = xf.shape[0]
    logits = (xf @ p['router_w'] + p['router_b']).astype(jnp.float32)
    top_val, top_idx = lax.top_k(logits, TOP_K)
    gates = jax.nn.softmax(top_val, axis=-1)
    n_pairs = n * TOP_K
    n_blocks = -(-n_pairs // MOE_BLOCK) + N_EXPERTS
    pair_expert = top_idx.reshape(n_pairs)
    pair_token = jnp.arange(n_pairs, dtype=jnp.int32) // TOP_K
    order = jnp.argsort(pair_expert)
    sorted_expert = pair_expert[order]
    counts = jnp.bincount(pair_expert, length=N_EXPERTS)
    padded = (counts + MOE_BLOCK - 1) // MOE_BLOCK * MOE_BLOCK
    padded_end = jnp.cumsum(padded)
    rank = jnp.arange(n_pairs, dtype=jnp.int32) - (jnp.cumsum(counts) - counts)[sorted_expert]
    dest = (padded_end - padded)[sorted_expert] + rank
    n_slots = n_blocks * MOE_BLOCK
    slot_token = jnp.full((n_slots,), n, jnp.int32).at[dest].set(pair_token[order])
    slot_gate = jnp.zeros((n_slots,), jnp.float32).at[dest].set(gates.reshape(n_pairs)[order])
    block_start = jnp.arange(n_blocks, dtype=padded_end.dtype) * MOE_BLOCK
    block_expert = jnp.minimum(jnp.searchsorted(padded_end, block_start, side='right'), N_EXPERTS - 1)
    x_pad = jnp.concatenate([xf, jnp.zeros((1, d), xf.dtype)], axis=0)

    def expert_block(args):
        tokens, e = args
        xb = x_pad[tokens]
        gate = jnp.minimum(xb @ p['moe_w_gate'][e] + p['moe_b_gate'][e], SWIGLU_LIMIT)
        up = jnp.clip(xb @ p['moe_w_up'][e] + p['moe_b_up'][e], -SWIGLU_LIMIT, SWIGLU_LIMIT)
        act = (up + 1.0) * gate * jax.nn.sigmoid(SWIGLU_ALPHA * gate)
        return act @ p['moe_w_down'][e] + p['moe_b_down'][e]

    y = lax.map(expert_block, (slot_token.reshape(n_blocks, MOE_BLOCK), block_expert))
    y = y.reshape(n_slots, d) * slot_gate[:, None].astype(y.dtype)
    return jax.ops.segment_sum(y, slot_token, num_segments=n + 1)[:n].reshape(b_, l, d)


def hybrid_layer(h_lat, h_ctx, c, c_ctx, p, with_ctx_out):
    sh_m, sc_m, g_m, sh_f, sc_f, g_f = ada_modulation(c, p['ada_w'], p['ada_b'])
    csh_m, csc_m, cg_m, csh_f, csc_f, cg_f = ada_modulation(c_ctx[None, :], p['ada_w'], p['ada_b'])
    u_lat = rms_norm(h_lat, p['norm_mix_w']) * (1.0 + sc_m) + sh_m
    u_ctx = rms_norm(h_ctx, p['norm_mix_w']) * (1.0 + csc_m) + csh_m
    proj_lat = u_lat @ p['w_in']
    s5_l, xbc_l, dt_l, z_l, ga_l, gb_l = jnp.split(proj_lat, IN_SPLITS, axis=-1)
    if with_ctx_out:
        proj_ctx = u_ctx @ p['w_in']
        s5_c, xbc_c, dt_c, z_c, ga_c, gb_c = jnp.split(proj_ctx, IN_SPLITS, axis=-1)
    else:
        proj_ctx = u_ctx @ p['w_in'][:, :IN_STATE_COLS]
        s5_c, xbc_c, dt_c = jnp.split(proj_ctx, IN_SPLITS[:2], axis=-1)
        z_c = None
    ya_l, ya_c = s5_mixer(s5_c, s5_l, p, with_ctx_out)
    yb_l, yb_c = ssd_mixer(xbc_c, dt_c, z_c, xbc_l, dt_l, z_l, p, with_ctx_out)

    def merge(ya, yb, ga, gb):
        branch_a = ya @ p['w_branch_a']
        branch_b = yb @ p['w_branch_b']
        return (jax.nn.sigmoid(ga) * branch_a + jax.nn.sigmoid(gb) * branch_b) @ p['w_out']

    def ffn(h, shift, scale):
        return moe_ffn(rms_norm(h, p['norm_ffn_w']) * (1.0 + scale) + shift, p)

    h_lat = h_lat + g_m * merge(ya_l, yb_l, ga_l, gb_l)
    h_lat = h_lat + g_f * ffn(h_lat, sh_f, sc_f)
    if with_ctx_out:
        h_ctx = h_ctx + cg_m * merge(ya_c, yb_c, ga_c, gb_c)
        h_ctx = h_ctx + cg_f * ffn(h_ctx, csh_f, csc_f)
    return h_lat, h_ctx


def setup_inputs(seed: int = 0) -> dict:
    key = jax.random.key(seed)
    ks = iter(jax.random.split(key, 40))
    f32 = jnp.float32
    L, D = DEPTH, D_MODEL

    def nrm(shape, scale=1.0):
        return scale * jax.random.normal(next(ks), shape, f32)

    def unif(shape, lo, hi):
        return jax.random.uniform(next(ks), shape, f32, lo, hi)

    inp = {}
    inp['x'] = nrm((BATCH, SEQ, D))
    inp['c'] = nrm((BATCH, D))
    inp['ctx'] = nrm((BATCH, CTX_LEN, D))
    inp['c_ctx'] = nrm((D,))
    inp['ada_w'] = nrm((L, D, 6 * D), 0.5 * D ** -0.5)
    inp['ada_b'] = nrm((L, 6 * D), 0.01)
    inp['norm_mix_w'] = 1.0 + nrm((L, D), 0.02)
    inp['w_in'] = nrm((L, D, IN_COLS), D ** -0.5)
    n_idx = jnp.arange(S5_STATE, dtype=f32)
    inp['s5_lam_re'] = -0.5 + nrm((L, 2, S5_GROUPS, S5_STATE), 0.01)
    inp['s5_lam_im'] = math.pi * n_idx + nrm((L, 2, S5_GROUPS, S5_STATE), 0.01)
    inp['s5_log_dt'] = unif((L, 2, S5_GROUPS), math.log(S5_DT_MIN), math.log(S5_DT_MAX))
    inp['s5_b_re'] = nrm((L, 2, S5_GROUPS, S5_STATE, S5_GROUP), (2 * S5_GROUP) ** -0.5)
    inp['s5_b_im'] = nrm((L, 2, S5_GROUPS, S5_STATE, S5_GROUP), (2 * S5_GROUP) ** -0.5)
    inp['s5_c_re'] = nrm((L, 2, S5_GROUPS, S5_GROUP, S5_STATE), (2 * S5_STATE) ** -0.5)
    inp['s5_c_im'] = nrm((L, 2, S5_GROUPS, S5_GROUP, S5_STATE), (2 * S5_STATE) ** -0.5)
    inp['s5_d'] = nrm((L, S5_WIDTH))
    inp['s5_glu_w'] = nrm((L, S5_WIDTH, S5_WIDTH), S5_WIDTH ** -0.5)
    inp['s5_glu_b'] = nrm((L, S5_WIDTH), 0.01)
    inp['ssd_conv_w'] = nrm((L, SSD_CONV, SSD_CONV_DIM), SSD_CONV ** -0.5)
    inp['ssd_conv_b'] = nrm((L, SSD_CONV_DIM), 0.01)
    dt0 = jnp.exp(unif((L, 2, SSD_HEADS), math.log(SSD_DT_MIN), math.log(SSD_DT_MAX)))
    inp['ssd_dt_bias'] = dt0 + jnp.log(-jnp.expm1(-dt0))
    inp['ssd_a_log'] = jnp.log(unif((L, 2, SSD_HEADS), 1.0, 16.0))
    inp['ssd_d'] = 1.0 + nrm((L, SSD_HEADS), 0.1)
    inp['ssd_norm_w'] = 1.0 + nrm((L, SSD_WIDTH), 0.02)
    inp['w_branch_a'] = nrm((L, S5_WIDTH, D), S5_WIDTH ** -0.5)
    inp['w_branch_b'] = nrm((L, SSD_WIDTH, D), SSD_WIDTH ** -0.5)
    inp['w_out'] = nrm((L, D, D), D ** -0.5)
    inp['norm_ffn_w'] = 1.0 + nrm((L, D), 0.02)
    inp['router_w'] = nrm((L, D, N_EXPERTS), D ** -0.5)
    inp['router_b'] = nrm((L, N_EXPERTS), 0.01)
    inp['moe_w_gate'] = nrm((L, N_EXPERTS, D, D_EXPERT), D ** -0.5)
    inp['moe_b_gate'] = nrm((L, N_EXPERTS, D_EXPERT), 0.01)
    inp['moe_w_up'] = nrm((L, N_EXPERTS, D, D_EXPERT), D ** -0.5)
    inp['moe_b_up'] = nrm((L, N_EXPERTS, D_EXPERT), 0.01)
    inp['moe_w_down'] = nrm((L, N_EXPERTS, D_EXPERT, D), D_EXPERT ** -0.5)
    inp['moe_b_down'] = nrm((L, N_EXPERTS, D), 0.01)
    inp['final_norm_w'] = 1.0 + nrm((D,), 0.02)
    return inp


def reference(x, c, ctx, c_ctx, ada_w, ada_b, norm_mix_w, w_in, s5_lam_re, s5_lam_im, s5_log_dt,
              s5_b_re, s5_b_im, s5_c_re, s5_c_im, s5_d, s5_glu_w, s5_glu_b, ssd_conv_w, ssd_conv_b,
              ssd_dt_bias, ssd_a_log, ssd_d, ssd_norm_w, w_branch_a, w_branch_b, w_out, norm_ffn_w,
              router_w, router_b, moe_w_gate, moe_b_gate, moe_w_up, moe_b_up, moe_w_down, moe_b_down,
              final_norm_w):
    h_lat, h_ctx = x, ctx
    for layer in range(DEPTH):
        p = dict(
            ada_w=ada_w[layer], ada_b=ada_b[layer], norm_mix_w=norm_mix_w[layer], w_in=w_in[layer],
            s5_lam_re=s5_lam_re[layer], s5_lam_im=s5_lam_im[layer], s5_log_dt=s5_log_dt[layer],
            s5_b_re=s5_b_re[layer], s5_b_im=s5_b_im[layer], s5_c_re=s5_c_re[layer], s5_c_im=s5_c_im[layer],
            s5_d=s5_d[layer], s5_glu_w=s5_glu_w[layer], s5_glu_b=s5_glu_b[layer],
            ssd_conv_w=ssd_conv_w[layer], ssd_conv_b=ssd_conv_b[layer], ssd_dt_bias=ssd_dt_bias[layer],
            ssd_a_log=ssd_a_log[layer], ssd_d=ssd_d[layer], ssd_norm_w=ssd_norm_w[layer],
            w_branch_a=w_branch_a[layer], w_branch_b=w_branch_b[layer], w_out=w_out[layer],
            norm_ffn_w=norm_ffn_w[layer], router_w=router_w[layer], router_b=router_b[layer],
            moe_w_gate=moe_w_gate[layer], moe_b_gate=moe_b_gate[layer], moe_w_up=moe_w_up[layer],
            moe_b_up=moe_b_up[layer], moe_w_down=moe_w_down[layer], moe_b_down=moe_b_down[layer],
        )
        h_lat, h_ctx = hybrid_layer(h_lat, h_ctx, c, c_ctx, p, layer < DEPTH - 1)
    return rms_norm(h_lat, final_norm_w)
```

```python
import os
import numpy as np
from contextlib import ExitStack
import concourse.bass as bass
import concourse.mybir as mybir
from concourse.bass_utils import run_bass_kernel_spmd

F32 = mybir.dt.float32
BF16 = mybir.dt.bfloat16
I32 = mybir.dt.int32
U32 = mybir.dt.uint32
U8 = mybir.dt.uint8
ALU = mybir.AluOpType
AF = mybir.ActivationFunctionType
AX = mybir.AxisListType

NDSEM = 8
D = 1024
L = 4096
LC = 256
T = L + LC
EPS = 1e-6


class Buf:
    __slots__ = ("name", "last_w", "readers", "excl")

    def __init__(self, name, excl=False):
        self.name = name
        self.last_w = None
        self.readers = []
        self.excl = excl


class Op:
    __slots__ = ("id", "eng", "fn", "deps", "dma", "eidx", "needs_inc", "semval", "dslot", "dval", "name")


COMPUTE = ("pe", "dve", "act", "pool")
ENGS = ("sp", "pe", "dve", "act", "pool")


class Sched:
    def __init__(self, nc):
        self.nc = nc
        self.ops = []
        self.per_eng = {e: [] for e in ENGS}
        self.ndma = {e: 0 for e in ENGS}
        self.barrier_floor = -1

    def op(self, eng, fn, reads=(), writes=(), dma=False, name=None):
        o = Op()
        o.id = len(self.ops)
        o.eng = eng
        o.fn = fn
        o.dma = dma
        o.name = name
        o.needs_inc = False
        o.semval = None
        o.dslot = None
        o.dval = None
        deps = {}
        if any(b.excl for b in reads):
            writes = list(writes) + [b for b in reads if b.excl and b not in writes]
            reads = [b for b in reads if not b.excl]
        for b in reads:
            if b.last_w is not None:
                deps[b.last_w.id] = (b.last_w, "raw")
        for b in writes:
            if b.last_w is not None and b.last_w.id not in deps:
                deps[b.last_w.id] = (b.last_w, "waw")
            for r in b.readers:
                if r.id not in deps:
                    deps[r.id] = (r, "war")
        o.deps = [(p, k) for (p, k) in deps.values() if p.id > self.barrier_floor]
        for b in reads:
            b.readers.append(o)
        for b in writes:
            b.last_w = o
            b.readers = []
        o.eidx = len(self.per_eng[eng])
        self.per_eng[eng].append(o)
        if dma:
            i = self.ndma[eng]
            self.ndma[eng] += 1
            o.dslot = i % NDSEM
            o.dval = 16 * (i // NDSEM + 1)
        self.ops.append(o)
        return o

    def barrier(self):
        lasts = []
        for e in ENGS:
            comp = [o for o in self.per_eng[e] if not o.dma and o.fn is not None]
            if comp:
                lasts.append(comp[-1])
            dm = [o for o in self.per_eng[e] if o.dma]
            lasts.extend(dm[-NDSEM:])
        lasts = [o for o in lasts if o.id > self.barrier_floor]
        for e in ENGS:
            o = self.op(e, None, name="barrier")
            o.deps = [(p, "raw") for p in lasts if p.eng != e or p.dma]
        self.barrier_floor = len(self.ops) - 1

    @staticmethod
    def _skip(o, p, kind):
        if p.dma or o.dma or p.eng != o.eng:
            return False
        if p.eng == "pe":
            return True
        if kind != "raw":
            return True
        return o.eidx - p.eidx > 2

    def emit(self):
        nc = self.nc
        for o in self.ops:
            for (p, kind) in o.deps:
                if p.dma or self._skip(o, p, kind):
                    continue
                p.needs_inc = True
        for e in ENGS:
            c = 0
            for o in self.per_eng[e]:
                if o.dma:
                    continue
                if o.needs_inc:
                    c += 1
                    o.semval = c
        with ExitStack() as es:
            csem = {e: es.enter_context(nc.semaphore(f"c_{e}")) for e in COMPUTE}
            dsem = {e: [es.enter_context(nc.semaphore(f"d_{e}{i}")) for i in range(NDSEM)]
                    for e in ENGS if self.ndma[e] > 0}
            block = es.enter_context(nc.Block())
            handles = {"sp": block.sync, "pe": block.tensor, "dve": block.vector,
                       "act": block.scalar, "pool": block.gpsimd}

            FUSE = os.environ.get("FUSEWAIT", "1") == "1"

            class _Rec:
                def __init__(self, eng):
                    self._eng = eng
                    self.first = None

                def __getattr__(self, name):
                    attr = getattr(self._eng, name)
                    if not callable(attr):
                        return attr

                    def w(*a, **k):
                        r = attr(*a, **k)
                        if self.first is None and hasattr(r, "then_inc"):
                            self.first = r
                        return r
                    return w

            def make(e):
                def body(eng):
                    waited = {}

                    def need(lst, sem, key, val):
                        if waited.get(key, 0) >= val:
                            return
                        for i, (s_, k_, v_) in enumerate(lst):
                            if k_ == key:
                                if v_ < val:
                                    lst[i] = (sem, key, val)
                                return
                        lst.append((sem, key, val))

                    for o in self.per_eng[e]:
                        lst = []
                        for (p, kind) in o.deps:
                            if p.dma:
                                need(lst, dsem[p.eng][p.dslot], ("d", p.eng, p.dslot), p.dval)
                            elif not self._skip(o, p, kind):
                                need(lst, csem[p.eng], ("c", p.eng), p.semval)
                        if o.dma and o.dval > 16:
                            need(lst, dsem[e][o.dslot], ("d", e, o.dslot), o.dval - 16)
                        for (s_, k_, v_) in lst:
                            waited[k_] = v_
                        fuse = None
                        if FUSE and lst and o.fn is not None and not o.dma:
                            fuse = lst.pop()
                        for (s_, k_, v_) in lst:
                            eng.wait_ge(s_, v_)
                        if o.fn is None:
                            continue
                        if fuse is not None:
                            rec = _Rec(eng)
                            ins = o.fn(rec)
                            rec.first._wait_ge(fuse[0], fuse[2])
                        else:
                            ins = o.fn(eng)
                        if o.dma:
                            ins.then_inc(dsem[e][o.dslot], 16)
                        elif o.needs_inc:
                            ins.then_inc(csem[e], 1)
                    if e == "sp":
                        for q in dsem:
                            dm = [o for o in self.per_eng[q] if o.dma]
                            for o in dm[-NDSEM:]:
                                if waited.get(("d", q, o.dslot), 0) < o.dval:
                                    eng.wait_ge(dsem[q][o.dslot], o.dval)
                                    waited[("d", q, o.dslot)] = o.dval
                return body

            for e in ENGS:
                if self.per_eng[e] or e == "sp":
                    handles[e](make(e))


ESZ = {F32: 4, BF16: 2, I32: 4, U32: 4, U8: 1}


class Arena:
    def __init__(self, nc, es, nbytes, name="arena"):
        self.t = es.enter_context(nc.sbuf_tensor(name, [128, nbytes], U8))
        self.nbytes = nbytes
        self.off = 0
        self.marks = []

    def alloc(self, shape, dtype, name="t"):
        if isinstance(shape, int):
            shape = [shape]
        n = int(np.prod(shape))
        nb = n * ESZ[dtype]
        self.off = (self.off + 63) // 64 * 64
        assert self.off + nb <= self.nbytes, f"arena overflow {name}: {self.off}+{nb}>{self.nbytes}"
        ap = self.t[:, self.off:self.off + nb]
        if dtype != U8:
            ap = ap.bitcast(dtype)
        if len(shape) > 1:
            names = " ".join(f"d{i}" for i in range(len(shape)))
            kw = {f"d{i}": int(shape[i]) for i in range(1, len(shape))}
            ap = ap.rearrange(f"p ({names}) -> p {names}", **kw)
        self.off += nb
        return ap, Buf(name)

    def mark(self):
        self.marks.append(self.off)

    def release(self):
        self.off = self.marks.pop()


IN_S5 = (0, 1024)
IN_XBC = (1024, 5120)
IN_DT = (5120, 5184)
IN_Z = (5184, 7232)
IN_GA = (7232, 8256)
IN_GB = (8256, 9280)


class K:
    def __init__(self, stage=99, debug=False):
        self.stage = stage
        self.debug = debug
        self.nc = bass.Bass("TRN2", target_bir_lowering=False)
        self.ins = {}
        self.dbg = {}

    def inp(self, name, shape, dt=F32):
        self.ins[name] = self.nc.dram_tensor(name, list(shape), dt, kind="ExternalInput").ap()
        return self.ins[name]

    def scratch(self, name, shape, dt, dump=False):
        if self.debug and dump:
            ap = self.nc.dram_tensor(name, list(shape), dt, kind="ExternalOutput").ap()
            self.dbg[name] = ap
        else:
            ap = self.nc.dram_tensor(name, list(shape), dt).ap()
        return ap, Buf(name)

    def build(self):
        nc = self.nc
        inp = self.inp
        xT_cm = inp("xT_cm", [D, L])
        xT_rm = inp("xT_rm", [D, L])
        ctxT = inp("ctxT", [D, LC])
        cT = inp("cT", [128, 8])
        cctxT = inp("cctxT", [128, 8])
        ada_w = inp("ada_w", [D, 6 * D])
        ada_b = inp("ada_b", [1, 6 * D])
        nmw = inp("norm_mix_w", [128, 8])
        w_in = inp("w_in", [D, 9280])
        out = nc.dram_tensor("out", [L, D], F32, kind="ExternalOutput").ap()
        self.out = out

        with ExitStack() as es:
            self.es = es
            ar = self.ar = Arena(nc, es, 206 * 1024)
            self.ps = [es.enter_context(nc.psum_tensor(f"ps{i}", [128, 512], F32)) for i in range(8)]
            self.psb = [Buf(f"ps{i}", excl=True) for i in range(8)]
            S = self.S = Sched(nc)

            ones_bf, ones_bf_b = ar.alloc([128], BF16, "ones_bf")
            S.op("dve", lambda e: e.memset(ones_bf, 1.0), writes=[ones_bf_b])
            ones_f, ones_f_b = ar.alloc([128], F32, "ones_f")
            S.op("dve", lambda e: e.memset(ones_f, 1.0), writes=[ones_f_b])
            ident_f, ident_f_b = ar.alloc([128], F32, "ident_f")
            S.op("pool", lambda e: e.affine_select(out=ident_f, in_=ones_f, pattern=[[-1, 128]],
                                                  compare_op=ALU.is_equal, fill=0.0, base=0,
                                                  channel_multiplier=1),
                 reads=[ones_f_b], writes=[ident_f_b])
            self.consts = dict(ones_bf=(ones_bf, ones_bf_b), ones_f=(ones_f, ones_f_b),
                               ident_f=(ident_f, ident_f_b))

            self.phase0(cT, cctxT, ada_w, ada_b, nmw)
            if self.stage >= 1:
                self.phase1(xT_rm, xT_cm, ctxT, w_in)
            if self.stage >= 2:
                self.phase2(inp("convw", [128, 32, 5]), inp("convb", [128, 32]), inp("dtbias", [1, 64]), inp("alog", [1, 64]),
                            inp("ssd_d", [128, 16]), inp("ssd_nw", [128, 16]))
            if self.stage >= 3:
                self.phase3_setup(inp("s5F", [2, 128, 5, 8, 64]), inp("s5S", [2, 128, 3 * 32 + 4 * 512]))
                self.phase3_main(inp("s5_d", [128, 8]))
            if self.stage >= 4:
                self.phase4(inp("glu_w", [D, D]), inp("glu_b", [128, 8]), inp("w_a", [D, D]), inp("w_b", [2 * D, D]), inp("w_o", [D, D]),
                            inp("x_tok", [L, D]), inp("nfw", [1, D]), inp("r_w", [D, 32]), inp("r_b", [1, 32]))
            if self.stage >= 5:
                self.phase5(inp("moe_wg", [4096, 8192]), inp("moe_wu", [4096, 8192]), inp("moe_wd", [4096, 8192]),
                            inp("moe_bg", [32, D]), inp("moe_bu", [32, D]), inp("moe_bd", [32, D]), inp("fnw", [1, D]))
            S.barrier()
            S.emit()
        return nc

    def phase0(self, cT, cctxT, ada_w, ada_b, nmw):
        nc, S, ar = self.nc, self.S, self.ar
        ps, psb = self.ps, self.psb
        ident_f, ident_f_b = self.consts["ident_f"]
        self.mod_rep = []
        self.Am = [ar.alloc([8], F32, f"Am{w}") for w in range(2)]
        self.Bm = [ar.alloc([8], F32, f"Bm{w}") for w in range(2)]
        nmw_t, nmw_b = ar.alloc([8], F32, "nmw")
        S.op("sp", lambda e: e.dma_start(out=nmw_t, in_=nmw), writes=[nmw_b], dma=True)
        ar.mark()
        self.mod_rep.append(ar.alloc([6 * D], F32, "mod_rep0"))
        self.mod_rep.append(ar.alloc([6 * D], F32, "mod_rep1"))
        self.mod_s, self.mod_sb = self.scratch("mod_s", [128, 6 * D], F32)
        adab, adab_b = ar.alloc([6 * D], F32, "adab")
        S.op("sp", lambda e: e.dma_start(out=adab, in_=ada_b.broadcast_to([128, 6 * D])), writes=[adab_b], dma=True)
        lhs = []
        for w, src in enumerate((cT, cctxT)):
            c_t, c_b = ar.alloc([8], F32, f"c{w}")
            S.op("sp", lambda e, c_t=c_t, src=src: e.dma_start(out=c_t, in_=src), writes=[c_b], dma=True)
            s_t, s_b = ar.alloc([8], F32, f"s{w}")
            S.op("act", lambda e, c_t=c_t, s_t=s_t: e.activation(out=s_t, in_=c_t, func=AF.Silu),
                 reads=[c_b], writes=[s_b])
            l_t, l_b = ar.alloc([8, 128], BF16, f"l{w}")
            S.op("dve", lambda e, l_t=l_t, s_t=s_t: e.tensor_copy(out=l_t, in_=s_t.unsqueeze(2).broadcast_to([128, 8, 128])),
                 reads=[s_b], writes=[l_b])
            lhs.append((l_t, l_b))
        wsrc = ada_w.rearrange("(kc p) n -> p kc n", p=128)
        wbufs = [ar.alloc([8, 512], BF16, f"adaw{i}") for i in range(2)]
        for blk in range(12):
            wt, wb = wbufs[blk % 2]
            S.op("pool", lambda e, wt=wt, blk=blk: e.dma_start(out=wt, in_=wsrc[:, :, blk * 512:(blk + 1) * 512]),
                 writes=[wb], dma=True)
            for w in range(2):
                l_t, l_b = lhs[w]
                pi = (blk * 2 + w) % 8

                def mm(e, l_t=l_t, wt=wt, pi=pi):
                    ins = None
                    for kc in range(8):
                        ins = e.matmul(ps[pi][:], lhsT=l_t[:, kc, :], rhs=wt[:, kc, :], start=(kc == 0), stop=(kc == 7))
                    return ins
                S.op("pe", mm, reads=[l_b, wb], writes=[psb[pi]])
                mt, mb = self.mod_rep[w]
                S.op("dve", lambda e, mt=mt, pi=pi, blk=blk: e.tensor_tensor(
                    out=mt[:, blk * 512:(blk + 1) * 512], in0=ps[pi][:], in1=adab[:, blk * 512:(blk + 1) * 512], op=ALU.add),
                    reads=[psb[pi], adab_b], writes=[mb])
        tmp, tmp_b = ar.alloc([8, 128], F32, "diagtmp")
        for w in range(2):
            mt, mb = self.mod_rep[w]
            for seg, (dst, dst_b) in ((0, self.Bm[w]), (1, self.Am[w])):
                view = mt[:, seg * D:(seg + 1) * D].rearrange("p (k q) -> p k q", k=8)
                S.op("dve", lambda e, view=view: e.tensor_tensor(
                    out=tmp, in0=view, in1=ident_f.unsqueeze(1).broadcast_to([128, 8, 128]), op=ALU.mult),
                    reads=[mb, ident_f_b], writes=[tmp_b])
                S.op("dve", lambda e, dst=dst: e.reduce_sum(out=dst, in_=tmp, axis=AX.X), reads=[tmp_b], writes=[dst_b])
            at, ab = self.Am[w]
            S.op("dve", lambda e, at=at: e.scalar_tensor_tensor(out=at, in0=at, scalar=1.0, in1=nmw_t, op0=ALU.add, op1=ALU.mult),
                 reads=[ab, nmw_b], writes=[ab])
        S.op("sp", lambda e: e.dma_start(out=self.mod_s, in_=self.mod_rep[0][0]), reads=[self.mod_rep[0][1]], writes=[self.mod_sb], dma=True)
        if self.debug:
            for w in range(2):
                o, ob = self.scratch(f"dbg_mod{w}", [128, 6 * D], F32, dump=True)
                mt, mb = self.mod_rep[w]
                S.op("sp", lambda e, o=o, mt=mt: e.dma_start(out=o, in_=mt), reads=[mb], writes=[ob], dma=True)
                o, ob = self.scratch(f"dbg_AB{w}", [128, 16], F32, dump=True)
                S.op("sp", lambda e, o=o, w=w: e.dma_start(out=o[:, 0:8], in_=self.Am[w][0]), reads=[self.Am[w][1]], writes=[ob], dma=True)
                S.op("sp", lambda e, o=o, w=w: e.dma_start(out=o[:, 8:16], in_=self.Bm[w][0]), reads=[self.Bm[w][1]], writes=[ob], dma=True)
        S.barrier()
        ar.release()

    def norm_bufs(self):
        ar = self.ar
        return dict(xt=[ar.alloc([8, 512], F32, f"xt{i}") for i in range(2)],
                    sq=[ar.alloc([8, 512], BF16, f"sq{i}") for i in range(2)],
                    rs=[ar.alloc([512], F32, f"rs{i}") for i in range(2)],
                    tm=[ar.alloc([512], F32, f"tm{i}") for i in range(4)], cnt=[0])

    def norm_tokens(self, src, ntok, u_t, u_b, col0, w, nb):
        nc, S, ar = self.nc, self.S, self.ar
        ps, psb = self.ps, self.psb
        ones_bf, ones_bf_b = self.consts["ones_bf"]
        At, Ab = self.Am[w]
        Bt, Bb = self.Bm[w]
        srcv = src.rearrange("(kc p) n -> p kc n", p=128)
        xt, sq, rs, tm = nb["xt"], nb["sq"], nb["rs"], nb["tm"]
        ntile = (ntok + 511) // 512
        for ti in range(ntile):
            n = min(512, ntok - ti * 512)
            ci = nb["cnt"][0]
            nb["cnt"][0] += 1
            x_t, x_b = xt[ci % 2]
            q_t, q_b = sq[ci % 2]
            r_t, r_b = rs[ci % 2]
            S.op("sp", lambda e, x_t=x_t, ti=ti, n=n: e.dma_start(out=x_t[:, :, 0:n], in_=srcv[:, :, ti * 512:ti * 512 + n]),
                 writes=[x_b], dma=True)
            S.op("act", lambda e, x_t=x_t, q_t=q_t, n=n: e.activation(out=q_t[:, :, 0:n], in_=x_t[:, :, 0:n], func=AF.Square),
                 reads=[x_b], writes=[q_b])
            pi = ci % 2

            def mm(e, q_t=q_t, pi=pi, n=n):
                ins = None
                for kc in range(8):
                    ins = e.matmul(ps[pi][:, 0:n], lhsT=ones_bf, rhs=q_t[:, kc, 0:n], start=(kc == 0), stop=(kc == 7))
                return ins
            S.op("pe", mm, reads=[q_b, ones_bf_b], writes=[psb[pi]])
            S.op("act", lambda e, r_t=r_t, pi=pi, n=n: e.activation(out=r_t[:, 0:n], in_=ps[pi][:, 0:n], func=AF.Sqrt,
                                                                  scale=1.0 / D, bias=EPS),
                 reads=[psb[pi]], writes=[r_b])
            S.op("dve", lambda e, r_t=r_t, n=n: e.reciprocal(out=r_t[:, 0:n], in_=r_t[:, 0:n]), reads=[r_b], writes=[r_b])
            for kc in range(8):
                t_t, t_b = tm[kc % 4]
                S.op("dve", lambda e, t_t=t_t, x_t=x_t, r_t=r_t, kc=kc, n=n: e.tensor_tensor(
                    out=t_t[:, 0:n], in0=x_t[:, kc, 0:n], in1=r_t[:, 0:n], op=ALU.mult),
                    reads=[x_b, r_b], writes=[t_b])
                S.op("act", lambda e, t_t=t_t, kc=kc, ti=ti, n=n: e.activation(
                    out=u_t[:, kc, col0 + ti * 512:col0 + ti * 512 + n], in_=t_t[:, 0:n], func=AF.Identity,
                    scale=At[:, kc:kc + 1], bias=Bt[:, kc:kc + 1]),
                    reads=[t_b, Ab, Bb], writes=[u_b])

    def proj_block(self, wsrc, c0, ncols, u_t, u_b, tok_ranges, dst, dst_b, row0, wbufs, stg, cnt):
        S = self.S
        ps, psb = self.ps, self.psb
        wt, wb = wbufs[cnt[0] % 2]
        cnt[0] += 1
        S.op("pool", lambda e: e.dma_start(out=wt[:, :, 0:ncols], in_=wsrc[:, :, c0:c0 + ncols]), writes=[wb], dma=True)
        for (ucol, n, dcol) in tok_ranges:
            for mc in range(ncols // 128):
                pi = cnt[1] % 8
                cnt[1] += 1

                def mm(e, pi=pi, mc=mc, ucol=ucol, n=n):
                    ins = None
                    for kc in range(8):
                        ins = e.matmul(ps[pi][:, 0:n], lhsT=wt[:, kc, mc * 128:(mc + 1) * 128],
                                       rhs=u_t[:, kc, ucol:ucol + n], start=(kc == 0), stop=(kc == 7))
                    return ins
                S.op("pe", mm, reads=[wb, u_b], writes=[psb[pi]])
                st, sb = stg[cnt[2] % len(stg)]
                eng = "act" if cnt[2] % 2 == 0 else "dve"
                cnt[2] += 1
                if eng == "act":
                    S.op("act", lambda e, st=st, pi=pi, n=n: e.activation(out=st[:, 0:n], in_=ps[pi][:, 0:n], func=AF.Copy),
                         reads=[psb[pi]], writes=[sb])
                else:
                    S.op("dve", lambda e, st=st, pi=pi, n=n: e.tensor_copy(out=st[:, 0:n], in_=ps[pi][:, 0:n]),
                         reads=[psb[pi]], writes=[sb])
                r = row0 + mc * 128
                S.op("sp", lambda e, st=st, r=r, dcol=dcol, n=n: e.dma_start(out=dst[r:r + 128, dcol:dcol + n], in_=st[:, 0:n]),
                     reads=[sb], writes=[dst_b], dma=True)

    def phase1(self, xT_rm, xT_cm, ctxT, w_in):
        nc, S, ar = self.nc, self.S, self.ar
        ps, psb = self.ps, self.psb
        dbg = self.debug
        self.s5u, self.s5u_b = self.scratch("s5u", [D, T], BF16, dump=dbg)
        self.xbc_s, self.xbc_sb = self.scratch("xbc_s", [4096, T], BF16, dump=dbg)
        self.z_s, self.z_sb = self.scratch("z_s", [2048, L], BF16, dump=dbg)
        self.gab_s, self.gab_sb = self.scratch("gab_s", [2048, L], BF16, dump=dbg)
        self.dt_s, self.dt_sb = self.scratch("dt_s", [128, 34 * 64], F32, dump=dbg)
        wsrc = w_in.rearrange("(kc p) n -> p kc n", p=128)
        ar.mark()
        u_t, u_b = ar.alloc([8, T], BF16, "u")
        wbufs = [ar.alloc([8, 512], BF16, f"wb{i}") for i in range(2)]
        stg = [ar.alloc([512], BF16, f"stg{i}") for i in range(4)]
        cnt = [0, 0, 0]
        nb = self.norm_bufs()
        self.norm_tokens(ctxT, LC, u_t, u_b, 0, 1, nb)
        self.norm_tokens(xT_rm, L, u_t, u_b, LC, 0, nb)
        if dbg:
            o, ob = self.scratch("dbg_u_rm", [128, 8 * T], BF16, dump=True)
            S.op("sp", lambda e: e.dma_start(out=o, in_=u_t.rearrange("p a b -> p (a b)")), reads=[u_b], writes=[ob], dma=True)
        toks = [(0, LC, 0)] + [(LC + i * 512, 512, LC + i * 512) for i in range(8)]
        for blk in range(2):
            self.proj_block(wsrc, IN_S5[0] + blk * 512, 512, u_t, u_b, toks, self.s5u, self.s5u_b, blk * 512, wbufs, stg, cnt)
        self.norm_tokens(xT_cm, L, u_t, u_b, LC, 0, nb)
        for blk in range(8):
            self.proj_block(wsrc, IN_XBC[0] + blk * 512, 512, u_t, u_b, toks, self.xbc_s, self.xbc_sb, blk * 512, wbufs, stg, cnt)
        ltoks = [(LC + i * 512, 512, i * 512) for i in range(8)]
        for blk in range(4):
            self.proj_block(wsrc, IN_Z[0] + blk * 512, 512, u_t, u_b, ltoks, self.z_s, self.z_sb, blk * 512, wbufs, stg, cnt)
        for blk in range(4):
            self.proj_block(wsrc, IN_GA[0] + blk * 512, 512, u_t, u_b, ltoks, self.gab_s, self.gab_sb, blk * 512, wbufs, stg, cnt)
        wdt, wdt_b = ar.alloc([8, 64], BF16, "wdt")
        S.op("pool", lambda e: e.dma_start(out=wdt, in_=wsrc[:, :, IN_DT[0]:IN_DT[1]]), writes=[wdt_b], dma=True)
        dtt, dtt_b = ar.alloc([34, 64], F32, "dtt")
        for c in range(34):
            pi = c % 2

            def mm(e, c=c, pi=pi):
                ins = None
                for kc in range(8):
                    ins = e.matmul(ps[pi][:, 0:64], lhsT=u_t[:, kc, c * 128:(c + 1) * 128], rhs=wdt[:, kc, :],
                                   start=(kc == 0), stop=(kc == 7))
                return ins
            S.op("pe", mm, reads=[u_b, wdt_b], writes=[psb[pi]])
            S.op("dve", lambda e, c=c, pi=pi: e.tensor_copy(out=dtt[:, c, :], in_=ps[pi][:, 0:64]), reads=[psb[pi]], writes=[dtt_b])
        S.op("sp", lambda e: e.dma_start(out=self.dt_s, in_=dtt.rearrange("p a b -> p (a b)")), reads=[dtt_b], writes=[self.dt_sb], dma=True)
        S.barrier()
        ar.release()


    def pq(self, i, j0, j1=None):
        if j1 is None:
            j1 = j0 + 1
        return self.ps[i][:, j0 * 128:j1 * 128], [self.psb[i]]

    def phase2(self, convw, convb, dtbias, alog, ssd_d, ssd_nw):
        nc, S, ar = self.nc, self.S, self.ar
        ps = self.ps
        dbg = self.debug
        ident_f, ident_f_b = self.consts["ident_f"]
        ones_f, ones_f_b = self.consts["ones_f"]
        ones_bf, ones_bf_b = self.consts["ones_bf"]
        self.yb_s, self.yb_sb = self.scratch("yb_s", [2048, L], BF16, dump=dbg)
        ar.mark()
        ident_bf, ident_bf_b = ar.alloc([128], BF16, "ident_bf")
        S.op("dve", lambda e: e.tensor_copy(out=ident_bf, in_=ident_f), reads=[ident_f_b], writes=[ident_bf_b])
        tri, tri_b = ar.alloc([128], F32, "tri")
        triT, triT_b = ar.alloc([128], F32, "triT")
        S.op("pool", lambda e: e.affine_select(out=tri, in_=ones_f, pattern=[[1, 128]], compare_op=ALU.is_ge, fill=0.0,
                                              base=0, channel_multiplier=-1), reads=[ones_f_b], writes=[tri_b])
        S.op("pool", lambda e: e.affine_select(out=triT, in_=ones_f, pattern=[[-1, 128]], compare_op=ALU.is_ge, fill=0.0,
                                              base=0, channel_multiplier=1), reads=[ones_f_b], writes=[triT_b])
        zer, zer_b = ar.alloc([128], F32, "zer")
        S.op("dve", lambda e: e.memset(zer, 0.0), writes=[zer_b])
        NEG = [ar.alloc([128], BF16, f"NEG{d}") for d in range(2)]
        S.op("pool", lambda e: e.affine_select(out=NEG[0][0], in_=zer, pattern=[[1, 128]], compare_op=ALU.is_ge, fill=-60000.0,
                                              base=0, channel_multiplier=-1), reads=[zer_b], writes=[NEG[0][1]])
        S.op("pool", lambda e: e.affine_select(out=NEG[1][0], in_=zer, pattern=[[-1, 128]], compare_op=ALU.is_ge, fill=-60000.0,
                                              base=0, channel_multiplier=1), reads=[zer_b], writes=[NEG[1][1]])
        oh2, oh2_b = ar.alloc([64], F32, "oh2")
        S.op("dve", lambda e: e.tensor_tensor(out=oh2[:, 0:32], in0=ident_f[:, 0:32], in1=ident_f[:, 32:64], op=ALU.add),
             reads=[ident_f_b], writes=[oh2_b])
        S.op("dve", lambda e: e.tensor_tensor(out=oh2[:, 32:64], in0=ident_f[:, 64:96], in1=ident_f[:, 96:128], op=ALU.add),
             reads=[ident_f_b], writes=[oh2_b])
        sel2, sel2_b = ar.alloc([64, 128], BF16, "sel2")
        S.op("dve", lambda e: e.tensor_copy(out=sel2, in_=oh2.unsqueeze(2).broadcast_to([128, 64, 128])), reads=[oh2_b], writes=[sel2_b])
        mhi, mhi_b = ar.alloc([1], F32, "mhi")
        mlo, mlo_b = ar.alloc([1], F32, "mlo")
        mt_, mt_b = ar.alloc([1], F32, "mtmp")
        S.op("dve", lambda e: e.reduce_sum(out=mhi, in_=ident_f[:, 0:32], axis=AX.X), reads=[ident_f_b], writes=[mhi_b])
        S.op("dve", lambda e: e.reduce_sum(out=mt_, in_=ident_f[:, 64:96], axis=AX.X), reads=[ident_f_b], writes=[mt_b])
        S.op("dve", lambda e: e.tensor_tensor(out=mhi, in0=mhi, in1=mt_, op=ALU.add), reads=[mhi_b, mt_b], writes=[mhi_b])
        S.op("dve", lambda e: e.tensor_scalar(out=mlo, in0=mhi, scalar1=-1.0, scalar2=1.0, op0=ALU.mult, op1=ALU.add),
             reads=[mhi_b], writes=[mlo_b])
        cw, cw_b = ar.alloc([32, 5], F32, "convw")
        cb, cb_b = ar.alloc([32], F32, "convb")
        dcol, dcol_b = ar.alloc([16], F32, "ssd_d")
        nwc, nwc_b = ar.alloc([16], F32, "ssd_nw")
        S.op("sp", lambda e: e.dma_start(out=cw, in_=convw), writes=[cw_b], dma=True)
        S.op("sp", lambda e: e.dma_start(out=cb, in_=convb), writes=[cb_b], dma=True)
        S.op("sp", lambda e: e.dma_start(out=dcol, in_=ssd_d), writes=[dcol_b], dma=True)
        S.op("sp", lambda e: e.dma_start(out=nwc, in_=ssd_nw), writes=[nwc_b], dma=True)
        dt, dt_b = ar.alloc([34, 2, 32], F32, "dt")
        decay, decay_b = ar.alloc([34, 2, 32], F32, "decay")
        wend, wend_b = ar.alloc([34, 2, 32], F32, "wend")
        HL, HL_b = ar.alloc([34, 128], BF16, "HL")
        HLn, HLn_b = ar.alloc([34, 128], BF16, "HLn")
        ar.mark()
        nacum, nacum_b = ar.alloc([34, 2, 32], F32, "nacum")
        adtd, adtd_b = ar.alloc([34, 4, 32], F32, "adtd")
        tot, tot_b = ar.alloc([34, 2, 32], F32, "tot")
        dtb, dtb_b = ar.alloc([2, 32], F32, "dtb")
        arep, arep_b = ar.alloc([2, 32], F32, "arep")
        S.op("sp", lambda e: e.dma_start(out=dt.rearrange("p a b c -> p (a b c)"), in_=self.dt_s), reads=[self.dt_sb], writes=[dt_b], dma=True)
        S.op("sp", lambda e: e.dma_start(out=dtb.rearrange("p a b -> p (a b)"), in_=dtbias.broadcast_to([128, 64])), writes=[dtb_b], dma=True)
        S.op("sp", lambda e: e.dma_start(out=arep.rearrange("p a b -> p (a b)"), in_=alog.broadcast_to([128, 64])), writes=[arep_b], dma=True)
        S.op("dve", lambda e: e.tensor_tensor(out=dt, in0=dt, in1=dtb.unsqueeze(1).broadcast_to([128, 34, 2, 32]), op=ALU.add),
             reads=[dt_b, dtb_b], writes=[dt_b])
        S.op("act", lambda e: e.activation(out=dt, in_=dt, func=AF.Exp), reads=[dt_b], writes=[dt_b])
        S.op("act", lambda e: e.activation(out=dt, in_=dt, func=AF.Ln, bias=1.0), reads=[dt_b], writes=[dt_b])
        S.op("act", lambda e: e.activation(out=arep, in_=arep, func=AF.Exp), reads=[arep_b], writes=[arep_b])
        S.op("dve", lambda e: e.tensor_scalar(out=arep, in0=arep, scalar1=-1.0, scalar2=None, op0=ALU.mult), reads=[arep_b], writes=[arep_b])
        for d in range(2):
            for j in range(2):
                S.op("dve", lambda e, d=d, j=j: e.tensor_tensor(
                    out=adtd[:, :, 2 * d + j, :], in0=dt[:, :, d, :], in1=arep[:, d, :].unsqueeze(1).broadcast_to([128, 34, 32]), op=ALU.mult),
                    reads=[dt_b, arep_b], writes=[adtd_b])
        k = 0
        for d in range(2):
            lhs, lhs_b = (tri, tri_b) if d == 0 else (triT, triT_b)
            for (c0, ncn) in ((0, 16), (16, 16), (32, 2)):
                pa, pb = self.pq(k % 4, 0, 4)
                k += 1
                S.op("pe", lambda e, pa=pa, lhs=lhs, c0=c0, ncn=ncn, d=d: e.matmul(
                    pa[:, 0:ncn * 32].rearrange("p (a b) -> p a b", b=32), lhsT=lhs, rhs=adtd[:, c0:c0 + ncn, 2 * d, :], start=True, stop=True),
                    reads=[lhs_b, adtd_b], writes=pb)
                S.op("dve", lambda e, pa=pa, c0=c0, ncn=ncn, d=d: e.tensor_scalar(
                    out=nacum[:, c0:c0 + ncn, d, :], in0=pa[:, 0:ncn * 32].rearrange("p (a b) -> p a b", b=32),
                    scalar1=-1.0, scalar2=None, op0=ALU.mult), reads=pb, writes=[nacum_b])
                pa, pb = self.pq(k % 4, 0, 4)
                k += 1
                S.op("pe", lambda e, pa=pa, c0=c0, ncn=ncn, d=d: e.matmul(
                    pa[:, 0:ncn * 32].rearrange("p (a b) -> p a b", b=32), lhsT=ones_f, rhs=adtd[:, c0:c0 + ncn, 2 * d, :], start=True, stop=True),
                    reads=[ones_f_b, adtd_b], writes=pb)
                S.op("dve", lambda e, pa=pa, c0=c0, ncn=ncn, d=d: e.tensor_copy(
                    out=tot[:, c0:c0 + ncn, d, :], in_=pa[:, 0:ncn * 32].rearrange("p (a b) -> p a b", b=32)), reads=pb, writes=[tot_b])
        S.op("act", lambda e: e.activation(out=decay, in_=tot, func=AF.Exp), reads=[tot_b], writes=[decay_b])
        S.op("dve", lambda e: e.tensor_tensor(out=wend, in0=tot, in1=nacum, op=ALU.add), reads=[tot_b, nacum_b], writes=[wend_b])
        S.op("act", lambda e: e.activation(out=wend, in_=wend, func=AF.Exp), reads=[wend_b], writes=[wend_b])
        S.op("dve", lambda e: e.tensor_tensor(out=wend, in0=wend, in1=dt, op=ALU.mult), reads=[wend_b, dt_b], writes=[wend_b])
        acT = [ar.alloc([128], F32, f"acT{i}") for i in range(2)]
        hi_ = [ar.alloc([128], BF16, f"hi{i}") for i in range(2)]
        lo_ = [ar.alloc([128], F32, f"lo{i}") for i in range(2)]
        for c in range(34):
            pa, pab = self.pq(4 + (c % 2), 0)
            pb_, pbb = self.pq(4 + (c % 2), 1)
            lhsT = adtd[:, c, :, :].rearrange("p a b -> p (a b)")
            S.op("pe", lambda e, pa=pa, lhsT=lhsT: e.matmul(pa, lhsT=lhsT, rhs=tri, start=True, stop=True),
                 reads=[adtd_b, tri_b], writes=pab)
            S.op("pe", lambda e, pb_=pb_, lhsT=lhsT: e.matmul(pb_, lhsT=lhsT, rhs=triT, start=True, stop=True),
                 reads=[adtd_b, triT_b], writes=pbb)
            a_t, a_b = acT[c % 2]
            h_t, h_b = hi_[c % 2]
            l_t, l_b = lo_[c % 2]
            S.op("act", lambda e, a_t=a_t, pa=pa: e.activation(out=a_t[0:64, :], in_=pa[0:64, :], func=AF.Copy), reads=pab, writes=[a_b])
            S.op("act", lambda e, a_t=a_t, pb_=pb_: e.activation(out=a_t[64:128, :], in_=pb_[64:128, :], func=AF.Copy), reads=pbb, writes=[a_b])
            S.op("dve", lambda e, a_t=a_t, h_t=h_t: e.tensor_copy(out=h_t, in_=a_t), reads=[a_b], writes=[h_b])
            S.op("dve", lambda e, a_t=a_t, h_t=h_t, l_t=l_t: e.tensor_tensor(out=l_t, in0=a_t, in1=h_t, op=ALU.subtract), reads=[a_b, h_b], writes=[l_b])
            S.op("dve", lambda e, l_t=l_t: e.tensor_scalar(out=l_t, in0=l_t, scalar1=mlo[:, 0:1], scalar2=None, op0=ALU.mult), reads=[l_b, mlo_b], writes=[l_b])
            S.op("dve", lambda e, h_t=h_t, l_t=l_t, c=c: e.scalar_tensor_tensor(out=HL[:, c, :], in0=h_t, scalar=mhi[:, 0:1], in1=l_t, op0=ALU.mult, op1=ALU.add),
                 reads=[h_b, l_b, mhi_b], writes=[HL_b])
        S.op("dve", lambda e: e.tensor_scalar(out=HLn, in0=HL, scalar1=-1.0, scalar2=None, op0=ALU.mult), reads=[HL_b], writes=[HLn_b])
        if dbg:
            for nm, src_, sb_, dtp, ncol in (("dbg_dt", dt.rearrange("p a b c -> p (a b c)"), dt_b, F32, 34 * 64),
                                             ("dbg_nacum", nacum.rearrange("p a b c -> p (a b c)"), nacum_b, F32, 34 * 64),
                                             ("dbg_wend", wend.rearrange("p a b c -> p (a b c)"), wend_b, F32, 34 * 64),
                                             ("dbg_HL", HL.rearrange("p a b -> p (a b)"), HL_b, BF16, 34 * 128)):
                o, ob = self.scratch(nm, [128, ncol], dtp, dump=True)
                S.op("sp", lambda e, o=o, src_=src_: e.dma_start(out=o, in_=src_), reads=[sb_], writes=[ob], dma=True)
        S.barrier()
        ar.release()
        P2STOP = int(os.environ.get("P2STOP", "99"))
        P2SKIP = os.environ.get("P2SKIP", "")
        if P2STOP <= 1:
            ar.release()
            return
        TP = 4360
        RA, RA_b = ar.alloc([4 * TP], BF16, "RA")
        R = RA.rearrange("p (a b) -> p a b", a=4)
        prevs = RA[:, 0:2 * 34 * 256].rearrange("p (d c n) -> p d c n", d=2, c=34)
        xc, xc_b = ar.alloc([4, T], BF16, "xc")
        x_tok, x_tok_b = ar.alloc([34, 256], BF16, "x_tok")
        B_tok, B_tok_b = ar.alloc([34, 128], BF16, "B_tok")
        dg, dg_b = ar.alloc([20, 128], BF16, "dg")
        st = [ar.alloc([256], F32, f"st{d}") for d in range(2)]
        xw = [ar.alloc([256], BF16, f"xw{i}") for i in range(4)]
        xdt = [ar.alloc([256], BF16, f"xdt{i}") for i in range(4)]
        E_ = [ar.alloc([4, 128], F32, f"E{i}") for i in range(2)]
        Lt = [ar.alloc([4, 128], F32, f"Lt{i}") for i in range(2)]
        Gt = [ar.alloc([4, 128], BF16, f"Gt{i}") for i in range(4)]
        Cp = [ar.alloc([4, 128], BF16, f"Cp{i}") for i in range(4)]
        zt = [ar.alloc([2, 512], BF16, f"zt{i}") for i in range(1)]
        sz = [ar.alloc([2, 512], F32, f"sz{i}") for i in range(1)]
        yz = [ar.alloc([2, 512], F32, f"yz{i}") for i in range(1)]
        sqy = [ar.alloc([2, 512], BF16, f"sqy{i}") for i in range(1)]
        rsy = [ar.alloc([512], F32, f"rsy{i}") for i in range(1)]
        ybo = [ar.alloc([2, 512], BF16, f"ybo{i}") for i in range(1)]
        bwd_order = [1, 0] + list(range(33, 1, -1))
        cnt = dict(e=0, a=0, g=0, cp=0, xw=0, cs=0, y=0, cb=0, post=0, conv=0, tr=0)
        for g in range(int(os.environ.get('P2G', '8'))):
            tiles = [2 * g, 2 * g + 1, 16 + g, 24 + g]
            for i, tix in enumerate(tiles):
                S.op("sp", lambda e, i=i, tix=tix: e.dma_start(out=R[:, i, 2:258], in_=self.xbc_s[tix * 128:(tix + 1) * 128, 0:256]),
                     reads=[self.xbc_sb], writes=[RA_b], dma=True)
                S.op("sp", lambda e, i=i, tix=tix: e.dma_start(out=R[:, i, 262:4358], in_=self.xbc_s[tix * 128:(tix + 1) * 128, 256:T]),
                     reads=[self.xbc_sb], writes=[RA_b], dma=True)
            for (a0, a1) in ((0, 2), (258, 262), (4358, 4360)):
                S.op("dve", lambda e, a0=a0, a1=a1: e.memset(R[:, :, a0:a1], 0.0), writes=[RA_b])
            for i, tix in enumerate(tiles):
                for kk in range(5):
                    S.op("dve", lambda e, i=i, tix=tix, kk=kk: e.tensor_scalar(
                        out=dg[:, i * 5 + kk, :], in0=ident_f, scalar1=cw[:, tix, kk:kk + 1], scalar2=None, op0=ALU.mult),
                        reads=[ident_f_b, cw_b], writes=[dg_b])
            for i, tix in enumerate(tiles):
                for (t0, n, base) in [(0, 256, 0)] + [(256 + j * 512, 512, 260 + j * 512) for j in range(8)]:
                    pa, pb = self.pq(cnt["conv"] % 2, 0, 4)
                    cnt["conv"] += 1

                    def mm(e, pa=pa, i=i, n=n, base=base):
                        ins = None
                        for kk in range(5):
                            ins = e.matmul(pa[:, 0:n], lhsT=dg[:, i * 5 + kk, :], rhs=R[:, i, base + kk:base + kk + n],
                                           start=(kk == 0), stop=(kk == 4))
                        return ins
                    S.op("pe", mm, reads=[dg_b, RA_b], writes=pb)
                    S.op("act", lambda e, pa=pa, i=i, tix=tix, t0=t0, n=n: e.activation(
                        out=xc[:, i, t0:t0 + n], in_=pa[:, 0:n], func=AF.Silu, bias=cb[:, tix:tix + 1]),
                        reads=pb + [cb_b], writes=[xc_b])
            if P2STOP <= 2:
                break
            for c in range(34):
                pa, pb = self.pq(2 + (cnt["tr"] % 2), 0, 4)
                cnt["tr"] += 1
                pab = pa.bitcast(BF16)

                def tr(e, pab=pab, c=c):
                    ins = None
                    for j in range(3):
                        ins = e.transpose(out=pab[:, j * 128:(j + 1) * 128], in_=xc[:, j, c * 128:(c + 1) * 128], identity=ident_bf)
                    return ins
                S.op("pe", tr, reads=[xc_b, ident_bf_b], writes=pb)
                S.op("dve", lambda e, pab=pab, c=c: e.tensor_copy(out=x_tok[:, c, :], in_=pab[:, 0:256]), reads=pb, writes=[x_tok_b])
                S.op("dve", lambda e, pab=pab, c=c: e.tensor_copy(out=B_tok[:, c, :], in_=pab[:, 256:384]), reads=pb, writes=[B_tok_b])
            if P2STOP <= 3:
                break
            for d in range(2):
                S.op("dve", lambda e, d=d: e.memset(st[d][0], 0.0), writes=[st[d][1]])
            for step in range(34):
                for d in range(2):
                    c = step if d == 0 else bwd_order[step]
                    xw_t, xw_b = xw[cnt["xw"] % 4]
                    cnt["xw"] += 1
                    S.op("dve", lambda e, xw_t=xw_t, c=c, d=d, g=g: e.tensor_tensor(
                        out=xw_t.rearrange("p (r q) -> p r q", r=4), in0=x_tok[:, c, :].rearrange("p (r q) -> p r q", r=4),
                        in1=wend[:, c, d, 4 * g:4 * g + 4].unsqueeze(2).broadcast_to([128, 4, 64]), op=ALU.mult),
                        reads=[x_tok_b, wend_b], writes=[xw_b])
                    k2 = cnt["cs"] % 4
                    cnt["cs"] += 1
                    pa, pb = self.pq(4 + k2, 0, 2)
                    S.op("pe", lambda e, pa=pa, xw_t=xw_t, c=c: e.matmul(pa, lhsT=B_tok[:, c, :], rhs=xw_t, start=True, stop=True),
                         reads=[B_tok_b, xw_b], writes=pb)
                    st_t, st_b = st[d]
                    S.op("act", lambda e, st_t=st_t, d=d, c=c: e.activation(out=prevs[:, d, c, :], in_=st_t, func=AF.Copy),
                         reads=[st_b], writes=[RA_b])
                    S.op("dve", lambda e, st_t=st_t, c=c, d=d, g=g: e.tensor_tensor(
                        out=st_t.rearrange("p (r q) -> p r q", r=4), in0=st_t.rearrange("p (r q) -> p r q", r=4),
                        in1=decay[:, c, d, 4 * g:4 * g + 4].unsqueeze(2).broadcast_to([128, 4, 64]), op=ALU.mult),
                        reads=[st_b, decay_b], writes=[st_b])
                    S.op("dve", lambda e, st_t=st_t, pa=pa: e.tensor_tensor(out=st_t, in0=st_t, in1=pa, op=ALU.add),
                         reads=[st_b] + pb, writes=[st_b])
            if dbg and g == 0:
                o, ob = self.scratch("dbg_xc", [128, 4 * T], BF16, dump=True)
                S.op("sp", lambda e, o=o: e.dma_start(out=o, in_=xc.rearrange("p a b -> p (a b)")), reads=[xc_b], writes=[ob], dma=True)
                o2, ob2 = self.scratch("dbg_prev", [128, 2 * 34 * 256], BF16, dump=True)
                S.op("sp", lambda e, o2=o2: e.dma_start(out=o2, in_=RA[:, 0:2 * 34 * 256]), reads=[RA_b], writes=[ob2], dma=True)
            if P2STOP <= 4:
                continue
            for cb4 in range(8):
                if P2STOP <= 5 and cb4 >= 1:
                    break
                pi = cnt["post"] % 2
                cnt["post"] += 1
                z_t, z_b = zt[0]
                sz_t, sz_b = sz[0]
                yz_t, yz_b = yz[0]
                for i in range(2):
                    r0 = 256 * g + 128 * i
                    S.op("sp", lambda e, z_t=z_t, i=i, r0=r0, cb4=cb4: e.dma_start(out=z_t[:, i, :], in_=self.z_s[r0:r0 + 128, cb4 * 512:(cb4 + 1) * 512]),
                         reads=[self.z_sb], writes=[z_b], dma=True)
                if "Z" not in P2SKIP:
                    S.op("act", lambda e, z_t=z_t, sz_t=sz_t: e.activation(out=sz_t, in_=z_t, func=AF.Silu), reads=[z_b], writes=[sz_b])
                for cl in range(4):
                    lc = cb4 * 4 + cl
                    c = 2 + lc
                    tsl = slice(c * 128, (c + 1) * 128)
                    pcb, pcb_b = self.pq(4 + cnt["cb"] % 2, 0)
                    cnt["cb"] += 1
                    S.op("pe", lambda e, pcb=pcb, tsl=tsl: e.matmul(pcb, lhsT=xc[:, 2, tsl], rhs=xc[:, 3, tsl], start=True, stop=True),
                         reads=[xc_b], writes=pcb_b)
                    gts, cps, xds = {}, {}, {}
                    for d in range(2):
                        xd_t, xd_b = xdt[cnt["xw"] % 4]
                        cnt["xw"] += 1
                        S.op("dve", lambda e, xd_t=xd_t, c=c, d=d, g=g: e.tensor_tensor(
                            out=xd_t.rearrange("p (r q) -> p r q", r=4), in0=x_tok[:, c, :].rearrange("p (r q) -> p r q", r=4),
                            in1=dt[:, c, d, 4 * g:4 * g + 4].unsqueeze(2).broadcast_to([128, 4, 64]), op=ALU.mult),
                            reads=[x_tok_b, dt_b], writes=[xd_b])
                        xds[d] = (xd_t, xd_b)
                        pe_, pe_b = self.pq(cnt["e"] % 2, 0, 4)
                        cnt["e"] += 1

                        def mme(e, pe_=pe_, c=c, d=d, g=g):
                            ins = None
                            for r in range(4):
                                hh = d * 32 + 4 * g + r
                                ins = e.matmul(pe_[:, r * 128:(r + 1) * 128], lhsT=sel2[:, hh, :], rhs=HL[:, c, :], start=True, stop=True)
                            return ins
                        S.op("pe", mme, reads=[sel2_b, HL_b], writes=pe_b)
                        E_t, E_b = E_[cnt["e"] % 2]
                        S.op("act", lambda e, E_t=E_t, pe_=pe_: e.activation(out=E_t.rearrange("p a b -> p (a b)"), in_=pe_, func=AF.Exp),
                             reads=pe_b, writes=[E_b])
                        cp_t, cp_b = Cp[cnt["cp"] % 4]
                        cnt["cp"] += 1
                        S.op("dve", lambda e, cp_t=cp_t, E_t=E_t, tsl=tsl: e.tensor_tensor(
                            out=cp_t, in0=E_t, in1=xc[:, 3, tsl].unsqueeze(1).broadcast_to([128, 4, 128]), op=ALU.mult),
                            reads=[xc_b, E_b], writes=[cp_b])
                        cps[d] = (cp_t, cp_b)
                        pa_, pa_b = self.pq(2 + cnt["a"] % 2, 0, 4)
                        cnt["a"] += 1

                        def mma(e, pa_=pa_, c=c, d=d, g=g):
                            ins = None
                            for r in range(4):
                                hh = d * 32 + 4 * g + r
                                o_ = pa_[:, r * 128:(r + 1) * 128]
                                e.matmul(o_, lhsT=sel2[:, hh, :], rhs=HL[:, c, :], start=True, stop=False)
                                e.matmul(o_, lhsT=HLn[:, c, :], rhs=sel2[:, hh, :], start=False, stop=False)
                                ins = e.matmul(o_, lhsT=ident_bf, rhs=NEG[d][0], start=False, stop=True)
                            return ins
                        S.op("pe", mma, reads=[sel2_b, HL_b, HLn_b, ident_bf_b, NEG[d][1]], writes=pa_b)
                        L_t, L_b = Lt[cnt["a"] % 2]
                        S.op("act", lambda e, L_t=L_t, pa_=pa_: e.activation(out=L_t.rearrange("p a b -> p (a b)"), in_=pa_, func=AF.Exp),
                             reads=pa_b, writes=[L_b])
                        g_t, g_b = Gt[cnt["g"] % 4]
                        cnt["g"] += 1
                        S.op("dve", lambda e, g_t=g_t, L_t=L_t, pcb=pcb: e.tensor_tensor(
                            out=g_t, in0=L_t, in1=pcb.unsqueeze(1).broadcast_to([128, 4, 128]), op=ALU.mult),
                            reads=[L_b] + pcb_b, writes=[g_b])
                        gts[d] = (g_t, g_b)
                    yb_i = 6 + cnt["y"] % 2
                    cnt["y"] += 1
                    pys = [self.pq(yb_i, i) for i in range(2)]

                    def mmy(e, pys=pys, c=c, gts=gts, cps=cps, xds=xds):
                        ins = None
                        for i in range(2):
                            py = pys[i][0]
                            for hf in range(2):
                                r = 2 * i + hf
                                o_ = py[64 * hf:64 * hf + 64, :]
                                tp = (0, 64 * hf)
                                e.matmul(o_, lhsT=xds[0][0][:, r * 64:(r + 1) * 64], rhs=gts[0][0][:, r, :], start=True, stop=False, tile_position=tp)
                                e.matmul(o_, lhsT=prevs[:, 0, c, r * 64:(r + 1) * 64], rhs=cps[0][0][:, r, :], start=False, stop=False, tile_position=tp)
                                e.matmul(o_, lhsT=xds[1][0][:, r * 64:(r + 1) * 64], rhs=gts[1][0][:, r, :], start=False, stop=False, tile_position=tp)
                                ins = e.matmul(o_, lhsT=prevs[:, 1, c, r * 64:(r + 1) * 64], rhs=cps[1][0][:, r, :], start=False, stop=True, tile_position=tp)
                        return ins
                    rd = [RA_b]
                    for d in range(2):
                        rd += [gts[d][1], cps[d][1], xds[d][1]]
                    S.op("pe", mmy, reads=rd, writes=pys[0][1])
                    for i in range(2):
                        py = pys[i][0]
                        S.op("dve", lambda e, py=py, i=i, g=g, tsl=tsl, yz_t=yz_t, cl=cl: e.scalar_tensor_tensor(
                            out=yz_t[:, i, cl * 128:(cl + 1) * 128], in0=xc[:, i, tsl], scalar=dcol[:, 2 * g + i:2 * g + i + 1], in1=py,
                            op0=ALU.mult, op1=ALU.add), reads=[xc_b, dcol_b] + pys[i][1], writes=[yz_b])
                if "O" in P2SKIP:
                    continue
                q_t, q_b = sqy[0]
                r_t, r_b = rsy[0]
                o_t, o_b = ybo[0]
                S.op("dve", lambda e, yz_t=yz_t, sz_t=sz_t: e.tensor_tensor(out=yz_t, in0=yz_t, in1=sz_t, op=ALU.mult), reads=[yz_b, sz_b], writes=[yz_b])
                S.op("act", lambda e, yz_t=yz_t, q_t=q_t: e.activation(out=q_t, in_=yz_t, func=AF.Square), reads=[yz_b], writes=[q_b])
                pp, pp_b = self.pq(pi, 0, 4)

                def mmq(e, pp=pp, q_t=q_t):
                    e.matmul(pp, lhsT=ones_bf, rhs=q_t[:, 0, :], start=True, stop=False)
                    return e.matmul(pp, lhsT=ones_bf, rhs=q_t[:, 1, :], start=False, stop=True)
                S.op("pe", mmq, reads=[q_b, ones_bf_b], writes=pp_b)
                S.op("act", lambda e, pp=pp, r_t=r_t: e.activation(out=r_t, in_=pp, func=AF.Sqrt, scale=1.0 / 256, bias=EPS), reads=pp_b, writes=[r_b])
                S.op("dve", lambda e, r_t=r_t: e.reciprocal(out=r_t, in_=r_t), reads=[r_b], writes=[r_b])
                for i in range(2):
                    S.op("dve", lambda e, i=i, yz_t=yz_t, r_t=r_t, o_t=o_t, g=g: e.scalar_tensor_tensor(
                        out=o_t[:, i, :], in0=yz_t[:, i, :], scalar=nwc[:, 2 * g + i:2 * g + i + 1], in1=r_t, op0=ALU.mult, op1=ALU.mult),
                        reads=[yz_b, r_b, nwc_b], writes=[o_b])
                    r0 = 256 * g + 128 * i
                    S.op("sp", lambda e, o_t=o_t, i=i, r0=r0, cb4=cb4: e.dma_start(out=self.yb_s[r0:r0 + 128, cb4 * 512:(cb4 + 1) * 512], in_=o_t[:, i, :]),
                         reads=[o_b], writes=[self.yb_sb], dma=True)
        S.barrier()
        ar.release()


    def trig(self, ar, src, n, add, out, out_b, src_b, tag):
        S = self.S
        t, t_b = ar.alloc([n], F32, f"trg_t{tag}")
        ti, ti_b = ar.alloc([n], I32, f"trg_i{tag}")
        S.op("dve", lambda e: e.tensor_scalar(out=t, in0=src, scalar1=64.0 + add, scalar2=None, op0=ALU.add), reads=[src_b], writes=[t_b])
        S.op("dve", lambda e: e.tensor_copy(out=ti, in_=t), reads=[t_b], writes=[ti_b])
        S.op("dve", lambda e: e.tensor_copy(out=out, in_=ti), reads=[ti_b], writes=[out_b])
        S.op("dve", lambda e: e.tensor_tensor(out=t, in0=t, in1=out, op=ALU.subtract), reads=[t_b, out_b], writes=[t_b])
        S.op("act", lambda e: e.activation(out=out, in_=t, func=AF.Sin, scale=2.0 * np.pi), reads=[t_b], writes=[out_b])

    def cpow_tables(self, ar, lre, lim, ldt, n, bufs, tag):
        S = self.S
        TB = Buf(f"cpow{tag}")
        step, _ = ar.alloc([n], F32, "step")
        lr, _ = ar.alloc([n], F32, "lr")
        th, _ = ar.alloc([n], F32, "th")
        S.op("act", lambda e: e.activation(out=step, in_=ldt, func=AF.Exp), reads=bufs, writes=[TB])
        S.op("dve", lambda e: e.tensor_tensor(out=lr, in0=lre, in1=step, op=ALU.mult), reads=bufs + [TB], writes=[TB])
        S.op("dve", lambda e: e.scalar_tensor_tensor(out=th, in0=lim, scalar=1.0 / (2.0 * np.pi), in1=step, op0=ALU.mult, op1=ALU.mult),
             reads=bufs + [TB], writes=[TB])
        are, _ = ar.alloc([9, n], F32, "are")
        aim, _ = ar.alloc([9, n], F32, "aim")
        mag, _ = ar.alloc([n], F32, "mag")
        ph, _ = ar.alloc([n], F32, "ph")
        ck, ck_b = ar.alloc([n], F32, "ck")
        sk, sk_b = ar.alloc([n], F32, "sk")
        for k in range(9):
            ar.mark()
            S.op("act", lambda e, k=k: e.activation(out=mag, in_=lr, func=AF.Exp, scale=float(k)), reads=[TB], writes=[TB])
            S.op("dve", lambda e, k=k: e.tensor_scalar(out=ph, in0=th, scalar1=float(k), scalar2=None, op0=ALU.mult), reads=[TB], writes=[TB])
            self.trig(ar, ph, n, 0.25, ck, ck_b, TB, f"{tag}c{k}")
            self.trig(ar, ph, n, 0.0, sk, sk_b, TB, f"{tag}s{k}")
            S.op("dve", lambda e, k=k: e.tensor_tensor(out=are[:, k, :], in0=mag, in1=ck, op=ALU.mult), reads=[TB, ck_b], writes=[TB])
            S.op("dve", lambda e, k=k: e.tensor_tensor(out=aim[:, k, :], in0=mag, in1=sk, op=ALU.mult), reads=[TB, sk_b], writes=[TB])
            ar.release()
        return dict(are=are, aim=aim, lr=lr, th=th, buf=TB)

    def cmul(self, eng, out_re, out_im, a_re, a_im, b_re, b_im, t1, t2, reads, writes):
        S = self.S
        S.op(eng, lambda e: e.tensor_tensor(out=t1, in0=a_re, in1=b_re, op=ALU.mult), reads=reads, writes=writes)
        S.op(eng, lambda e: e.tensor_tensor(out=t2, in0=a_im, in1=b_im, op=ALU.mult), reads=reads, writes=writes)
        S.op(eng, lambda e: e.tensor_tensor(out=out_re, in0=t1, in1=t2, op=ALU.subtract), reads=reads, writes=writes)
        S.op(eng, lambda e: e.tensor_tensor(out=t1, in0=a_re, in1=b_im, op=ALU.mult), reads=reads, writes=writes)
        S.op(eng, lambda e: e.tensor_tensor(out=t2, in0=a_im, in1=b_re, op=ALU.mult), reads=reads, writes=writes)
        S.op(eng, lambda e: e.tensor_tensor(out=out_im, in0=t1, in1=t2, op=ALU.add), reads=reads, writes=writes)

    def bc_coef(self, ar, pw, lre, lim, n, bufs):
        S = self.S
        TB = pw["buf"]
        rd = bufs + [TB]
        nre, _ = ar.alloc([n], F32, "nre")
        den, _ = ar.alloc([n], F32, "den")
        t1, _ = ar.alloc([n], F32, "bct1")
        bcr, _ = ar.alloc([n], F32, "bcr")
        bci, _ = ar.alloc([n], F32, "bci")
        are1, aim1 = pw["are"][:, 1, :], pw["aim"][:, 1, :]
        S.op("dve", lambda e: e.tensor_scalar(out=nre, in0=are1, scalar1=-1.0, scalar2=None, op0=ALU.add), reads=rd, writes=[TB])
        S.op("dve", lambda e: e.tensor_tensor(out=den, in0=lre, in1=lre, op=ALU.mult), reads=rd, writes=[TB])
        S.op("dve", lambda e: e.tensor_tensor(out=t1, in0=lim, in1=lim, op=ALU.mult), reads=rd, writes=[TB])
        S.op("dve", lambda e: e.tensor_tensor(out=den, in0=den, in1=t1, op=ALU.add), reads=rd, writes=[TB])
        S.op("dve", lambda e: e.reciprocal(out=den, in_=den), reads=rd, writes=[TB])
        S.op("dve", lambda e: e.tensor_tensor(out=bcr, in0=nre, in1=lre, op=ALU.mult), reads=rd, writes=[TB])
        S.op("dve", lambda e: e.tensor_tensor(out=t1, in0=aim1, in1=lim, op=ALU.mult), reads=rd, writes=[TB])
        S.op("dve", lambda e: e.tensor_tensor(out=bcr, in0=bcr, in1=t1, op=ALU.add), reads=rd, writes=[TB])
        S.op("dve", lambda e: e.tensor_tensor(out=bcr, in0=bcr, in1=den, op=ALU.mult), reads=rd, writes=[TB])
        S.op("dve", lambda e: e.tensor_tensor(out=bci, in0=aim1, in1=lre, op=ALU.mult), reads=rd, writes=[TB])
        S.op("dve", lambda e: e.tensor_tensor(out=t1, in0=nre, in1=lim, op=ALU.mult), reads=rd, writes=[TB])
        S.op("dve", lambda e: e.tensor_tensor(out=bci, in0=bci, in1=t1, op=ALU.subtract), reads=rd, writes=[TB])
        S.op("dve", lambda e: e.tensor_tensor(out=bci, in0=bci, in1=den, op=ALU.mult), reads=rd, writes=[TB])
        return bcr, bci

    def phase3_setup(self, s5F, s5S):
        nc, S, ar = self.nc, self.S, self.ar
        dbg = self.debug
        ident_f, ident_f_b = self.consts["ident_f"]
        self.SI_s, self.SI_sb = self.scratch("SI_s", [2, 8, 128, 2048], BF16, dump=dbg)
        self.RO_s, self.RO_sb = self.scratch("RO_s", [2, 8, 128, 2048], BF16, dump=dbg)
        self.FIR_s, self.FIR_sb = self.scratch("FIR_s", [2, 8, 128, 1024], BF16, dump=dbg)
        self.RS, self.RS_b = ar.alloc([2, 2, 32], F32, "RS")
        mF = [ar.alloc([1], F32, f"mF{i}") for i in range(2)]
        mS = [ar.alloc([1], F32, f"mS{i}") for i in range(2)]
        mSn = [ar.alloc([1], F32, f"mSn{i}") for i in range(2)]
        self.rowmask = [ar.alloc([1], F32, f"rowm{i}") for i in range(4)]
        for i in range(4):
            S.op("dve", lambda e, i=i: e.reduce_sum(out=self.rowmask[i][0], in_=ident_f[:, 32 * i:32 * i + 32], axis=AX.X),
                 reads=[ident_f_b], writes=[self.rowmask[i][1]])
        for i in range(2):
            S.op("dve", lambda e, i=i: e.reduce_sum(out=mS[i][0], in_=ident_f[:, 64 * i:64 * i + 64], axis=AX.X), reads=[ident_f_b], writes=[mS[i][1]])
            S.op("dve", lambda e, i=i: e.tensor_scalar(out=mSn[i][0], in0=mS[i][0], scalar1=-1.0, scalar2=None, op0=ALU.mult), reads=[mS[i][1]], writes=[mSn[i][1]])
            S.op("dve", lambda e, i=i: e.reduce_sum(out=mF[i][0], in_=ident_f.rearrange("p (a b c) -> p a b c", a=4, b=2)[:, :, i, :], axis=AX.XY),
                 reads=[ident_f_b], writes=[mF[i][1]])
        for d in range(2):
            ar.mark()
            Ft, Ft_b = ar.alloc([5, 512], F32, "Ft")
            S.op("sp", lambda e, d=d: e.dma_start(out=Ft, in_=s5F[d].rearrange("p a f q -> p a (f q)")), writes=[Ft_b], dma=True)
            pw = self.cpow_tables(ar, Ft[:, 0, :], Ft[:, 1, :], Ft[:, 2, :], 512, [Ft_b], f"F{d}")
            TB = pw["buf"]
            bcr, bci = self.bc_coef(ar, pw, Ft[:, 0, :], Ft[:, 1, :], 512, [Ft_b])
            t1, _ = ar.alloc([512], F32, "ft1")
            t2, _ = ar.alloc([512], F32, "ft2")
            bbr, _ = ar.alloc([512], F32, "bbr")
            bbi, _ = ar.alloc([512], F32, "bbi")
            self.cmul("dve", bbr, bbi, bcr, bci, Ft[:, 3, :], Ft[:, 4, :], t1, t2, [Ft_b, TB], [TB])
            wr_, _ = ar.alloc([512], F32, "wr_")
            wi_, _ = ar.alloc([512], F32, "wi_")
            SIt, SIt_b = ar.alloc([8, 8, 2, 2, 64], BF16, "SIt")
            for j in range(8):
                kj = 7 - j if d == 0 else j
                self.cmul("dve", wr_, wi_, pw["are"][:, kj, :], pw["aim"][:, kj, :], bbr, bbi, t1, t2, [TB], [TB])
                for ri, w_ in enumerate((wr_, wi_)):
                    for gq in range(2):
                        S.op("dve", lambda e, j=j, ri=ri, gq=gq, w_=w_: e.tensor_scalar(
                            out=SIt[:, :, j, ri, gq, :], in0=w_.rearrange("p (f q) -> p f q", f=8), scalar1=mF[gq][0][:, 0:1], scalar2=None, op0=ALU.mult),
                            reads=[TB, mF[gq][1]], writes=[SIt_b])
            S.op("sp", lambda e, d=d: e.dma_start(out=self.SI_s[d].rearrange("f p n -> p f n"), in_=SIt.rearrange("p f j r g q -> p f (j r g q)")),
                 reads=[SIt_b], writes=[self.SI_sb], dma=True)
            S.barrier()
            ar.release()
            ar.mark()
            NS = 3 * 32 + 4 * 512
            St, St_b = ar.alloc([NS], F32, "St")
            S.op("sp", lambda e, d=d: e.dma_start(out=St, in_=s5S[d]), writes=[St_b], dma=True)
            lre, lim, ldt = St[:, 0:32], St[:, 32:64], St[:, 64:96]
            Bre = St[:, 96:96 + 512].rearrange("p (g h) -> p g h", h=16)
            Bim = St[:, 96 + 512:96 + 1024].rearrange("p (g h) -> p g h", h=16)
            Cre = St[:, 96 + 1024:96 + 1536].rearrange("p (g h) -> p g h", h=16)
            Cim = St[:, 96 + 1536:96 + 2048].rearrange("p (g h) -> p g h", h=16)
            pw = self.cpow_tables(ar, lre, lim, ldt, 32, [St_b], f"S{d}")
            TB = pw["buf"]
            bcr, bci = self.bc_coef(ar, pw, lre, lim, 32, [St_b])
            bc3 = lambda a: a.unsqueeze(2).broadcast_to([128, 32, 16])
            t1, _ = ar.alloc([32, 16], F32, "st1")
            t2, _ = ar.alloc([32, 16], F32, "st2")
            bbr, _ = ar.alloc([32, 16], F32, "sbbr")
            bbi, _ = ar.alloc([32, 16], F32, "sbbi")
            self.cmul("dve", bbr, bbi, bc3(bcr), bc3(bci), Bre, Bim, t1, t2, [St_b, TB], [TB])
            S.op("act", lambda e, d=d: e.activation(out=self.RS[:, d, 0, :], in_=pw["lr"], func=AF.Exp, scale=8.0), reads=[TB], writes=[self.RS_b])
            p8, p8_b = ar.alloc([32], F32, "p8")
            p8i, p8i_b = ar.alloc([32], I32, "p8i")
            p8f, p8f_b = ar.alloc([32], F32, "p8f")
            S.op("dve", lambda e: e.tensor_scalar(out=p8, in0=pw["th"], scalar1=8.0, scalar2=64.0, op0=ALU.mult, op1=ALU.add), reads=[TB], writes=[p8_b])
            S.op("dve", lambda e: e.tensor_copy(out=p8i, in_=p8), reads=[p8_b], writes=[p8i_b])
            S.op("dve", lambda e: e.tensor_copy(out=p8f, in_=p8i), reads=[p8i_b], writes=[p8f_b])
            S.op("dve", lambda e, d=d: e.tensor_tensor(out=self.RS[:, d, 1, :], in0=p8, in1=p8f, op=ALU.subtract), reads=[p8_b, p8f_b], writes=[self.RS_b])
            vr_, _ = ar.alloc([32, 16], F32, "vr_")
            vi_, _ = ar.alloc([32, 16], F32, "vi_")
            ROt, ROt_b = ar.alloc([32, 8, 2, 2, 16], BF16, "ROt")
            for j in range(8):
                kj = j + 1 if d == 0 else 8 - j
                self.cmul("dve", vr_, vi_, Cre, Cim, bc3(pw["are"][:, kj, :]), bc3(pw["aim"][:, kj, :]), t1, t2, [St_b, TB], [TB])
                for ri, (v_, ms) in enumerate(((vr_, mS), (vi_, mSn))):
                    for gq in range(2):
                        S.op("dve", lambda e, j=j, ri=ri, gq=gq, v_=v_, ms=ms: e.tensor_scalar(
                            out=ROt[:, :, j, ri, gq, :], in0=v_, scalar1=ms[gq][0][:, 0:1], scalar2=None, op0=ALU.mult),
                            reads=[TB, ms[gq][1]], writes=[ROt_b])
            S.op("sp", lambda e, d=d: e.dma_start(out=self.RO_s[d].rearrange("f p n -> p f n"),
                                                in_=ROt.rearrange("p (f g) j r q h -> p f (g j r q h)", f=8)),
                 reads=[ROt_b], writes=[self.RO_sb], dma=True)
            W2p = [ar.alloc([8, 4, 8, 16], BF16, f"W2p{ri}") for ri in range(2)]
            W1p = [[ar.alloc([8, 4, 8, 16], BF16, f"W1p{b}{ri}") for ri in range(2)] for b in range(2)]
            for t_, tb_ in W2p + W1p[0] + W1p[1]:
                S.op("dve", lambda e, t_=t_: e.memset(t_, 0.0), writes=[tb_])
            g4 = lambda a: a.rearrange("p (f g) h -> p f g h", g=4)
            for ri, (c_, ms) in enumerate(((Cre, mS), (Cim, mSn))):
                for gp4 in range(4):
                    for gq in range(2):
                        S.op("dve", lambda e, ri=ri, gp4=gp4, gq=gq, c_=c_, ms=ms: e.tensor_scalar(
                            out=W2p[ri][0][:, :, gp4, 2 * gp4 + gq, :], in0=g4(c_)[:, :, gp4, :], scalar1=ms[gq][0][:, 0:1], scalar2=None, op0=ALU.mult),
                            reads=[St_b, ms[gq][1]], writes=[W2p[ri][1]])
            FIRt, FIRt_b = ar.alloc([8, 8, 128], BF16, "FIRt")
            w1r, _ = ar.alloc([32, 16], F32, "w1r")
            w1i, _ = ar.alloc([32, 16], F32, "w1i")
            cnt = 0
            for tau in range(8):
                self.cmul("dve", w1r, w1i, bc3(pw["are"][:, tau, :]), bc3(pw["aim"][:, tau, :]), bbr, bbi, t1, t2, [TB], [TB])
                W1 = W1p[tau % 2]
                for ri, w_ in enumerate((w1r, w1i)):
                    for gp4 in range(4):
                        for gq in range(2):
                            S.op("dve", lambda e, ri=ri, gp4=gp4, gq=gq, w_=w_, W1=W1: e.tensor_scalar(
                                out=W1[ri][0][:, :, gp4, 2 * gp4 + gq, :], in0=g4(w_)[:, :, gp4, :], scalar1=mS[gq][0][:, 0:1], scalar2=None, op0=ALU.mult),
                                reads=[TB, mS[gq][1]], writes=[W1[ri][1]])
                for fc in range(8):
                    pa, pb = self.pq(cnt % 8, 0)
                    cnt += 1

                    def mm(e, pa=pa, fc=fc, W1=W1):
                        ins = None
                        k = 0
                        for ri in range(2):
                            for gp4 in range(4):
                                ins = e.matmul(pa, lhsT=W1[ri][0][:, fc, gp4, :, :].rearrange("p a b -> p (a b)"),
                                               rhs=W2p[ri][0][:, fc, gp4, :, :].rearrange("p a b -> p (a b)"), start=(k == 0), stop=(k == 7))
                                k += 1
                        return ins
                    S.op("pe", mm, reads=[W1[0][1], W1[1][1], W2p[0][1], W2p[1][1]], writes=pb)
                    eng = "act" if cnt % 2 == 0 else "dve"
                    if eng == "act":
                        S.op("act", lambda e, pa=pa, fc=fc, tau=tau: e.activation(out=FIRt[:, fc, tau, :], in_=pa, func=AF.Copy), reads=pb, writes=[FIRt_b])
                    else:
                        S.op("dve", lambda e, pa=pa, fc=fc, tau=tau: e.tensor_copy(out=FIRt[:, fc, tau, :], in_=pa), reads=pb, writes=[FIRt_b])
            S.op("sp", lambda e, d=d: e.dma_start(out=self.FIR_s[d].rearrange("f p n -> p f n"), in_=FIRt.rearrange("p f t n -> p f (t n)")),
                 reads=[FIRt_b], writes=[self.FIR_sb], dma=True)
            S.barrier()
            ar.release()
        if dbg:
            o, ob = self.scratch("dbg_RS", [128, 128], F32, dump=True)
            S.op("sp", lambda e: e.dma_start(out=o, in_=self.RS.rearrange("p a b c -> p (a b c)")), reads=[self.RS_b], writes=[ob], dma=True)


    def phase3_main(self, s5_d):
        nc, S, ar = self.nc, self.S, self.ar
        ps = self.ps
        dbg = self.debug
        self.g_s, self.g_sb = self.scratch("g_s", [D, L], BF16, dump=dbg)
        RS, RS_b = self.RS, self.RS_b
        NCH = 544
        ar.mark()
        dcol, dcol_b = ar.alloc([8], F32, "s5d")
        S.op("sp", lambda e: e.dma_start(out=dcol, in_=s5_d), writes=[dcol_b], dma=True)
        iot_i, iot_ib = ar.alloc([NCH], I32, "iota_i")
        iot, iot_b = ar.alloc([NCH], F32, "iota_f")
        S.op("pool", lambda e: e.iota(iot_i, pattern=[[1, NCH]], base=0, channel_multiplier=0), writes=[iot_ib])
        S.op("dve", lambda e: e.tensor_copy(out=iot, in_=iot_i), reads=[iot_ib], writes=[iot_b])
        u_t, u_b = ar.alloc([T], BF16, "u_fc")
        um, um_b = ar.alloc([4, T], BF16, "um")
        SI_t, SI_b = ar.alloc([2, 2048], BF16, "SI_t")
        RO_t, RO_b = ar.alloc([2, 2048], BF16, "RO_t")
        FIR_t, FIR_b = ar.alloc([2, 1024], BF16, "FIR_t")
        cn, cn_b = ar.alloc([4, NCH], F32, "cn")
        sn, sn_b = ar.alloc([4, NCH], F32, "sn")
        pht, pht_b = ar.alloc([4, NCH], F32, "pht")
        phi_, phi_b = ar.alloc([4, NCH], I32, "phi_")
        self._s5_frac, self._s5_frac_b = ar.alloc([4, NCH], F32, "s5frac")
        Ssb = [[ar.alloc([NCH], F32, f"Ssb{k}{ri}") for ri in range(2)] for k in range(2)]
        V = [ar.alloc([NCH], F32, f"V{ri}") for ri in range(2)]
        W = [ar.alloc([NCH], F32, f"W{ri}") for ri in range(2)]
        tmp = [ar.alloc([NCH], F32, f"s5t{i}") for i in range(4)]
        Zp = [[[ar.alloc([512], BF16, f"Zp{d}{g}{ri}") for ri in range(2)] for g in range(4)] for d in range(2)]
        gst = [ar.alloc([L], BF16, f"gst{i}") for i in range(2)]
        ys = [ar.alloc([2, 256], F32, f"ys{i}") for i in range(2)]
        kset = 0
        for fc in range(8):
            S.op("sp", lambda e, fc=fc: e.dma_start(out=u_t, in_=self.s5u[fc * 128:(fc + 1) * 128, :]), reads=[self.s5u_b], writes=[u_b], dma=True)
            S.op("sp", lambda e, fc=fc: e.dma_start(out=SI_t, in_=self.SI_s[:, fc].rearrange("d p n -> p d n")), reads=[self.SI_sb], writes=[SI_b], dma=True)
            S.op("sp", lambda e, fc=fc: e.dma_start(out=RO_t, in_=self.RO_s[:, fc].rearrange("d p n -> p d n")), reads=[self.RO_sb], writes=[RO_b], dma=True)
            S.op("sp", lambda e, fc=fc: e.dma_start(out=FIR_t, in_=self.FIR_s[:, fc].rearrange("d p n -> p d n")), reads=[self.FIR_sb], writes=[FIR_b], dma=True)
            for i in range(4):
                S.op("act", lambda e, i=i: e.activation(out=um[:, i, :], in_=u_t, func=AF.Copy, scale=self.rowmask[i][0][:, 0:1]),
                     reads=[u_b, self.rowmask[i][1]], writes=[um_b])
            for d in range(2):
                S.op("dve", lambda e, d=d, fc=fc: e.tensor_tensor(
                    out=pht, in0=iot.unsqueeze(1).broadcast_to([128, 4, NCH]),
                    in1=RS[:, d, 1, 4 * fc:4 * fc + 4].unsqueeze(2).broadcast_to([128, 4, NCH]), op=ALU.mult),
                    reads=[iot_b, RS_b], writes=[pht_b])
                for (dst, dst_b, add) in ((cn, cn_b, 0.25), (sn, sn_b, 0.0)):
                    S.op("dve", lambda e, dst=dst, add=add: e.tensor_scalar(out=dst, in0=pht, scalar1=64.0 + add, scalar2=None, op0=ALU.add),
                         reads=[pht_b], writes=[dst_b])
                    S.op("dve", lambda e, dst=dst: e.tensor_copy(out=phi_, in_=dst), reads=[dst_b], writes=[phi_b])
                    S.op("dve", lambda e, dst=dst: e.tensor_copy(out=self._s5_frac, in_=phi_), reads=[phi_b], writes=[self._s5_frac_b])
                    S.op("dve", lambda e, dst=dst: e.tensor_tensor(out=dst, in0=dst, in1=self._s5_frac, op=ALU.subtract),
                         reads=[dst_b, self._s5_frac_b], writes=[dst_b])
                    S.op("act", lambda e, dst=dst: e.activation(out=dst, in_=dst, func=AF.Sin, scale=2.0 * np.pi), reads=[dst_b], writes=[dst_b])
                for gp4 in range(4):
                    gp = 4 * fc + gp4
                    b0 = 3 * (kset % 2)
                    kset += 1
                    bufs3 = [self.psb[b0], self.psb[b0 + 1], self.psb[b0 + 2]]

                    def mm(e, d=d, gp4=gp4, b0=b0):
                        ins = None
                        for ri in range(2):
                            for j in range(8):
                                ins = e.matmul(ps[b0 + ri][:, 0:512], lhsT=SI_t[:, d, (j * 2 + ri) * 128:(j * 2 + ri + 1) * 128],
                                               rhs=um[:, gp4, LC + j:T:8], start=(j == 0), stop=(j == 7))
                        for ri in range(2):
                            for j in range(8):
                                ins = e.matmul(ps[b0 + 2][:, ri * 32:(ri + 1) * 32], lhsT=SI_t[:, d, (j * 2 + ri) * 128:(j * 2 + ri + 1) * 128],
                                               rhs=um[:, gp4, j:LC:8], start=(j == 0), stop=(j == 7))
                        return ins
                    S.op("pe", mm, reads=[SI_b, um_b], writes=bufs3)
                    Sk = Ssb[kset % 2]
                    for ri in range(2):
                        s_t, s_b = Sk[ri]
                        src_c = ps[b0 + 2][:, ri * 32:(ri + 1) * 32]
                        src_l = ps[b0 + ri][:, 0:512]
                        if d == 1:
                            src_c = src_c[:, ::-1]
                            src_l = src_l[:, ::-1]
                        S.op("act", lambda e, s_t=s_t, src_c=src_c: e.activation(out=s_t[:, 0:32], in_=src_c, func=AF.Copy),
                             reads=[self.psb[b0 + 2]], writes=[s_b])
                        S.op("act", lambda e, s_t=s_t, src_l=src_l: e.activation(out=s_t[:, 32:NCH], in_=src_l, func=AF.Copy),
                             reads=[self.psb[b0 + ri]], writes=[s_b])
                    (Sr, Sr_b), (Si, Si_b) = Sk
                    cnv, snv = cn[:, gp4, :], sn[:, gp4, :]
                    (t1, t1b), (t2, t2b), (t3, t3b), (t4, t4b) = tmp
                    (Vr, Vr_b), (Vi, Vi_b) = V
                    (Wr, Wr_b), (Wi, Wi_b) = W
                    S.op("dve", lambda e, cnv=cnv, Sr=Sr: e.tensor_tensor(out=t1, in0=cnv, in1=Sr, op=ALU.mult), reads=[cn_b, Sr_b], writes=[t1b])
                    S.op("dve", lambda e, snv=snv, Si=Si: e.tensor_tensor(out=t2, in0=snv, in1=Si, op=ALU.mult), reads=[sn_b, Si_b], writes=[t2b])
                    S.op("dve", lambda e, cnv=cnv, Si=Si: e.tensor_tensor(out=t3, in0=cnv, in1=Si, op=ALU.mult), reads=[cn_b, Si_b], writes=[t3b])
                    S.op("dve", lambda e, snv=snv, Sr=Sr: e.tensor_tensor(out=t4, in0=snv, in1=Sr, op=ALU.mult), reads=[sn_b, Sr_b], writes=[t4b])
                    S.op("dve", lambda e: e.tensor_tensor(out=Vr, in0=t1, in1=t2, op=ALU.add), reads=[t1b, t2b], writes=[Vr_b])
                    S.op("dve", lambda e: e.tensor_tensor(out=Vi, in0=t3, in1=t4, op=ALU.subtract), reads=[t3b, t4b], writes=[Vi_b])
                    Rb = RS[:, d, 0, gp:gp + 1].broadcast_to([128, NCH])
                    S.op("dve", lambda e, Rb=Rb: e.tensor_tensor_scan(out=Wr, data0=Rb, data1=Vr, initial=0.0, op0=ALU.mult, op1=ALU.add),
                         reads=[RS_b, Vr_b], writes=[Wr_b])
                    S.op("dve", lambda e, Rb=Rb: e.tensor_tensor_scan(out=Wi, data0=Rb, data1=Vi, initial=0.0, op0=ALU.mult, op1=ALU.add),
                         reads=[RS_b, Vi_b], writes=[Wi_b])
                    sl = slice(31, 543)
                    (zr, zr_b), (zi, zi_b) = Zp[d][gp4]
                    zro = zr if d == 0 else zr[:, ::-1]
                    zio = zi if d == 0 else zi[:, ::-1]
                    S.op("dve", lambda e, cnv=cnv: e.tensor_tensor(out=t1[:, sl], in0=cnv[:, sl], in1=Wr[:, sl], op=ALU.mult), reads=[cn_b, Wr_b], writes=[t1b])
                    S.op("dve", lambda e, snv=snv: e.tensor_tensor(out=t2[:, sl], in0=snv[:, sl], in1=Wi[:, sl], op=ALU.mult), reads=[sn_b, Wi_b], writes=[t2b])
                    S.op("dve", lambda e, cnv=cnv: e.tensor_tensor(out=t3[:, sl], in0=cnv[:, sl], in1=Wi[:, sl], op=ALU.mult), reads=[cn_b, Wi_b], writes=[t3b])
                    S.op("dve", lambda e, snv=snv: e.tensor_tensor(out=t4[:, sl], in0=snv[:, sl], in1=Wr[:, sl], op=ALU.mult), reads=[sn_b, Wr_b], writes=[t4b])
                    S.op("dve", lambda e, zro=zro: e.tensor_tensor(out=zro, in0=t1[:, sl], in1=t2[:, sl], op=ALU.subtract), reads=[t1b, t2b], writes=[zr_b])
                    S.op("dve", lambda e, zio=zio: e.tensor_tensor(out=zio, in0=t3[:, sl], in1=t4[:, sl], op=ALU.add), reads=[t3b, t4b], writes=[zi_b])
            g_t, g_b = gst[fc % 2]
            for hb in range(2):
                c0 = 256 * hb
                for j in range(8):
                    bank = 4 + j // 2
                    reg = ps[bank][:, (j % 2) * 256:(j % 2) * 256 + 256]

                    def mmy(e, j=j, reg=reg, c0=c0):
                        ops = []
                        for tau in range(0, j + 1):
                            st0 = LC + 8 * c0 + (j - tau)
                            ops.append((reg, FIR_t[:, 0, tau * 128:(tau + 1) * 128], u_t[:, st0:st0 + 2041:8], None))
                        for tau in range(0, 8 - j):
                            st0 = LC + 8 * c0 + (j + tau)
                            ops.append((reg, FIR_t[:, 1, tau * 128:(tau + 1) * 128], u_t[:, st0:st0 + 2041:8], None))
                        for d in range(2):
                            for gp4 in range(4):
                                for ri in range(2):
                                    o0 = ((gp4 * 8 + j) * 2 + ri) * 32
                                    ops.append((reg[32 * gp4:32 * gp4 + 32, :], RO_t[:, d, o0:o0 + 32], Zp[d][gp4][ri][0][:, c0:c0 + 256], (0, 32 * gp4)))
                        ins = None
                        for k, (o_, l_, r_, tp) in enumerate(ops):
                            if tp is None:
                                ins = e.matmul(o_, lhsT=l_, rhs=r_, start=(k == 0), stop=(k == len(ops) - 1))
                            else:
                                ins = e.matmul(o_, lhsT=l_, rhs=r_, start=(k == 0), stop=(k == len(ops) - 1), tile_position=tp)
                        return ins
                    rd = [FIR_b, RO_b, u_b] + [Zp[d][g][ri][1] for d in range(2) for g in range(4) for ri in range(2)]
                    S.op("pe", mmy, reads=rd, writes=[self.psb[bank]])
                    if j % 2 == 1:
                        j0 = j - 1
                        y_t, y_b = ys[(j // 2) % 2]
                        base = LC + 8 * c0
                        uv = u_t[:, base:base + 2048].rearrange("p (c j) -> p j c", j=8)[:, j0:j0 + 2, :]
                        S.op("dve", lambda e, y_t=y_t, uv=uv, bank=bank, fc=fc: e.scalar_tensor_tensor(
                            out=y_t, in0=uv, scalar=dcol[:, fc:fc + 1], in1=ps[bank][:].rearrange("p (j c) -> p j c", j=2),
                            op0=ALU.mult, op1=ALU.add), reads=[u_b, dcol_b, self.psb[bank]], writes=[y_b])
                        outv = g_t.rearrange("p (c8 j row) -> p j row c8", c8=8, j=8)[:, j0:j0 + 2, 32 * hb:32 * hb + 32, :]
                        S.op("act", lambda e, y_t=y_t, outv=outv: e.activation(
                            out=outv, in_=y_t.rearrange("p j (r c8) -> p j r c8", c8=8), func=AF.Gelu_apprx_tanh),
                            reads=[y_b], writes=[g_b])
            S.op("sp", lambda e, fc=fc, g_t=g_t: e.dma_start(out=self.g_s[fc * 128:(fc + 1) * 128, :], in_=g_t), reads=[g_b], writes=[self.g_sb], dma=True)
        S.barrier()
        ar.release()


    def phase4(self, glu_w, glu_b, w_a, w_b, w_o, x_tok, nfw, r_w, r_b):
        nc, S, ar = self.nc, self.S, self.ar
        ps, psb = self.ps, self.psb
        dbg = self.debug
        ident_f, ident_f_b = self.consts["ident_f"]
        self.h1_s, self.h1_sb = self.scratch("h1_s", [L, D], F32, dump=dbg)
        self.uf_s, self.uf_sb = self.scratch("uf_s", [L, D], BF16, dump=dbg)
        self.logits, self.logits_b = ar.alloc([32, 32], F32, "logits")
        ar.mark()
        wv = lambda w: w.rearrange("(kc p) n -> p kc n", p=128)
        Wg, Wg_b = ar.alloc([8, D], BF16, "Wglu")
        Wa, Wa_b = ar.alloc([8, D], BF16, "Wa")
        Wb, Wb_b = ar.alloc([16, D], BF16, "Wb")
        Wo, Wo_b = ar.alloc([8, D], BF16, "Wo")
        for (t_, tb_, src, nk) in ((Wg, Wg_b, glu_w, 8), (Wa, Wa_b, w_a, 8), (Wb, Wb_b, w_b, 16), (Wo, Wo_b, w_o, 8)):
            for k0 in range(0, nk, 4):
                S.op("pool", lambda e, t_=t_, src=src, k0=k0: e.dma_start(out=t_[:, k0:k0 + 4, :], in_=wv(src)[:, k0:k0 + 4, :]), writes=[tb_], dma=True)
        Wr, Wr_b = ar.alloc([8, 32], F32, "Wr")
        S.op("sp", lambda e: e.dma_start(out=Wr, in_=r_w.rearrange("(kc p) n -> p kc n", p=128)), writes=[Wr_b], dma=True)
        rb_rep, rb_rep_b = ar.alloc([32], F32, "rb_rep")
        S.op("sp", lambda e: e.dma_start(out=rb_rep, in_=r_b.broadcast_to([128, 32])), writes=[rb_rep_b], dma=True)
        gb_col, gb_col_b = ar.alloc([8], F32, "glu_b")
        S.op("sp", lambda e: e.dma_start(out=gb_col, in_=glu_b), writes=[gb_col_b], dma=True)
        gm_rep, gm_rep_b = ar.alloc([D], F32, "gm_rep")
        Af_rep, Af_rep_b = ar.alloc([D], F32, "Af_rep")
        Bf_rep, Bf_rep_b = ar.alloc([D], F32, "Bf_rep")
        S.op("sp", lambda e: e.dma_start(out=gm_rep, in_=self.mod_s[:, 2 * D:3 * D]), reads=[self.mod_sb], writes=[gm_rep_b], dma=True)
        S.op("sp", lambda e: e.dma_start(out=Bf_rep, in_=self.mod_s[:, 3 * D:4 * D]), reads=[self.mod_sb], writes=[Bf_rep_b], dma=True)
        S.op("sp", lambda e: e.dma_start(out=Af_rep, in_=self.mod_s[:, 4 * D:5 * D]), reads=[self.mod_sb], writes=[Af_rep_b], dma=True)
        nf_rep, nf_rep_b = ar.alloc([D], F32, "nf_rep")
        S.op("sp", lambda e: e.dma_start(out=nf_rep, in_=nfw.broadcast_to([128, D])), writes=[nf_rep_b], dma=True)
        S.op("dve", lambda e: e.scalar_tensor_tensor(out=Af_rep, in0=Af_rep, scalar=1.0, in1=nf_rep, op0=ALU.add, op1=ALU.mult),
             reads=[Af_rep_b, nf_rep_b], writes=[Af_rep_b])
        g_t, g_b = ar.alloc([8, 512], BF16, "m_g")
        yb_t, yb_b = ar.alloc([16, 512], BF16, "m_yb")
        gab_t, gab_b = ar.alloc([16, 512], BF16, "m_gab")
        ya_t, ya_b = ar.alloc([8, 512], BF16, "m_ya")
        m1_t, m1_b = ar.alloc([8, 512], F32, "m_m1")
        mg_t, mg_b = ar.alloc([8, 512], BF16, "m_mg")
        sg = [ar.alloc([512], F32, f"m_sg{i}") for i in range(2)]
        xk = [ar.alloc([D], F32, f"m_xk{i}") for i in range(1)]
        h1 = [ar.alloc([D], F32, f"m_h1{i}") for i in range(2)]
        uft = [ar.alloc([D], F32, f"m_uft{i}") for i in range(1)]
        ufb = [ar.alloc([D], BF16, f"m_ufb{i}") for i in range(2)]
        ufT = [ar.alloc([8, 128], F32, f"m_ufT{i}") for i in range(1)]
        sq_junk, sq_junk_b = ar.alloc([D], BF16, "m_sqj")
        ss = [ar.alloc([1], F32, f"m_ss{i}") for i in range(2)]
        pc = [0]

        def bank():
            b = pc[0] % 8
            pc[0] += 1
            return b
        for tt in range(8):
            cs = slice(tt * 512, (tt + 1) * 512)
            S.op("sp", lambda e, cs=cs: e.dma_start(out=g_t, in_=self.g_s.rearrange("(kc p) n -> p kc n", p=128)[:, :, cs]), reads=[self.g_sb], writes=[g_b], dma=True)
            S.op("sp", lambda e, cs=cs: e.dma_start(out=yb_t, in_=self.yb_s.rearrange("(kc p) n -> p kc n", p=128)[:, :, cs]), reads=[self.yb_sb], writes=[yb_b], dma=True)
            S.op("sp", lambda e, cs=cs: e.dma_start(out=gab_t, in_=self.gab_s.rearrange("(kc p) n -> p kc n", p=128)[:, :, cs]), reads=[self.gab_sb], writes=[gab_b], dma=True)
            for mc in range(8):
                b_ = bank()

                def mm(e, b_=b_, mc=mc):
                    ins = None
                    for kc in range(8):
                        ins = e.matmul(ps[b_][:], lhsT=Wg[:, kc, mc * 128:(mc + 1) * 128], rhs=g_t[:, kc, :], start=(kc == 0), stop=(kc == 7))
                    return ins
                S.op("pe", mm, reads=[Wg_b, g_b], writes=[psb[b_]])
                s_t, s_b = sg[mc % 2]
                S.op("act", lambda e, s_t=s_t, b_=b_, mc=mc: e.activation(out=s_t, in_=ps[b_][:], func=AF.Sigmoid, bias=gb_col[:, mc:mc + 1]),
                     reads=[psb[b_], gb_col_b], writes=[s_b])
                S.op("dve", lambda e, s_t=s_t, mc=mc: e.tensor_tensor(out=ya_t[:, mc, :], in0=s_t, in1=g_t[:, mc, :], op=ALU.mult),
                     reads=[s_b, g_b], writes=[ya_b])
            for mc in range(8):
                b_ = bank()

                def mm(e, b_=b_, mc=mc):
                    ins = None
                    for kc in range(8):
                        ins = e.matmul(ps[b_][:], lhsT=Wa[:, kc, mc * 128:(mc + 1) * 128], rhs=ya_t[:, kc, :], start=(kc == 0), stop=(kc == 7))
                    return ins
                S.op("pe", mm, reads=[Wa_b, ya_b], writes=[psb[b_]])
                s_t, s_b = sg[mc % 2]
                S.op("act", lambda e, s_t=s_t, mc=mc: e.activation(out=s_t, in_=gab_t[:, mc, :], func=AF.Sigmoid), reads=[gab_b], writes=[s_b])
                S.op("dve", lambda e, s_t=s_t, b_=b_, mc=mc: e.tensor_tensor(out=m1_t[:, mc, :], in0=s_t, in1=ps[b_][:], op=ALU.mult),
                     reads=[s_b, psb[b_]], writes=[m1_b])
            for mc in range(8):
                b_ = bank()

                def mm(e, b_=b_, mc=mc):
                    ins = None
                    for kc in range(16):
                        ins = e.matmul(ps[b_][:], lhsT=Wb[:, kc, mc * 128:(mc + 1) * 128], rhs=yb_t[:, kc, :], start=(kc == 0), stop=(kc == 15))
                    return ins
                S.op("pe", mm, reads=[Wb_b, yb_b], writes=[psb[b_]])
                s_t, s_b = sg[mc % 2]
                S.op("act", lambda e, s_t=s_t, mc=mc: e.activation(out=s_t, in_=gab_t[:, 8 + mc, :], func=AF.Sigmoid), reads=[gab_b], writes=[s_b])
                S.op("dve", lambda e, s_t=s_t, b_=b_: e.tensor_tensor(out=s_t, in0=s_t, in1=ps[b_][:], op=ALU.mult), reads=[s_b, psb[b_]], writes=[s_b])
                S.op("dve", lambda e, s_t=s_t, mc=mc: e.tensor_tensor(out=mg_t[:, mc, :], in0=s_t, in1=m1_t[:, mc, :], op=ALU.add),
                     reads=[s_b, m1_b], writes=[mg_b])
            for sub in range(4):
                ti = tt * 4 + sub
                x_t, x_b = xk[0]
                h_t, h_b = h1[ti % 2]
                S.op("sp", lambda e, x_t=x_t, ti=ti: e.dma_start(out=x_t, in_=x_tok[ti * 128:(ti + 1) * 128, :]), writes=[x_b], dma=True)
                for half in range(2):
                    b_ = bank()

                    def mm(e, b_=b_, sub=sub, half=half):
                        ins = None
                        for kc in range(8):
                            ins = e.matmul(ps[b_][:], lhsT=mg_t[:, kc, sub * 128:(sub + 1) * 128], rhs=Wo[:, kc, half * 512:(half + 1) * 512],
                                           start=(kc == 0), stop=(kc == 7))
                        return ins
                    S.op("pe", mm, reads=[mg_b, Wo_b], writes=[psb[b_]])
                    hs = slice(half * 512, (half + 1) * 512)
                    S.op("dve", lambda e, h_t=h_t, b_=b_, hs=hs: e.tensor_tensor(out=h_t[:, hs], in0=ps[b_][:], in1=gm_rep[:, hs], op=ALU.mult),
                         reads=[psb[b_], gm_rep_b], writes=[h_b])
                    S.op("dve", lambda e, h_t=h_t, x_t=x_t, hs=hs: e.tensor_tensor(out=h_t[:, hs], in0=h_t[:, hs], in1=x_t[:, hs], op=ALU.add),
                         reads=[h_b, x_b], writes=[h_b])
                S.op("sp", lambda e, h_t=h_t, ti=ti: e.dma_start(out=self.h1_s[ti * 128:(ti + 1) * 128, :], in_=h_t), reads=[h_b], writes=[self.h1_sb], dma=True)
                s_t, s_b = ss[ti % 2]
                S.op("act", lambda e, h_t=h_t, s_t=s_t: e.activation(out=sq_junk, in_=h_t, func=AF.Square, accum_out=s_t), reads=[h_b], writes=[sq_junk_b, s_b])
                S.op("act", lambda e, s_t=s_t: e.activation(out=s_t, in_=s_t, func=AF.Sqrt, scale=1.0 / D, bias=EPS), reads=[s_b], writes=[s_b])
                S.op("dve", lambda e, s_t=s_t: e.reciprocal(out=s_t, in_=s_t), reads=[s_b], writes=[s_b])
                u_t, u_b = uft[0]
                ub_t, ub_b = ufb[ti % 2]
                S.op("dve", lambda e, u_t=u_t, h_t=h_t, s_t=s_t: e.scalar_tensor_tensor(out=u_t, in0=h_t, scalar=s_t[:, 0:1], in1=Af_rep, op0=ALU.mult, op1=ALU.mult),
                     reads=[h_b, s_b, Af_rep_b], writes=[u_b])
                S.op("dve", lambda e, u_t=u_t: e.tensor_tensor(out=u_t, in0=u_t, in1=Bf_rep, op=ALU.add), reads=[u_b, Bf_rep_b], writes=[u_b])
                S.op("act", lambda e, u_t=u_t, ub_t=ub_t: e.activation(out=ub_t, in_=u_t, func=AF.Copy), reads=[u_b], writes=[ub_b])
                S.op("sp", lambda e, ub_t=ub_t, ti=ti: e.dma_start(out=self.uf_s[ti * 128:(ti + 1) * 128, :], in_=ub_t), reads=[ub_b], writes=[self.uf_sb], dma=True)
                T_t, T_b = ufT[0]
                for half in range(2):
                    b_ = bank()

                    def tr(e, b_=b_, u_t=u_t, half=half):
                        ins = None
                        for q in range(4):
                            kc = half * 4 + q
                            ins = e.transpose(out=ps[b_][:, q * 128:(q + 1) * 128], in_=u_t[:, kc * 128:(kc + 1) * 128], identity=ident_f)
                        return ins
                    S.op("pe", tr, reads=[u_b, ident_f_b], writes=[psb[b_]])
                    S.op("act", lambda e, T_t=T_t, b_=b_, half=half: e.activation(
                        out=T_t[:, half * 4:half * 4 + 4, :].rearrange("p a b -> p (a b)"), in_=ps[b_][:], func=AF.Copy), reads=[psb[b_]], writes=[T_b])
                b_ = bank()

                def mml(e, b_=b_, T_t=T_t):
                    ins = None
                    for kc in range(8):
                        ins = e.matmul(ps[b_][:, 0:32], lhsT=T_t[:, kc, :], rhs=Wr[:, kc, :], start=(kc == 0), stop=(kc == 7))
                    return ins
                S.op("pe", mml, reads=[T_b, Wr_b], writes=[psb[b_]])
                S.op("dve", lambda e, b_=b_, ti=ti: e.tensor_tensor(out=self.logits[:, ti, :], in0=ps[b_][:, 0:32], in1=rb_rep, op=ALU.add),
                     reads=[psb[b_], rb_rep_b], writes=[self.logits_b])
        if dbg:
            o, ob = self.scratch("dbg_logits", [128, 1024], F32, dump=True)
            S.op("sp", lambda e: e.dma_start(out=o, in_=self.logits.rearrange("p a b -> p (a b)")), reads=[self.logits_b], writes=[ob], dma=True)
        S.barrier()
        ar.release()


    def phase5(self, wg_d, wu_d, wd_d, bg_d, bu_d, bd_d, fnw):
        nc, S, ar = self.nc, self.S, self.ar
        ps, psb = self.ps, self.psb
        dbg = self.debug
        ident_f, ident_f_b = self.consts["ident_f"]
        ones_f, ones_f_b = self.consts["ones_f"]
        ones_bf, ones_bf_b = self.consts["ones_bf"]
        BLK = int(os.environ.get('MOE_BLK', '256'))
        NSUB = BLK // 128
        NT, NE = 32, 32
        NB = -(-(16384 + 32 * (BLK - 1)) // BLK)
        NSLOT = NB * BLK
        self.xs, self.xs_b = self.scratch("xs", [NSLOT, D], BF16)
        self.ys, self.ys_b = self.scratch("ys", [NSLOT, D], BF16)
        logits, logits_b = self.logits, self.logits_b
        ar.mark()
        dest_i, dest_ib = ar.alloc([NT, 4], I32, "dest_i")
        gate4, gate4_b = ar.alloc([NT, 4], F32, "gate4")
        blk_i, blk_ib = ar.alloc([NB], I32, "blk_i")
        chg_i, chg_ib = ar.alloc([NB], I32, "chg_i")
        widx, widx_b = ar.alloc([NB], I32, "widx")
        bidx, bidx_b = ar.alloc([NB], I32, "bidx")
        ident_bf, ident_bf_b = ar.alloc([128], BF16, "ident_bf5")
        S.op("dve", lambda e: e.tensor_copy(out=ident_bf, in_=ident_f), reads=[ident_f_b], writes=[ident_bf_b])
        ar.mark()
        top8, top8_b = ar.alloc([NT, 8], F32, "top8")
        mask, mask_b = ar.alloc([NT, NE], F32, "mask")
        mask_bf, mask_bfb = ar.alloc([NT, NE], BF16, "mask_bf")
        gatef, gatef_b = ar.alloc([NT, NE], F32, "gatef")
        pos, pos_b = ar.alloc([NT, NE], F32, "pos")
        cnta, cnta_b = ar.alloc([NT, NE], F32, "cnta")
        base, base_b = ar.alloc([NT, NE], F32, "base")
        rsum, rsum_b = ar.alloc([NT], F32, "rsum")
        stri, stri_b = ar.alloc([128], BF16, "stri")
        S.op("pool", lambda e: e.affine_select(out=stri, in_=ones_f, pattern=[[1, 128]], compare_op=ALU.is_ge, fill=0.0,
                                              base=-1, channel_multiplier=-1), reads=[ones_f_b], writes=[stri_b])
        for i in range(NT):
            S.op("dve", lambda e, i=i: e.max(out=top8[:, i, :], in_=logits[:, i, :]), reads=[logits_b], writes=[top8_b])
            S.op("dve", lambda e, i=i: e.tensor_scalar(out=mask[:, i, :], in0=logits[:, i, :], scalar1=top8[:, i, 3:4], scalar2=None, op0=ALU.is_ge),
                 reads=[logits_b, top8_b], writes=[mask_b])
        S.op("act", lambda e: e.activation(out=mask_bf, in_=mask, func=AF.Copy), reads=[mask_b], writes=[mask_bfb])
        S.op("dve", lambda e: e.tensor_tensor(out=gatef, in0=logits, in1=top8[:, :, 0:1].broadcast_to([128, NT, NE]), op=ALU.subtract),
             reads=[logits_b, top8_b], writes=[gatef_b])
        S.op("act", lambda e: e.activation(out=gatef, in_=gatef, func=AF.Exp), reads=[gatef_b], writes=[gatef_b])
        S.op("dve", lambda e: e.tensor_tensor(out=gatef, in0=gatef, in1=mask, op=ALU.mult), reads=[gatef_b, mask_b], writes=[gatef_b])
        S.op("dve", lambda e: e.reduce_sum(out=rsum, in_=gatef, axis=AX.X), reads=[gatef_b], writes=[rsum_b])
        S.op("dve", lambda e: e.reciprocal(out=rsum, in_=rsum), reads=[rsum_b], writes=[rsum_b])
        S.op("dve", lambda e: e.tensor_tensor(out=gatef, in0=gatef, in1=rsum.unsqueeze(2).broadcast_to([128, NT, NE]), op=ALU.mult),
             reads=[gatef_b, rsum_b], writes=[gatef_b])
        mflat = mask_bf.rearrange("p a b -> p (a b)")
        for half in range(2):
            b_ = half

            def mm(e, b_=b_, half=half):
                return e.matmul(ps[b_][:], lhsT=ones_bf, rhs=mflat[:, half * 512:(half + 1) * 512], start=True, stop=True)
            S.op("pe", mm, reads=[mask_bfb, ones_bf_b], writes=[psb[b_]])
            S.op("dve", lambda e, b_=b_, half=half: e.tensor_copy(out=cnta.rearrange("p a b -> p (a b)")[:, half * 512:(half + 1) * 512], in_=ps[b_][:]),
                 reads=[psb[b_]], writes=[cnta_b])
            b2 = 2 + half

            def mm2(e, b2=b2, half=half):
                return e.matmul(ps[b2][:], lhsT=stri, rhs=mflat[:, half * 512:(half + 1) * 512], start=True, stop=True)
            S.op("pe", mm2, reads=[mask_bfb, stri_b], writes=[psb[b2]])
            S.op("dve", lambda e, b2=b2, half=half: e.tensor_copy(out=pos.rearrange("p a b -> p (a b)")[:, half * 512:(half + 1) * 512], in_=ps[b2][:]),
                 reads=[psb[b2]], writes=[pos_b])
        for ee in range(NE):
            S.op("dve", lambda e, ee=ee: e.tensor_tensor_scan(out=base[:, :, ee], data0=ones_f[:, 0:NT], data1=cnta[:, :, ee], initial=0.0,
                                                             op0=ALU.mult, op1=ALU.add), reads=[cnta_b, ones_f_b], writes=[base_b])
        tot, tot_b = ar.alloc([NE], F32, "tot")
        S.op("dve", lambda e: e.tensor_copy(out=tot, in_=base[:, NT - 1, :]), reads=[base_b], writes=[tot_b])
        S.op("dve", lambda e: e.tensor_tensor(out=base, in0=base, in1=cnta, op=ALU.subtract), reads=[base_b, cnta_b], writes=[base_b])
        S.op("dve", lambda e: e.tensor_tensor(out=pos, in0=pos, in1=base, op=ALU.add), reads=[pos_b, base_b], writes=[pos_b])
        nb_f, nb_fb = ar.alloc([NE], F32, "nb_f")
        nb_i, nb_ib = ar.alloc([NE], I32, "nb_i")
        pend, pend_b = ar.alloc([NE], F32, "pend")
        pstart, pstart_b = ar.alloc([NE], F32, "pstart")
        S.op("dve", lambda e: e.tensor_scalar(out=nb_f, in0=tot, scalar1=1.0 / BLK, scalar2=(BLK - 1.0) / BLK - 0.5 + 0.25 / BLK, op0=ALU.mult, op1=ALU.add),
             reads=[tot_b], writes=[nb_fb])
        S.op("dve", lambda e: e.tensor_copy(out=nb_i, in_=nb_f), reads=[nb_fb], writes=[nb_ib])
        S.op("dve", lambda e: e.tensor_copy(out=nb_f, in_=nb_i), reads=[nb_ib], writes=[nb_fb])
        S.op("dve", lambda e: e.tensor_scalar(out=nb_f, in0=nb_f, scalar1=float(BLK), scalar2=None, op0=ALU.mult), reads=[nb_fb], writes=[nb_fb])
        S.op("dve", lambda e: e.tensor_tensor_scan(out=pend, data0=ones_f[:, 0:NE], data1=nb_f, initial=0.0, op0=ALU.mult, op1=ALU.add),
             reads=[nb_fb, ones_f_b], writes=[pend_b])
        S.op("dve", lambda e: e.tensor_tensor(out=pstart, in0=pend, in1=nb_f, op=ALU.subtract), reads=[pend_b, nb_fb], writes=[pstart_b])
        S.op("dve", lambda e: e.tensor_tensor(out=pos, in0=pos, in1=pstart.unsqueeze(1).broadcast_to([128, NT, NE]), op=ALU.add),
             reads=[pos_b, pstart_b], writes=[pos_b])
        dest4, dest4_b = ar.alloc([NT, 4], F32, "dest4")
        junk, junk_b = ar.alloc([NE], F32, "junk")
        for i in range(NT):
            for k in range(4):
                S.op("dve", lambda e, i=i, k=k: e.scalar_tensor_tensor(out=junk, in0=logits[:, i, :], scalar=top8[:, i, k:k + 1], in1=pos[:, i, :],
                                                                     op0=ALU.is_equal, op1=ALU.mult, accum_out=dest4[:, i, k:k + 1]),
                     reads=[logits_b, top8_b, pos_b], writes=[junk_b, dest4_b])
                S.op("dve", lambda e, i=i, k=k: e.scalar_tensor_tensor(out=junk, in0=logits[:, i, :], scalar=top8[:, i, k:k + 1], in1=gatef[:, i, :],
                                                                     op0=ALU.is_equal, op1=ALU.mult, accum_out=gate4[:, i, k:k + 1]),
                     reads=[logits_b, top8_b, gatef_b], writes=[junk_b, gate4_b])
        S.op("dve", lambda e: e.tensor_copy(out=dest_i, in_=dest4), reads=[dest4_b], writes=[dest_ib])
        bv_i, bv_ib = ar.alloc([NB], I32, "bv_i")
        bv, bv_b = ar.alloc([NB], F32, "bv")
        cmp_, cmp_b = ar.alloc([NB, NE], F32, "cmp")
        blk_f, blk_fb = ar.alloc([NB], F32, "blk_f")
        chg_f, chg_fb = ar.alloc([NB], F32, "chg_f")
        S.op("pool", lambda e: e.iota(bv_i, pattern=[[BLK, NB]], base=0, channel_multiplier=0), writes=[bv_ib])
        S.op("dve", lambda e: e.tensor_copy(out=bv, in_=bv_i), reads=[bv_ib], writes=[bv_b])
        S.op("dve", lambda e: e.tensor_tensor(out=cmp_, in0=pend.unsqueeze(1).broadcast_to([128, NB, NE]),
                                              in1=bv.unsqueeze(2).broadcast_to([128, NB, NE]), op=ALU.is_le), reads=[pend_b, bv_b], writes=[cmp_b])
        S.op("dve", lambda e: e.reduce_sum(out=blk_f, in_=cmp_, axis=AX.X), reads=[cmp_b], writes=[blk_fb])
        S.op("dve", lambda e: e.tensor_scalar(out=blk_f, in0=blk_f, scalar1=float(NE - 1), scalar2=None, op0=ALU.min), reads=[blk_fb], writes=[blk_fb])
        S.op("dve", lambda e: e.memset(chg_f[:, 0:1], 1.0), writes=[chg_fb])
        S.op("dve", lambda e: e.tensor_tensor(out=chg_f[:, 1:NB], in0=blk_f[:, 1:NB], in1=blk_f[:, 0:NB - 1], op=ALU.not_equal), reads=[blk_fb], writes=[chg_fb])
        S.op("dve", lambda e: e.tensor_copy(out=blk_i, in_=blk_f), reads=[blk_fb], writes=[blk_ib])
        S.op("dve", lambda e: e.tensor_copy(out=chg_i, in_=chg_f), reads=[chg_fb], writes=[chg_ib])
        pio_i, pio_ib = ar.alloc([1], I32, "pio_i")
        pio, pio_b = ar.alloc([1], F32, "pio")
        S.op("pool", lambda e: e.iota(pio_i, pattern=[[0, 1]], base=0, channel_multiplier=1), writes=[pio_ib])
        S.op("dve", lambda e: e.tensor_copy(out=pio, in_=pio_i), reads=[pio_ib], writes=[pio_b])
        nchg, nchg_b = ar.alloc([NB], F32, "nchg")
        S.op("dve", lambda e: e.tensor_scalar(out=nchg, in0=chg_f, scalar1=-1.0e7, scalar2=1.0e7, op0=ALU.mult, op1=ALU.add), reads=[chg_fb], writes=[nchg_b])
        wb_f, wb_fb = ar.alloc([NB], F32, "wb_f")
        S.op("dve", lambda e: e.tensor_scalar(out=wb_f, in0=blk_f, scalar1=128.0, scalar2=pio[:, 0:1], op0=ALU.mult, op1=ALU.add), reads=[blk_fb, pio_b], writes=[wb_fb])
        S.op("dve", lambda e: e.tensor_tensor(out=wb_f, in0=wb_f, in1=chg_f, op=ALU.mult), reads=[wb_fb, chg_fb], writes=[wb_fb])
        S.op("dve", lambda e: e.tensor_tensor(out=wb_f, in0=wb_f, in1=nchg, op=ALU.add), reads=[wb_fb, nchg_b], writes=[wb_fb])
        S.op("dve", lambda e: e.tensor_copy(out=widx, in_=wb_f), reads=[wb_fb], writes=[widx_b])
        bi_f, bi_fb = ar.alloc([NB], F32, "bi_f")
        S.op("dve", lambda e: e.tensor_tensor(out=bi_f, in0=blk_f, in1=chg_f, op=ALU.mult), reads=[blk_fb, chg_fb], writes=[bi_fb])
        S.op("dve", lambda e: e.tensor_tensor(out=bi_f, in0=bi_f, in1=nchg, op=ALU.add), reads=[bi_fb, nchg_b], writes=[bi_fb])
        S.op("dve", lambda e: e.tensor_copy(out=bidx, in_=bi_f), reads=[bi_fb], writes=[bidx_b])
        if dbg:
            for nm, t_, tb_, n, dtp in (("dbg_dest", dest_i.rearrange("p a b -> p (a b)"), dest_ib, NT * 4, I32),
                                        ("dbg_gate4", gate4.rearrange("p a b -> p (a b)"), gate4_b, NT * 4, F32),
                                        ("dbg_blk", blk_i, blk_ib, NB, I32), ("dbg_chg", chg_i, chg_ib, NB, I32)):
                o, ob = self.scratch(nm, [128, n], dtp, dump=True)
                S.op("sp", lambda e, o=o, t_=t_: e.dma_start(out=o, in_=t_), reads=[tb_], writes=[ob], dma=True)
        S.barrier()
        ar.release()
        ar.mark()
        zt, zt_b = ar.alloc([4, D], BF16, "zt")
        S.op("dve", lambda e: e.memset(zt, 0.0), writes=[zt_b])
        xsv = self.xs.rearrange("(b p) n -> p b n", p=128)
        for q in range(NSLOT // 512):
            S.op("sp", lambda e, q=q: e.dma_start(out=xsv[:, 4 * q:4 * q + 4, :], in_=zt), reads=[zt_b], writes=[self.xs_b], dma=True)
        uft = [ar.alloc([D], BF16, f"sc_u{i}") for i in range(2)]
        for i in range(NT):
            u_t, u_b = uft[i % 2]
            S.op("sp", lambda e, u_t=u_t, i=i: e.dma_start(out=u_t, in_=self.uf_s[i * 128:(i + 1) * 128, :]), reads=[self.uf_sb], writes=[u_b], dma=True)
            for k in range(4):
                S.op("pool", lambda e, u_t=u_t, i=i, k=k: e.indirect_dma_start(
                    out=self.xs, out_offset=bass.IndirectOffsetOnAxis(ap=dest_i[:, i, k:k + 1].bitcast(U32), axis=0), in_=u_t, in_offset=None),
                    reads=[u_b, dest_ib], writes=[self.xs_b], dma=True)
        S.barrier()
        ar.release()
        ar.mark()
        Wt = [ar.alloc([8, D], BF16, f"moe_w{m}") for m in range(3)]
        brow, brow_b = ar.alloc([3, D], BF16, "moe_brow")
        WS = [ar.alloc([8, D], F32, f"moe_ws{m}") for m in range(3)]
        browS, browS_b = ar.alloc([3, D], BF16, "moe_browS")
        wds = [wg_d, wu_d, wd_d]
        bds = [bg_d, bu_d, bd_d]
        xb = [ar.alloc([D], BF16, f"moe_xb{i}") for i in range(2)]
        xT = [ar.alloc([8, 128], BF16, f"moe_xT{i}") for i in range(2)]
        gc = [ar.alloc([512], F32, f"moe_gc{i}") for i in range(2)]
        uc = [ar.alloc([512], F32, f"moe_uc{i}") for i in range(2)]
        sgm = [ar.alloc([512], F32, f"moe_sg{i}") for i in range(2)]
        hb_ = [ar.alloc([D], BF16, f"moe_h{i}") for i in range(2)]
        hT = [ar.alloc([8, 128], BF16, f"moe_hT{i}") for i in range(2)]
        yo = [ar.alloc([D], BF16, f"moe_yo{i}") for i in range(2)]
        NBLK = int(os.environ.get("MOE_NB", str(NB)))
        regs = {}

        def breg(e, val):
            if val not in regs:
                regs[val] = e.to_reg(val)
            return regs[val]
        for b in range(NBLK):
            for m in range(3):
                S.op("pool", lambda e, m=m, b=b: e.indirect_dma_start(
                    out=WS[m][0].rearrange("p a b -> p (a b)"), out_offset=None, in_=wds[m],
                    in_offset=bass.IndirectOffsetOnAxis(ap=widx[:, b:b + 1].bitcast(U32), axis=0),
                    bounds_check=breg(e, NE * 128 - 1), oob_is_err=False),
                    reads=[widx_b], writes=[WS[m][1]], dma=True)
            for m in range(3):
                S.op("pool", lambda e, m=m, b=b: e.indirect_dma_start(
                    out=browS[:, m, :], out_offset=None, in_=bds[m],
                    in_offset=bass.IndirectOffsetOnAxis(ap=bidx[:, b:b + 1].bitcast(U32), axis=0),
                    bounds_check=breg(e, NE - 1), oob_is_err=False),
                    reads=[bidx_b], writes=[browS_b], dma=True)
            for m in range(3):
                eng = "dve" if m < 2 else "act"
                if eng == "dve":
                    S.op("dve", lambda e, m=m: e.tensor_copy(out=Wt[m][0], in_=WS[m][0]), reads=[WS[m][1]], writes=[Wt[m][1]])
                else:
                    S.op("act", lambda e, m=m: e.activation(out=Wt[m][0], in_=WS[m][0], func=AF.Copy), reads=[WS[m][1]], writes=[Wt[m][1]])
            S.op("act", lambda e: e.activation(out=brow[0:1], in_=browS[0:1], func=AF.Copy), reads=[browS_b], writes=[brow_b])
            subs = [b * NSUB + s_ for s_ in range(NSUB)]

            def stageA(sb):
                x_t, x_b = xb[sb % 2]
                S.op("sp", lambda e, x_t=x_t, sb=sb: e.dma_start(out=x_t, in_=self.xs[sb * 128:(sb + 1) * 128, :]), reads=[self.xs_b], writes=[x_b], dma=True)
                xT_t, xT_b = xT[sb % 2]
                pT = ps[0][:].bitcast(BF16)

                def trx(e, x_t=x_t, pT=pT):
                    ins = None
                    for kc in range(8):
                        ins = e.transpose(out=pT[:, kc * 128:(kc + 1) * 128], in_=x_t[:, kc * 128:(kc + 1) * 128], identity=ident_bf)
                    return ins
                S.op("pe", trx, reads=[x_b, ident_bf_b], writes=[psb[0]])
                S.op("act", lambda e, xT_t=xT_t, pT=pT: e.activation(out=xT_t.rearrange("p a b -> p (a b)"), in_=pT, func=AF.Copy), reads=[psb[0]], writes=[xT_b])
                h_t, h_b = hb_[sb % 2]
                for half in range(2):
                    hs = slice(half * 512, (half + 1) * 512)
                    for m, bk in ((0, 1 + half), (1, 3 + half)):
                        def mm(e, m=m, bk=bk, hs=hs, xT_t=xT_t):
                            e.matmul(ps[bk][:], lhsT=ones_bf[0:1, 0:128], rhs=brow[0:1, m, hs], start=True, stop=False)
                            ins = None
                            for kc in range(8):
                                ins = e.matmul(ps[bk][:], lhsT=xT_t[:, kc, :], rhs=Wt[m][0][:, kc, hs], start=False, stop=(kc == 7))
                            return ins
                        S.op("pe", mm, reads=[xT_b, Wt[m][1], brow_b, ones_bf_b], writes=[psb[bk]])
                    g_t, g_b = gc[half]
                    u_t, u_b = uc[half]
                    s_t, s_b = sgm[half]
                    S.op("dve", lambda e, g_t=g_t, half=half: e.tensor_scalar(out=g_t, in0=ps[1 + half][:], scalar1=7.0, scalar2=None, op0=ALU.min),
                         reads=[psb[1 + half]], writes=[g_b])
                    S.op("dve", lambda e, u_t=u_t, half=half: e.tensor_scalar(out=u_t, in0=ps[3 + half][:], scalar1=7.0, scalar2=-7.0, op0=ALU.min, op1=ALU.max),
                         reads=[psb[3 + half]], writes=[u_b])
                    S.op("act", lambda e, s_t=s_t, g_t=g_t: e.activation(out=s_t, in_=g_t, func=AF.Sigmoid, scale=1.702), reads=[g_b], writes=[s_b])
                    S.op("dve", lambda e, u_t=u_t, g_t=g_t: e.scalar_tensor_tensor(out=u_t, in0=u_t, scalar=1.0, in1=g_t, op0=ALU.add, op1=ALU.mult),
                         reads=[u_b, g_b], writes=[u_b])
                    S.op("dve", lambda e, h_t=h_t, u_t=u_t, s_t=s_t, hs=hs: e.tensor_tensor(out=h_t[:, hs], in0=u_t, in1=s_t, op=ALU.mult),
                         reads=[u_b, s_b], writes=[h_b])

            def stageB(sb):
                h_t, h_b = hb_[sb % 2]
                hT_t, hT_b = hT[sb % 2]
                pT5 = ps[5][:].bitcast(BF16)

                def trh(e, h_t=h_t, pT5=pT5):
                    ins = None
                    for kc in range(8):
                        ins = e.transpose(out=pT5[:, kc * 128:(kc + 1) * 128], in_=h_t[:, kc * 128:(kc + 1) * 128], identity=ident_bf)
                    return ins
                S.op("pe", trh, reads=[h_b, ident_bf_b], writes=[psb[5]])
                S.op("act", lambda e, hT_t=hT_t, pT5=pT5: e.activation(out=hT_t.rearrange("p a b -> p (a b)"), in_=pT5, func=AF.Copy), reads=[psb[5]], writes=[hT_b])
                y_t, y_b = yo[sb % 2]
                for half in range(2):
                    hs = slice(half * 512, (half + 1) * 512)
                    bk = 6 + half

                    def mmd(e, bk=bk, hs=hs, hT_t=hT_t):
                        e.matmul(ps[bk][:], lhsT=ones_bf[0:1, 0:128], rhs=brow[0:1, 2, hs], start=True, stop=False)
                        ins = None
                        for kc in range(8):
                            ins = e.matmul(ps[bk][:], lhsT=hT_t[:, kc, :], rhs=Wt[2][0][:, kc, hs], start=False, stop=(kc == 7))
                        return ins
                    S.op("pe", mmd, reads=[hT_b, Wt[2][1], brow_b, ones_bf_b], writes=[psb[bk]])
                    if half == 0:
                        S.op("dve", lambda e, y_t=y_t, bk=bk, hs=hs: e.tensor_copy(out=y_t[:, hs], in_=ps[bk][:]), reads=[psb[bk]], writes=[y_b])
                    else:
                        S.op("act", lambda e, y_t=y_t, bk=bk, hs=hs: e.activation(out=y_t[:, hs], in_=ps[bk][:], func=AF.Copy), reads=[psb[bk]], writes=[y_b])
                S.op("sp", lambda e, y_t=y_t, sb=sb: e.dma_start(out=self.ys[sb * 128:(sb + 1) * 128, :], in_=y_t), reads=[y_b], writes=[self.ys_b], dma=True)

            stageA(subs[0])
            for i_ in range(1, NSUB):
                stageA(subs[i_])
                stageB(subs[i_ - 1])
            stageB(subs[-1])
        S.barrier()
        ar.release()
        ar.mark()
        gf_rep, gf_rep_b = ar.alloc([D], F32, "gf_rep")
        fn_rep, fn_rep_b = ar.alloc([D], F32, "fn_rep")
        S.op("sp", lambda e: e.dma_start(out=gf_rep, in_=self.mod_s[:, 5 * D:6 * D]), reads=[self.mod_sb], writes=[gf_rep_b], dma=True)
        S.op("sp", lambda e: e.dma_start(out=fn_rep, in_=fnw.broadcast_to([128, D])), writes=[fn_rep_b], dma=True)
        yk = [[ar.alloc([D], BF16, f"cb_y{j}{k}") for k in range(4)] for j in range(2)]
        hh = [ar.alloc([D], F32, f"cb_h{j}") for j in range(2)]
        acc = [ar.alloc([D], F32, f"cb_a{j}") for j in range(2)]
        oo = [ar.alloc([D], F32, f"cb_o{j}") for j in range(2)]
        sqj, sqj_b = ar.alloc([D], BF16, "cb_sq")
        ssq = [ar.alloc([1], F32, f"cb_ss{j}") for j in range(2)]
        for i in range(NT):
            j = i % 2
            h_t, h_b = hh[j]
            a_t, a_b = acc[j]
            o_t, o_b = oo[j]
            s_t, s_b = ssq[j]
            S.op("sp", lambda e, h_t=h_t, i=i: e.dma_start(out=h_t, in_=self.h1_s[i * 128:(i + 1) * 128, :]), reads=[self.h1_sb], writes=[h_b], dma=True)
            for k in range(4):
                y_t, y_b = yk[j][k]
                S.op("pool", lambda e, y_t=y_t, i=i, k=k: e.indirect_dma_start(
                    out=y_t, out_offset=None, in_=self.ys, in_offset=bass.IndirectOffsetOnAxis(ap=dest_i[:, i, k:k + 1].bitcast(U32), axis=0)),
                    reads=[self.ys_b, dest_ib], writes=[y_b], dma=True)
            S.op("dve", lambda e, a_t=a_t, i=i, j=j: e.tensor_scalar(out=a_t, in0=yk[j][0][0], scalar1=gate4[:, i, 0:1], scalar2=None, op0=ALU.mult),
                 reads=[yk[j][0][1], gate4_b], writes=[a_b])
            for k in range(1, 4):
                S.op("dve", lambda e, a_t=a_t, i=i, j=j, k=k: e.scalar_tensor_tensor(out=a_t, in0=yk[j][k][0], scalar=gate4[:, i, k:k + 1], in1=a_t,
                                                                                 op0=ALU.mult, op1=ALU.add), reads=[yk[j][k][1], gate4_b, a_b], writes=[a_b])
            S.op("dve", lambda e, a_t=a_t: e.tensor_tensor(out=a_t, in0=a_t, in1=gf_rep, op=ALU.mult), reads=[a_b, gf_rep_b], writes=[a_b])
            S.op("dve", lambda e, a_t=a_t, h_t=h_t: e.tensor_tensor(out=a_t, in0=a_t, in1=h_t, op=ALU.add), reads=[a_b, h_b], writes=[a_b])
            S.op("act", lambda e, a_t=a_t, s_t=s_t: e.activation(out=sqj, in_=a_t, func=AF.Square, accum_out=s_t), reads=[a_b], writes=[sqj_b, s_b])
            S.op("act", lambda e, s_t=s_t: e.activation(out=s_t, in_=s_t, func=AF.Sqrt, scale=1.0 / D, bias=EPS), reads=[s_b], writes=[s_b])
            S.op("dve", lambda e, s_t=s_t: e.reciprocal(out=s_t, in_=s_t), reads=[s_b], writes=[s_b])
            S.op("dve", lambda e, o_t=o_t, a_t=a_t, s_t=s_t: e.scalar_tensor_tensor(out=o_t, in0=a_t, scalar=s_t[:, 0:1], in1=fn_rep, op0=ALU.mult, op1=ALU.mult),
                 reads=[a_b, s_b, fn_rep_b], writes=[o_b])
            S.op("sp", lambda e, o_t=o_t, i=i: e.dma_start(out=self.out[i * 128:(i + 1) * 128, :], in_=o_t), reads=[o_b], dma=True)
        S.barrier()
        ar.release()
        ar.release()

def to_cm(a):
    return a.reshape(64, 64, *a.shape[1:]).swapaxes(0, 1).reshape(a.shape)


def from_cm(a):
    return a.reshape(64, 64, *a.shape[1:]).swapaxes(0, 1).reshape(a.shape)


def make_in_maps(inputs, cores):
    x = np.asarray(inputs["x"], np.float32)
    c = np.asarray(inputs["c"], np.float32)
    ctx = np.asarray(inputs["ctx"], np.float32)
    c_ctx = np.asarray(inputs["c_ctx"], np.float32)
    shared = {
        "cctxT": np.ascontiguousarray(c_ctx.reshape(8, 128).T),
        "ada_w": np.ascontiguousarray(inputs["ada_w"][0]),
        "ada_b": np.ascontiguousarray(inputs["ada_b"][0].reshape(1, -1)),
        "norm_mix_w": np.ascontiguousarray(np.asarray(inputs["norm_mix_w"][0]).reshape(8, 128).T),
        "w_in": np.ascontiguousarray(inputs["w_in"][0]),
        "convw": np.ascontiguousarray(np.transpose(np.asarray(inputs["ssd_conv_w"][0]).reshape(5, 32, 128), (2, 1, 0))),
        "convb": np.ascontiguousarray(np.asarray(inputs["ssd_conv_b"][0]).reshape(32, 128).T),
        "dtbias": np.ascontiguousarray(np.asarray(inputs["ssd_dt_bias"][0]).reshape(1, 64)),
        "alog": np.ascontiguousarray(np.asarray(inputs["ssd_a_log"][0]).reshape(1, 64)),
        "ssd_d": np.ascontiguousarray(np.repeat(np.asarray(inputs["ssd_d"][0]), 64).reshape(16, 128).T),
        "ssd_nw": np.ascontiguousarray(np.asarray(inputs["ssd_norm_w"][0]).reshape(16, 128).T),
    }
    lam_re = np.asarray(inputs["s5_lam_re"][0]); lam_im = np.asarray(inputs["s5_lam_im"][0]); ldt = np.asarray(inputs["s5_log_dt"][0])
    b_re = np.asarray(inputs["s5_b_re"][0]); b_im = np.asarray(inputs["s5_b_im"][0])
    c_re = np.asarray(inputs["s5_c_re"][0]); c_im = np.asarray(inputs["s5_c_im"][0])
    s5F = np.zeros((2, 128, 5, 8, 64), np.float32)
    s5S = np.zeros((2, 128, 3 * 32 + 4 * 512), np.float32)
    for d in range(2):
        def F_gp(a):
            t = a.reshape(8, 8, 64).transpose(1, 0, 2)
            return np.repeat(t[:, None], 16, axis=1).reshape(128, 8, 64)
        s5F[d, :, 0] = F_gp(lam_re[d]); s5F[d, :, 1] = F_gp(lam_im[d])
        s5F[d, :, 2] = F_gp(np.repeat(ldt[d][:, None], 64, axis=1))
        s5F[d, :, 3] = b_re[d].reshape(8, 8, 64, 16).transpose(1, 3, 0, 2).reshape(128, 8, 64)
        s5F[d, :, 4] = b_im[d].reshape(8, 8, 64, 16).transpose(1, 3, 0, 2).reshape(128, 8, 64)
        def S_gp(a):
            return a.reshape(32, 2, 64).transpose(1, 2, 0).reshape(128, 32)
        s5S[d, :, 0:32] = S_gp(lam_re[d]); s5S[d, :, 32:64] = S_gp(lam_im[d])
        s5S[d, :, 64:96] = S_gp(np.repeat(ldt[d][:, None], 64, axis=1))
        s5S[d, :, 96:96 + 512] = b_re[d].reshape(32, 2, 64, 16).transpose(1, 2, 0, 3).reshape(128, 512)
        s5S[d, :, 96 + 512:96 + 1024] = b_im[d].reshape(32, 2, 64, 16).transpose(1, 2, 0, 3).reshape(128, 512)
        s5S[d, :, 96 + 1024:96 + 1536] = c_re[d].reshape(32, 2, 16, 64).transpose(1, 3, 0, 2).reshape(128, 512)
        s5S[d, :, 96 + 1536:96 + 2048] = c_im[d].reshape(32, 2, 16, 64).transpose(1, 3, 0, 2).reshape(128, 512)
    shared["glu_w"] = np.ascontiguousarray(inputs["s5_glu_w"][0])
    shared["glu_b"] = np.ascontiguousarray(np.asarray(inputs["s5_glu_b"][0]).reshape(8, 128).T)
    shared["w_a"] = np.ascontiguousarray(inputs["w_branch_a"][0])
    shared["w_b"] = np.ascontiguousarray(inputs["w_branch_b"][0])
    shared["w_o"] = np.ascontiguousarray(inputs["w_out"][0])
    shared["nfw"] = np.ascontiguousarray(np.asarray(inputs["norm_ffn_w"][0]).reshape(1, -1))
    shared["r_w"] = np.ascontiguousarray(inputs["router_w"][0])
    shared["r_b"] = np.ascontiguousarray(np.asarray(inputs["router_b"][0]).reshape(1, -1))
    def wperm(w):
        return np.ascontiguousarray(np.asarray(w).reshape(32, 8, 128, D).transpose(0, 2, 1, 3)).reshape(4096, 8192)
    shared["moe_wg"] = wperm(inputs["moe_w_gate"][0])
    shared["moe_wu"] = wperm(inputs["moe_w_up"][0])
    shared["moe_wd"] = wperm(inputs["moe_w_down"][0])
    shared["moe_bg"] = np.ascontiguousarray(inputs["moe_b_gate"][0])
    shared["moe_bu"] = np.ascontiguousarray(inputs["moe_b_up"][0])
    shared["moe_bd"] = np.ascontiguousarray(inputs["moe_b_down"][0])
    shared["fnw"] = np.ascontiguousarray(np.asarray(inputs["final_norm_w"]).reshape(1, -1))
    shared["s5F"] = s5F
    shared["s5S"] = s5S
    shared["s5_d"] = np.ascontiguousarray(np.asarray(inputs["s5_d"][0]).reshape(8, 128).T)
    maps = []
    for b in cores:
        m = dict(shared)
        m["xT_rm"] = np.ascontiguousarray(x[b].T)
        m["xT_cm"] = np.ascontiguousarray(to_cm(x[b]).T)
        m["ctxT"] = np.ascontiguousarray(ctx[b].T)
        m["cT"] = np.ascontiguousarray(c[b].reshape(8, 128).T)
        m["x_tok"] = np.ascontiguousarray(to_cm(x[b]))
        maps.append(m)
    return maps


_NC_CACHE = {}


def kernel(**inputs):
    if "nc" not in _NC_CACHE:
        _NC_CACHE["nc"] = K().build()
    nc = _NC_CACHE["nc"]
    maps = make_in_maps(inputs, list(range(8)))
    res = run_bass_kernel_spmd(nc, maps, core_ids=list(range(8)))
    outs = [from_cm(np.asarray(r["out"])) for r in res.results]
    return np.stack(outs, 0).astype(np.float32)
```

```python
import os
import numpy as np
from contextlib import ExitStack
import concourse.bass as bass
import concourse.mybir as mybir
from concourse.bass_utils import run_bass_kernel_spmd

F32 = mybir.dt.float32
BF16 = mybir.dt.bfloat16
I32 = mybir.dt.int32
U32 = mybir.dt.uint32
U8 = mybir.dt.uint8
ALU = mybir.AluOpType
AF = mybir.ActivationFunctionType
AX = mybir.AxisListType

NDSEM = 8
D = 1024
L = 4096
LC = 256
T = L + LC
EPS = 1e-6


class Buf:
    __slots__ = ("name", "last_w", "readers", "excl")

    def __init__(self, name, excl=False):
        self.name = name
        self.last_w = None
        self.readers = []
        self.excl = excl


class Op:
    __slots__ = ("id", "eng", "fn", "deps", "dma", "eidx", "needs_inc", "semval", "dslot", "dval", "name")


COMPUTE = ("pe", "dve", "act", "pool")
ENGS = ("sp", "pe", "dve", "act", "pool")


class Sched:
    def __init__(self, nc):
        self.nc = nc
        self.ops = []
        self.per_eng = {e: [] for e in ENGS}
        self.ndma = {e: 0 for e in ENGS}
        self.barrier_floor = -1

    def op(self, eng, fn, reads=(), writes=(), dma=False, name=None):
        o = Op()
        o.id = len(self.ops)
        o.eng = eng
        o.fn = fn
        o.dma = dma
        o.name = name
        o.needs_inc = False
        o.semval = None
        o.dslot = None
        o.dval = None
        deps = {}
        if any(b.excl for b in reads):
            writes = list(writes) + [b for b in reads if b.excl and b not in writes]
            reads = [b for b in reads if not b.excl]
        for b in reads:
            if b.last_w is not None:
                deps[b.last_w.id] = (b.last_w, "raw")
        for b in writes:
            if b.last_w is not None and b.last_w.id not in deps:
                deps[b.last_w.id] = (b.last_w, "waw")
            for r in b.readers:
                if r.id not in deps:
                    deps[r.id] = (r, "war")
        o.deps = [(p, k) for (p, k) in deps.values() if p.id > self.barrier_floor]
        for b in reads:
            b.readers.append(o)
        for b in writes:
            b.last_w = o
            b.readers = []
        o.eidx = len(self.per_eng[eng])
        self.per_eng[eng].append(o)
        if dma:
            i = self.ndma[eng]
            self.ndma[eng] += 1
            o.dslot = i % NDSEM
            o.dval = 16 * (i // NDSEM + 1)
        self.ops.append(o)
        return o

    def barrier(self):
        lasts = []
        for e in ENGS:
            comp = [o for o in self.per_eng[e] if not o.dma and o.fn is not None]
            if comp:
                lasts.append(comp[-1])
            dm = [o for o in self.per_eng[e] if o.dma]
            lasts.extend(dm[-NDSEM:])
        lasts = [o for o in lasts if o.id > self.barrier_floor]
        for e in ENGS:
            o = self.op(e, None, name="barrier")
            o.deps = [(p, "raw") for p in lasts if p.eng != e or p.dma]
        self.barrier_floor = len(self.ops) - 1

    @staticmethod
    def _skip(o, p, kind):
        if p.dma or o.dma or p.eng != o.eng:
            return False
        if p.eng == "pe":
            return True
        if kind != "raw":
            return True
        return o.eidx - p.eidx > 2

    def emit(self):
        nc = self.nc
        for o in self.ops:
            for (p, kind) in o.deps:
                if p.dma or self._skip(o, p, kind):
                    continue
                p.needs_inc = True
        for e in ENGS:
            c = 0
            for o in self.per_eng[e]:
                if o.dma:
                    continue
                if o.needs_inc:
                    c += 1
                    o.semval = c
        with ExitStack() as es:
            csem = {e: es.enter_context(nc.semaphore(f"c_{e}")) for e in COMPUTE}
            dsem = {e: [es.enter_context(nc.semaphore(f"d_{e}{i}")) for i in range(NDSEM)]
                    for e in ENGS if self.ndma[e] > 0}
            block = es.enter_context(nc.Block())
            handles = {"sp": block.sync, "pe": block.tensor, "dve": block.vector,
                       "act": block.scalar, "pool": block.gpsimd}

            FUSE = os.environ.get("FUSEWAIT", "1") == "1"

            class _Rec:
                def __init__(self, eng):
                    self._eng = eng
                    self.first = None

                def __getattr__(self, name):
                    attr = getattr(self._eng, name)
                    if not callable(attr):
                        return attr

                    def w(*a, **k):
                        r = attr(*a, **k)
                        if self.first is None and hasattr(r, "then_inc"):
                            self.first = r
                        return r
                    return w

            def make(e):
                def body(eng):
                    waited = {}

                    def need(lst, sem, key, val):
                        if waited.get(key, 0) >= val:
                            return
                        for i, (s_, k_, v_) in enumerate(lst):
                            if k_ == key:
                                if v_ < val:
                                    lst[i] = (sem, key, val)
                                return
                        lst.append((sem, key, val))

                    for o in self.per_eng[e]:
                        lst = []
                        for (p, kind) in o.deps:
                            if p.dma:
                                need(lst, dsem[p.eng][p.dslot], ("d", p.eng, p.dslot), p.dval)
                            elif not self._skip(o, p, kind):
                                need(lst, csem[p.eng], ("c", p.eng), p.semval)
                        if o.dma and o.dval > 16:
                            need(lst, dsem[e][o.dslot], ("d", e, o.dslot), o.dval - 16)
                        for (s_, k_, v_) in lst:
                            waited[k_] = v_
                        fuse = None
                        if FUSE and lst and o.fn is not None and not o.dma:
                            fuse = lst.pop()
                        for (s_, k_, v_) in lst:
                            eng.wait_ge(s_, v_)
                        if o.fn is None:
                            continue
                        if fuse is not None:
                            rec = _Rec(eng)
                            ins = o.fn(rec)
                            rec.first._wait_ge(fuse[0], fuse[2])
                        else:
                            ins = o.fn(eng)
                        if o.dma:
                            ins.then_inc(dsem[e][o.dslot], 16)
                        elif o.needs_inc:
                            ins.then_inc(csem[e], 1)
                    if e == "sp":
                        for q in dsem:
                            dm = [o for o in self.per_eng[q] if o.dma]
                            for o in dm[-NDSEM:]:
                                if waited.get(("d", q, o.dslot), 0) < o.dval:
                                    eng.wait_ge(dsem[q][o.dslot], o.dval)
                                    waited[("d", q, o.dslot)] = o.dval
                return body

            for e in ENGS:
                if self.per_eng[e] or e == "sp":
                    handles[e](make(e))


ESZ = {F32: 4, BF16: 2, I32: 4, U32: 4, U8: 1}


class Arena:
    def __init__(self, nc, es, nbytes, name="arena"):
        self.t = es.enter_context(nc.sbuf_tensor(name, [128, nbytes], U8))
        self.nbytes = nbytes
        self.off = 0
        self.marks = []

    def alloc(self, shape, dtype, name="t"):
        if isinstance(shape, int):
            shape = [shape]
        n = int(np.prod(shape))
        nb = n * ESZ[dtype]
        self.off = (self.off + 63) // 64 * 64
        assert self.off + nb <= self.nbytes, f"arena overflow {name}: {self.off}+{nb}>{self.nbytes}"
        ap = self.t[:, self.off:self.off + nb]
        if dtype != U8:
            ap = ap.bitcast(dtype)
        if len(shape) > 1:
            names = " ".join(f"d{i}" for i in range(len(shape)))
            kw = {f"d{i}": int(shape[i]) for i in range(1, len(shape))}
            ap = ap.rearrange(f"p ({names}) -> p {names}", **kw)
        self.off += nb
        return ap, Buf(name)

    def mark(self):
        self.marks.append(self.off)

    def release(self):
        self.off = self.marks.pop()


IN_S5 = (0, 1024)
IN_XBC = (1024, 5120)
IN_DT = (5120, 5184)
IN_Z = (5184, 7232)
IN_GA = (7232, 8256)
IN_GB = (8256, 9280)


class K:
    def __init__(self, stage=99, debug=False):
        self.stage = stage
        self.debug = debug
        self.nc = bass.Bass("TRN2", target_bir_lowering=False)
        self.ins = {}
        self.dbg = {}

    def inp(self, name, shape, dt=F32):
        self.ins[name] = self.nc.dram_tensor(name, list(shape), dt, kind="ExternalInput").ap()
        return self.ins[name]

    def scratch(self, name, shape, dt, dump=False):
        if self.debug and dump:
            ap = self.nc.dram_tensor(name, list(shape), dt, kind="ExternalOutput").ap()
            self.dbg[name] = ap
        else:
            ap = self.nc.dram_tensor(name, list(shape), dt).ap()
        return ap, Buf(name)

    def build(self):
        nc = self.nc
        inp = self.inp
        xT_cm = inp("xT_cm", [D, L])
        xT_rm = inp("xT_rm", [D, L])
        ctxT = inp("ctxT", [D, LC])
        cT = inp("cT", [128, 8])
        cctxT = inp("cctxT", [128, 8])
        ada_w = inp("ada_w", [D, 6 * D])
        ada_b = inp("ada_b", [1, 6 * D])
        nmw = inp("norm_mix_w", [128, 8])
        w_in = inp("w_in", [D, 9280])
        out = nc.dram_tensor("out", [L, D], F32, kind="ExternalOutput").ap()
        self.out = out

        with ExitStack() as es:
            self.es = es
            ar = self.ar = Arena(nc, es, 206 * 1024)
            self.ps = [es.enter_context(nc.psum_tensor(f"ps{i}", [128, 512], F32)) for i in range(8)]
            self.psb = [Buf(f"ps{i}", excl=True) for i in range(8)]
            S = self.S = Sched(nc)

            ones_bf, ones_bf_b = ar.alloc([128], BF16, "ones_bf")
            S.op("dve", lambda e: e.memset(ones_bf, 1.0), writes=[ones_bf_b])
            ones_f, ones_f_b = ar.alloc([128], F32, "ones_f")
            S.op("dve", lambda e: e.memset(ones_f, 1.0), writes=[ones_f_b])
            ident_f, ident_f_b = ar.alloc([128], F32, "ident_f")
            S.op("pool", lambda e: e.affine_select(out=ident_f, in_=ones_f, pattern=[[-1, 128]],
                                                  compare_op=ALU.is_equal, fill=0.0, base=0,
                                                  channel_multiplier=1),
                 reads=[ones_f_b], writes=[ident_f_b])
            self.consts = dict(ones_bf=(ones_bf, ones_bf_b), ones_f=(ones_f, ones_f_b),
                               ident_f=(ident_f, ident_f_b))

            self.phase0(cT, cctxT, ada_w, ada_b, nmw)
            if self.stage >= 1:
                self.phase1(xT_rm, xT_cm, ctxT, w_in)
            if self.stage >= 2:
                self.phase2(inp("convw", [128, 32, 5]), inp("convb", [128, 32]), inp("dtbias", [1, 64]), inp("alog", [1, 64]),
                            inp("ssd_d", [128, 16]), inp("ssd_nw", [128, 16]))
            if self.stage >= 3:
                self.phase3_setup(inp("s5F", [2, 128, 5, 8, 64]), inp("s5S", [2, 128, 3 * 32 + 4 * 512]))
                self.phase3_main(inp("s5_d", [128, 8]))
            if self.stage >= 4:
                self.phase4(inp("glu_w", [D, D]), inp("glu_b", [128, 8]), inp("w_a", [D, D]), inp("w_b", [2 * D, D]), inp("w_o", [D, D]),
                            inp("x_tok", [L, D]), inp("nfw", [1, D]), inp("r_w", [D, 32]), inp("r_b", [1, 32]))
            if self.stage >= 5:
                self.phase5(inp("moe_wg", [4096, 8192]), inp("moe_wu", [4096, 8192]), inp("moe_wd", [4096, 8192]),
                            inp("moe_bg", [32, D]), inp("moe_bu", [32, D]), inp("moe_bd", [32, D]), inp("fnw", [1, D]))
            S.barrier()
            S.emit()
        return nc

    def phase0(self, cT, cctxT, ada_w, ada_b, nmw):
        nc, S, ar = self.nc, self.S, self.ar
        ps, psb = self.ps, self.psb
        ident_f, ident_f_b = self.consts["ident_f"]
        self.mod_rep = []
        self.Am = [ar.alloc([8], F32, f"Am{w}") for w in range(2)]
        self.Bm = [ar.alloc([8], F32, f"Bm{w}") for w in range(2)]
        nmw_t, nmw_b = ar.alloc([8], F32, "nmw")
        S.op("sp", lambda e: e.dma_start(out=nmw_t, in_=nmw), writes=[nmw_b], dma=True)
        ar.mark()
        self.mod_rep.append(ar.alloc([6 * D], F32, "mod_rep0"))
        self.mod_rep.append(ar.alloc([6 * D], F32, "mod_rep1"))
        self.mod_s, self.mod_sb = self.scratch("mod_s", [128, 6 * D], F32)
        adab, adab_b = ar.alloc([6 * D], F32, "adab")
        S.op("sp", lambda e: e.dma_start(out=adab, in_=ada_b.broadcast_to([128, 6 * D])), writes=[adab_b], dma=True)
        lhs = []
        for w, src in enumerate((cT, cctxT)):
            c_t, c_b = ar.alloc([8], F32, f"c{w}")
            S.op("sp", lambda e, c_t=c_t, src=src: e.dma_start(out=c_t, in_=src), writes=[c_b], dma=True)
            s_t, s_b = ar.alloc([8], F32, f"s{w}")
            S.op("act", lambda e, c_t=c_t, s_t=s_t: e.activation(out=s_t, in_=c_t, func=AF.Silu),
                 reads=[c_b], writes=[s_b])
            l_t, l_b = ar.alloc([8, 128], BF16, f"l{w}")
            S.op("dve", lambda e, l_t=l_t, s_t=s_t: e.tensor_copy(out=l_t, in_=s_t.unsqueeze(2).broadcast_to([128, 8, 128])),
                 reads=[s_b], writes=[l_b])
            lhs.append((l_t, l_b))
        wsrc = ada_w.rearrange("(kc p) n -> p kc n", p=128)
        wbufs = [ar.alloc([8, 512], BF16, f"adaw{i}") for i in range(2)]
        for blk in range(12):
            wt, wb = wbufs[blk % 2]
            S.op("pool", lambda e, wt=wt, blk=blk: e.dma_start(out=wt, in_=wsrc[:, :, blk * 512:(blk + 1) * 512]),
                 writes=[wb], dma=True)
            for w in range(2):
                l_t, l_b = lhs[w]
                pi = (blk * 2 + w) % 8

                def mm(e, l_t=l_t, wt=wt, pi=pi):
                    ins = None
                    for kc in range(8):
                        ins = e.matmul(ps[pi][:], lhsT=l_t[:, kc, :], rhs=wt[:, kc, :], start=(kc == 0), stop=(kc == 7))
                    return ins
                S.op("pe", mm, reads=[l_b, wb], writes=[psb[pi]])
                mt, mb = self.mod_rep[w]
                S.op("dve", lambda e, mt=mt, pi=pi, blk=blk: e.tensor_tensor(
                    out=mt[:, blk * 512:(blk + 1) * 512], in0=ps[pi][:], in1=adab[:, blk * 512:(blk + 1) * 512], op=ALU.add),
                    reads=[psb[pi], adab_b], writes=[mb])
        tmp, tmp_b = ar.alloc([8, 128], F32, "diagtmp")
        for w in range(2):
            mt, mb = self.mod_rep[w]
            for seg, (dst, dst_b) in ((0, self.Bm[w]), (1, self.Am[w])):
                view = mt[:, seg * D:(seg + 1) * D].rearrange("p (k q) -> p k q", k=8)
                S.op("dve", lambda e, view=view: e.tensor_tensor(
                    out=tmp, in0=view, in1=ident_f.unsqueeze(1).broadcast_to([128, 8, 128]), op=ALU.mult),
                    reads=[mb, ident_f_b], writes=[tmp_b])
                S.op("dve", lambda e, dst=dst: e.reduce_sum(out=dst, in_=tmp, axis=AX.X), reads=[tmp_b], writes=[dst_b])
            at, ab = self.Am[w]
            S.op("dve", lambda e, at=at: e.scalar_tensor_tensor(out=at, in0=at, scalar=1.0, in1=nmw_t, op0=ALU.add, op1=ALU.mult),
                 reads=[ab, nmw_b], writes=[ab])
        S.op("sp", lambda e: e.dma_start(out=self.mod_s, in_=self.mod_rep[0][0]), reads=[self.mod_rep[0][1]], writes=[self.mod_sb], dma=True)
        if self.debug:
            for w in range(2):
                o, ob = self.scratch(f"dbg_mod{w}", [128, 6 * D], F32, dump=True)
                mt, mb = self.mod_rep[w]
                S.op("sp", lambda e, o=o, mt=mt: e.dma_start(out=o, in_=mt), reads=[mb], writes=[ob], dma=True)
                o, ob = self.scratch(f"dbg_AB{w}", [128, 16], F32, dump=True)
                S.op("sp", lambda e, o=o, w=w: e.dma_start(out=o[:, 0:8], in_=self.Am[w][0]), reads=[self.Am[w][1]], writes=[ob], dma=True)
                S.op("sp", lambda e, o=o, w=w: e.dma_start(out=o[:, 8:16], in_=self.Bm[w][0]), reads=[self.Bm[w][1]], writes=[ob], dma=True)
        S.barrier()
        ar.release()

    def norm_bufs(self):
        ar = self.ar
        return dict(xt=[ar.alloc([8, 512], F32, f"xt{i}") for i in range(2)],
                    sq=[ar.alloc([8, 512], BF16, f"sq{i}") for i in range(2)],
                    rs=[ar.alloc([512], F32, f"rs{i}") for i in range(2)],
                    tm=[ar.alloc([512], F32, f"tm{i}") for i in range(4)], cnt=[0])

    def norm_tokens(self, src, ntok, u_t, u_b, col0, w, nb):
        nc, S, ar = self.nc, self.S, self.ar
        ps, psb = self.ps, self.psb
        ones_bf, ones_bf_b = self.consts["ones_bf"]
        At, Ab = self.Am[w]
        Bt, Bb = self.Bm[w]
        srcv = src.rearrange("(kc p) n -> p kc n", p=128)
        xt, sq, rs, tm = nb["xt"], nb["sq"], nb["rs"], nb["tm"]
        ntile = (ntok + 511) // 512
        for ti in range(ntile):
            n = min(512, ntok - ti * 512)
            ci = nb["cnt"][0]
            nb["cnt"][0] += 1
            x_t, x_b = xt[ci % 2]
            q_t, q_b = sq[ci % 2]
            r_t, r_b = rs[ci % 2]
            S.op("sp", lambda e, x_t=x_t, ti=ti, n=n: e.dma_start(out=x_t[:, :, 0:n], in_=srcv[:, :, ti * 512:ti * 512 + n]),
                 writes=[x_b], dma=True)
            S.op("act", lambda e, x_t=x_t, q_t=q_t, n=n: e.activation(out=q_t[:, :, 0:n], in_=x_t[:, :, 0:n], func=AF.Square),
                 reads=[x_b], writes=[q_b])
            pi = ci % 2

            def mm(e, q_t=q_t, pi=pi, n=n):
                ins = None
                for kc in range(8):
                    ins = e.matmul(ps[pi][:, 0:n], lhsT=ones_bf, rhs=q_t[:, kc, 0:n], start=(kc == 0), stop=(kc == 7))
                return ins
            S.op("pe", mm, reads=[q_b, ones_bf_b], writes=[psb[pi]])
            S.op("act", lambda e, r_t=r_t, pi=pi, n=n: e.activation(out=r_t[:, 0:n], in_=ps[pi][:, 0:n], func=AF.Sqrt,
                                                                  scale=1.0 / D, bias=EPS),
                 reads=[psb[pi]], writes=[r_b])
            S.op("dve", lambda e, r_t=r_t, n=n: e.reciprocal(out=r_t[:, 0:n], in_=r_t[:, 0:n]), reads=[r_b], writes=[r_b])
            for kc in range(8):
                t_t, t_b = tm[kc % 4]
                S.op("dve", lambda e, t_t=t_t, x_t=x_t, r_t=r_t, kc=kc, n=n: e.tensor_tensor(
                    out=t_t[:, 0:n], in0=x_t[:, kc, 0:n], in1=r_t[:, 0:n], op=ALU.mult),
                    reads=[x_b, r_b], writes=[t_b])
                S.op("act", lambda e, t_t=t_t, kc=kc, ti=ti, n=n: e.activation(
                    out=u_t[:, kc, col0 + ti * 512:col0 + ti * 512 + n], in_=t_t[:, 0:n], func=AF.Identity,
                    scale=At[:, kc:kc + 1], bias=Bt[:, kc:kc + 1]),
                    reads=[t_b, Ab, Bb], writes=[u_b])

    def proj_block(self, wsrc, c0, ncols, u_t, u_b, tok_ranges, dst, dst_b, row0, wbufs, stg, cnt):
        S = self.S
        ps, psb = self.ps, self.psb
        wt, wb = wbufs[cnt[0] % 2]
        cnt[0] += 1
        S.op("pool", lambda e: e.dma_start(out=wt[:, :, 0:ncols], in_=wsrc[:, :, c0:c0 + ncols]), writes=[wb], dma=True)
        for (ucol, n, dcol) in tok_ranges:
            for mc in range(ncols // 128):
                pi = cnt[1] % 8
                cnt[1] += 1

                def mm(e, pi=pi, mc=mc, ucol=ucol, n=n):
                    ins = None
                    for kc in range(8):
                        ins = e.matmul(ps[pi][:, 0:n], lhsT=wt[:, kc, mc * 128:(mc + 1) * 128],
                                       rhs=u_t[:, kc, ucol:ucol + n], start=(kc == 0), stop=(kc == 7))
                    return ins
                S.op("pe", mm, reads=[wb, u_b], writes=[psb[pi]])
                st, sb = stg[cnt[2] % len(stg)]
                eng = "act" if cnt[2] % 2 == 0 else "dve"
                cnt[2] += 1
                if eng == "act":
                    S.op("act", lambda e, st=st, pi=pi, n=n: e.activation(out=st[:, 0:n], in_=ps[pi][:, 0:n], func=AF.Copy),
                         reads=[psb[pi]], writes=[sb])
                else:
                    S.op("dve", lambda e, st=st, pi=pi, n=n: e.tensor_copy(out=st[:, 0:n], in_=ps[pi][:, 0:n]),
                         reads=[psb[pi]], writes=[sb])
                r = row0 + mc * 128
                S.op("sp", lambda e, st=st, r=r, dcol=dcol, n=n: e.dma_start(out=dst[r:r + 128, dcol:dcol + n], in_=st[:, 0:n]),
                     reads=[sb], writes=[dst_b], dma=True)

    def phase1(self, xT_rm, xT_cm, ctxT, w_in):
        nc, S, ar = self.nc, self.S, self.ar
        ps, psb = self.ps, self.psb
        dbg = self.debug
        self.s5u, self.s5u_b = self.scratch("s5u", [D, T], BF16, dump=dbg)
        self.xbc_s, self.xbc_sb = self.scratch("xbc_s", [4096, T], BF16, dump=dbg)
        self.z_s, self.z_sb = self.scratch("z_s", [2048, L], BF16, dump=dbg)
        self.gab_s, self.gab_sb = self.scratch("gab_s", [2048, L], BF16, dump=dbg)
        self.dt_s, self.dt_sb = self.scratch("dt_s", [128, 34 * 64], F32, dump=dbg)
        wsrc = w_in.rearrange("(kc p) n -> p kc n", p=128)
        ar.mark()
        u_t, u_b = ar.alloc([8, T], BF16, "u")
        wbufs = [ar.alloc([8, 512], BF16, f"wb{i}") for i in range(2)]
        stg = [ar.alloc([512], BF16, f"stg{i}") for i in range(4)]
        cnt = [0, 0, 0]
        nb = self.norm_bufs()
        self.norm_tokens(ctxT, LC, u_t, u_b, 0, 1, nb)
        self.norm_tokens(xT_rm, L, u_t, u_b, LC, 0, nb)
        if dbg:
            o, ob = self.scratch("dbg_u_rm", [128, 8 * T], BF16, dump=True)
            S.op("sp", lambda e: e.dma_start(out=o, in_=u_t.rearrange("p a b -> p (a b)")), reads=[u_b], writes=[ob], dma=True)
        toks = [(0, LC, 0)] + [(LC + i * 512, 512, LC + i * 512) for i in range(8)]
        for blk in range(2):
            self.proj_block(wsrc, IN_S5[0] + blk * 512, 512, u_t, u_b, toks, self.s5u, self.s5u_b, blk * 512, wbufs, stg, cnt)
        self.norm_tokens(xT_cm, L, u_t, u_b, LC, 0, nb)
        for blk in range(8):
            self.proj_block(wsrc, IN_XBC[0] + blk * 512, 512, u_t, u_b, toks, self.xbc_s, self.xbc_sb, blk * 512, wbufs, stg, cnt)
        ltoks = [(LC + i * 512, 512, i * 512) for i in range(8)]
        for blk in range(4):
            self.proj_block(wsrc, IN_Z[0] + blk * 512, 512, u_t, u_b, ltoks, self.z_s, self.z_sb, blk * 512, wbufs, stg, cnt)
        for blk in range(4):
            self.proj_block(wsrc, IN_GA[0] + blk * 512, 512, u_t, u_b, ltoks, self.gab_s, self.gab_sb, blk * 512, wbufs, stg, cnt)
        wdt, wdt_b = ar.alloc([8, 64], BF16, "wdt")
        S.op("pool", lambda e: e.dma_start(out=wdt, in_=wsrc[:, :, IN_DT[0]:IN_DT[1]]), writes=[wdt_b], dma=True)
        dtt, dtt_b = ar.alloc([34, 64], F32, "dtt")
        for c in range(34):
            pi = c % 2

            def mm(e, c=c, pi=pi):
                ins = None
                for kc in range(8):
                    ins = e.matmul(ps[pi][:, 0:64], lhsT=u_t[:, kc, c * 128:(c + 1) * 128], rhs=wdt[:, kc, :],
                                   start=(kc == 0), stop=(kc == 7))
                return ins
            S.op("pe", mm, reads=[u_b, wdt_b], writes=[psb[pi]])
            S.op("dve", lambda e, c=c, pi=pi: e.tensor_copy(out=dtt[:, c, :], in_=ps[pi][:, 0:64]), reads=[psb[pi]], writes=[dtt_b])
        S.op("sp", lambda e: e.dma_start(out=self.dt_s, in_=dtt.rearrange("p a b -> p (a b)")), reads=[dtt_b], writes=[self.dt_sb], dma=True)
        S.barrier()
        ar.release()


    def pq(self, i, j0, j1=None):
        if j1 is None:
            j1 = j0 + 1
        return self.ps[i][:, j0 * 128:j1 * 128], [self.psb[i]]

    def phase2(self, convw, convb, dtbias, alog, ssd_d, ssd_nw):
        nc, S, ar = self.nc, self.S, self.ar
        ps = self.ps
        dbg = self.debug
        ident_f, ident_f_b = self.consts["ident_f"]
        ones_f, ones_f_b = self.consts["ones_f"]
        ones_bf, ones_bf_b = self.consts["ones_bf"]
        self.yb_s, self.yb_sb = self.scratch("yb_s", [2048, L], BF16, dump=dbg)
        ar.mark()
        ident_bf, ident_bf_b = ar.alloc([128], BF16, "ident_bf")
        S.op("dve", lambda e: e.tensor_copy(out=ident_bf, in_=ident_f), reads=[ident_f_b], writes=[ident_bf_b])
        tri, tri_b = ar.alloc([128], F32, "tri")
        triT, triT_b = ar.alloc([128], F32, "triT")
        S.op("pool", lambda e: e.affine_select(out=tri, in_=ones_f, pattern=[[1, 128]], compare_op=ALU.is_ge, fill=0.0,
                                              base=0, channel_multiplier=-1), reads=[ones_f_b], writes=[tri_b])
        S.op("pool", lambda e: e.affine_select(out=triT, in_=ones_f, pattern=[[-1, 128]], compare_op=ALU.is_ge, fill=0.0,
                                              base=0, channel_multiplier=1), reads=[ones_f_b], writes=[triT_b])
        zer, zer_b = ar.alloc([128], F32, "zer")
        S.op("dve", lambda e: e.memset(zer, 0.0), writes=[zer_b])
        NEG = [ar.alloc([128], BF16, f"NEG{d}") for d in range(2)]
        S.op("pool", lambda e: e.affine_select(out=NEG[0][0], in_=zer, pattern=[[1, 128]], compare_op=ALU.is_ge, fill=-60000.0,
                                              base=0, channel_multiplier=-1), reads=[zer_b], writes=[NEG[0][1]])
        S.op("pool", lambda e: e.affine_select(out=NEG[1][0], in_=zer, pattern=[[-1, 128]], compare_op=ALU.is_ge, fill=-60000.0,
                                              base=0, channel_multiplier=1), reads=[zer_b], writes=[NEG[1][1]])
        oh2, oh2_b = ar.alloc([64], F32, "oh2")
        S.op("dve", lambda e: e.tensor_tensor(out=oh2[:, 0:32], in0=ident_f[:, 0:32], in1=ident_f[:, 32:64], op=ALU.add),
             reads=[ident_f_b], writes=[oh2_b])
        S.op("dve", lambda e: e.tensor_tensor(out=oh2[:, 32:64], in0=ident_f[:, 64:96], in1=ident_f[:, 96:128], op=ALU.add),
             reads=[ident_f_b], writes=[oh2_b])
        sel2, sel2_b = ar.alloc([64, 128], BF16, "sel2")
        S.op("dve", lambda e: e.tensor_copy(out=sel2, in_=oh2.unsqueeze(2).broadcast_to([128, 64, 128])), reads=[oh2_b], writes=[sel2_b])
        mhi, mhi_b = ar.alloc([1], F32, "mhi")
        mlo, mlo_b = ar.alloc([1], F32, "mlo")
        mt_, mt_b = ar.alloc([1], F32, "mtmp")
        S.op("dve", lambda e: e.reduce_sum(out=mhi, in_=ident_f[:, 0:32], axis=AX.X), reads=[ident_f_b], writes=[mhi_b])
        S.op("dve", lambda e: e.reduce_sum(out=mt_, in_=ident_f[:, 64:96], axis=AX.X), reads=[ident_f_b], writes=[mt_b])
        S.op("dve", lambda e: e.tensor_tensor(out=mhi, in0=mhi, in1=mt_, op=ALU.add), reads=[mhi_b, mt_b], writes=[mhi_b])
        S.op("dve", lambda e: e.tensor_scalar(out=mlo, in0=mhi, scalar1=-1.0, scalar2=1.0, op0=ALU.mult, op1=ALU.add),
             reads=[mhi_b], writes=[mlo_b])
        cw, cw_b = ar.alloc([32, 5], F32, "convw")
        cb, cb_b = ar.alloc([32], F32, "convb")
        dcol, dcol_b = ar.alloc([16], F32, "ssd_d")
        nwc, nwc_b = ar.alloc([16], F32, "ssd_nw")
        S.op("sp", lambda e: e.dma_start(out=cw, in_=convw), writes=[cw_b], dma=True)
        S.op("sp", lambda e: e.dma_start(out=cb, in_=convb), writes=[cb_b], dma=True)
        S.op("sp", lambda e: e.dma_start(out=dcol, in_=ssd_d), writes=[dcol_b], dma=True)
        S.op("sp", lambda e: e.dma_start(out=nwc, in_=ssd_nw), writes=[nwc_b], dma=True)
        dt, dt_b = ar.alloc([34, 2, 32], F32, "dt")
        decay, decay_b = ar.alloc([34, 2, 32], F32, "decay")
        wend, wend_b = ar.alloc([34, 2, 32], F32, "wend")
        HL, HL_b = ar.alloc([34, 128], BF16, "HL")
        HLn, HLn_b = ar.alloc([34, 128], BF16, "HLn")
        ar.mark()
        nacum, nacum_b = ar.alloc([34, 2, 32], F32, "nacum")
        adtd, adtd_b = ar.alloc([34, 4, 32], F32, "adtd")
        tot, tot_b = ar.alloc([34, 2, 32], F32, "tot")
        dtb, dtb_b = ar.alloc([2, 32], F32, "dtb")
        arep, arep_b = ar.alloc([2, 32], F32, "arep")
        S.op("sp", lambda e: e.dma_start(out=dt.rearrange("p a b c -> p (a b c)"), in_=self.dt_s), reads=[self.dt_sb], writes=[dt_b], dma=True)
        S.op("sp", lambda e: e.dma_start(out=dtb.rearrange("p a b -> p (a b)"), in_=dtbias.broadcast_to([128, 64])), writes=[dtb_b], dma=True)
        S.op("sp", lambda e: e.dma_start(out=arep.rearrange("p a b -> p (a b)"), in_=alog.broadcast_to([128, 64])), writes=[arep_b], dma=True)
        S.op("dve", lambda e: e.tensor_tensor(out=dt, in0=dt, in1=dtb.unsqueeze(1).broadcast_to([128, 34, 2, 32]), op=ALU.add),
             reads=[dt_b, dtb_b], writes=[dt_b])
        S.op("act", lambda e: e.activation(out=dt, in_=dt, func=AF.Exp), reads=[dt_b], writes=[dt_b])
        S.op("act", lambda e: e.activation(out=dt, in_=dt, func=AF.Ln, bias=1.0), reads=[dt_b], writes=[dt_b])
        S.op("act", lambda e: e.activation(out=arep, in_=arep, func=AF.Exp), reads=[arep_b], writes=[arep_b])
        S.op("dve", lambda e: e.tensor_scalar(out=arep, in0=arep, scalar1=-1.0, scalar2=None, op0=ALU.mult), reads=[arep_b], writes=[arep_b])
        for d in range(2):
            for j in range(2):
                S.op("dve", lambda e, d=d, j=j: e.tensor_tensor(
                    out=adtd[:, :, 2 * d + j, :], in0=dt[:, :, d, :], in1=arep[:, d, :].unsqueeze(1).broadcast_to([128, 34, 32]), op=ALU.mult),
                    reads=[dt_b, arep_b], writes=[adtd_b])
        k = 0
        for d in range(2):
            lhs, lhs_b = (tri, tri_b) if d == 0 else (triT, triT_b)
            for (c0, ncn) in ((0, 16), (16, 16), (32, 2)):
                pa, pb = self.pq(k % 4, 0, 4)
                k += 1
                S.op("pe", lambda e, pa=pa, lhs=lhs, c0=c0, ncn=ncn, d=d: e.matmul(
                    pa[:, 0:ncn * 32].rearrange("p (a b) -> p a b", b=32), lhsT=lhs, rhs=adtd[:, c0:c0 + ncn, 2 * d, :], start=True, stop=True),
                    reads=[lhs_b, adtd_b], writes=pb)
                S.op("dve", lambda e, pa=pa, c0=c0, ncn=ncn, d=d: e.tensor_scalar(
                    out=nacum[:, c0:c0 + ncn, d, :], in0=pa[:, 0:ncn * 32].rearrange("p (a b) -> p a b", b=32),
                    scalar1=-1.0, scalar2=None, op0=ALU.mult), reads=pb, writes=[nacum_b])
                pa, pb = self.pq(k % 4, 0, 4)
                k += 1
                S.op("pe", lambda e, pa=pa, c0=c0, ncn=ncn, d=d: e.matmul(
                    pa[:, 0:ncn * 32].rearrange("p (a b) -> p a b", b=32), lhsT=ones_f, rhs=adtd[:, c0:c0 + ncn, 2 * d, :], start=True, stop=True),
                    reads=[ones_f_b, adtd_b], writes=pb)
                S.op("dve", lambda e, pa=pa, c0=c0, ncn=ncn, d=d: e.tensor_copy(
                    out=tot[:, c0:c0 + ncn, d, :], in_=pa[:, 0:ncn * 32].rearrange("p (a b) -> p a b", b=32)), reads=pb, writes=[tot_b])
        S.op("act", lambda e: e.activation(out=decay, in_=tot, func=AF.Exp), reads=[tot_b], writes=[decay_b])
        S.op("dve", lambda e: e.tensor_tensor(out=wend, in0=tot, in1=nacum, op=ALU.add), reads=[tot_b, nacum_b], writes=[wend_b])
        S.op("act", lambda e: e.activation(out=wend, in_=wend, func=AF.Exp), reads=[wend_b], writes=[wend_b])
        S.op("dve", lambda e: e.tensor_tensor(out=wend, in0=wend, in1=dt, op=ALU.mult), reads=[wend_b, dt_b], writes=[wend_b])
        acT = [ar.alloc([128], F32, f"acT{i}") for i in range(2)]
        hi_ = [ar.alloc([128], BF16, f"hi{i}") for i in range(2)]
        lo_ = [ar.alloc([128], F32, f"lo{i}") for i in range(2)]
        for c in range(34):
            pa, pab = self.pq(4 + (c % 2), 0)
            pb_, pbb = self.pq(4 + (c % 2), 1)
            lhsT = adtd[:, c, :, :].rearrange("p a b -> p (a b)")
            S.op("pe", lambda e, pa=pa, lhsT=lhsT: e.matmul(pa, lhsT=lhsT, rhs=tri, start=True, stop=True),
                 reads=[adtd_b, tri_b], writes=pab)
            S.op("pe", lambda e, pb_=pb_, lhsT=lhsT: e.matmul(pb_, lhsT=lhsT, rhs=triT, start=True, stop=True),
                 reads=[adtd_b, triT_b], writes=pbb)
            a_t, a_b = acT[c % 2]
            h_t, h_b = hi_[c % 2]
            l_t, l_b = lo_[c % 2]
            S.op("act", lambda e, a_t=a_t, pa=pa: e.activation(out=a_t[0:64, :], in_=pa[0:64, :], func=AF.Copy), reads=pab, writes=[a_b])
            S.op("act", lambda e, a_t=a_t, pb_=pb_: e.activation(out=a_t[64:128, :], in_=pb_[64:128, :], func=AF.Copy), reads=pbb, writes=[a_b])
            S.op("dve", lambda e, a_t=a_t, h_t=h_t: e.tensor_copy(out=h_t, in_=a_t), reads=[a_b], writes=[h_b])
            S.op("dve", lambda e, a_t=a_t, h_t=h_t, l_t=l_t: e.tensor_tensor(out=l_t, in0=a_t, in1=h_t, op=ALU.subtract), reads=[a_b, h_b], writes=[l_b])
            S.op("dve", lambda e, l_t=l_t: e.tensor_scalar(out=l_t, in0=l_t, scalar1=mlo[:, 0:1], scalar2=None, op0=ALU.mult), reads=[l_b, mlo_b], writes=[l_b])
            S.op("dve", lambda e, h_t=h_t, l_t=l_t, c=c: e.scalar_tensor_tensor(out=HL[:, c, :], in0=h_t, scalar=mhi[:, 0:1], in1=l_t, op0=ALU.mult, op1=ALU.add),
                 reads=[h_b, l_b, mhi_b], writes=[HL_b])
        S.op("dve", lambda e: e.tensor_scalar(out=HLn, in0=HL, scalar1=-1.0, scalar2=None, op0=ALU.mult), reads=[HL_b], writes=[HLn_b])
        if dbg:
            for nm, src_, sb_, dtp, ncol in (("dbg_dt", dt.rearrange("p a b c -> p (a b c)"), dt_b, F32, 34 * 64),
                                             ("dbg_nacum", nacum.rearrange("p a b c -> p (a b c)"), nacum_b, F32, 34 * 64),
                                             ("dbg_wend", wend.rearrange("p a b c -> p (a b c)"), wend_b, F32, 34 * 64),
                                             ("dbg_HL", HL.rearrange("p a b -> p (a b)"), HL_b, BF16, 34 * 128)):
                o, ob = self.scratch(nm, [128, ncol], dtp, dump=True)
                S.op("sp", lambda e, o=o, src_=src_: e.dma_start(out=o, in_=src_), reads=[sb_], writes=[ob], dma=True)
        S.barrier()
        ar.release()
        P2STOP = int(os.environ.get("P2STOP", "99"))
        P2SKIP = os.environ.get("P2SKIP", "")
        if P2STOP <= 1:
            ar.release()
            return
        TP = 4360
        RA, RA_b = ar.alloc([4 * TP], BF16, "RA")
        R = RA.rearrange("p (a b) -> p a b", a=4)
        prevs = RA[:, 0:2 * 34 * 256].rearrange("p (d c n) -> p d c n", d=2, c=34)
        xc, xc_b = ar.alloc([4, T], BF16, "xc")
        x_tok, x_tok_b = ar.alloc([34, 256], BF16, "x_tok")
        B_tok, B_tok_b = ar.alloc([34, 128], BF16, "B_tok")
        dg, dg_b = ar.alloc([20, 128], BF16, "dg")
        st = [ar.alloc([256], F32, f"st{d}") for d in range(2)]
        xw = [ar.alloc([256], BF16, f"xw{i}") for i in range(4)]
        xdt = [ar.alloc([256], BF16, f"xdt{i}") for i in range(4)]
        E_ = [ar.alloc([4, 128], F32, f"E{i}") for i in range(2)]
        Lt = [ar.alloc([4, 128], F32, f"Lt{i}") for i in range(2)]
        Gt = [ar.alloc([4, 128], BF16, f"Gt{i}") for i in range(4)]
        Cp = [ar.alloc([4, 128], BF16, f"Cp{i}") for i in range(4)]
        zt = [ar.alloc([2, 512], BF16, f"zt{i}") for i in range(1)]
        sz = [ar.alloc([2, 512], F32, f"sz{i}") for i in range(1)]
        yz = [ar.alloc([2, 512], F32, f"yz{i}") for i in range(1)]
        sqy = [ar.alloc([2, 512], BF16, f"sqy{i}") for i in range(1)]
        rsy = [ar.alloc([512], F32, f"rsy{i}") for i in range(1)]
        ybo = [ar.alloc([2, 512], BF16, f"ybo{i}") for i in range(1)]
        bwd_order = [1, 0] + list(range(33, 1, -1))
        cnt = dict(e=0, a=0, g=0, cp=0, xw=0, cs=0, y=0, cb=0, post=0, conv=0, tr=0)
        for g in range(int(os.environ.get('P2G', '8'))):
            tiles = [2 * g, 2 * g + 1, 16 + g, 24 + g]
            for i, tix in enumerate(tiles):
                S.op("sp", lambda e, i=i, tix=tix: e.dma_start(out=R[:, i, 2:258], in_=self.xbc_s[tix * 128:(tix + 1) * 128, 0:256]),
                     reads=[self.xbc_sb], writes=[RA_b], dma=True)
                S.op("sp", lambda e, i=i, tix=tix: e.dma_start(out=R[:, i, 262:4358], in_=self.xbc_s[tix * 128:(tix + 1) * 128, 256:T]),
                     reads=[self.xbc_sb], writes=[RA_b], dma=True)
            for (a0, a1) in ((0, 2), (258, 262), (4358, 4360)):
                S.op("dve", lambda e, a0=a0, a1=a1: e.memset(R[:, :, a0:a1], 0.0), writes=[RA_b])
            for i, tix in enumerate(tiles):
                for kk in range(5):
                    S.op("dve", lambda e, i=i, tix=tix, kk=kk: e.tensor_scalar(
                        out=dg[:, i * 5 + kk, :], in0=ident_f, scalar1=cw[:, tix, kk:kk + 1], scalar2=None, op0=ALU.mult),
                        reads=[ident_f_b, cw_b], writes=[dg_b])
            for i, tix in enumerate(tiles):
                for (t0, n, base) in [(0, 256, 0)] + [(256 + j * 512, 512, 260 + j * 512) for j in range(8)]:
                    pa, pb = self.pq(cnt["conv"] % 2, 0, 4)
                    cnt["conv"] += 1

                    def mm(e, pa=pa, i=i, n=n, base=base):
                        ins = None
                        for kk in range(5):
                            ins = e.matmul(pa[:, 0:n], lhsT=dg[:, i * 5 + kk, :], rhs=R[:, i, base + kk:base + kk + n],
                                           start=(kk == 0), stop=(kk == 4))
                        return ins
                    S.op("pe", mm, reads=[dg_b, RA_b], writes=pb)
                    S.op("act", lambda e, pa=pa, i=i, tix=tix, t0=t0, n=n: e.activation(
                        out=xc[:, i, t0:t0 + n], in_=pa[:, 0:n], func=AF.Silu, bias=cb[:, tix:tix + 1]),
                        reads=pb + [cb_b], writes=[xc_b])
            if P2STOP <= 2:
                break
            for c in range(34):
                pa, pb = self.pq(2 + (cnt["tr"] % 2), 0, 4)
                cnt["tr"] += 1
                pab = pa.bitcast(BF16)

                def tr(e, pab=pab, c=c):
                    ins = None
                    for j in range(3):
                        ins = e.transpose(out=pab[:, j * 128:(j + 1) * 128], in_=xc[:, j, c * 128:(c + 1) * 128], identity=ident_bf)
                    return ins
                S.op("pe", tr, reads=[xc_b, ident_bf_b], writes=pb)
                S.op("dve", lambda e, pab=pab, c=c: e.tensor_copy(out=x_tok[:, c, :], in_=pab[:, 0:256]), reads=pb, writes=[x_tok_b])
                S.op("dve", lambda e, pab=pab, c=c: e.tensor_copy(out=B_tok[:, c, :], in_=pab[:, 256:384]), reads=pb, writes=[B_tok_b])
            if P2STOP <= 3:
                break
            for d in range(2):
                S.op("dve", lambda e, d=d: e.memset(st[d][0], 0.0), writes=[st[d][1]])
            for step in range(34):
                for d in range(2):
                    c = step if d == 0 else bwd_order[step]
                    xw_t, xw_b = xw[cnt["xw"] % 4]
                    cnt["xw"] += 1
                    S.op("dve", lambda e, xw_t=xw_t, c=c, d=d, g=g: e.tensor_tensor(
                        out=xw_t.rearrange("p (r q) -> p r q", r=4), in0=x_tok[:, c, :].rearrange("p (r q) -> p r q", r=4),
                        in1=wend[:, c, d, 4 * g:4 * g + 4].unsqueeze(2).broadcast_to([128, 4, 64]), op=ALU.mult),
                        reads=[x_tok_b, wend_b], writes=[xw_b])
                    k2 = cnt["cs"] % 4
                    cnt["cs"] += 1
                    pa, pb = self.pq(4 + k2, 0, 2)
                    S.op("pe", lambda e, pa=pa, xw_t=xw_t, c=c: e.matmul(pa, lhsT=B_tok[:, c, :], rhs=xw_t, start=True, stop=True),
                         reads=[B_tok_b, xw_b], writes=pb)
                    st_t, st_b = st[d]
                    S.op("act", lambda e, st_t=st_t, d=d, c=c: e.activation(out=prevs[:, d, c, :], in_=st_t, func=AF.Copy),
                         reads=[st_b], writes=[RA_b])
                    S.op("dve", lambda e, st_t=st_t, c=c, d=d, g=g: e.tensor_tensor(
                        out=st_t.rearrange("p (r q) -> p r q", r=4), in0=st_t.rearrange("p (r q) -> p r q", r=4),
                        in1=decay[:, c, d, 4 * g:4 * g + 4].unsqueeze(2).broadcast_to([128, 4, 64]), op=ALU.mult),
                        reads=[st_b, decay_b], writes=[st_b])
                    S.op("dve", lambda e, st_t=st_t, pa=pa: e.tensor_tensor(out=st_t, in0=st_t, in1=pa, op=ALU.add),
                         reads=[st_b] + pb, writes=[st_b])
            if dbg and g == 0:
                o, ob = self.scratch("dbg_xc", [128, 4 * T], BF16, dump=True)
                S.op("sp", lambda e, o=o: e.dma_start(out=o, in_=xc.rearrange("p a b -> p (a b)")), reads=[xc_b], writes=[ob], dma=True)
                o2, ob2 = self.scratch("dbg_prev", [128, 2 * 34 * 256], BF16, dump=True)
                S.op("sp", lambda e, o2=o2: e.dma_start(out=o2, in_=RA[:, 0:2 * 34 * 256]), reads=[RA_b], writes=[ob2], dma=True)
            if P2STOP <= 4:
                continue
            for cb4 in range(8):
                if P2STOP <= 5 and cb4 >= 1:
                    break
                pi = cnt["post"] % 2
                cnt["post"] += 1
                z_t, z_b = zt[0]
                sz_t, sz_b = sz[0]
                yz_t, yz_b = yz[0]
                for i in range(2):
                    r0 = 256 * g + 128 * i
                    S.op("sp", lambda e, z_t=z_t, i=i, r0=r0, cb4=cb4: e.dma_start(out=z_t[:, i, :], in_=self.z_s[r0:r0 + 128, cb4 * 512:(cb4 + 1) * 512]),
                         reads=[self.z_sb], writes=[z_b], dma=True)
                if "Z" not in P2SKIP:
                    S.op("act", lambda e, z_t=z_t, sz_t=sz_t: e.activation(out=sz_t, in_=z_t, func=AF.Silu), reads=[z_b], writes=[sz_b])
                for cl in range(4):
                    lc = cb4 * 4 + cl
                    c = 2 + lc
                    tsl = slice(c * 128, (c + 1) * 128)
                    pcb, pcb_b = self.pq(4 + cnt["cb"] % 2, 0)
                    cnt["cb"] += 1
                    S.op("pe", lambda e, pcb=pcb, tsl=tsl: e.matmul(pcb, lhsT=xc[:, 2, tsl], rhs=xc[:, 3, tsl], start=True, stop=True),
                         reads=[xc_b], writes=pcb_b)
                    gts, cps, xds = {}, {}, {}
                    for d in range(2):
                        xd_t, xd_b = xdt[cnt["xw"] % 4]
                        cnt["xw"] += 1
                        S.op("dve", lambda e, xd_t=xd_t, c=c, d=d, g=g: e.tensor_tensor(
                            out=xd_t.rearrange("p (r q) -> p r q", r=4), in0=x_tok[:, c, :].rearrange("p (r q) -> p r q", r=4),
                            in1=dt[:, c, d, 4 * g:4 * g + 4].unsqueeze(2).broadcast_to([128, 4, 64]), op=ALU.mult),
                            reads=[x_tok_b, dt_b], writes=[xd_b])
                        xds[d] = (xd_t, xd_b)
                        pe_, pe_b = self.pq(cnt["e"] % 2, 0, 4)
                        cnt["e"] += 1

                        def mme(e, pe_=pe_, c=c, d=d, g=g):
                            ins = None
                            for r in range(4):
                                hh = d * 32 + 4 * g + r
                                ins = e.matmul(pe_[:, r * 128:(r + 1) * 128], lhsT=sel2[:, hh, :], rhs=HL[:, c, :], start=True, stop=True)
                            return ins
                        S.op("pe", mme, reads=[sel2_b, HL_b], writes=pe_b)
                        E_t, E_b = E_[cnt["e"] % 2]
                        S.op("act", lambda e, E_t=E_t, pe_=pe_: e.activation(out=E_t.rearrange("p a b -> p (a b)"), in_=pe_, func=AF.Exp),
                             reads=pe_b, writes=[E_b])
                        cp_t, cp_b = Cp[cnt["cp"] % 4]
                        cnt["cp"] += 1
                        S.op("dve", lambda e, cp_t=cp_t, E_t=E_t, tsl=tsl: e.tensor_tensor(
                            out=cp_t, in0=E_t, in1=xc[:, 3, tsl].unsqueeze(1).broadcast_to([128, 4, 128]), op=ALU.mult),
                            reads=[xc_b, E_b], writes=[cp_b])
                        cps[d] = (cp_t, cp_b)
                        pa_, pa_b = self.pq(2 + cnt["a"] % 2, 0, 4)
                        cnt["a"] += 1

                        def mma(e, pa_=pa_, c=c, d=d, g=g):
                            ins = None
                            for r in range(4):
                                hh = d * 32 + 4 * g + r
                                o_ = pa_[:, r * 128:(r + 1) * 128]
                                e.matmul(o_, lhsT=sel2[:, hh, :], rhs=HL[:, c, :], start=True, stop=False)
                                e.matmul(o_, lhsT=HLn[:, c, :], rhs=sel2[:, hh, :], start=False, stop=False)
                                ins = e.matmul(o_, lhsT=ident_bf, rhs=NEG[d][0], start=False, stop=True)
                            return ins
                        S.op("pe", mma, reads=[sel2_b, HL_b, HLn_b, ident_bf_b, NEG[d][1]], writes=pa_b)
                        L_t, L_b = Lt[cnt["a"] % 2]
                        S.op("act", lambda e, L_t=L_t, pa_=pa_: e.activation(out=L_t.rearrange("p a b -> p (a b)"), in_=pa_, func=AF.Exp),
                             reads=pa_b, writes=[L_b])
                        g_t, g_b = Gt[cnt["g"] % 4]
                        cnt["g"] += 1
                        S.op("dve", lambda e, g_t=g_t, L_t=L_t, pcb=pcb: e.tensor_tensor(
                            out=g_t, in0=L_t, in1=pcb.unsqueeze(1).broadcast_to([128, 4, 128]), op=ALU.mult),
                            reads=[L_b] + pcb_b, writes=[g_b])
                        gts[d] = (g_t, g_b)
                    yb_i = 6 + cnt["y"] % 2
                    cnt["y"] += 1
                    pys = [self.pq(yb_i, i) for i in range(2)]

                    def mmy(e, pys=pys, c=c, gts=gts, cps=cps, xds=xds):
                        ins = None
                        for i in range(2):
                            py = pys[i][0]
                            for hf in range(2):
                                r = 2 * i + hf
                                o_ = py[64 * hf:64 * hf + 64, :]
                                tp = (0, 64 * hf)
                                e.matmul(o_, lhsT=xds[0][0][:, r * 64:(r + 1) * 64], rhs=gts[0][0][:, r, :], start=True, stop=False, tile_position=tp)
                                e.matmul(o_, lhsT=prevs[:, 0, c, r * 64:(r + 1) * 64], rhs=cps[0][0][:, r, :], start=False, stop=False, tile_position=tp)
                                e.matmul(o_, lhsT=xds[1][0][:, r * 64:(r + 1) * 64], rhs=gts[1][0][:, r, :], start=False, stop=False, tile_position=tp)
                                ins = e.matmul(o_, lhsT=prevs[:, 1, c, r * 64:(r + 1) * 64], rhs=cps[1][0][:, r, :], start=False, stop=True, tile_position=tp)
                        return ins
                    rd = [RA_b]
                    for d in range(2):
                        rd += [gts[d][1], cps[d][1], xds[d][1]]
                    S.op("pe", mmy, reads=rd, writes=pys[0][1])
                    for i in range(2):
                        py = pys[i][0]
                        S.op("dve", lambda e, py=py, i=i, g=g, tsl=tsl, yz_t=yz_t, cl=cl: e.scalar_tensor_tensor(
                            out=yz_t[:, i, cl * 128:(cl + 1) * 128], in0=xc[:, i, tsl], scalar=dcol[:, 2 * g + i:2 * g + i + 1], in1=py,
                            op0=ALU.mult, op1=ALU.add), reads=[xc_b, dcol_b] + pys[i][1], writes=[yz_b])
                if "O" in P2SKIP:
                    continue
                q_t, q_b = sqy[0]
                r_t, r_b = rsy[0]
                o_t, o_b = ybo[0]
                S.op("dve", lambda e, yz_t=yz_t, sz_t=sz_t: e.tensor_tensor(out=yz_t, in0=yz_t, in1=sz_t, op=ALU.mult), reads=[yz_b, sz_b], writes=[yz_b])
                S.op("act", lambda e, yz_t=yz_t, q_t=q_t: e.activation(out=q_t, in_=yz_t, func=AF.Square), reads=[yz_b], writes=[q_b])
                pp, pp_b = self.pq(pi, 0, 4)

                def mmq(e, pp=pp, q_t=q_t):
                    e.matmul(pp, lhsT=ones_bf, rhs=q_t[:, 0, :], start=True, stop=False)
                    return e.matmul(pp, lhsT=ones_bf, rhs=q_t[:, 1, :], start=False, stop=True)
                S.op("pe", mmq, reads=[q_b, ones_bf_b], writes=pp_b)
                S.op("act", lambda e, pp=pp, r_t=r_t: e.activation(out=r_t, in_=pp, func=AF.Sqrt, scale=1.0 / 256, bias=EPS), reads=pp_b, writes=[r_b])
                S.op("dve", lambda e, r_t=r_t: e.reciprocal(out=r_t, in_=r_t), reads=[r_b], writes=[r_b])
                for i in range(2):
                    S.op("dve", lambda e, i=i, yz_t=yz_t, r_t=r_t, o_t=o_t, g=g: e.scalar_tensor_tensor(
                        out=o_t[:, i, :], in0=yz_t[:, i, :], scalar=nwc[:, 2 * g + i:2 * g + i + 1], in1=r_t, op0=ALU.mult, op1=ALU.mult),
                        reads=[yz_b, r_b, nwc_b], writes=[o_b])
                    r0 = 256 * g + 128 * i
                    S.op("sp", lambda e, o_t=o_t, i=i, r0=r0, cb4=cb4: e.dma_start(out=self.yb_s[r0:r0 + 128, cb4 * 512:(cb4 + 1) * 512], in_=o_t[:, i, :]),
                         reads=[o_b], writes=[self.yb_sb], dma=True)
        S.barrier()
        ar.release()


    def trig(self, ar, src, n, add, out, out_b, src_b, tag):
        S = self.S
        t, t_b = ar.alloc([n], F32, f"trg_t{tag}")
        ti, ti_b = ar.alloc([n], I32, f"trg_i{tag}")
        S.op("dve", lambda e: e.tensor_scalar(out=t, in0=src, scalar1=64.0 + add, scalar2=None, op0=ALU.add), reads=[src_b], writes=[t_b])
        S.op("dve", lambda e: e.tensor_copy(out=ti, in_=t), reads=[t_b], writes=[ti_b])
        S.op("dve", lambda e: e.tensor_copy(out=out, in_=ti), reads=[ti_b], writes=[out_b])
        S.op("dve", lambda e: e.tensor_tensor(out=t, in0=t, in1=out, op=ALU.subtract), reads=[t_b, out_b], writes=[t_b])
        S.op("act", lambda e: e.activation(out=out, in_=t, func=AF.Sin, scale=2.0 * np.pi), reads=[t_b], writes=[out_b])

    def cpow_tables(self, ar, lre, lim, ldt, n, bufs, tag):
        S = self.S
        TB = Buf(f"cpow{tag}")
        step, _ = ar.alloc([n], F32, "step")
        lr, _ = ar.alloc([n], F32, "lr")
        th, _ = ar.alloc([n], F32, "th")
        S.op("act", lambda e: e.activation(out=step, in_=ldt, func=AF.Exp), reads=bufs, writes=[TB])
        S.op("dve", lambda e: e.tensor_tensor(out=lr, in0=lre, in1=step, op=ALU.mult), reads=bufs + [TB], writes=[TB])
        S.op("dve", lambda e: e.scalar_tensor_tensor(out=th, in0=lim, scalar=1.0 / (2.0 * np.pi), in1=step, op0=ALU.mult, op1=ALU.mult),
             reads=bufs + [TB], writes=[TB])
        are, _ = ar.alloc([9, n], F32, "are")
        aim, _ = ar.alloc([9, n], F32, "aim")
        mag, _ = ar.alloc([n], F32, "mag")
        ph, _ = ar.alloc([n], F32, "ph")
        ck, ck_b = ar.alloc([n], F32, "ck")
        sk, sk_b = ar.alloc([n], F32, "sk")
        for k in range(9):
            ar.mark()
            S.op("act", lambda e, k=k: e.activation(out=mag, in_=lr, func=AF.Exp, scale=float(k)), reads=[TB], writes=[TB])
            S.op("dve", lambda e, k=k: e.tensor_scalar(out=ph, in0=th, scalar1=float(k), scalar2=None, op0=ALU.mult), reads=[TB], writes=[TB])
            self.trig(ar, ph, n, 0.25, ck, ck_b, TB, f"{tag}c{k}")
            self.trig(ar, ph, n, 0.0, sk, sk_b, TB, f"{tag}s{k}")
            S.op("dve", lambda e, k=k: e.tensor_tensor(out=are[:, k, :], in0=mag, in1=ck, op=ALU.mult), reads=[TB, ck_b], writes=[TB])
            S.op("dve", lambda e, k=k: e.tensor_tensor(out=aim[:, k, :], in0=mag, in1=sk, op=ALU.mult), reads=[TB, sk_b], writes=[TB])
            ar.release()
        return dict(are=are, aim=aim, lr=lr, th=th, buf=TB)

    def cmul(self, eng, out_re, out_im, a_re, a_im, b_re, b_im, t1, t2, reads, writes):
        S = self.S
        S.op(eng, lambda e: e.tensor_tensor(out=t1, in0=a_re, in1=b_re, op=ALU.mult), reads=reads, writes=writes)
        S.op(eng, lambda e: e.tensor_tensor(out=t2, in0=a_im, in1=b_im, op=ALU.mult), reads=reads, writes=writes)
        S.op(eng, lambda e: e.tensor_tensor(out=out_re, in0=t1, in1=t2, op=ALU.subtract), reads=reads, writes=writes)
        S.op(eng, lambda e: e.tensor_tensor(out=t1, in0=a_re, in1=b_im, op=ALU.mult), reads=reads, writes=writes)
        S.op(eng, lambda e: e.tensor_tensor(out=t2, in0=a_im, in1=b_re, op=ALU.mult), reads=reads, writes=writes)
        S.op(eng, lambda e: e.tensor_tensor(out=out_im, in0=t1, in1=t2, op=ALU.add), reads=reads, writes=writes)

    def bc_coef(self, ar, pw, lre, lim, n, bufs):
        S = self.S
        TB = pw["buf"]
        rd = bufs + [TB]
        nre, _ = ar.alloc([n], F32, "nre")
        den, _ = ar.alloc([n], F32, "den")
        t1, _ = ar.alloc([n], F32, "bct1")
        bcr, _ = ar.alloc([n], F32, "bcr")
        bci, _ = ar.alloc([n], F32, "bci")
        are1, aim1 = pw["are"][:, 1, :], pw["aim"][:, 1, :]
        S.op("dve", lambda e: e.tensor_scalar(out=nre, in0=are1, scalar1=-1.0, scalar2=None, op0=ALU.add), reads=rd, writes=[TB])
        S.op("dve", lambda e: e.tensor_tensor(out=den, in0=lre, in1=lre, op=ALU.mult), reads=rd, writes=[TB])
        S.op("dve", lambda e: e.tensor_tensor(out=t1, in0=lim, in1=lim, op=ALU.mult), reads=rd, writes=[TB])
        S.op("dve", lambda e: e.tensor_tensor(out=den, in0=den, in1=t1, op=ALU.add), reads=rd, writes=[TB])
        S.op("dve", lambda e: e.reciprocal(out=den, in_=den), reads=rd, writes=[TB])
        S.op("dve", lambda e: e.tensor_tensor(out=bcr, in0=nre, in1=lre, op=ALU.mult), reads=rd, writes=[TB])
        S.op("dve", lambda e: e.tensor_tensor(out=t1, in0=aim1, in1=lim, op=ALU.mult), reads=rd, writes=[TB])
        S.op("dve", lambda e: e.tensor_tensor(out=bcr, in0=bcr, in1=t1, op=ALU.add), reads=rd, writes=[TB])
        S.op("dve", lambda e: e.tensor_tensor(out=bcr, in0=bcr, in1=den, op=ALU.mult), reads=rd, writes=[TB])
        S.op("dve", lambda e: e.tensor_tensor(out=bci, in0=aim1, in1=lre, op=ALU.mult), reads=rd, writes=[TB])
        S.op("dve", lambda e: e.tensor_tensor(out=t1, in0=nre, in1=lim, op=ALU.mult), reads=rd, writes=[TB])
        S.op("dve", lambda e: e.tensor_tensor(out=bci, in0=bci, in1=t1, op=ALU.subtract), reads=rd, writes=[TB])
        S.op("dve", lambda e: e.tensor_tensor(out=bci, in0=bci, in1=den, op=ALU.mult), reads=rd, writes=[TB])
        return bcr, bci

    def phase3_setup(self, s5F, s5S):
        nc, S, ar = self.nc, self.S, self.ar
        dbg = self.debug
        ident_f, ident_f_b = self.consts["ident_f"]
        self.SI_s, self.SI_sb = self.scratch("SI_s", [2, 8, 128, 2048], BF16, dump=dbg)
        self.RO_s, self.RO_sb = self.scratch("RO_s", [2, 8, 128, 2048], BF16, dump=dbg)
        self.FIR_s, self.FIR_sb = self.scratch("FIR_s", [2, 8, 128, 1024], BF16, dump=dbg)
        self.RS, self.RS_b = ar.alloc([2, 2, 32], F32, "RS")
        mF = [ar.alloc([1], F32, f"mF{i}") for i in range(2)]
        mS = [ar.alloc([1], F32, f"mS{i}") for i in range(2)]
        mSn = [ar.alloc([1], F32, f"mSn{i}") for i in range(2)]
        self.rowmask = [ar.alloc([1], F32, f"rowm{i}") for i in range(4)]
        for i in range(4):
            S.op("dve", lambda e, i=i: e.reduce_sum(out=self.rowmask[i][0], in_=ident_f[:, 32 * i:32 * i + 32], axis=AX.X),
                 reads=[ident_f_b], writes=[self.rowmask[i][1]])
        for i in range(2):
            S.op("dve", lambda e, i=i: e.reduce_sum(out=mS[i][0], in_=ident_f[:, 64 * i:64 * i + 64], axis=AX.X), reads=[ident_f_b], writes=[mS[i][1]])
            S.op("dve", lambda e, i=i: e.tensor_scalar(out=mSn[i][0], in0=mS[i][0], scalar1=-1.0, scalar2=None, op0=ALU.mult), reads=[mS[i][1]], writes=[mSn[i][1]])
            S.op("dve", lambda e, i=i: e.reduce_sum(out=mF[i][0], in_=ident_f.rearrange("p (a b c) -> p a b c", a=4, b=2)[:, :, i, :], axis=AX.XY),
                 reads=[ident_f_b], writes=[mF[i][1]])
        for d in range(2):
            ar.mark()
            Ft, Ft_b = ar.alloc([5, 512], F32, "Ft")
            S.op("sp", lambda e, d=d: e.dma_start(out=Ft, in_=s5F[d].rearrange("p a f q -> p a (f q)")), writes=[Ft_b], dma=True)
            pw = self.cpow_tables(ar, Ft[:, 0, :], Ft[:, 1, :], Ft[:, 2, :], 512, [Ft_b], f"F{d}")
            TB = pw["buf"]
            bcr, bci = self.bc_coef(ar, pw, Ft[:, 0, :], Ft[:, 1, :], 512, [Ft_b])
            t1, _ = ar.alloc([512], F32, "ft1")
            t2, _ = ar.alloc([512], F32, "ft2")
            bbr, _ = ar.alloc([512], F32, "bbr")
            bbi, _ = ar.alloc([512], F32, "bbi")
            self.cmul("dve", bbr, bbi, bcr, bci, Ft[:, 3, :], Ft[:, 4, :], t1, t2, [Ft_b, TB], [TB])
            wr_, _ = ar.alloc([512], F32, "wr_")
            wi_, _ = ar.alloc([512], F32, "wi_")
            SIt, SIt_b = ar.alloc([8, 8, 2, 2, 64], BF16, "SIt")
            for j in range(8):
                kj = 7 - j if d == 0 else j
                self.cmul("dve", wr_, wi_, pw["are"][:, kj, :], pw["aim"][:, kj, :], bbr, bbi, t1, t2, [TB], [TB])
                for ri, w_ in enumerate((wr_, wi_)):
                    for gq in range(2):
                        S.op("dve", lambda e, j=j, ri=ri, gq=gq, w_=w_: e.tensor_scalar(
                            out=SIt[:, :, j, ri, gq, :], in0=w_.rearrange("p (f q) -> p f q", f=8), scalar1=mF[gq][0][:, 0:1], scalar2=None, op0=ALU.mult),
                            reads=[TB, mF[gq][1]], writes=[SIt_b])
            S.op("sp", lambda e, d=d: e.dma_start(out=self.SI_s[d].rearrange("f p n -> p f n"), in_=SIt.rearrange("p f j r g q -> p f (j r g q)")),
                 reads=[SIt_b], writes=[self.SI_sb], dma=True)
            S.barrier()
            ar.release()
            ar.mark()
            NS = 3 * 32 + 4 * 512
            St, St_b = ar.alloc([NS], F32, "St")
            S.op("sp", lambda e, d=d: e.dma_start(out=St, in_=s5S[d]), writes=[St_b], dma=True)
            lre, lim, ldt = St[:, 0:32], St[:, 32:64], St[:, 64:96]
            Bre = St[:, 96:96 + 512].rearrange("p (g h) -> p g h", h=16)
            Bim = St[:, 96 + 512:96 + 1024].rearrange("p (g h) -> p g h", h=16)
            Cre = St[:, 96 + 1024:96 + 1536].rearrange("p (g h) -> p g h", h=16)
            Cim = St[:, 96 + 1536:96 + 2048].rearrange("p (g h) -> p g h", h=16)
            pw = self.cpow_tables(ar, lre, lim, ldt, 32, [St_b], f"S{d}")
            TB = pw["buf"]
            bcr, bci = self.bc_coef(ar, pw, lre, lim, 32, [St_b])
            bc3 = lambda a: a.unsqueeze(2).broadcast_to([128, 32, 16])
            t1, _ = ar.alloc([32, 16], F32, "st1")
            t2, _ = ar.alloc([32, 16], F32, "st2")
            bbr, _ = ar.alloc([32, 16], F32, "sbbr")
            bbi, _ = ar.alloc([32, 16], F32, "sbbi")
            self.cmul("dve", bbr, bbi, bc3(bcr), bc3(bci), Bre, Bim, t1, t2, [St_b, TB], [TB])
            S.op("act", lambda e, d=d: e.activation(out=self.RS[:, d, 0, :], in_=pw["lr"], func=AF.Exp, scale=8.0), reads=[TB], writes=[self.RS_b])
            p8, p8_b = ar.alloc([32], F32, "p8")
            p8i, p8i_b = ar.alloc([32], I32, "p8i")
            p8f, p8f_b = ar.alloc([32], F32, "p8f")
            S.op("dve", lambda e: e.tensor_scalar(out=p8, in0=pw["th"], scalar1=8.0, scalar2=64.0, op0=ALU.mult, op1=ALU.add), reads=[TB], writes=[p8_b])
            S.op("dve", lambda e: e.tensor_copy(out=p8i, in_=p8), reads=[p8_b], writes=[p8i_b])
            S.op("dve", lambda e: e.tensor_copy(out=p8f, in_=p8i), reads=[p8i_b], writes=[p8f_b])
            S.op("dve", lambda e, d=d: e.tensor_tensor(out=self.RS[:, d, 1, :], in0=p8, in1=p8f, op=ALU.subtract), reads=[p8_b, p8f_b], writes=[self.RS_b])
            vr_, _ = ar.alloc([32, 16], F32, "vr_")
            vi_, _ = ar.alloc([32, 16], F32, "vi_")
            ROt, ROt_b = ar.alloc([32, 8, 2, 2, 16], BF16, "ROt")
            for j in range(8):
                kj = j + 1 if d == 0 else 8 - j
                self.cmul("dve", vr_, vi_, Cre, Cim, bc3(pw["are"][:, kj, :]), bc3(pw["aim"][:, kj, :]), t1, t2, [St_b, TB], [TB])
                for ri, (v_, ms) in enumerate(((vr_, mS), (vi_, mSn))):
                    for gq in range(2):
                        S.op("dve", lambda e, j=j, ri=ri, gq=gq, v_=v_, ms=ms: e.tensor_scalar(
                            out=ROt[:, :, j, ri, gq, :], in0=v_, scalar1=ms[gq][0][:, 0:1], scalar2=None, op0=ALU.mult),
                            reads=[TB, ms[gq][1]], writes=[ROt_b])
            S.op("sp", lambda e, d=d: e.dma_start(out=self.RO_s[d].rearrange("f p n -> p f n"),
                                                in_=ROt.rearrange("p (f g) j r q h -> p f (g j r q h)", f=8)),
                 reads=[ROt_b], writes=[self.RO_sb], dma=True)
            W2p = [ar.alloc([8, 4, 8, 16], BF16, f"W2p{ri}") for ri in range(2)]
            W1p = [[ar.alloc([8, 4, 8, 16], BF16, f"W1p{b}{ri}") for ri in range(2)] for b in range(2)]
            for t_, tb_ in W2p + W1p[0] + W1p[1]:
                S.op("dve", lambda e, t_=t_: e.memset(t_, 0.0), writes=[tb_])
            g4 = lambda a: a.rearrange("p (f g) h -> p f g h", g=4)
            for ri, (c_, ms) in enumerate(((Cre, mS), (Cim, mSn))):
                for gp4 in range(4):
                    for gq in range(2):
                        S.op("dve", lambda e, ri=ri, gp4=gp4, gq=gq, c_=c_, ms=ms: e.tensor_scalar(
                            out=W2p[ri][0][:, :, gp4, 2 * gp4 + gq, :], in0=g4(c_)[:, :, gp4, :], scalar1=ms[gq][0][:, 0:1], scalar2=None, op0=ALU.mult),
                            reads=[St_b, ms[gq][1]], writes=[W2p[ri][1]])
            FIRt, FIRt_b = ar.alloc([8, 8, 128], BF16, "FIRt")
            w1r, _ = ar.alloc([32, 16], F32, "w1r")
            w1i, _ = ar.alloc([32, 16], F32, "w1i")
            cnt = 0
            for tau in range(8):
                self.cmul("dve", w1r, w1i, bc3(pw["are"][:, tau, :]), bc3(pw["aim"][:, tau, :]), bbr, bbi, t1, t2, [TB], [TB])
                W1 = W1p[tau % 2]
                for ri, w_ in enumerate((w1r, w1i)):
                    for gp4 in range(4):
                        for gq in range(2):
                            S.op("dve", lambda e, ri=ri, gp4=gp4, gq=gq, w_=w_, W1=W1: e.tensor_scalar(
                                out=W1[ri][0][:, :, gp4, 2 * gp4 + gq, :], in0=g4(w_)[:, :, gp4, :], scalar1=mS[gq][0][:, 0:1], scalar2=None, op0=ALU.mult),
                                reads=[TB, mS[gq][1]], writes=[W1[ri][1]])
                for fc in range(8):
                    pa, pb = self.pq(cnt % 8, 0)
                    cnt += 1

                    def mm(e, pa=pa, fc=fc, W1=W1):
                        ins = None
                        k = 0
                        for ri in range(2):
                            for gp4 in range(4):
                                ins = e.matmul(pa, lhsT=W1[ri][0][:, fc, gp4, :, :].rearrange("p a b -> p (a b)"),
                                               rhs=W2p[ri][0][:, fc, gp4, :, :].rearrange("p a b -> p (a b)"), start=(k == 0), stop=(k == 7))
                                k += 1
                        return ins
                    S.op("pe", mm, reads=[W1[0][1], W1[1][1], W2p[0][1], W2p[1][1]], writes=pb)
                    eng = "act" if cnt % 2 == 0 else "dve"
                    if eng == "act":
                        S.op("act", lambda e, pa=pa, fc=fc, tau=tau: e.activation(out=FIRt[:, fc, tau, :], in_=pa, func=AF.Copy), reads=pb, writes=[FIRt_b])
                    else:
                        S.op("dve", lambda e, pa=pa, fc=fc, tau=tau: e.tensor_copy(out=FIRt[:, fc, tau, :], in_=pa), reads=pb, writes=[FIRt_b])
            S.op("sp", lambda e, d=d: e.dma_start(out=self.FIR_s[d].rearrange("f p n -> p f n"), in_=FIRt.rearrange("p f t n -> p f (t n)")),
                 reads=[FIRt_b], writes=[self.FIR_sb], dma=True)
            S.barrier()
            ar.release()
        if dbg:
            o, ob = self.scratch("dbg_RS", [128, 128], F32, dump=True)
            S.op("sp", lambda e: e.dma_start(out=o, in_=self.RS.rearrange("p a b c -> p (a b c)")), reads=[self.RS_b], writes=[ob], dma=True)


    def phase3_main(self, s5_d):
        nc, S, ar = self.nc, self.S, self.ar
        ps = self.ps
        dbg = self.debug
        self.g_s, self.g_sb = self.scratch("g_s", [D, L], BF16, dump=dbg)
        RS, RS_b = self.RS, self.RS_b
        NCH = 544
        ar.mark()
        dcol, dcol_b = ar.alloc([8], F32, "s5d")
        S.op("sp", lambda e: e.dma_start(out=dcol, in_=s5_d), writes=[dcol_b], dma=True)
        iot_i, iot_ib = ar.alloc([NCH], I32, "iota_i")
        iot, iot_b = ar.alloc([NCH], F32, "iota_f")
        S.op("pool", lambda e: e.iota(iot_i, pattern=[[1, NCH]], base=0, channel_multiplier=0), writes=[iot_ib])
        S.op("dve", lambda e: e.tensor_copy(out=iot, in_=iot_i), reads=[iot_ib], writes=[iot_b])
        u_t, u_b = ar.alloc([T], BF16, "u_fc")
        um, um_b = ar.alloc([4, T], BF16, "um")
        SI_t, SI_b = ar.alloc([2, 2048], BF16, "SI_t")
        RO_t, RO_b = ar.alloc([2, 2048], BF16, "RO_t")
        FIR_t, FIR_b = ar.alloc([2, 1024], BF16, "FIR_t")
        cn, cn_b = ar.alloc([4, NCH], F32, "cn")
        sn, sn_b = ar.alloc([4, NCH], F32, "sn")
        pht, pht_b = ar.alloc([4, NCH], F32, "pht")
        phi_, phi_b = ar.alloc([4, NCH], I32, "phi_")
        self._s5_frac, self._s5_frac_b = ar.alloc([4, NCH], F32, "s5frac")
        Ssb = [[ar.alloc([NCH], F32, f"Ssb{k}{ri}") for ri in range(2)] for k in range(2)]
        V = [ar.alloc([NCH], F32, f"V{ri}") for ri in range(2)]
        W = [ar.alloc([NCH], F32, f"W{ri}") for ri in range(2)]
        tmp = [ar.alloc([NCH], F32, f"s5t{i}") for i in range(4)]
        Zp = [[[ar.alloc([512], BF16, f"Zp{d}{g}{ri}") for ri in range(2)] for g in range(4)] for d in range(2)]
        gst = [ar.alloc([L], BF16, f"gst{i}") for i in range(2)]
        ys = [ar.alloc([2, 256], F32, f"ys{i}") for i in range(2)]
        kset = 0
        for fc in range(8):
            S.op("sp", lambda e, fc=fc: e.dma_start(out=u_t, in_=self.s5u[fc * 128:(fc + 1) * 128, :]), reads=[self.s5u_b], writes=[u_b], dma=True)
            S.op("sp", lambda e, fc=fc: e.dma_start(out=SI_t, in_=self.SI_s[:, fc].rearrange("d p n -> p d n")), reads=[self.SI_sb], writes=[SI_b], dma=True)
            S.op("sp", lambda e, fc=fc: e.dma_start(out=RO_t, in_=self.RO_s[:, fc].rearrange("d p n -> p d n")), reads=[self.RO_sb], writes=[RO_b], dma=True)
            S.op("sp", lambda e, fc=fc: e.dma_start(out=FIR_t, in_=self.FIR_s[:, fc].rearrange("d p n -> p d n")), reads=[self.FIR_sb], writes=[FIR_b], dma=True)
            for i in range(4):
                S.op("act", lambda e, i=i: e.activation(out=um[:, i, :], in_=u_t, func=AF.Copy, scale=self.rowmask[i][0][:, 0:1]),
                     reads=[u_b, self.rowmask[i][1]], writes=[um_b])
            for d in range(2):
                S.op("dve", lambda e, d=d, fc=fc: e.tensor_tensor(
                    out=pht, in0=iot.unsqueeze(1).broadcast_to([128, 4, NCH]),
                    in1=RS[:, d, 1, 4 * fc:4 * fc + 4].unsqueeze(2).broadcast_to([128, 4, NCH]), op=ALU.mult),
                    reads=[iot_b, RS_b], writes=[pht_b])
                for (dst, dst_b, add) in ((cn, cn_b, 0.25), (sn, sn_b, 0.0)):
                    S.op("dve", lambda e, dst=dst, add=add: e.tensor_scalar(out=dst, in0=pht, scalar1=64.0 + add, scalar2=None, op0=ALU.add),
                         reads=[pht_b], writes=[dst_b])
                    S.op("dve", lambda e, dst=dst: e.tensor_copy(out=phi_, in_=dst), reads=[dst_b], writes=[phi_b])
                    S.op("dve", lambda e, dst=dst: e.tensor_copy(out=self._s5_frac, in_=phi_), reads=[phi_b], writes=[self._s5_frac_b])
                    S.op("dve", lambda e, dst=dst: e.tensor_tensor(out=dst, in0=dst, in1=self._s5_frac, op=ALU.subtract),
                         reads=[dst_b, self._s5_frac_b], writes=[dst_b])
                    S.op("act", lambda e, dst=dst: e.activation(out=dst, in_=dst, func=AF.Sin, scale=2.0 * np.pi), reads=[dst_b], writes=[dst_b])
                for gp4 in range(4):
                    gp = 4 * fc + gp4
                    b0 = 3 * (kset % 2)
                    kset += 1
                    bufs3 = [self.psb[b0], self.psb[b0 + 1], self.psb[b0 + 2]]

                    def mm(e, d=d, gp4=gp4, b0=b0):
                        ins = None
                        for ri in range(2):
                            for j in range(8):
                                ins = e.matmul(ps[b0 + ri][:, 0:512], lhsT=SI_t[:, d, (j * 2 + ri) * 128:(j * 2 + ri + 1) * 128],
                                               rhs=um[:, gp4, LC + j:T:8], start=(j == 0), stop=(j == 7))
                        for ri in range(2):
                            for j in range(8):
                                ins = e.matmul(ps[b0 + 2][:, ri * 32:(ri + 1) * 32], lhsT=SI_t[:, d, (j * 2 + ri) * 128:(j * 2 + ri + 1) * 128],
                                               rhs=um[:, gp4, j:LC:8], start=(j == 0), stop=(j == 7))
                        return ins
                    S.op("pe", mm, reads=[SI_b, um_b], writes=bufs3)
                    Sk = Ssb[kset % 2]
                    for ri in range(2):
                        s_t, s_b = Sk[ri]
                        src_c = ps[b0 + 2][:, ri * 32:(ri + 1) * 32]
                        src_l = ps[b0 + ri][:, 0:512]
                        if d == 1:
                            src_c = src_c[:, ::-1]
                            src_l = src_l[:, ::-1]
                        S.op("act", lambda e, s_t=s_t, src_c=src_c: e.activation(out=s_t[:, 0:32], in_=src_c, func=AF.Copy),
                             reads=[self.psb[b0 + 2]], writes=[s_b])
                        S.op("act", lambda e, s_t=s_t, src_l=src_l: e.activation(out=s_t[:, 32:NCH], in_=src_l, func=AF.Copy),
                             reads=[self.psb[b0 + ri]], writes=[s_b])
                    (Sr, Sr_b), (Si, Si_b) = Sk
                    cnv, snv = cn[:, gp4, :], sn[:, gp4, :]
                    (t1, t1b), (t2, t2b), (t3, t3b), (t4, t4b) = tmp
                    (Vr, Vr_b), (Vi, Vi_b) = V
                    (Wr, Wr_b), (Wi, Wi_b) = W
                    S.op("dve", lambda e, cnv=cnv, Sr=Sr: e.tensor_tensor(out=t1, in0=cnv, in1=Sr, op=ALU.mult), reads=[cn_b, Sr_b], writes=[t1b])
                    S.op("dve", lambda e, snv=snv, Si=Si: e.tensor_tensor(out=t2, in0=snv, in1=Si, op=ALU.mult), reads=[sn_b, Si_b], writes=[t2b])
                    S.op("dve", lambda e, cnv=cnv, Si=Si: e.tensor_tensor(out=t3, in0=cnv, in1=Si, op=ALU.mult), reads=[cn_b, Si_b], writes=[t3b])
                    S.op("dve", lambda e, snv=snv, Sr=Sr: e.tensor_tensor(out=t4, in0=snv, in1=Sr, op=ALU.mult), reads=[sn_b, Sr_b], writes=[t4b])
                    S.op("dve", lambda e: e.tensor_tensor(out=Vr, in0=t1, in1=t2, op=ALU.add), reads=[t1b, t2b], writes=[Vr_b])
                    S.op("dve", lambda e: e.tensor_tensor(out=Vi, in0=t3, in1=t4, op=ALU.subtract), reads=[t3b, t4b], writes=[Vi_b])
                    Rb = RS[:, d, 0, gp:gp + 1].broadcast_to([128, NCH])
                    S.op("dve", lambda e, Rb=Rb: e.tensor_tensor_scan(out=Wr, data0=Rb, data1=Vr, initial=0.0, op0=ALU.mult, op1=ALU.add),
                         reads=[RS_b, Vr_b], writes=[Wr_b])
                    S.op("dve", lambda e, Rb=Rb: e.tensor_tensor_scan(out=Wi, data0=Rb, data1=Vi, initial=0.0, op0=ALU.mult, op1=ALU.add),
                         reads=[RS_b, Vi_b], writes=[Wi_b])
                    sl = slice(31, 543)
                    (zr, zr_b), (zi, zi_b) = Zp[d][gp4]
                    zro = zr if d == 0 else zr[:, ::-1]
                    zio = zi if d == 0 else zi[:, ::-1]
                    S.op("dve", lambda e, cnv=cnv: e.tensor_tensor(out=t1[:, sl], in0=cnv[:, sl], in1=Wr[:, sl], op=ALU.mult), reads=[cn_b, Wr_b], writes=[t1b])
                    S.op("dve", lambda e, snv=snv: e.tensor_tensor(out=t2[:, sl], in0=snv[:, sl], in1=Wi[:, sl], op=ALU.mult), reads=[sn_b, Wi_b], writes=[t2b])
                    S.op("dve", lambda e, cnv=cnv: e.tensor_tensor(out=t3[:, sl], in0=cnv[:, sl], in1=Wi[:, sl], op=ALU.mult), reads=[cn_b, Wi_b], writes=[t3b])
                    S.op("dve", lambda e, snv=snv: e.tensor_tensor(out=t4[:, sl], in0=snv[:, sl], in1=Wr[:, sl], op=ALU.mult), reads=[sn_b, Wr_b], writes=[t4b])
                    S.op("dve", lambda e, zro=zro: e.tensor_tensor(out=zro, in0=t1[:, sl], in1=t2[:, sl], op=ALU.subtract), reads=[t1b, t2b], writes=[zr_b])
                    S.op("dve", lambda e, zio=zio: e.tensor_tensor(out=zio, in0=t3[:, sl], in1=t4[:, sl], op=ALU.add), reads=[t3b, t4b], writes=[zi_b])
            g_t, g_b = gst[fc % 2]
            for hb in range(2):
                c0 = 256 * hb
                for j in range(8):
                    bank = 4 + j // 2
                    reg = ps[bank][:, (j % 2) * 256:(j % 2) * 256 + 256]

                    def mmy(e, j=j, reg=reg, c0=c0):
                        ops = []
                        for tau in range(0, j + 1):
                            st0 = LC + 8 * c0 + (j - tau)
                            ops.append((reg, FIR_t[:, 0, tau * 128:(tau + 1) * 128], u_t[:, st0:st0 + 2041:8], None))
                        for tau in range(0, 8 - j):
                            st0 = LC + 8 * c0 + (j + tau)
                            ops.append((reg, FIR_t[:, 1, tau * 128:(tau + 1) * 128], u_t[:, st0:st0 + 2041:8], None))
                        for d in range(2):
                            for gp4 in range(4):
                                for ri in range(2):
                                    o0 = ((gp4 * 8 + j) * 2 + ri) * 32
                                    ops.append((reg[32 * gp4:32 * gp4 + 32, :], RO_t[:, d, o0:o0 + 32], Zp[d][gp4][ri][0][:, c0:c0 + 256], (0, 32 * gp4)))
                        ins = None
                        for k, (o_, l_, r_, tp) in enumerate(ops):
                            if tp is None:
                                ins = e.matmul(o_, lhsT=l_, rhs=r_, start=(k == 0), stop=(k == len(ops) - 1))
                            else:
                                ins = e.matmul(o_, lhsT=l_, rhs=r_, start=(k == 0), stop=(k == len(ops) - 1), tile_position=tp)
                        return ins
                    rd = [FIR_b, RO_b, u_b] + [Zp[d][g][ri][1] for d in range(2) for g in range(4) for ri in range(2)]
                    S.op("pe", mmy, reads=rd, writes=[self.psb[bank]])
                    if j % 2 == 1:
                        j0 = j - 1
                        y_t, y_b = ys[(j // 2) % 2]
                        base = LC + 8 * c0
                        uv = u_t[:, base:base + 2048].rearrange("p (c j) -> p j c", j=8)[:, j0:j0 + 2, :]
                        S.op("dve", lambda e, y_t=y_t, uv=uv, bank=bank, fc=fc: e.scalar_tensor_tensor(
                            out=y_t, in0=uv, scalar=dcol[:, fc:fc + 1], in1=ps[bank][:].rearrange("p (j c) -> p j c", j=2),
                            op0=ALU.mult, op1=ALU.add), reads=[u_b, dcol_b, self.psb[bank]], writes=[y_b])
                        outv = g_t.rearrange("p (c8 j row) -> p j row c8", c8=8, j=8)[:, j0:j0 + 2, 32 * hb:32 * hb + 32, :]
                        S.op("act", lambda e, y_t=y_t, outv=outv: e.activation(
                            out=outv, in_=y_t.rearrange("p j (r c8) -> p j r c8", c8=8), func=AF.Gelu_apprx_tanh),
                            reads=[y_b], writes=[g_b])
            S.op("sp", lambda e, fc=fc, g_t=g_t: e.dma_start(out=self.g_s[fc * 128:(fc + 1) * 128, :], in_=g_t), reads=[g_b], writes=[self.g_sb], dma=True)
        S.barrier()
        ar.release()


    def phase4(self, glu_w, glu_b, w_a, w_b, w_o, x_tok, nfw, r_w, r_b):
        nc, S, ar = self.nc, self.S, self.ar
        ps, psb = self.ps, self.psb
        dbg = self.debug
        ident_f, ident_f_b = self.consts["ident_f"]
        self.h1_s, self.h1_sb = self.scratch("h1_s", [L, D], F32, dump=dbg)
        self.uf_s, self.uf_sb = self.scratch("uf_s", [L, D], BF16, dump=dbg)
        self.logits, self.logits_b = ar.alloc([32, 32], F32, "logits")
        ar.mark()
        wv = lambda w: w.rearrange("(kc p) n -> p kc n", p=128)
        Wg, Wg_b = ar.alloc([8, D], BF16, "Wglu")
        Wa, Wa_b = ar.alloc([8, D], BF16, "Wa")
        Wb, Wb_b = ar.alloc([16, D], BF16, "Wb")
        Wo, Wo_b = ar.alloc([8, D], BF16, "Wo")
        for (t_, tb_, src, nk) in ((Wg, Wg_b, glu_w, 8), (Wa, Wa_b, w_a, 8), (Wb, Wb_b, w_b, 16), (Wo, Wo_b, w_o, 8)):
            for k0 in range(0, nk, 4):
                S.op("pool", lambda e, t_=t_, src=src, k0=k0: e.dma_start(out=t_[:, k0:k0 + 4, :], in_=wv(src)[:, k0:k0 + 4, :]), writes=[tb_], dma=True)
        Wr, Wr_b = ar.alloc([8, 32], F32, "Wr")
        S.op("sp", lambda e: e.dma_start(out=Wr, in_=r_w.rearrange("(kc p) n -> p kc n", p=128)), writes=[Wr_b], dma=True)
        rb_rep, rb_rep_b = ar.alloc([32], F32, "rb_rep")
        S.op("sp", lambda e: e.dma_start(out=rb_rep, in_=r_b.broadcast_to([128, 32])), writes=[rb_rep_b], dma=True)
        gb_col, gb_col_b = ar.alloc([8], F32, "glu_b")
        S.op("sp", lambda e: e.dma_start(out=gb_col, in_=glu_b), writes=[gb_col_b], dma=True)
        gm_rep, gm_rep_b = ar.alloc([D], F32, "gm_rep")
        Af_rep, Af_rep_b = ar.alloc([D], F32, "Af_rep")
        Bf_rep, Bf_rep_b = ar.alloc([D], F32, "Bf_rep")
        S.op("sp", lambda e: e.dma_start(out=gm_rep, in_=self.mod_s[:, 2 * D:3 * D]), reads=[self.mod_sb], writes=[gm_rep_b], dma=True)
        S.op("sp", lambda e: e.dma_start(out=Bf_rep, in_=self.mod_s[:, 3 * D:4 * D]), reads=[self.mod_sb], writes=[Bf_rep_b], dma=True)
        S.op("sp", lambda e: e.dma_start(out=Af_rep, in_=self.mod_s[:, 4 * D:5 * D]), reads=[self.mod_sb], writes=[Af_rep_b], dma=True)
        nf_rep, nf_rep_b = ar.alloc([D], F32, "nf_rep")
        S.op("sp", lambda e: e.dma_start(out=nf_rep, in_=nfw.broadcast_to([128, D])), writes=[nf_rep_b], dma=True)
        S.op("dve", lambda e: e.scalar_tensor_tensor(out=Af_rep, in0=Af_rep, scalar=1.0, in1=nf_rep, op0=ALU.add, op1=ALU.mult),
             reads=[Af_rep_b, nf_rep_b], writes=[Af_rep_b])
        g_t, g_b = ar.alloc([8, 512], BF16, "m_g")
        yb_t, yb_b = ar.alloc([16, 512], BF16, "m_yb")
        gab_t, gab_b = ar.alloc([16, 512], BF16, "m_gab")
        ya_t, ya_b = ar.alloc([8, 512], BF16, "m_ya")
        m1_t, m1_b = ar.alloc([8, 512], F32, "m_m1")
        mg_t, mg_b = ar.alloc([8, 512], BF16, "m_mg")
        sg = [ar.alloc([512], F32, f"m_sg{i}") for i in range(2)]
        xk = [ar.alloc([D], F32, f"m_xk{i}") for i in range(1)]
        h1 = [ar.alloc([D], F32, f"m_h1{i}") for i in range(2)]
        uft = [ar.alloc([D], F32, f"m_uft{i}") for i in range(1)]
        ufb = [ar.alloc([D], BF16, f"m_ufb{i}") for i in range(2)]
        ufT = [ar.alloc([8, 128], F32, f"m_ufT{i}") for i in range(1)]
        sq_junk, sq_junk_b = ar.alloc([D], BF16, "m_sqj")
        ss = [ar.alloc([1], F32, f"m_ss{i}") for i in range(2)]
        pc = [0]

        def bank():
            b = pc[0] % 8
            pc[0] += 1
            return b
        for tt in range(8):
            cs = slice(tt * 512, (tt + 1) * 512)
            S.op("sp", lambda e, cs=cs: e.dma_start(out=g_t, in_=self.g_s.rearrange("(kc p) n -> p kc n", p=128)[:, :, cs]), reads=[self.g_sb], writes=[g_b], dma=True)
            S.op("sp", lambda e, cs=cs: e.dma_start(out=yb_t, in_=self.yb_s.rearrange("(kc p) n -> p kc n", p=128)[:, :, cs]), reads=[self.yb_sb], writes=[yb_b], dma=True)
            S.op("sp", lambda e, cs=cs: e.dma_start(out=gab_t, in_=self.gab_s.rearrange("(kc p) n -> p kc n", p=128)[:, :, cs]), reads=[self.gab_sb], writes=[gab_b], dma=True)
            for mc in range(8):
                b_ = bank()

                def mm(e, b_=b_, mc=mc):
                    ins = None
                    for kc in range(8):
                        ins = e.matmul(ps[b_][:], lhsT=Wg[:, kc, mc * 128:(mc + 1) * 128], rhs=g_t[:, kc, :], start=(kc == 0), stop=(kc == 7))
                    return ins
                S.op("pe", mm, reads=[Wg_b, g_b], writes=[psb[b_]])
                s_t, s_b = sg[mc % 2]
                S.op("act", lambda e, s_t=s_t, b_=b_, mc=mc: e.activation(out=s_t, in_=ps[b_][:], func=AF.Sigmoid, bias=gb_col[:, mc:mc + 1]),
                     reads=[psb[b_], gb_col_b], writes=[s_b])
                S.op("dve", lambda e, s_t=s_t, mc=mc: e.tensor_tensor(out=ya_t[:, mc, :], in0=s_t, in1=g_t[:, mc, :], op=ALU.mult),
                     reads=[s_b, g_b], writes=[ya_b])
            for mc in range(8):
                b_ = bank()

                def mm(e, b_=b_, mc=mc):
                    ins = None
                    for kc in range(8):
                        ins = e.matmul(ps[b_][:], lhsT=Wa[:, kc, mc * 128:(mc + 1) * 128], rhs=ya_t[:, kc, :], start=(kc == 0), stop=(kc == 7))
                    return ins
                S.op("pe", mm, reads=[Wa_b, ya_b], writes=[psb[b_]])
                s_t, s_b = sg[mc % 2]
                S.op("act", lambda e, s_t=s_t, mc=mc: e.activation(out=s_t, in_=gab_t[:, mc, :], func=AF.Sigmoid), reads=[gab_b], writes=[s_b])
                S.op("dve", lambda e, s_t=s_t, b_=b_, mc=mc: e.tensor_tensor(out=m1_t[:, mc, :], in0=s_t, in1=ps[b_][:], op=ALU.mult),
                     reads=[s_b, psb[b_]], writes=[m1_b])
            for mc in range(8):
                b_ = bank()

                def mm(e, b_=b_, mc=mc):
                    ins = None
                    for kc in range(16):
                        ins = e.matmul(ps[b_][:], lhsT=Wb[:, kc, mc * 128:(mc + 1) * 128], rhs=yb_t[:, kc, :], start=(kc == 0), stop=(kc == 15))
                    return ins
                S.op("pe", mm, reads=[Wb_b, yb_b], writes=[psb[b_]])
                s_t, s_b = sg[mc % 2]
                S.op("act", lambda e, s_t=s_t, mc=mc: e.activation(out=s_t, in_=gab_t[:, 8 + mc, :], func=AF.Sigmoid), reads=[gab_b], writes=[s_b])
                S.op("dve", lambda e, s_t=s_t, b_=b_: e.tensor_tensor(out=s_t, in0=s_t, in1=ps[b_][:], op=ALU.mult), reads=[s_b, psb[b_]], writes=[s_b])
                S.op("dve", lambda e, s_t=s_t, mc=mc: e.tensor_tensor(out=mg_t[:, mc, :], in0=s_t, in1=m1_t[:, mc, :], op=ALU.add),
                     reads=[s_b, m1_b], writes=[mg_b])
            for sub in range(4):
                ti = tt * 4 + sub
                x_t, x_b = xk[0]
                h_t, h_b = h1[ti % 2]
                S.op("sp", lambda e, x_t=x_t, ti=ti: e.dma_start(out=x_t, in_=x_tok[ti * 128:(ti + 1) * 128, :]), writes=[x_b], dma=True)
                for half in range(2):
                    b_ = bank()

                    def mm(e, b_=b_, sub=sub, half=half):
                        ins = None
                        for kc in range(8):
                            ins = e.matmul(ps[b_][:], lhsT=mg_t[:, kc, sub * 128:(sub + 1) * 128], rhs=Wo[:, kc, half * 512:(half + 1) * 512],
                                           start=(kc == 0), stop=(kc == 7))
                        return ins
                    S.op("pe", mm, reads=[mg_b, Wo_b], writes=[psb[b_]])
                    hs = slice(half * 512, (half + 1) * 512)
                    S.op("dve", lambda e, h_t=h_t, b_=b_, hs=hs: e.tensor_tensor(out=h_t[:, hs], in0=ps[b_][:], in1=gm_rep[:, hs], op=ALU.mult),
                         reads=[psb[b_], gm_rep_b], writes=[h_b])
                    S.op("dve", lambda e, h_t=h_t, x_t=x_t, hs=hs: e.tensor_tensor(out=h_t[:, hs], in0=h_t[:, hs], in1=x_t[:, hs], op=ALU.add),
                         reads=[h_b, x_b], writes=[h_b])
                S.op("sp", lambda e, h_t=h_t, ti=ti: e.dma_start(out=self.h1_s[ti * 128:(ti + 1) * 128, :], in_=h_t), reads=[h_b], writes=[self.h1_sb], dma=True)
                s_t, s_b = ss[ti % 2]
                S.op("act", lambda e, h_t=h_t, s_t=s_t: e.activation(out=sq_junk, in_=h_t, func=AF.Square, accum_out=s_t), reads=[h_b], writes=[sq_junk_b, s_b])
                S.op("act", lambda e, s_t=s_t: e.activation(out=s_t, in_=s_t, func=AF.Sqrt, scale=1.0 / D, bias=EPS), reads=[s_b], writes=[s_b])
                S.op("dve", lambda e, s_t=s_t: e.reciprocal(out=s_t, in_=s_t), reads=[s_b], writes=[s_b])
                u_t, u_b = uft[0]
                ub_t, ub_b = ufb[ti % 2]
                S.op("dve", lambda e, u_t=u_t, h_t=h_t, s_t=s_t: e.scalar_tensor_tensor(out=u_t, in0=h_t, scalar=s_t[:, 0:1], in1=Af_rep, op0=ALU.mult, op1=ALU.mult),
                     reads=[h_b, s_b, Af_rep_b], writes=[u_b])
                S.op("dve", lambda e, u_t=u_t: e.tensor_tensor(out=u_t, in0=u_t, in1=Bf_rep, op=ALU.add), reads=[u_b, Bf_rep_b], writes=[u_b])
                S.op("act", lambda e, u_t=u_t, ub_t=ub_t: e.activation(out=ub_t, in_=u_t, func=AF.Copy), reads=[u_b], writes=[ub_b])
                S.op("sp", lambda e, ub_t=ub_t, ti=ti: e.dma_start(out=self.uf_s[ti * 128:(ti + 1) * 128, :], in_=ub_t), reads=[ub_b], writes=[self.uf_sb], dma=True)
                T_t, T_b = ufT[0]
                for half in range(2):
                    b_ = bank()

                    def tr(e, b_=b_, u_t=u_t, half=half):
                        ins = None
                        for q in range(4):
                            kc = half * 4 + q
                            ins = e.transpose(out=ps[b_][:, q * 128:(q + 1) * 128], in_=u_t[:, kc * 128:(kc + 1) * 128], identity=ident_f)
                        return ins
                    S.op("pe", tr, reads=[u_b, ident_f_b], writes=[psb[b_]])
                    S.op("act", lambda e, T_t=T_t, b_=b_, half=half: e.activation(
                        out=T_t[:, half * 4:half * 4 + 4, :].rearrange("p a b -> p (a b)"), in_=ps[b_][:], func=AF.Copy), reads=[psb[b_]], writes=[T_b])
                b_ = bank()

                def mml(e, b_=b_, T_t=T_t):
                    ins = None
                    for kc in range(8):
                        ins = e.matmul(ps[b_][:, 0:32], lhsT=T_t[:, kc, :], rhs=Wr[:, kc, :], start=(kc == 0), stop=(kc == 7))
                    return ins
                S.op("pe", mml, reads=[T_b, Wr_b], writes=[psb[b_]])
                S.op("dve", lambda e, b_=b_, ti=ti: e.tensor_tensor(out=self.logits[:, ti, :], in0=ps[b_][:, 0:32], in1=rb_rep, op=ALU.add),
                     reads=[psb[b_], rb_rep_b], writes=[self.logits_b])
        if dbg:
            o, ob = self.scratch("dbg_logits", [128, 1024], F32, dump=True)
            S.op("sp", lambda e: e.dma_start(out=o, in_=self.logits.rearrange("p a b -> p (a b)")), reads=[self.logits_b], writes=[ob], dma=True)
        S.barrier()
        ar.release()


    def phase5(self, wg_d, wu_d, wd_d, bg_d, bu_d, bd_d, fnw):
        nc, S, ar = self.nc, self.S, self.ar
        ps, psb = self.ps, self.psb
        dbg = self.debug
        ident_f, ident_f_b = self.consts["ident_f"]
        ones_f, ones_f_b = self.consts["ones_f"]
        ones_bf, ones_bf_b = self.consts["ones_bf"]
        BLK = int(os.environ.get('MOE_BLK', '256'))
        NSUB = BLK // 128
        NT, NE = 32, 32
        NB = -(-(16384 + 32 * (BLK - 1)) // BLK)
        NSLOT = NB * BLK
        self.xs, self.xs_b = self.scratch("xs", [NSLOT, D], BF16)
        self.ys, self.ys_b = self.scratch("ys", [NSLOT, D], BF16)
        logits, logits_b = self.logits, self.logits_b
        ar.mark()
        dest_i, dest_ib = ar.alloc([NT, 4], I32, "dest_i")
        gate4, gate4_b = ar.alloc([NT, 4], F32, "gate4")
        blk_i, blk_ib = ar.alloc([NB], I32, "blk_i")
        chg_i, chg_ib = ar.alloc([NB], I32, "chg_i")
        widx, widx_b = ar.alloc([NB], I32, "widx")
        bidx, bidx_b = ar.alloc([NB], I32, "bidx")
        ident_bf, ident_bf_b = ar.alloc([128], BF16, "ident_bf5")
        S.op("dve", lambda e: e.tensor_copy(out=ident_bf, in_=ident_f), reads=[ident_f_b], writes=[ident_bf_b])
        ar.mark()
        top8, top8_b = ar.alloc([NT, 8], F32, "top8")
        mask, mask_b = ar.alloc([NT, NE], F32, "mask")
        mask_bf, mask_bfb = ar.alloc([NT, NE], BF16, "mask_bf")
        gatef, gatef_b = ar.alloc([NT, NE], F32, "gatef")
        pos, pos_b = ar.alloc([NT, NE], F32, "pos")
        cnta, cnta_b = ar.alloc([NT, NE], F32, "cnta")
        base, base_b = ar.alloc([NT, NE], F32, "base")
        rsum, rsum_b = ar.alloc([NT], F32, "rsum")
        stri, stri_b = ar.alloc([128], BF16, "stri")
        S.op("pool", lambda e: e.affine_select(out=stri, in_=ones_f, pattern=[[1, 128]], compare_op=ALU.is_ge, fill=0.0,
                                              base=-1, channel_multiplier=-1), reads=[ones_f_b], writes=[stri_b])
        for i in range(NT):
            S.op("dve", lambda e, i=i: e.max(out=top8[:, i, :], in_=logits[:, i, :]), reads=[logits_b], writes=[top8_b])
            S.op("dve", lambda e, i=i: e.tensor_scalar(out=mask[:, i, :], in0=logits[:, i, :], scalar1=top8[:, i, 3:4], scalar2=None, op0=ALU.is_ge),
                 reads=[logits_b, top8_b], writes=[mask_b])
        S.op("act", lambda e: e.activation(out=mask_bf, in_=mask, func=AF.Copy), reads=[mask_b], writes=[mask_bfb])
        S.op("dve", lambda e: e.tensor_tensor(out=gatef, in0=logits, in1=top8[:, :, 0:1].broadcast_to([128, NT, NE]), op=ALU.subtract),
             reads=[logits_b, top8_b], writes=[gatef_b])
        S.op("act", lambda e: e.activation(out=gatef, in_=gatef, func=AF.Exp), reads=[gatef_b], writes=[gatef_b])
        S.op("dve", lambda e: e.tensor_tensor(out=gatef, in0=gatef, in1=mask, op=ALU.mult), reads=[gatef_b, mask_b], writes=[gatef_b])
        S.op("dve", lambda e: e.reduce_sum(out=rsum, in_=gatef, axis=AX.X), reads=[gatef_b], writes=[rsum_b])
        S.op("dve", lambda e: e.reciprocal(out=rsum, in_=rsum), reads=[rsum_b], writes=[rsum_b])
        S.op("dve", lambda e: e.tensor_tensor(out=gatef, in0=gatef, in1=rsum.unsqueeze(2).broadcast_to([128, NT, NE]), op=ALU.mult),
             reads=[gatef_b, rsum_b], writes=[gatef_b])
        mflat = mask_bf.rearrange("p a b -> p (a b)")
        for half in range(2):
            b_ = half

            def mm(e, b_=b_, half=half):
                return e.matmul(ps[b_][:], lhsT=ones_bf, rhs=mflat[:, half * 512:(half + 1) * 512], start=True, stop=True)
            S.op("pe", mm, reads=[mask_bfb, ones_bf_b], writes=[psb[b_]])
            S.op("dve", lambda e, b_=b_, half=half: e.tensor_copy(out=cnta.rearrange("p a b -> p (a b)")[:, half * 512:(half + 1) * 512], in_=ps[b_][:]),
                 reads=[psb[b_]], writes=[cnta_b])
            b2 = 2 + half

            def mm2(e, b2=b2, half=half):
                return e.matmul(ps[b2][:], lhsT=stri, rhs=mflat[:, half * 512:(half + 1) * 512], start=True, stop=True)
            S.op("pe", mm2, reads=[mask_bfb, stri_b], writes=[psb[b2]])
            S.op("dve", lambda e, b2=b2, half=half: e.tensor_copy(out=pos.rearrange("p a b -> p (a b)")[:, half * 512:(half + 1) * 512], in_=ps[b2][:]),
                 reads=[psb[b2]], writes=[pos_b])
        for ee in range(NE):
            S.op("dve", lambda e, ee=ee: e.tensor_tensor_scan(out=base[:, :, ee], data0=ones_f[:, 0:NT], data1=cnta[:, :, ee], initial=0.0,
                                                             op0=ALU.mult, op1=ALU.add), reads=[cnta_b, ones_f_b], writes=[base_b])
        tot, tot_b = ar.alloc([NE], F32, "tot")
        S.op("dve", lambda e: e.tensor_copy(out=tot, in_=base[:, NT - 1, :]), reads=[base_b], writes=[tot_b])
        S.op("dve", lambda e: e.tensor_tensor(out=base, in0=base, in1=cnta, op=ALU.subtract), reads=[base_b, cnta_b], writes=[base_b])
        S.op("dve", lambda e: e.tensor_tensor(out=pos, in0=pos, in1=base, op=ALU.add), reads=[pos_b, base_b], writes=[pos_b])
        nb_f, nb_fb = ar.alloc([NE], F32, "nb_f")
        nb_i, nb_ib = ar.alloc([NE], I32, "nb_i")
        pend, pend_b = ar.alloc([NE], F32, "pend")
        pstart, pstart_b = ar.alloc([NE], F32, "pstart")
        S.op("dve", lambda e: e.tensor_scalar(out=nb_f, in0=tot, scalar1=1.0 / BLK, scalar2=(BLK - 1.0) / BLK - 0.5 + 0.25 / BLK, op0=ALU.mult, op1=ALU.add),
             reads=[tot_b], writes=[nb_fb])
        S.op("dve", lambda e: e.tensor_copy(out=nb_i, in_=nb_f), reads=[nb_fb], writes=[nb_ib])
        S.op("dve", lambda e: e.tensor_copy(out=nb_f, in_=nb_i), reads=[nb_ib], writes=[nb_fb])
        S.op("dve", lambda e: e.tensor_scalar(out=nb_f, in0=nb_f, scalar1=float(BLK), scalar2=None, op0=ALU.mult), reads=[nb_fb], writes=[nb_fb])
        S.op("dve", lambda e: e.tensor_tensor_scan(out=pend, data0=ones_f[:, 0:NE], data1=nb_f, initial=0.0, op0=ALU.mult, op1=ALU.add),
             reads=[nb_fb, ones_f_b], writes=[pend_b])
        S.op("dve", lambda e: e.tensor_tensor(out=pstart, in0=pend, in1=nb_f, op=ALU.subtract), reads=[pend_b, nb_fb], writes=[pstart_b])
        S.op("dve", lambda e: e.tensor_tensor(out=pos, in0=pos, in1=pstart.unsqueeze(1).broadcast_to([128, NT, NE]), op=ALU.add),
             reads=[pos_b, pstart_b], writes=[pos_b])
        dest4, dest4_b = ar.alloc([NT, 4], F32, "dest4")
        junk, junk_b = ar.alloc([NE], F32, "junk")
        for i in range(NT):
            for k in range(4):
                S.op("dve", lambda e, i=i, k=k: e.scalar_tensor_tensor(out=junk, in0=logits[:, i, :], scalar=top8[:, i, k:k + 1], in1=pos[:, i, :],
                                                                     op0=ALU.is_equal, op1=ALU.mult, accum_out=dest4[:, i, k:k + 1]),
                     reads=[logits_b, top8_b, pos_b], writes=[junk_b, dest4_b])
                S.op("dve", lambda e, i=i, k=k: e.scalar_tensor_tensor(out=junk, in0=logits[:, i, :], scalar=top8[:, i, k:k + 1], in1=gatef[:, i, :],
                                                                     op0=ALU.is_equal, op1=ALU.mult, accum_out=gate4[:, i, k:k + 1]),
                     reads=[logits_b, top8_b, gatef_b], writes=[junk_b, gate4_b])
        S.op("dve", lambda e: e.tensor_copy(out=dest_i, in_=dest4), reads=[dest4_b], writes=[dest_ib])
        bv_i, bv_ib = ar.alloc([NB], I32, "bv_i")
        bv, bv_b = ar.alloc([NB], F32, "bv")
        cmp_, cmp_b = ar.alloc([NB, NE], F32, "cmp")
        blk_f, blk_fb = ar.alloc([NB], F32, "blk_f")
        chg_f, chg_fb = ar.alloc([NB], F32, "chg_f")
        S.op("pool", lambda e: e.iota(bv_i, pattern=[[BLK, NB]], base=0, channel_multiplier=0), writes=[bv_ib])
        S.op("dve", lambda e: e.tensor_copy(out=bv, in_=bv_i), reads=[bv_ib], writes=[bv_b])
        S.op("dve", lambda e: e.tensor_tensor(out=cmp_, in0=pend.unsqueeze(1).broadcast_to([128, NB, NE]),
                                              in1=bv.unsqueeze(2).broadcast_to([128, NB, NE]), op=ALU.is_le), reads=[pend_b, bv_b], writes=[cmp_b])
        S.op("dve", lambda e: e.reduce_sum(out=blk_f, in_=cmp_, axis=AX.X), reads=[cmp_b], writes=[blk_fb])
        S.op("dve", lambda e: e.tensor_scalar(out=blk_f, in0=blk_f, scalar1=float(NE - 1), scalar2=None, op0=ALU.min), reads=[blk_fb], writes=[blk_fb])
        S.op("dve", lambda e: e.memset(chg_f[:, 0:1], 1.0), writes=[chg_fb])
        S.op("dve", lambda e: e.tensor_tensor(out=chg_f[:, 1:NB], in0=blk_f[:, 1:NB], in1=blk_f[:, 0:NB - 1], op=ALU.not_equal), reads=[blk_fb], writes=[chg_fb])
        S.op("dve", lambda e: e.tensor_copy(out=blk_i, in_=blk_f), reads=[blk_fb], writes=[blk_ib])
        S.op("dve", lambda e: e.tensor_copy(out=chg_i, in_=chg_f), reads=[chg_fb], writes=[chg_ib])
        pio_i, pio_ib = ar.alloc([1], I32, "pio_i")
        pio, pio_b = ar.alloc([1], F32, "pio")
        S.op("pool", lambda e: e.iota(pio_i, pattern=[[0, 1]], base=0, channel_multiplier=1), writes=[pio_ib])
        S.op("dve", lambda e: e.tensor_copy(out=pio, in_=pio_i), reads=[pio_ib], writes=[pio_b])
        nchg, nchg_b = ar.alloc([NB], F32, "nchg")
        S.op("dve", lambda e: e.tensor_scalar(out=nchg, in0=chg_f, scalar1=-1.0e7, scalar2=1.0e7, op0=ALU.mult, op1=ALU.add), reads=[chg_fb], writes=[nchg_b])
        wb_f, wb_fb = ar.alloc([NB], F32, "wb_f")
        S.op("dve", lambda e: e.tensor_scalar(out=wb_f, in0=blk_f, scalar1=128.0, scalar2=pio[:, 0:1], op0=ALU.mult, op1=ALU.add), reads=[blk_fb, pio_b], writes=[wb_fb])
        S.op("dve", lambda e: e.tensor_tensor(out=wb_f, in0=wb_f, in1=chg_f, op=ALU.mult), reads=[wb_fb, chg_fb], writes=[wb_fb])
        S.op("dve", lambda e: e.tensor_tensor(out=wb_f, in0=wb_f, in1=nchg, op=ALU.add), reads=[wb_fb, nchg_b], writes=[wb_fb])
        S.op("dve", lambda e: e.tensor_copy(out=widx, in_=wb_f), reads=[wb_fb], writes=[widx_b])
        bi_f, bi_fb = ar.alloc([NB], F32, "bi_f")
        S.op("dve", lambda e: e.tensor_tensor(out=bi_f, in0=blk_f, in1=chg_f, op=ALU.mult), reads=[blk_fb, chg_fb], writes=[bi_fb])
        S.op("dve", lambda e: e.tensor_tensor(out=bi_f, in0=bi_f, in1=nchg, op=ALU.add), reads=[bi_fb, nchg_b], writes=[bi_fb])
        S.op("dve", lambda e: e.tensor_copy(out=bidx, in_=bi_f), reads=[bi_fb], writes=[bidx_b])
        if dbg:
            for nm, t_, tb_, n, dtp in (("dbg_dest", dest_i.rearrange("p a b -> p (a b)"), dest_ib, NT * 4, I32),
                                        ("dbg_gate4", gate4.rearrange("p a b -> p (a b)"), gate4_b, NT * 4, F32),
                                        ("dbg_blk", blk_i, blk_ib, NB, I32), ("dbg_chg", chg_i, chg_ib, NB, I32)):
                o, ob = self.scratch(nm, [128, n], dtp, dump=True)
                S.op("sp", lambda e, o=o, t_=t_: e.dma_start(out=o, in_=t_), reads=[tb_], writes=[ob], dma=True)
        S.barrier()
        ar.release()
        ar.mark()
        zt, zt_b = ar.alloc([4, D], BF16, "zt")
        S.op("dve", lambda e: e.memset(zt, 0.0), writes=[zt_b])
        xsv = self.xs.rearrange("(b p) n -> p b n", p=128)
        for q in range(NSLOT // 512):
            S.op("sp", lambda e, q=q: e.dma_start(out=xsv[:, 4 * q:4 * q + 4, :], in_=zt), reads=[zt_b], writes=[self.xs_b], dma=True)
        uft = [ar.alloc([D], BF16, f"sc_u{i}") for i in range(2)]
        for i in range(NT):
            u_t, u_b = uft[i % 2]
            S.op("sp", lambda e, u_t=u_t, i=i: e.dma_start(out=u_t, in_=self.uf_s[i * 128:(i + 1) * 128, :]), reads=[self.uf_sb], writes=[u_b], dma=True)
            for k in range(4):
                S.op("pool", lambda e, u_t=u_t, i=i, k=k: e.indirect_dma_start(
                    out=self.xs, out_offset=bass.IndirectOffsetOnAxis(ap=dest_i[:, i, k:k + 1].bitcast(U32), axis=0), in_=u_t, in_offset=None),
                    reads=[u_b, dest_ib], writes=[self.xs_b], dma=True)
        S.barrier()
        ar.release()
        ar.mark()
        Wt = [ar.alloc([8, D], BF16, f"moe_w{m}") for m in range(3)]
        brow, brow_b = ar.alloc([3, D], BF16, "moe_brow")
        WS = [ar.alloc([8, D], F32, f"moe_ws{m}") for m in range(3)]
        browS, browS_b = ar.alloc([3, D], BF16, "moe_browS")
        wds = [wg_d, wu_d, wd_d]
        bds = [bg_d, bu_d, bd_d]
        xb = [ar.alloc([D], BF16, f"moe_xb{i}") for i in range(2)]
        xT = [ar.alloc([8, 128], BF16, f"moe_xT{i}") for i in range(2)]
        gc = [ar.alloc([512], F32, f"moe_gc{i}") for i in range(2)]
        uc = [ar.alloc([512], F32, f"moe_uc{i}") for i in range(2)]
        sgm = [ar.alloc([512], F32, f"moe_sg{i}") for i in range(2)]
        hb_ = [ar.alloc([D], BF16, f"moe_h{i}") for i in range(2)]
        hT = [ar.alloc([8, 128], BF16, f"moe_hT{i}") for i in range(2)]
        yo = [ar.alloc([D], BF16, f"moe_yo{i}") for i in range(2)]
        NBLK = int(os.environ.get("MOE_NB", str(NB)))
        regs = {}

        def breg(e, val):
            if val not in regs:
                regs[val] = e.to_reg(val)
            return regs[val]
        for b in range(NBLK):
            for m in range(3):
                S.op("pool", lambda e, m=m, b=b: e.indirect_dma_start(
                    out=WS[m][0].rearrange("p a b -> p (a b)"), out_offset=None, in_=wds[m],
                    in_offset=bass.IndirectOffsetOnAxis(ap=widx[:, b:b + 1].bitcast(U32), axis=0),
                    bounds_check=breg(e, NE * 128 - 1), oob_is_err=False),
                    reads=[widx_b], writes=[WS[m][1]], dma=True)
            for m in range(3):
                S.op("pool", lambda e, m=m, b=b: e.indirect_dma_start(
                    out=browS[:, m, :], out_offset=None, in_=bds[m],
                    in_offset=bass.IndirectOffsetOnAxis(ap=bidx[:, b:b + 1].bitcast(U32), axis=0),
                    bounds_check=breg(e, NE - 1), oob_is_err=False),
                    reads=[bidx_b], writes=[browS_b], dma=True)
            for m in range(3):
                eng = ("dve", "dve", "act")[m]
                if eng == "act":
                    S.op("act", lambda e, m=m: e.activation(out=Wt[m][0], in_=WS[m][0], func=AF.Copy), reads=[WS[m][1]], writes=[Wt[m][1]])
                else:
                    S.op(eng, lambda e, m=m: e.tensor_copy(out=Wt[m][0], in_=WS[m][0]), reads=[WS[m][1]], writes=[Wt[m][1]])
            S.op("act", lambda e: e.activation(out=brow[0:1], in_=browS[0:1], func=AF.Copy), reads=[browS_b], writes=[brow_b])
            subs = [b * NSUB + s_ for s_ in range(NSUB)]

            def stageA(sb):
                x_t, x_b = xb[sb % 2]
                S.op("sp", lambda e, x_t=x_t, sb=sb: e.dma_start(out=x_t, in_=self.xs[sb * 128:(sb + 1) * 128, :]), reads=[self.xs_b], writes=[x_b], dma=True)
                xT_t, xT_b = xT[sb % 2]
                pT = ps[0][:].bitcast(BF16)

                def trx(e, x_t=x_t, pT=pT):
                    ins = None
                    for kc in range(8):
                        ins = e.transpose(out=pT[:, kc * 128:(kc + 1) * 128], in_=x_t[:, kc * 128:(kc + 1) * 128], identity=ident_bf)
                    return ins
                S.op("pe", trx, reads=[x_b, ident_bf_b], writes=[psb[0]])
                S.op("act", lambda e, xT_t=xT_t, pT=pT: e.activation(out=xT_t.rearrange("p a b -> p (a b)"), in_=pT, func=AF.Copy), reads=[psb[0]], writes=[xT_b])
                h_t, h_b = hb_[sb % 2]
                for half in range(2):
                    hs = slice(half * 512, (half + 1) * 512)
                    for m, bk in ((0, 1 + half), (1, 3 + half)):
                        def mm(e, m=m, bk=bk, hs=hs, xT_t=xT_t):
                            e.matmul(ps[bk][:], lhsT=ones_bf[0:1, 0:128], rhs=brow[0:1, m, hs], start=True, stop=False)
                            ins = None
                            for kc in range(8):
                                ins = e.matmul(ps[bk][:], lhsT=xT_t[:, kc, :], rhs=Wt[m][0][:, kc, hs], start=False, stop=(kc == 7))
                            return ins
                        S.op("pe", mm, reads=[xT_b, Wt[m][1], brow_b, ones_bf_b], writes=[psb[bk]])
                    g_t, g_b = gc[half]
                    u_t, u_b = uc[half]
                    s_t, s_b = sgm[half]
                    S.op("dve", lambda e, g_t=g_t, half=half: e.tensor_scalar(out=g_t, in0=ps[1 + half][:], scalar1=7.0, scalar2=None, op0=ALU.min),
                         reads=[psb[1 + half]], writes=[g_b])
                    S.op("dve", lambda e, u_t=u_t, half=half: e.tensor_scalar(out=u_t, in0=ps[3 + half][:], scalar1=7.0, scalar2=-7.0, op0=ALU.min, op1=ALU.max),
                         reads=[psb[3 + half]], writes=[u_b])
                    S.op("act", lambda e, s_t=s_t, g_t=g_t: e.activation(out=s_t, in_=g_t, func=AF.Sigmoid, scale=1.702), reads=[g_b], writes=[s_b])
                    S.op("dve", lambda e, u_t=u_t, g_t=g_t: e.scalar_tensor_tensor(out=u_t, in0=u_t, scalar=1.0, in1=g_t, op0=ALU.add, op1=ALU.mult),
                         reads=[u_b, g_b], writes=[u_b])
                    S.op("dve", lambda e, h_t=h_t, u_t=u_t, s_t=s_t, hs=hs: e.tensor_tensor(out=h_t[:, hs], in0=u_t, in1=s_t, op=ALU.mult),
                         reads=[u_b, s_b], writes=[h_b])

            def stageB(sb):
                h_t, h_b = hb_[sb % 2]
                hT_t, hT_b = hT[sb % 2]
                pT5 = ps[5][:].bitcast(BF16)

                def trh(e, h_t=h_t, pT5=pT5):
                    ins = None
                    for kc in range(8):
                        ins = e.transpose(out=pT5[:, kc * 128:(kc + 1) * 128], in_=h_t[:, kc * 128:(kc + 1) * 128], identity=ident_bf)
                    return ins
                S.op("pe", trh, reads=[h_b, ident_bf_b], writes=[psb[5]])
                S.op("act", lambda e, hT_t=hT_t, pT5=pT5: e.activation(out=hT_t.rearrange("p a b -> p (a b)"), in_=pT5, func=AF.Copy), reads=[psb[5]], writes=[hT_b])
                y_t, y_b = yo[sb % 2]
                for half in range(2):
                    hs = slice(half * 512, (half + 1) * 512)
                    bk = 6 + half

                    def mmd(e, bk=bk, hs=hs, hT_t=hT_t):
                        e.matmul(ps[bk][:], lhsT=ones_bf[0:1, 0:128], rhs=brow[0:1, 2, hs], start=True, stop=False)
                        ins = None
                        for kc in range(8):
                            ins = e.matmul(ps[bk][:], lhsT=hT_t[:, kc, :], rhs=Wt[2][0][:, kc, hs], start=False, stop=(kc == 7))
                        return ins
                    S.op("pe", mmd, reads=[hT_b, Wt[2][1], brow_b, ones_bf_b], writes=[psb[bk]])
                    if half == 0:
                        S.op("dve", lambda e, y_t=y_t, bk=bk, hs=hs: e.tensor_copy(out=y_t[:, hs], in_=ps[bk][:]), reads=[psb[bk]], writes=[y_b])
                    else:
                        S.op("act", lambda e, y_t=y_t, bk=bk, hs=hs: e.activation(out=y_t[:, hs], in_=ps[bk][:], func=AF.Copy), reads=[psb[bk]], writes=[y_b])
                S.op("sp", lambda e, y_t=y_t, sb=sb: e.dma_start(out=self.ys[sb * 128:(sb + 1) * 128, :], in_=y_t), reads=[y_b], writes=[self.ys_b], dma=True)

            stageA(subs[0])
            for i_ in range(1, NSUB):
                stageA(subs[i_])
                stageB(subs[i_ - 1])
            stageB(subs[-1])
        S.barrier()
        ar.release()
        ar.mark()
        gf_rep, gf_rep_b = ar.alloc([D], F32, "gf_rep")
        fn_rep, fn_rep_b = ar.alloc([D], F32, "fn_rep")
        S.op("sp", lambda e: e.dma_start(out=gf_rep, in_=self.mod_s[:, 5 * D:6 * D]), reads=[self.mod_sb], writes=[gf_rep_b], dma=True)
        S.op("sp", lambda e: e.dma_start(out=fn_rep, in_=fnw.broadcast_to([128, D])), writes=[fn_rep_b], dma=True)
        yk = [[ar.alloc([D], BF16, f"cb_y{j}{k}") for k in range(4)] for j in range(2)]
        hh = [ar.alloc([D], F32, f"cb_h{j}") for j in range(2)]
        acc = [ar.alloc([D], F32, f"cb_a{j}") for j in range(2)]
        oo = [ar.alloc([D], F32, f"cb_o{j}") for j in range(2)]
        sqj, sqj_b = ar.alloc([D], BF16, "cb_sq")
        ssq = [ar.alloc([1], F32, f"cb_ss{j}") for j in range(2)]
        for i in range(NT):
            j = i % 2
            h_t, h_b = hh[j]
            a_t, a_b = acc[j]
            o_t, o_b = oo[j]
            s_t, s_b = ssq[j]
            S.op("sp", lambda e, h_t=h_t, i=i: e.dma_start(out=h_t, in_=self.h1_s[i * 128:(i + 1) * 128, :]), reads=[self.h1_sb], writes=[h_b], dma=True)
            for k in range(4):
                y_t, y_b = yk[j][k]
                S.op("pool", lambda e, y_t=y_t, i=i, k=k: e.indirect_dma_start(
                    out=y_t, out_offset=None, in_=self.ys, in_offset=bass.IndirectOffsetOnAxis(ap=dest_i[:, i, k:k + 1].bitcast(U32), axis=0)),
                    reads=[self.ys_b, dest_ib], writes=[y_b], dma=True)
            S.op("dve", lambda e, a_t=a_t, i=i, j=j: e.tensor_scalar(out=a_t, in0=yk[j][0][0], scalar1=gate4[:, i, 0:1], scalar2=None, op0=ALU.mult),
                 reads=[yk[j][0][1], gate4_b], writes=[a_b])
            for k in range(1, 4):
                S.op("dve", lambda e, a_t=a_t, i=i, j=j, k=k: e.scalar_tensor_tensor(out=a_t, in0=yk[j][k][0], scalar=gate4[:, i, k:k + 1], in1=a_t,
                                                                                 op0=ALU.mult, op1=ALU.add), reads=[yk[j][k][1], gate4_b, a_b], writes=[a_b])
            S.op("dve", lambda e, a_t=a_t: e.tensor_tensor(out=a_t, in0=a_t, in1=gf_rep, op=ALU.mult), reads=[a_b, gf_rep_b], writes=[a_b])
            S.op("dve", lambda e, a_t=a_t, h_t=h_t: e.tensor_tensor(out=a_t, in0=a_t, in1=h_t, op=ALU.add), reads=[a_b, h_b], writes=[a_b])
            S.op("act", lambda e, a_t=a_t, s_t=s_t: e.activation(out=sqj, in_=a_t, func=AF.Square, accum_out=s_t), reads=[a_b], writes=[sqj_b, s_b])
            S.op("act", lambda e, s_t=s_t: e.activation(out=s_t, in_=s_t, func=AF.Sqrt, scale=1.0 / D, bias=EPS), reads=[s_b], writes=[s_b])
            S.op("dve", lambda e, s_t=s_t: e.reciprocal(out=s_t, in_=s_t), reads=[s_b], writes=[s_b])
            S.op("dve", lambda e, o_t=o_t, a_t=a_t, s_t=s_t: e.scalar_tensor_tensor(out=o_t, in0=a_t, scalar=s_t[:, 0:1], in1=fn_rep, op0=ALU.mult, op1=ALU.mult),
                 reads=[a_b, s_b, fn_rep_b], writes=[o_b])
            S.op("sp", lambda e, o_t=o_t, i=i: e.dma_start(out=self.out[i * 128:(i + 1) * 128, :], in_=o_t), reads=[o_b], dma=True)
        S.barrier()
        ar.release()
        ar.release()

def to_cm(a):
    return a.reshape(64, 64, *a.shape[1:]).swapaxes(0, 1).reshape(a.shape)


def from_cm(a):
    return a.reshape(64, 64, *a.shape[1:]).swapaxes(0, 1).reshape(a.shape)


def make_in_maps(inputs, cores):
    x = np.asarray(inputs["x"], np.float32)
    c = np.asarray(inputs["c"], np.float32)
    ctx = np.asarray(inputs["ctx"], np.float32)
    c_ctx = np.asarray(inputs["c_ctx"], np.float32)
    shared = {
        "cctxT": np.ascontiguousarray(c_ctx.reshape(8, 128).T),
        "ada_w": np.ascontiguousarray(inputs["ada_w"][0]),
        "ada_b": np.ascontiguousarray(inputs["ada_b"][0].reshape(1, -1)),
        "norm_mix_w": np.ascontiguousarray(np.asarray(inputs["norm_mix_w"][0]).reshape(8, 128).T),
        "w_in": np.ascontiguousarray(inputs["w_in"][0]),
        "convw": np.ascontiguousarray(np.transpose(np.asarray(inputs["ssd_conv_w"][0]).reshape(5, 32, 128), (2, 1, 0))),
        "convb": np.ascontiguousarray(np.asarray(inputs["ssd_conv_b"][0]).reshape(32, 128).T),
        "dtbias": np.ascontiguousarray(np.asarray(inputs["ssd_dt_bias"][0]).reshape(1, 64)),
        "alog": np.ascontiguousarray(np.asarray(inputs["ssd_a_log"][0]).reshape(1, 64)),
        "ssd_d": np.ascontiguousarray(np.repeat(np.asarray(inputs["ssd_d"][0]), 64).reshape(16, 128).T),
        "ssd_nw": np.ascontiguousarray(np.asarray(inputs["ssd_norm_w"][0]).reshape(16, 128).T),
    }
    lam_re = np.asarray(inputs["s5_lam_re"][0]); lam_im = np.asarray(inputs["s5_lam_im"][0]); ldt = np.asarray(inputs["s5_log_dt"][0])
    b_re = np.asarray(inputs["s5_b_re"][0]); b_im = np.asarray(inputs["s5_b_im"][0])
    c_re = np.asarray(inputs["s5_c_re"][0]); c_im = np.asarray(inputs["s5_c_im"][0])
    s5F = np.zeros((2, 128, 5, 8, 64), np.float32)
    s5S = np.zeros((2, 128, 3 * 32 + 4 * 512), np.float32)
    for d in range(2):
        def F_gp(a):
            t = a.reshape(8, 8, 64).transpose(1, 0, 2)
            return np.repeat(t[:, None], 16, axis=1).reshape(128, 8, 64)
        s5F[d, :, 0] = F_gp(lam_re[d]); s5F[d, :, 1] = F_gp(lam_im[d])
        s5F[d, :, 2] = F_gp(np.repeat(ldt[d][:, None], 64, axis=1))
        s5F[d, :, 3] = b_re[d].reshape(8, 8, 64, 16).transpose(1, 3, 0, 2).reshape(128, 8, 64)
        s5F[d, :, 4] = b_im[d].reshape(8, 8, 64, 16).transpose(1, 3, 0, 2).reshape(128, 8, 64)
        def S_gp(a):
            return a.reshape(32, 2, 64).transpose(1, 2, 0).reshape(128, 32)
        s5S[d, :, 0:32] = S_gp(lam_re[d]); s5S[d, :, 32:64] = S_gp(lam_im[d])
        s5S[d, :, 64:96] = S_gp(np.repeat(ldt[d][:, None], 64, axis=1))
        s5S[d, :, 96:96 + 512] = b_re[d].reshape(32, 2, 64, 16).transpose(1, 2, 0, 3).reshape(128, 512)
        s5S[d, :, 96 + 512:96 + 1024] = b_im[d].reshape(32, 2, 64, 16).transpose(1, 2, 0, 3).reshape(128, 512)
        s5S[d, :, 96 + 1024:96 + 1536] = c_re[d].reshape(32, 2, 16, 64).transpose(1, 3, 0, 2).reshape(128, 512)
        s5S[d, :, 96 + 1536:96 + 2048] = c_im[d].reshape(32, 2, 16, 64).transpose(1, 3, 0, 2).reshape(128, 512)
    shared["glu_w"] = np.ascontiguousarray(inputs["s5_glu_w"][0])
    shared["glu_b"] = np.ascontiguousarray(np.asarray(inputs["s5_glu_b"][0]).reshape(8, 128).T)
    shared["w_a"] = np.ascontiguousarray(inputs["w_branch_a"][0])
    shared["w_b"] = np.ascontiguousarray(inputs["w_branch_b"][0])
    shared["w_o"] = np.ascontiguousarray(inputs["w_out"][0])
    shared["nfw"] = np.ascontiguousarray(np.asarray(inputs["norm_ffn_w"][0]).reshape(1, -1))
    shared["r_w"] = np.ascontiguousarray(inputs["router_w"][0])
    shared["r_b"] = np.ascontiguousarray(np.asarray(inputs["router_b"][0]).reshape(1, -1))
    def wperm(w):
        return np.ascontiguousarray(np.asarray(w).reshape(32, 8, 128, D).transpose(0, 2, 1, 3)).reshape(4096, 8192)
    shared["moe_wg"] = wperm(inputs["moe_w_gate"][0])
    shared["moe_wu"] = wperm(inputs["moe_w_up"][0])
    shared["moe_wd"] = wperm(inputs["moe_w_down"][0])
    shared["moe_bg"] = np.ascontiguousarray(inputs["moe_b_gate"][0])
    shared["moe_bu"] = np.ascontiguousarray(inputs["moe_b_up"][0])
    shared["moe_bd"] = np.ascontiguousarray(inputs["moe_b_down"][0])
    shared["fnw"] = np.ascontiguousarray(np.asarray(inputs["final_norm_w"]).reshape(1, -1))
    shared["s5F"] = s5F
    shared["s5S"] = s5S
    shared["s5_d"] = np.ascontiguousarray(np.asarray(inputs["s5_d"][0]).reshape(8, 128).T)
    maps = []
    for b in cores:
        m = dict(shared)
        m["xT_rm"] = np.ascontiguousarray(x[b].T)
        m["xT_cm"] = np.ascontiguousarray(to_cm(x[b]).T)
        m["ctxT"] = np.ascontiguousarray(ctx[b].T)
        m["cT"] = np.ascontiguousarray(c[b].reshape(8, 128).T)
        m["x_tok"] = np.ascontiguousarray(to_cm(x[b]))
        maps.append(m)
    return maps


_NC_CACHE = {}


def kernel(**inputs):
    if "nc" not in _NC_CACHE:
        _NC_CACHE["nc"] = K().build()
    nc = _NC_CACHE["nc"]
    maps = make_in_maps(inputs, list(range(8)))
    res = run_bass_kernel_spmd(nc, maps, core_ids=list(range(8)))
    outs = [from_cm(np.asarray(r["out"])) for r in res.results]
    return np.stack(outs, 0).astype(np.float32)
```

```python
import os
import numpy as np
from contextlib import ExitStack
import concourse.bass as bass
import concourse.mybir as mybir
from concourse.bass_utils import run_bass_kernel_spmd

F32 = mybir.dt.float32
BF16 = mybir.dt.bfloat16
I32 = mybir.dt.int32
U32 = mybir.dt.uint32
U8 = mybir.dt.uint8
ALU = mybir.AluOpType
AF = mybir.ActivationFunctionType
AX = mybir.AxisListType

NDSEM = 8
D = 1024
L = 4096
LC = 256
T = L + LC
EPS = 1e-6


class Buf:
    __slots__ = ("name", "last_w", "readers", "excl")

    def __init__(self, name, excl=False):
        self.name = name
        self.last_w = None
        self.readers = []
        self.excl = excl


class Op:
    __slots__ = ("id", "eng", "fn", "deps", "dma", "eidx", "needs_inc", "semval", "dslot", "dval", "name")


COMPUTE = ("pe", "dve", "act", "pool")
ENGS = ("sp", "pe", "dve", "act", "pool")


class Sched:
    def __init__(self, nc):
        self.nc = nc
        self.ops = []
        self.per_eng = {e: [] for e in ENGS}
        self.ndma = {e: 0 for e in ENGS}
        self.barrier_floor = -1

    def op(self, eng, fn, reads=(), writes=(), dma=False, name=None):
        o = Op()
        o.id = len(self.ops)
        o.eng = eng
        o.fn = fn
        o.dma = dma
        o.name = name
        o.needs_inc = False
        o.semval = None
        o.dslot = None
        o.dval = None
        deps = {}
        if any(b.excl for b in reads):
            writes = list(writes) + [b for b in reads if b.excl and b not in writes]
            reads = [b for b in reads if not b.excl]
        for b in reads:
            if b.last_w is not None:
                deps[b.last_w.id] = (b.last_w, "raw")
        for b in writes:
            if b.last_w is not None and b.last_w.id not in deps:
                deps[b.last_w.id] = (b.last_w, "waw")
            for r in b.readers:
                if r.id not in deps:
                    deps[r.id] = (r, "war")
        o.deps = [(p, k) for (p, k) in deps.values() if p.id > self.barrier_floor]
        for b in reads:
            b.readers.append(o)
        for b in writes:
            b.last_w = o
            b.readers = []
        o.eidx = len(self.per_eng[eng])
        self.per_eng[eng].append(o)
        if dma:
            i = self.ndma[eng]
            self.ndma[eng] += 1
            o.dslot = i % NDSEM
            o.dval = 16 * (i // NDSEM + 1)
        self.ops.append(o)
        return o

    def barrier(self):
        lasts = []
        for e in ENGS:
            comp = [o for o in self.per_eng[e] if not o.dma and o.fn is not None]
            if comp:
                lasts.append(comp[-1])
            dm = [o for o in self.per_eng[e] if o.dma]
            lasts.extend(dm[-NDSEM:])
        lasts = [o for o in lasts if o.id > self.barrier_floor]
        for e in ENGS:
            o = self.op(e, None, name="barrier")
            o.deps = [(p, "raw") for p in lasts if p.eng != e or p.dma]
        self.barrier_floor = len(self.ops) - 1

    @staticmethod
    def _skip(o, p, kind):
        if p.dma or o.dma or p.eng != o.eng:
            return False
        if p.eng == "pe":
            return True
        if kind != "raw":
            return True
        return o.eidx - p.eidx > 2

    def emit(self):
        nc = self.nc
        for o in self.ops:
            for (p, kind) in o.deps:
                if p.dma or self._skip(o, p, kind):
                    continue
                p.needs_inc = True
        for e in ENGS:
            c = 0
            for o in self.per_eng[e]:
                if o.dma:
                    continue
                if o.needs_inc:
                    c += 1
                    o.semval = c
        with ExitStack() as es:
            csem = {e: es.enter_context(nc.semaphore(f"c_{e}")) for e in COMPUTE}
            dsem = {e: [es.enter_context(nc.semaphore(f"d_{e}{i}")) for i in range(NDSEM)]
                    for e in ENGS if self.ndma[e] > 0}
            block = es.enter_context(nc.Block())
            handles = {"sp": block.sync, "pe": block.tensor, "dve": block.vector,
                       "act": block.scalar, "pool": block.gpsimd}

            FUSE = os.environ.get("FUSEWAIT", "1") == "1"

            class _Rec:
                def __init__(self, eng):
                    self._eng = eng
                    self.first = None

                def __getattr__(self, name):
                    attr = getattr(self._eng, name)
                    if not callable(attr):
                        return attr

                    def w(*a, **k):
                        r = attr(*a, **k)
                        if self.first is None and hasattr(r, "then_inc"):
                            self.first = r
                        return r
                    return w

            def make(e):
                def body(eng):
                    waited = {}

                    def need(lst, sem, key, val):
                        if waited.get(key, 0) >= val:
                            return
                        for i, (s_, k_, v_) in enumerate(lst):
                            if k_ == key:
                                if v_ < val:
                                    lst[i] = (sem, key, val)
                                return
                        lst.append((sem, key, val))

                    for o in self.per_eng[e]:
                        lst = []
                        for (p, kind) in o.deps:
                            if p.dma:
                                need(lst, dsem[p.eng][p.dslot], ("d", p.eng, p.dslot), p.dval)
                            elif not self._skip(o, p, kind):
                                need(lst, csem[p.eng], ("c", p.eng), p.semval)
                        if o.dma and o.dval > 16:
                            need(lst, dsem[e][o.dslot], ("d", e, o.dslot), o.dval - 16)
                        for (s_, k_, v_) in lst:
                            waited[k_] = v_
                        fuse = None
                        if FUSE and lst and o.fn is not None and not o.dma:
                            fuse = lst.pop()
                        for (s_, k_, v_) in lst:
                            eng.wait_ge(s_, v_)
                        if o.fn is None:
                            continue
                        if fuse is not None:
                            rec = _Rec(eng)
                            ins = o.fn(rec)
                            rec.first._wait_ge(fuse[0], fuse[2])
                        else:
                            ins = o.fn(eng)
                        if o.dma:
                            ins.then_inc(dsem[e][o.dslot], 16)
                        elif o.needs_inc:
                            ins.then_inc(csem[e], 1)
                    if e == "sp":
                        for q in dsem:
                            dm = [o for o in self.per_eng[q] if o.dma]
                            for o in dm[-NDSEM:]:
                                if waited.get(("d", q, o.dslot), 0) < o.dval:
                                    eng.wait_ge(dsem[q][o.dslot], o.dval)
                                    waited[("d", q, o.dslot)] = o.dval
                return body

            for e in ENGS:
                if self.per_eng[e] or e == "sp":
                    handles[e](make(e))


ESZ = {F32: 4, BF16: 2, I32: 4, U32: 4, U8: 1}


class Arena:
    def __init__(self, nc, es, nbytes, name="arena"):
        self.t = es.enter_context(nc.sbuf_tensor(name, [128, nbytes], U8))
        self.nbytes = nbytes
        self.off = 0
        self.marks = []

    def alloc(self, shape, dtype, name="t"):
        if isinstance(shape, int):
            shape = [shape]
        n = int(np.prod(shape))
        nb = n * ESZ[dtype]
        self.off = (self.off + 63) // 64 * 64
        assert self.off + nb <= self.nbytes, f"arena overflow {name}: {self.off}+{nb}>{self.nbytes}"
        ap = self.t[:, self.off:self.off + nb]
        if dtype != U8:
            ap = ap.bitcast(dtype)
        if len(shape) > 1:
            names = " ".join(f"d{i}" for i in range(len(shape)))
            kw = {f"d{i}": int(shape[i]) for i in range(1, len(shape))}
            ap = ap.rearrange(f"p ({names}) -> p {names}", **kw)
        self.off += nb
        return ap, Buf(name)

    def mark(self):
        self.marks.append(self.off)

    def release(self):
        self.off = self.marks.pop()


IN_S5 = (0, 1024)
IN_XBC = (1024, 5120)
IN_DT = (5120, 5184)
IN_Z = (5184, 7232)
IN_GA = (7232, 8256)
IN_GB = (8256, 9280)


class K:
    def __init__(self, stage=99, debug=False):
        self.stage = stage
        self.debug = debug
        self.nc = bass.Bass("TRN2", target_bir_lowering=False)
        self.ins = {}
        self.dbg = {}

    def inp(self, name, shape, dt=F32):
        self.ins[name] = self.nc.dram_tensor(name, list(shape), dt, kind="ExternalInput").ap()
        return self.ins[name]

    def scratch(self, name, shape, dt, dump=False):
        if self.debug and dump:
            ap = self.nc.dram_tensor(name, list(shape), dt, kind="ExternalOutput").ap()
            self.dbg[name] = ap
        else:
            ap = self.nc.dram_tensor(name, list(shape), dt).ap()
        return ap, Buf(name)

    def build(self):
        nc = self.nc
        inp = self.inp
        xT_cm = inp("xT_cm", [D, L])
        xT_rm = inp("xT_rm", [D, L])
        ctxT = inp("ctxT", [D, LC])
        cT = inp("cT", [128, 8])
        cctxT = inp("cctxT", [128, 8])
        ada_w = inp("ada_w", [D, 6 * D])
        ada_b = inp("ada_b", [1, 6 * D])
        nmw = inp("norm_mix_w", [128, 8])
        w_in = inp("w_in", [D, 9280])
        out = nc.dram_tensor("out", [L, D], F32, kind="ExternalOutput").ap()
        self.out = out

        with ExitStack() as es:
            self.es = es
            ar = self.ar = Arena(nc, es, 206 * 1024)
            self.ps = [es.enter_context(nc.psum_tensor(f"ps{i}", [128, 512], F32)) for i in range(8)]
            self.psb = [Buf(f"ps{i}", excl=True) for i in range(8)]
            S = self.S = Sched(nc)

            ones_bf, ones_bf_b = ar.alloc([128], BF16, "ones_bf")
            S.op("dve", lambda e: e.memset(ones_bf, 1.0), writes=[ones_bf_b])
            ones_f, ones_f_b = ar.alloc([128], F32, "ones_f")
            S.op("dve", lambda e: e.memset(ones_f, 1.0), writes=[ones_f_b])
            ident_f, ident_f_b = ar.alloc([128], F32, "ident_f")
            S.op("pool", lambda e: e.affine_select(out=ident_f, in_=ones_f, pattern=[[-1, 128]],
                                                  compare_op=ALU.is_equal, fill=0.0, base=0,
                                                  channel_multiplier=1),
                 reads=[ones_f_b], writes=[ident_f_b])
            self.consts = dict(ones_bf=(ones_bf, ones_bf_b), ones_f=(ones_f, ones_f_b),
                               ident_f=(ident_f, ident_f_b))

            self.phase0(cT, cctxT, ada_w, ada_b, nmw)
            if self.stage >= 1:
                self.phase1(xT_rm, xT_cm, ctxT, w_in)
            if self.stage >= 2:
                self.phase2(inp("convw", [128, 32, 5]), inp("convb", [128, 32]), inp("dtbias", [1, 64]), inp("alog", [1, 64]),
                            inp("ssd_d", [128, 16]), inp("ssd_nw", [128, 16]))
            if self.stage >= 3:
                self.phase3_setup(inp("s5F", [2, 128, 5, 8, 64]), inp("s5S", [2, 128, 3 * 32 + 4 * 512]))
                self.phase3_main(inp("s5_d", [128, 8]))
            if self.stage >= 4:
                self.phase4(inp("glu_w", [D, D]), inp("glu_b", [128, 8]), inp("w_a", [D, D]), inp("w_b", [2 * D, D]), inp("w_o", [D, D]),
                            inp("x_tok", [L, D]), inp("nfw", [1, D]), inp("r_w", [D, 32]), inp("r_b", [1, 32]))
            if self.stage >= 5:
                self.phase5(inp("moe_wg", [4096, 8192]), inp("moe_wu", [4096, 8192]), inp("moe_wd", [4096, 8192]),
                            inp("moe_bg", [32, D]), inp("moe_bu", [32, D]), inp("moe_bd", [32, D]), inp("fnw", [1, D]))
            S.barrier()
            S.emit()
        return nc

    def phase0(self, cT, cctxT, ada_w, ada_b, nmw):
        nc, S, ar = self.nc, self.S, self.ar
        ps, psb = self.ps, self.psb
        ident_f, ident_f_b = self.consts["ident_f"]
        self.mod_rep = []
        self.Am = [ar.alloc([8], F32, f"Am{w}") for w in range(2)]
        self.Bm = [ar.alloc([8], F32, f"Bm{w}") for w in range(2)]
        nmw_t, nmw_b = ar.alloc([8], F32, "nmw")
        S.op("sp", lambda e: e.dma_start(out=nmw_t, in_=nmw), writes=[nmw_b], dma=True)
        ar.mark()
        self.mod_rep.append(ar.alloc([6 * D], F32, "mod_rep0"))
        self.mod_rep.append(ar.alloc([6 * D], F32, "mod_rep1"))
        self.mod_s, self.mod_sb = self.scratch("mod_s", [128, 6 * D], F32)
        adab, adab_b = ar.alloc([6 * D], F32, "adab")
        S.op("sp", lambda e: e.dma_start(out=adab, in_=ada_b.broadcast_to([128, 6 * D])), writes=[adab_b], dma=True)
        lhs = []
        for w, src in enumerate((cT, cctxT)):
            c_t, c_b = ar.alloc([8], F32, f"c{w}")
            S.op("sp", lambda e, c_t=c_t, src=src: e.dma_start(out=c_t, in_=src), writes=[c_b], dma=True)
            s_t, s_b = ar.alloc([8], F32, f"s{w}")
            S.op("act", lambda e, c_t=c_t, s_t=s_t: e.activation(out=s_t, in_=c_t, func=AF.Silu),
                 reads=[c_b], writes=[s_b])
            l_t, l_b = ar.alloc([8, 128], BF16, f"l{w}")
            S.op("dve", lambda e, l_t=l_t, s_t=s_t: e.tensor_copy(out=l_t, in_=s_t.unsqueeze(2).broadcast_to([128, 8, 128])),
                 reads=[s_b], writes=[l_b])
            lhs.append((l_t, l_b))
        wsrc = ada_w.rearrange("(kc p) n -> p kc n", p=128)
        wbufs = [ar.alloc([8, 512], BF16, f"adaw{i}") for i in range(2)]
        for blk in range(12):
            wt, wb = wbufs[blk % 2]
            S.op("pool", lambda e, wt=wt, blk=blk: e.dma_start(out=wt, in_=wsrc[:, :, blk * 512:(blk + 1) * 512]),
                 writes=[wb], dma=True)
            for w in range(2):
                l_t, l_b = lhs[w]
                pi = (blk * 2 + w) % 8

                def mm(e, l_t=l_t, wt=wt, pi=pi):
                    ins = None
                    for kc in range(8):
                        ins = e.matmul(ps[pi][:], lhsT=l_t[:, kc, :], rhs=wt[:, kc, :], start=(kc == 0), stop=(kc == 7))
                    return ins
                S.op("pe", mm, reads=[l_b, wb], writes=[psb[pi]])
                mt, mb = self.mod_rep[w]
                S.op("dve", lambda e, mt=mt, pi=pi, blk=blk: e.tensor_tensor(
                    out=mt[:, blk * 512:(blk + 1) * 512], in0=ps[pi][:], in1=adab[:, blk * 512:(blk + 1) * 512], op=ALU.add),
                    reads=[psb[pi], adab_b], writes=[mb])
        tmp, tmp_b = ar.alloc([8, 128], F32, "diagtmp")
        for w in range(2):
            mt, mb = self.mod_rep[w]
            for seg, (dst, dst_b) in ((0, self.Bm[w]), (1, self.Am[w])):
                view = mt[:, seg * D:(seg + 1) * D].rearrange("p (k q) -> p k q", k=8)
                S.op("dve", lambda e, view=view: e.tensor_tensor(
                    out=tmp, in0=view, in1=ident_f.unsqueeze(1).broadcast_to([128, 8, 128]), op=ALU.mult),
                    reads=[mb, ident_f_b], writes=[tmp_b])
                S.op("dve", lambda e, dst=dst: e.reduce_sum(out=dst, in_=tmp, axis=AX.X), reads=[tmp_b], writes=[dst_b])
            at, ab = self.Am[w]
            S.op("dve", lambda e, at=at: e.scalar_tensor_tensor(out=at, in0=at, scalar=1.0, in1=nmw_t, op0=ALU.add, op1=ALU.mult),
                 reads=[ab, nmw_b], writes=[ab])
        S.op("sp", lambda e: e.dma_start(out=self.mod_s, in_=self.mod_rep[0][0]), reads=[self.mod_rep[0][1]], writes=[self.mod_sb], dma=True)
        if self.debug:
            for w in range(2):
                o, ob = self.scratch(f"dbg_mod{w}", [128, 6 * D], F32, dump=True)
                mt, mb = self.mod_rep[w]
                S.op("sp", lambda e, o=o, mt=mt: e.dma_start(out=o, in_=mt), reads=[mb], writes=[ob], dma=True)
                o, ob = self.scratch(f"dbg_AB{w}", [128, 16], F32, dump=True)
                S.op("sp", lambda e, o=o, w=w: e.dma_start(out=o[:, 0:8], in_=self.Am[w][0]), reads=[self.Am[w][1]], writes=[ob], dma=True)
                S.op("sp", lambda e, o=o, w=w: e.dma_start(out=o[:, 8:16], in_=self.Bm[w][0]), reads=[self.Bm[w][1]], writes=[ob], dma=True)
        S.barrier()
        ar.release()

    def norm_bufs(self):
        ar = self.ar
        return dict(xt=[ar.alloc([8, 512], F32, f"xt{i}") for i in range(2)],
                    sq=[ar.alloc([8, 512], BF16, f"sq{i}") for i in range(2)],
                    rs=[ar.alloc([512], F32, f"rs{i}") for i in range(2)],
                    tm=[ar.alloc([512], F32, f"tm{i}") for i in range(4)], cnt=[0])

    def norm_tokens(self, src, ntok, u_t, u_b, col0, w, nb):
        nc, S, ar = self.nc, self.S, self.ar
        ps, psb = self.ps, self.psb
        ones_bf, ones_bf_b = self.consts["ones_bf"]
        At, Ab = self.Am[w]
        Bt, Bb = self.Bm[w]
        srcv = src.rearrange("(kc p) n -> p kc n", p=128)
        xt, sq, rs, tm = nb["xt"], nb["sq"], nb["rs"], nb["tm"]
        ntile = (ntok + 511) // 512
        for ti in range(ntile):
            n = min(512, ntok - ti * 512)
            ci = nb["cnt"][0]
            nb["cnt"][0] += 1
            x_t, x_b = xt[ci % 2]
            q_t, q_b = sq[ci % 2]
            r_t, r_b = rs[ci % 2]
            S.op("sp", lambda e, x_t=x_t, ti=ti, n=n: e.dma_start(out=x_t[:, :, 0:n], in_=srcv[:, :, ti * 512:ti * 512 + n]),
                 writes=[x_b], dma=True)
            S.op("act", lambda e, x_t=x_t, q_t=q_t, n=n: e.activation(out=q_t[:, :, 0:n], in_=x_t[:, :, 0:n], func=AF.Square),
                 reads=[x_b], writes=[q_b])
            pi = ci % 2

            def mm(e, q_t=q_t, pi=pi, n=n):
                ins = None
                for kc in range(8):
                    ins = e.matmul(ps[pi][:, 0:n], lhsT=ones_bf, rhs=q_t[:, kc, 0:n], start=(kc == 0), stop=(kc == 7))
                return ins
            S.op("pe", mm, reads=[q_b, ones_bf_b], writes=[psb[pi]])
            S.op("act", lambda e, r_t=r_t, pi=pi, n=n: e.activation(out=r_t[:, 0:n], in_=ps[pi][:, 0:n], func=AF.Sqrt,
                                                                  scale=1.0 / D, bias=EPS),
                 reads=[psb[pi]], writes=[r_b])
            S.op("dve", lambda e, r_t=r_t, n=n: e.reciprocal(out=r_t[:, 0:n], in_=r_t[:, 0:n]), reads=[r_b], writes=[r_b])
            for kc in range(8):
                t_t, t_b = tm[kc % 4]
                S.op("dve", lambda e, t_t=t_t, x_t=x_t, r_t=r_t, kc=kc, n=n: e.tensor_tensor(
                    out=t_t[:, 0:n], in0=x_t[:, kc, 0:n], in1=r_t[:, 0:n], op=ALU.mult),
                    reads=[x_b, r_b], writes=[t_b])
                S.op("act", lambda e, t_t=t_t, kc=kc, ti=ti, n=n: e.activation(
                    out=u_t[:, kc, col0 + ti * 512:col0 + ti * 512 + n], in_=t_t[:, 0:n], func=AF.Identity,
                    scale=At[:, kc:kc + 1], bias=Bt[:, kc:kc + 1]),
                    reads=[t_b, Ab, Bb], writes=[u_b])

    def proj_block(self, wsrc, c0, ncols, u_t, u_b, tok_ranges, dst, dst_b, row0, wbufs, stg, cnt):
        S = self.S
        ps, psb = self.ps, self.psb
        wt, wb = wbufs[cnt[0] % 2]
        cnt[0] += 1
        S.op("pool", lambda e: e.dma_start(out=wt[:, :, 0:ncols], in_=wsrc[:, :, c0:c0 + ncols]), writes=[wb], dma=True)
        for (ucol, n, dcol) in tok_ranges:
            for mc in range(ncols // 128):
                pi = cnt[1] % 8
                cnt[1] += 1

                def mm(e, pi=pi, mc=mc, ucol=ucol, n=n):
                    ins = None
                    for kc in range(8):
                        ins = e.matmul(ps[pi][:, 0:n], lhsT=wt[:, kc, mc * 128:(mc + 1) * 128],
                                       rhs=u_t[:, kc, ucol:ucol + n], start=(kc == 0), stop=(kc == 7))
                    return ins
                S.op("pe", mm, reads=[wb, u_b], writes=[psb[pi]])
                st, sb = stg[cnt[2] % len(stg)]
                eng = "act" if cnt[2] % 2 == 0 else "dve"
                cnt[2] += 1
                if eng == "act":
                    S.op("act", lambda e, st=st, pi=pi, n=n: e.activation(out=st[:, 0:n], in_=ps[pi][:, 0:n], func=AF.Copy),
                         reads=[psb[pi]], writes=[sb])
                else:
                    S.op("dve", lambda e, st=st, pi=pi, n=n: e.tensor_copy(out=st[:, 0:n], in_=ps[pi][:, 0:n]),
                         reads=[psb[pi]], writes=[sb])
                r = row0 + mc * 128
                S.op("sp", lambda e, st=st, r=r, dcol=dcol, n=n: e.dma_start(out=dst[r:r + 128, dcol:dcol + n], in_=st[:, 0:n]),
                     reads=[sb], writes=[dst_b], dma=True)

    def phase1(self, xT_rm, xT_cm, ctxT, w_in):
        nc, S, ar = self.nc, self.S, self.ar
        ps, psb = self.ps, self.psb
        dbg = self.debug
        self.s5u, self.s5u_b = self.scratch("s5u", [D, T], BF16, dump=dbg)
        self.xbc_s, self.xbc_sb = self.scratch("xbc_s", [4096, T], BF16, dump=dbg)
        self.z_s, self.z_sb = self.scratch("z_s", [2048, L], BF16, dump=dbg)
        self.gab_s, self.gab_sb = self.scratch("gab_s", [2048, L], BF16, dump=dbg)
        self.dt_s, self.dt_sb = self.scratch("dt_s", [128, 34 * 64], F32, dump=dbg)
        wsrc = w_in.rearrange("(kc p) n -> p kc n", p=128)
        ar.mark()
        u_t, u_b = ar.alloc([8, T], BF16, "u")
        wbufs = [ar.alloc([8, 512], BF16, f"wb{i}") for i in range(2)]
        stg = [ar.alloc([512], BF16, f"stg{i}") for i in range(4)]
        cnt = [0, 0, 0]
        nb = self.norm_bufs()
        self.norm_tokens(ctxT, LC, u_t, u_b, 0, 1, nb)
        self.norm_tokens(xT_rm, L, u_t, u_b, LC, 0, nb)
        if dbg:
            o, ob = self.scratch("dbg_u_rm", [128, 8 * T], BF16, dump=True)
            S.op("sp", lambda e: e.dma_start(out=o, in_=u_t.rearrange("p a b -> p (a b)")), reads=[u_b], writes=[ob], dma=True)
        toks = [(0, LC, 0)] + [(LC + i * 512, 512, LC + i * 512) for i in range(8)]
        for blk in range(2):
            self.proj_block(wsrc, IN_S5[0] + blk * 512, 512, u_t, u_b, toks, self.s5u, self.s5u_b, blk * 512, wbufs, stg, cnt)
        self.norm_tokens(xT_cm, L, u_t, u_b, LC, 0, nb)
        for blk in range(8):
            self.proj_block(wsrc, IN_XBC[0] + blk * 512, 512, u_t, u_b, toks, self.xbc_s, self.xbc_sb, blk * 512, wbufs, stg, cnt)
        ltoks = [(LC + i * 512, 512, i * 512) for i in range(8)]
        for blk in range(4):
            self.proj_block(wsrc, IN_Z[0] + blk * 512, 512, u_t, u_b, ltoks, self.z_s, self.z_sb, blk * 512, wbufs, stg, cnt)
        for blk in range(4):
            self.proj_block(wsrc, IN_GA[0] + blk * 512, 512, u_t, u_b, ltoks, self.gab_s, self.gab_sb, blk * 512, wbufs, stg, cnt)
        wdt, wdt_b = ar.alloc([8, 64], BF16, "wdt")
        S.op("pool", lambda e: e.dma_start(out=wdt, in_=wsrc[:, :, IN_DT[0]:IN_DT[1]]), writes=[wdt_b], dma=True)
        dtt, dtt_b = ar.alloc([34, 64], F32, "dtt")
        for c in range(34):
            pi = c % 2

            def mm(e, c=c, pi=pi):
                ins = None
                for kc in range(8):
                    ins = e.matmul(ps[pi][:, 0:64], lhsT=u_t[:, kc, c * 128:(c + 1) * 128], rhs=wdt[:, kc, :],
                                   start=(kc == 0), stop=(kc == 7))
                return ins
            S.op("pe", mm, reads=[u_b, wdt_b], writes=[psb[pi]])
            S.op("dve", lambda e, c=c, pi=pi: e.tensor_copy(out=dtt[:, c, :], in_=ps[pi][:, 0:64]), reads=[psb[pi]], writes=[dtt_b])
        S.op("sp", lambda e: e.dma_start(out=self.dt_s, in_=dtt.rearrange("p a b -> p (a b)")), reads=[dtt_b], writes=[self.dt_sb], dma=True)
        S.barrier()
        ar.release()


    def pq(self, i, j0, j1=None):
        if j1 is None:
            j1 = j0 + 1
        return self.ps[i][:, j0 * 128:j1 * 128], [self.psb[i]]

    def phase2(self, convw, convb, dtbias, alog, ssd_d, ssd_nw):
        nc, S, ar = self.nc, self.S, self.ar
        ps = self.ps
        dbg = self.debug
        ident_f, ident_f_b = self.consts["ident_f"]
        ones_f, ones_f_b = self.consts["ones_f"]
        ones_bf, ones_bf_b = self.consts["ones_bf"]
        self.yb_s, self.yb_sb = self.scratch("yb_s", [2048, L], BF16, dump=dbg)
        ar.mark()
        ident_bf, ident_bf_b = ar.alloc([128], BF16, "ident_bf")
        S.op("dve", lambda e: e.tensor_copy(out=ident_bf, in_=ident_f), reads=[ident_f_b], writes=[ident_bf_b])
        tri, tri_b = ar.alloc([128], F32, "tri")
        triT, triT_b = ar.alloc([128], F32, "triT")
        S.op("pool", lambda e: e.affine_select(out=tri, in_=ones_f, pattern=[[1, 128]], compare_op=ALU.is_ge, fill=0.0,
                                              base=0, channel_multiplier=-1), reads=[ones_f_b], writes=[tri_b])
        S.op("pool", lambda e: e.affine_select(out=triT, in_=ones_f, pattern=[[-1, 128]], compare_op=ALU.is_ge, fill=0.0,
                                              base=0, channel_multiplier=1), reads=[ones_f_b], writes=[triT_b])
        zer, zer_b = ar.alloc([128], F32, "zer")
        S.op("dve", lambda e: e.memset(zer, 0.0), writes=[zer_b])
        NEG = [ar.alloc([128], BF16, f"NEG{d}") for d in range(2)]
        S.op("pool", lambda e: e.affine_select(out=NEG[0][0], in_=zer, pattern=[[1, 128]], compare_op=ALU.is_ge, fill=-60000.0,
                                              base=0, channel_multiplier=-1), reads=[zer_b], writes=[NEG[0][1]])
        S.op("pool", lambda e: e.affine_select(out=NEG[1][0], in_=zer, pattern=[[-1, 128]], compare_op=ALU.is_ge, fill=-60000.0,
                                              base=0, channel_multiplier=1), reads=[zer_b], writes=[NEG[1][1]])
        oh2, oh2_b = ar.alloc([64], F32, "oh2")
        S.op("dve", lambda e: e.tensor_tensor(out=oh2[:, 0:32], in0=ident_f[:, 0:32], in1=ident_f[:, 32:64], op=ALU.add),
             reads=[ident_f_b], writes=[oh2_b])
        S.op("dve", lambda e: e.tensor_tensor(out=oh2[:, 32:64], in0=ident_f[:, 64:96], in1=ident_f[:, 96:128], op=ALU.add),
             reads=[ident_f_b], writes=[oh2_b])
        sel2, sel2_b = ar.alloc([64, 128], BF16, "sel2")
        S.op("dve", lambda e: e.tensor_copy(out=sel2, in_=oh2.unsqueeze(2).broadcast_to([128, 64, 128])), reads=[oh2_b], writes=[sel2_b])
        mhi, mhi_b = ar.alloc([1], F32, "mhi")
        mlo, mlo_b = ar.alloc([1], F32, "mlo")
        mt_, mt_b = ar.alloc([1], F32, "mtmp")
        S.op("dve", lambda e: e.reduce_sum(out=mhi, in_=ident_f[:, 0:32], axis=AX.X), reads=[ident_f_b], writes=[mhi_b])
        S.op("dve", lambda e: e.reduce_sum(out=mt_, in_=ident_f[:, 64:96], axis=AX.X), reads=[ident_f_b], writes=[mt_b])
        S.op("dve", lambda e: e.tensor_tensor(out=mhi, in0=mhi, in1=mt_, op=ALU.add), reads=[mhi_b, mt_b], writes=[mhi_b])
        S.op("dve", lambda e: e.tensor_scalar(out=mlo, in0=mhi, scalar1=-1.0, scalar2=1.0, op0=ALU.mult, op1=ALU.add),
             reads=[mhi_b], writes=[mlo_b])
        cw, cw_b = ar.alloc([32, 5], F32, "convw")
        cb, cb_b = ar.alloc([32], F32, "convb")
        dcol, dcol_b = ar.alloc([16], F32, "ssd_d")
        nwc, nwc_b = ar.alloc([16], F32, "ssd_nw")
        S.op("sp", lambda e: e.dma_start(out=cw, in_=convw), writes=[cw_b], dma=True)
        S.op("sp", lambda e: e.dma_start(out=cb, in_=convb), writes=[cb_b], dma=True)
        S.op("sp", lambda e: e.dma_start(out=dcol, in_=ssd_d), writes=[dcol_b], dma=True)
        S.op("sp", lambda e: e.dma_start(out=nwc, in_=ssd_nw), writes=[nwc_b], dma=True)
        dt, dt_b = ar.alloc([34, 2, 32], F32, "dt")
        decay, decay_b = ar.alloc([34, 2, 32], F32, "decay")
        wend, wend_b = ar.alloc([34, 2, 32], F32, "wend")
        HL, HL_b = ar.alloc([34, 128], BF16, "HL")
        HLn, HLn_b = ar.alloc([34, 128], BF16, "HLn")
        ar.mark()
        nacum, nacum_b = ar.alloc([34, 2, 32], F32, "nacum")
        adtd, adtd_b = ar.alloc([34, 4, 32], F32, "adtd")
        tot, tot_b = ar.alloc([34, 2, 32], F32, "tot")
        dtb, dtb_b = ar.alloc([2, 32], F32, "dtb")
        arep, arep_b = ar.alloc([2, 32], F32, "arep")
        S.op("sp", lambda e: e.dma_start(out=dt.rearrange("p a b c -> p (a b c)"), in_=self.dt_s), reads=[self.dt_sb], writes=[dt_b], dma=True)
        S.op("sp", lambda e: e.dma_start(out=dtb.rearrange("p a b -> p (a b)"), in_=dtbias.broadcast_to([128, 64])), writes=[dtb_b], dma=True)
        S.op("sp", lambda e: e.dma_start(out=arep.rearrange("p a b -> p (a b)"), in_=alog.broadcast_to([128, 64])), writes=[arep_b], dma=True)
        S.op("dve", lambda e: e.tensor_tensor(out=dt, in0=dt, in1=dtb.unsqueeze(1).broadcast_to([128, 34, 2, 32]), op=ALU.add),
             reads=[dt_b, dtb_b], writes=[dt_b])
        S.op("act", lambda e: e.activation(out=dt, in_=dt, func=AF.Exp), reads=[dt_b], writes=[dt_b])
        S.op("act", lambda e: e.activation(out=dt, in_=dt, func=AF.Ln, bias=1.0), reads=[dt_b], writes=[dt_b])
        S.op("act", lambda e: e.activation(out=arep, in_=arep, func=AF.Exp), reads=[arep_b], writes=[arep_b])
        S.op("dve", lambda e: e.tensor_scalar(out=arep, in0=arep, scalar1=-1.0, scalar2=None, op0=ALU.mult), reads=[arep_b], writes=[arep_b])
        for d in range(2):
            for j in range(2):
                S.op("dve", lambda e, d=d, j=j: e.tensor_tensor(
                    out=adtd[:, :, 2 * d + j, :], in0=dt[:, :, d, :], in1=arep[:, d, :].unsqueeze(1).broadcast_to([128, 34, 32]), op=ALU.mult),
                    reads=[dt_b, arep_b], writes=[adtd_b])
        k = 0
        for d in range(2):
            lhs, lhs_b = (tri, tri_b) if d == 0 else (triT, triT_b)
            for (c0, ncn) in ((0, 16), (16, 16), (32, 2)):
                pa, pb = self.pq(k % 4, 0, 4)
                k += 1
                S.op("pe", lambda e, pa=pa, lhs=lhs, c0=c0, ncn=ncn, d=d: e.matmul(
                    pa[:, 0:ncn * 32].rearrange("p (a b) -> p a b", b=32), lhsT=lhs, rhs=adtd[:, c0:c0 + ncn, 2 * d, :], start=True, stop=True),
                    reads=[lhs_b, adtd_b], writes=pb)
                S.op("dve", lambda e, pa=pa, c0=c0, ncn=ncn, d=d: e.tensor_scalar(
                    out=nacum[:, c0:c0 + ncn, d, :], in0=pa[:, 0:ncn * 32].rearrange("p (a b) -> p a b", b=32),
                    scalar1=-1.0, scalar2=None, op0=ALU.mult), reads=pb, writes=[nacum_b])
                pa, pb = self.pq(k % 4, 0, 4)
                k += 1
                S.op("pe", lambda e, pa=pa, c0=c0, ncn=ncn, d=d: e.matmul(
                    pa[:, 0:ncn * 32].rearrange("p (a b) -> p a b", b=32), lhsT=ones_f, rhs=adtd[:, c0:c0 + ncn, 2 * d, :], start=True, stop=True),
                    reads=[ones_f_b, adtd_b], writes=pb)
                S.op("dve", lambda e, pa=pa, c0=c0, ncn=ncn, d=d: e.tensor_copy(
                    out=tot[:, c0:c0 + ncn, d, :], in_=pa[:, 0:ncn * 32].rearrange("p (a b) -> p a b", b=32)), reads=pb, writes=[tot_b])
        S.op("act", lambda e: e.activation(out=decay, in_=tot, func=AF.Exp), reads=[tot_b], writes=[decay_b])
        S.op("dve", lambda e: e.tensor_tensor(out=wend, in0=tot, in1=nacum, op=ALU.add), reads=[tot_b, nacum_b], writes=[wend_b])
        S.op("act", lambda e: e.activation(out=wend, in_=wend, func=AF.Exp), reads=[wend_b], writes=[wend_b])
        S.op("dve", lambda e: e.tensor_tensor(out=wend, in0=wend, in1=dt, op=ALU.mult), reads=[wend_b, dt_b], writes=[wend_b])
        acT = [ar.alloc([128], F32, f"acT{i}") for i in range(2)]
        hi_ = [ar.alloc([128], BF16, f"hi{i}") for i in range(2)]
        lo_ = [ar.alloc([128], F32, f"lo{i}") for i in range(2)]
        for c in range(34):
            pa, pab = self.pq(4 + (c % 2), 0)
            pb_, pbb = self.pq(4 + (c % 2), 1)
            lhsT = adtd[:, c, :, :].rearrange("p a b -> p (a b)")
            S.op("pe", lambda e, pa=pa, lhsT=lhsT: e.matmul(pa, lhsT=lhsT, rhs=tri, start=True, stop=True),
                 reads=[adtd_b, tri_b], writes=pab)
            S.op("pe", lambda e, pb_=pb_, lhsT=lhsT: e.matmul(pb_, lhsT=lhsT, rhs=triT, start=True, stop=True),
                 reads=[adtd_b, triT_b], writes=pbb)
            a_t, a_b = acT[c % 2]
            h_t, h_b = hi_[c % 2]
            l_t, l_b = lo_[c % 2]
            S.op("act", lambda e, a_t=a_t, pa=pa: e.activation(out=a_t[0:64, :], in_=pa[0:64, :], func=AF.Copy), reads=pab, writes=[a_b])
            S.op("act", lambda e, a_t=a_t, pb_=pb_: e.activation(out=a_t[64:128, :], in_=pb_[64:128, :], func=AF.Copy), reads=pbb, writes=[a_b])
            S.op("dve", lambda e, a_t=a_t, h_t=h_t: e.tensor_copy(out=h_t, in_=a_t), reads=[a_b], writes=[h_b])
            S.op("dve", lambda e, a_t=a_t, h_t=h_t, l_t=l_t: e.tensor_tensor(out=l_t, in0=a_t, in1=h_t, op=ALU.subtract), reads=[a_b, h_b], writes=[l_b])
            S.op("dve", lambda e, l_t=l_t: e.tensor_scalar(out=l_t, in0=l_t, scalar1=mlo[:, 0:1], scalar2=None, op0=ALU.mult), reads=[l_b, mlo_b], writes=[l_b])
            S.op("dve", lambda e, h_t=h_t, l_t=l_t, c=c: e.scalar_tensor_tensor(out=HL[:, c, :], in0=h_t, scalar=mhi[:, 0:1], in1=l_t, op0=ALU.mult, op1=ALU.add),
                 reads=[h_b, l_b, mhi_b], writes=[HL_b])
        S.op("dve", lambda e: e.tensor_scalar(out=HLn, in0=HL, scalar1=-1.0, scalar2=None, op0=ALU.mult), reads=[HL_b], writes=[HLn_b])
        if dbg:
            for nm, src_, sb_, dtp, ncol in (("dbg_dt", dt.rearrange("p a b c -> p (a b c)"), dt_b, F32, 34 * 64),
                                             ("dbg_nacum", nacum.rearrange("p a b c -> p (a b c)"), nacum_b, F32, 34 * 64),
                                             ("dbg_wend", wend.rearrange("p a b c -> p (a b c)"), wend_b, F32, 34 * 64),
                                             ("dbg_HL", HL.rearrange("p a b -> p (a b)"), HL_b, BF16, 34 * 128)):
                o, ob = self.scratch(nm, [128, ncol], dtp, dump=True)
                S.op("sp", lambda e, o=o, src_=src_: e.dma_start(out=o, in_=src_), reads=[sb_], writes=[ob], dma=True)
        S.barrier()
        ar.release()
        P2STOP = int(os.environ.get("P2STOP", "99"))
        P2SKIP = os.environ.get("P2SKIP", "")
        if P2STOP <= 1:
            ar.release()
            return
        TP = 4360
        RA, RA_b = ar.alloc([4 * TP], BF16, "RA")
        R = RA.rearrange("p (a b) -> p a b", a=4)
        prevs = RA[:, 0:2 * 34 * 256].rearrange("p (d c n) -> p d c n", d=2, c=34)
        xc, xc_b = ar.alloc([4, T], BF16, "xc")
        x_tok, x_tok_b = ar.alloc([34, 256], BF16, "x_tok")
        B_tok, B_tok_b = ar.alloc([34, 128], BF16, "B_tok")
        dg, dg_b = ar.alloc([20, 128], BF16, "dg")
        st = [ar.alloc([256], F32, f"st{d}") for d in range(2)]
        xw = [ar.alloc([256], BF16, f"xw{i}") for i in range(4)]
        xdt = [ar.alloc([256], BF16, f"xdt{i}") for i in range(4)]
        E_ = [ar.alloc([4, 128], F32, f"E{i}") for i in range(3)]
        Lt = [ar.alloc([4, 128], F32, f"Lt{i}") for i in range(3)]
        Gt = [ar.alloc([4, 128], BF16, f"Gt{i}") for i in range(4)]
        Cp = [ar.alloc([4, 128], BF16, f"Cp{i}") for i in range(4)]
        zt = [ar.alloc([2, 512], BF16, f"zt{i}") for i in range(1)]
        sz = [ar.alloc([2, 512], F32, f"sz{i}") for i in range(1)]
        yz = [ar.alloc([2, 512], F32, f"yz{i}") for i in range(1)]
        sqy = [ar.alloc([2, 512], BF16, f"sqy{i}") for i in range(1)]
        rsy = [ar.alloc([512], F32, f"rsy{i}") for i in range(1)]
        ybo = [ar.alloc([2, 512], BF16, f"ybo{i}") for i in range(1)]
        bwd_order = [1, 0] + list(range(33, 1, -1))
        cnt = dict(e=0, a=0, g=0, cp=0, xw=0, cs=0, y=0, cb=0, post=0, conv=0, tr=0)
        for g in range(int(os.environ.get('P2G', '8'))):
            tiles = [2 * g, 2 * g + 1, 16 + g, 24 + g]
            for i, tix in enumerate(tiles):
                S.op("sp", lambda e, i=i, tix=tix: e.dma_start(out=R[:, i, 2:258], in_=self.xbc_s[tix * 128:(tix + 1) * 128, 0:256]),
                     reads=[self.xbc_sb], writes=[RA_b], dma=True)
                S.op("sp", lambda e, i=i, tix=tix: e.dma_start(out=R[:, i, 262:4358], in_=self.xbc_s[tix * 128:(tix + 1) * 128, 256:T]),
                     reads=[self.xbc_sb], writes=[RA_b], dma=True)
            for (a0, a1) in ((0, 2), (258, 262), (4358, 4360)):
                S.op("dve", lambda e, a0=a0, a1=a1: e.memset(R[:, :, a0:a1], 0.0), writes=[RA_b])
            for i, tix in enumerate(tiles):
                for kk in range(5):
                    S.op("dve", lambda e, i=i, tix=tix, kk=kk: e.tensor_scalar(
                        out=dg[:, i * 5 + kk, :], in0=ident_f, scalar1=cw[:, tix, kk:kk + 1], scalar2=None, op0=ALU.mult),
                        reads=[ident_f_b, cw_b], writes=[dg_b])
            for i, tix in enumerate(tiles):
                for (t0, n, base) in [(0, 256, 0)] + [(256 + j * 512, 512, 260 + j * 512) for j in range(8)]:
                    pa, pb = self.pq(cnt["conv"] % 2, 0, 4)
                    cnt["conv"] += 1

                    def mm(e, pa=pa, i=i, n=n, base=base):
                        ins = None
                        for kk in range(5):
                            ins = e.matmul(pa[:, 0:n], lhsT=dg[:, i * 5 + kk, :], rhs=R[:, i, base + kk:base + kk + n],
                                           start=(kk == 0), stop=(kk == 4))
                        return ins
                    S.op("pe", mm, reads=[dg_b, RA_b], writes=pb)
                    S.op("act", lambda e, pa=pa, i=i, tix=tix, t0=t0, n=n: e.activation(
                        out=xc[:, i, t0:t0 + n], in_=pa[:, 0:n], func=AF.Silu, bias=cb[:, tix:tix + 1]),
                        reads=pb + [cb_b], writes=[xc_b])
            if P2STOP <= 2:
                break
            for c in range(34):
                pa, pb = self.pq(2 + (cnt["tr"] % 2), 0, 4)
                cnt["tr"] += 1
                pab = pa.bitcast(BF16)

                def tr(e, pab=pab, c=c):
                    ins = None
                    for j in range(3):
                        ins = e.transpose(out=pab[:, j * 128:(j + 1) * 128], in_=xc[:, j, c * 128:(c + 1) * 128], identity=ident_bf)
                    return ins
                S.op("pe", tr, reads=[xc_b, ident_bf_b], writes=pb)
                S.op("dve", lambda e, pab=pab, c=c: e.tensor_copy(out=x_tok[:, c, :], in_=pab[:, 0:256]), reads=pb, writes=[x_tok_b])
                S.op("dve", lambda e, pab=pab, c=c: e.tensor_copy(out=B_tok[:, c, :], in_=pab[:, 256:384]), reads=pb, writes=[B_tok_b])
            if P2STOP <= 3:
                break
            for d in range(2):
                S.op("dve", lambda e, d=d: e.memset(st[d][0], 0.0), writes=[st[d][1]])
            for step in range(34):
                for d in range(2):
                    c = step if d == 0 else bwd_order[step]
                    xw_t, xw_b = xw[cnt["xw"] % 4]
                    cnt["xw"] += 1
                    S.op("dve", lambda e, xw_t=xw_t, c=c, d=d, g=g: e.tensor_tensor(
                        out=xw_t.rearrange("p (r q) -> p r q", r=4), in0=x_tok[:, c, :].rearrange("p (r q) -> p r q", r=4),
                        in1=wend[:, c, d, 4 * g:4 * g + 4].unsqueeze(2).broadcast_to([128, 4, 64]), op=ALU.mult),
                        reads=[x_tok_b, wend_b], writes=[xw_b])
                    k2 = cnt["cs"] % 4
                    cnt["cs"] += 1
                    pa, pb = self.pq(4 + k2, 0, 2)
                    S.op("pe", lambda e, pa=pa, xw_t=xw_t, c=c: e.matmul(pa, lhsT=B_tok[:, c, :], rhs=xw_t, start=True, stop=True),
                         reads=[B_tok_b, xw_b], writes=pb)
                    st_t, st_b = st[d]
                    S.op("act", lambda e, st_t=st_t, d=d, c=c: e.activation(out=prevs[:, d, c, :], in_=st_t, func=AF.Copy),
                         reads=[st_b], writes=[RA_b])
                    S.op("dve", lambda e, st_t=st_t, c=c, d=d, g=g: e.tensor_tensor(
                        out=st_t.rearrange("p (r q) -> p r q", r=4), in0=st_t.rearrange("p (r q) -> p r q", r=4),
                        in1=decay[:, c, d, 4 * g:4 * g + 4].unsqueeze(2).broadcast_to([128, 4, 64]), op=ALU.mult),
                        reads=[st_b, decay_b], writes=[st_b])
                    S.op("dve", lambda e, st_t=st_t, pa=pa: e.tensor_tensor(out=st_t, in0=st_t, in1=pa, op=ALU.add),
                         reads=[st_b] + pb, writes=[st_b])
            if dbg and g == 0:
                o, ob = self.scratch("dbg_xc", [128, 4 * T], BF16, dump=True)
                S.op("sp", lambda e, o=o: e.dma_start(out=o, in_=xc.rearrange("p a b -> p (a b)")), reads=[xc_b], writes=[ob], dma=True)
                o2, ob2 = self.scratch("dbg_prev", [128, 2 * 34 * 256], BF16, dump=True)
                S.op("sp", lambda e, o2=o2: e.dma_start(out=o2, in_=RA[:, 0:2 * 34 * 256]), reads=[RA_b], writes=[ob2], dma=True)
            if P2STOP <= 4:
                continue
            for cb4 in range(8):
                if P2STOP <= 5 and cb4 >= 1:
                    break
                pi = cnt["post"] % 2
                cnt["post"] += 1
                z_t, z_b = zt[0]
                sz_t, sz_b = sz[0]
                yz_t, yz_b = yz[0]
                for i in range(2):
                    r0 = 256 * g + 128 * i
                    S.op("sp", lambda e, z_t=z_t, i=i, r0=r0, cb4=cb4: e.dma_start(out=z_t[:, i, :], in_=self.z_s[r0:r0 + 128, cb4 * 512:(cb4 + 1) * 512]),
                         reads=[self.z_sb], writes=[z_b], dma=True)
                if "Z" not in P2SKIP:
                    S.op("act", lambda e, z_t=z_t, sz_t=sz_t: e.activation(out=sz_t, in_=z_t, func=AF.Silu), reads=[z_b], writes=[sz_b])
                for cl in range(4):
                    lc = cb4 * 4 + cl
                    c = 2 + lc
                    tsl = slice(c * 128, (c + 1) * 128)
                    pcb, pcb_b = self.pq(4 + cnt["cb"] % 2, 0)
                    cnt["cb"] += 1
                    S.op("pe", lambda e, pcb=pcb, tsl=tsl: e.matmul(pcb, lhsT=xc[:, 2, tsl], rhs=xc[:, 3, tsl], start=True, stop=True),
                         reads=[xc_b], writes=pcb_b)
                    gts, cps, xds = {}, {}, {}
                    for d in range(2):
                        xd_t, xd_b = xdt[cnt["xw"] % 4]
                        cnt["xw"] += 1
                        S.op("dve", lambda e, xd_t=xd_t, c=c, d=d, g=g: e.tensor_tensor(
                            out=xd_t.rearrange("p (r q) -> p r q", r=4), in0=x_tok[:, c, :].rearrange("p (r q) -> p r q", r=4),
                            in1=dt[:, c, d, 4 * g:4 * g + 4].unsqueeze(2).broadcast_to([128, 4, 64]), op=ALU.mult),
                            reads=[x_tok_b, dt_b], writes=[xd_b])
                        xds[d] = (xd_t, xd_b)
                        pe_, pe_b = self.pq(cnt["e"] % 2, 0, 4)
                        cnt["e"] += 1

                        def mme(e, pe_=pe_, c=c, d=d, g=g):
                            ins = None
                            for r in range(4):
                                hh = d * 32 + 4 * g + r
                                ins = e.matmul(pe_[:, r * 128:(r + 1) * 128], lhsT=sel2[:, hh, :], rhs=HL[:, c, :], start=True, stop=True)
                            return ins
                        S.op("pe", mme, reads=[sel2_b, HL_b], writes=pe_b)
                        E_t, E_b = E_[cnt["e"] % 3]
                        S.op("act", lambda e, E_t=E_t, pe_=pe_: e.activation(out=E_t.rearrange("p a b -> p (a b)"), in_=pe_, func=AF.Exp),
                             reads=pe_b, writes=[E_b])
                        cp_t, cp_b = Cp[cnt["cp"] % 4]
                        cnt["cp"] += 1
                        S.op("dve", lambda e, cp_t=cp_t, E_t=E_t, tsl=tsl: e.tensor_tensor(
                            out=cp_t, in0=E_t, in1=xc[:, 3, tsl].unsqueeze(1).broadcast_to([128, 4, 128]), op=ALU.mult),
                            reads=[xc_b, E_b], writes=[cp_b])
                        cps[d] = (cp_t, cp_b)
                        pa_, pa_b = self.pq(2 + cnt["a"] % 2, 0, 4)
                        cnt["a"] += 1

                        def mma(e, pa_=pa_, c=c, d=d, g=g):
                            ins = None
                            for r in range(4):
                                hh = d * 32 + 4 * g + r
                                o_ = pa_[:, r * 128:(r + 1) * 128]
                                e.matmul(o_, lhsT=sel2[:, hh, :], rhs=HL[:, c, :], start=True, stop=False)
                                e.matmul(o_, lhsT=HLn[:, c, :], rhs=sel2[:, hh, :], start=False, stop=False)
                                ins = e.matmul(o_, lhsT=ident_bf, rhs=NEG[d][0], start=False, stop=True)
                            return ins
                        S.op("pe", mma, reads=[sel2_b, HL_b, HLn_b, ident_bf_b, NEG[d][1]], writes=pa_b)
                        L_t, L_b = Lt[cnt["a"] % 3]
                        S.op("act", lambda e, L_t=L_t, pa_=pa_: e.activation(out=L_t.rearrange("p a b -> p (a b)"), in_=pa_, func=AF.Exp),
                             reads=pa_b, writes=[L_b])
                        g_t, g_b = Gt[cnt["g"] % 4]
                        cnt["g"] += 1
                        S.op("dve", lambda e, g_t=g_t, L_t=L_t, pcb=pcb: e.tensor_tensor(
                            out=g_t, in0=L_t, in1=pcb.unsqueeze(1).broadcast_to([128, 4, 128]), op=ALU.mult),
                            reads=[L_b] + pcb_b, writes=[g_b])
                        gts[d] = (g_t, g_b)
                    yb_i = 6 + cnt["y"] % 2
                    cnt["y"] += 1
                    pys = [self.pq(yb_i, i) for i in range(2)]

                    def mmy(e, pys=pys, c=c, gts=gts, cps=cps, xds=xds):
                        ins = None
                        for i in range(2):
                            py = pys[i][0]
                            for hf in range(2):
                                r = 2 * i + hf
                                o_ = py[64 * hf:64 * hf + 64, :]
                                tp = (0, 64 * hf)
                                e.matmul(o_, lhsT=xds[0][0][:, r * 64:(r + 1) * 64], rhs=gts[0][0][:, r, :], start=True, stop=False, tile_position=tp)
                                e.matmul(o_, lhsT=prevs[:, 0, c, r * 64:(r + 1) * 64], rhs=cps[0][0][:, r, :], start=False, stop=False, tile_position=tp)
                                e.matmul(o_, lhsT=xds[1][0][:, r * 64:(r + 1) * 64], rhs=gts[1][0][:, r, :], start=False, stop=False, tile_position=tp)
                                ins = e.matmul(o_, lhsT=prevs[:, 1, c, r * 64:(r + 1) * 64], rhs=cps[1][0][:, r, :], start=False, stop=True, tile_position=tp)
                        return ins
                    rd = [RA_b]
                    for d in range(2):
                        rd += [gts[d][1], cps[d][1], xds[d][1]]
                    S.op("pe", mmy, reads=rd, writes=pys[0][1])
                    for i in range(2):
                        py = pys[i][0]
                        S.op("dve", lambda e, py=py, i=i, g=g, tsl=tsl, yz_t=yz_t, cl=cl: e.scalar_tensor_tensor(
                            out=yz_t[:, i, cl * 128:(cl + 1) * 128], in0=xc[:, i, tsl], scalar=dcol[:, 2 * g + i:2 * g + i + 1], in1=py,
                            op0=ALU.mult, op1=ALU.add), reads=[xc_b, dcol_b] + pys[i][1], writes=[yz_b])
                if "O" in P2SKIP:
                    continue
                q_t, q_b = sqy[0]
                r_t, r_b = rsy[0]
                o_t, o_b = ybo[0]
                S.op("dve", lambda e, yz_t=yz_t, sz_t=sz_t: e.tensor_tensor(out=yz_t, in0=yz_t, in1=sz_t, op=ALU.mult), reads=[yz_b, sz_b], writes=[yz_b])
                S.op("act", lambda e, yz_t=yz_t, q_t=q_t: e.activation(out=q_t, in_=yz_t, func=AF.Square), reads=[yz_b], writes=[q_b])
                pp, pp_b = self.pq(pi, 0, 4)

                def mmq(e, pp=pp, q_t=q_t):
                    e.matmul(pp, lhsT=ones_bf, rhs=q_t[:, 0, :], start=True, stop=False)
                    return e.matmul(pp, lhsT=ones_bf, rhs=q_t[:, 1, :], start=False, stop=True)
                S.op("pe", mmq, reads=[q_b, ones_bf_b], writes=pp_b)
                S.op("act", lambda e, pp=pp, r_t=r_t: e.activation(out=r_t, in_=pp, func=AF.Sqrt, scale=1.0 / 256, bias=EPS), reads=pp_b, writes=[r_b])
                S.op("dve", lambda e, r_t=r_t: e.reciprocal(out=r_t, in_=r_t), reads=[r_b], writes=[r_b])
                for i in range(2):
                    S.op("dve", lambda e, i=i, yz_t=yz_t, r_t=r_t, o_t=o_t, g=g: e.scalar_tensor_tensor(
                        out=o_t[:, i, :], in0=yz_t[:, i, :], scalar=nwc[:, 2 * g + i:2 * g + i + 1], in1=r_t, op0=ALU.mult, op1=ALU.mult),
                        reads=[yz_b, r_b, nwc_b], writes=[o_b])
                    r0 = 256 * g + 128 * i
                    S.op("sp", lambda e, o_t=o_t, i=i, r0=r0, cb4=cb4: e.dma_start(out=self.yb_s[r0:r0 + 128, cb4 * 512:(cb4 + 1) * 512], in_=o_t[:, i, :]),
                         reads=[o_b], writes=[self.yb_sb], dma=True)
        S.barrier()
        ar.release()


    def trig(self, ar, src, n, add, out, out_b, src_b, tag):
        S = self.S
        t, t_b = ar.alloc([n], F32, f"trg_t{tag}")
        ti, ti_b = ar.alloc([n], I32, f"trg_i{tag}")
        S.op("dve", lambda e: e.tensor_scalar(out=t, in0=src, scalar1=64.0 + add, scalar2=None, op0=ALU.add), reads=[src_b], writes=[t_b])
        S.op("dve", lambda e: e.tensor_copy(out=ti, in_=t), reads=[t_b], writes=[ti_b])
        S.op("dve", lambda e: e.tensor_copy(out=out, in_=ti), reads=[ti_b], writes=[out_b])
        S.op("dve", lambda e: e.tensor_tensor(out=t, in0=t, in1=out, op=ALU.subtract), reads=[t_b, out_b], writes=[t_b])
        S.op("act", lambda e: e.activation(out=out, in_=t, func=AF.Sin, scale=2.0 * np.pi), reads=[t_b], writes=[out_b])

    def cpow_tables(self, ar, lre, lim, ldt, n, bufs, tag):
        S = self.S
        TB = Buf(f"cpow{tag}")
        step, _ = ar.alloc([n], F32, "step")
        lr, _ = ar.alloc([n], F32, "lr")
        th, _ = ar.alloc([n], F32, "th")
        S.op("act", lambda e: e.activation(out=step, in_=ldt, func=AF.Exp), reads=bufs, writes=[TB])
        S.op("dve", lambda e: e.tensor_tensor(out=lr, in0=lre, in1=step, op=ALU.mult), reads=bufs + [TB], writes=[TB])
        S.op("dve", lambda e: e.scalar_tensor_tensor(out=th, in0=lim, scalar=1.0 / (2.0 * np.pi), in1=step, op0=ALU.mult, op1=ALU.mult),
             reads=bufs + [TB], writes=[TB])
        are, _ = ar.alloc([9, n], F32, "are")
        aim, _ = ar.alloc([9, n], F32, "aim")
        mag, _ = ar.alloc([n], F32, "mag")
        ph, _ = ar.alloc([n], F32, "ph")
        ck, ck_b = ar.alloc([n], F32, "ck")
        sk, sk_b = ar.alloc([n], F32, "sk")
        for k in range(9):
            ar.mark()
            S.op("act", lambda e, k=k: e.activation(out=mag, in_=lr, func=AF.Exp, scale=float(k)), reads=[TB], writes=[TB])
            S.op("dve", lambda e, k=k: e.tensor_scalar(out=ph, in0=th, scalar1=float(k), scalar2=None, op0=ALU.mult), reads=[TB], writes=[TB])
            self.trig(ar, ph, n, 0.25, ck, ck_b, TB, f"{tag}c{k}")
            self.trig(ar, ph, n, 0.0, sk, sk_b, TB, f"{tag}s{k}")
            S.op("dve", lambda e, k=k: e.tensor_tensor(out=are[:, k, :], in0=mag, in1=ck, op=ALU.mult), reads=[TB, ck_b], writes=[TB])
            S.op("dve", lambda e, k=k: e.tensor_tensor(out=aim[:, k, :], in0=mag, in1=sk, op=ALU.mult), reads=[TB, sk_b], writes=[TB])
            ar.release()
        return dict(are=are, aim=aim, lr=lr, th=th, buf=TB)

    def cmul(self, eng, out_re, out_im, a_re, a_im, b_re, b_im, t1, t2, reads, writes):
        S = self.S
        S.op(eng, lambda e: e.tensor_tensor(out=t1, in0=a_re, in1=b_re, op=ALU.mult), reads=reads, writes=writes)
        S.op(eng, lambda e: e.tensor_tensor(out=t2, in0=a_im, in1=b_im, op=ALU.mult), reads=reads, writes=writes)
        S.op(eng, lambda e: e.tensor_tensor(out=out_re, in0=t1, in1=t2, op=ALU.subtract), reads=reads, writes=writes)
        S.op(eng, lambda e: e.tensor_tensor(out=t1, in0=a_re, in1=b_im, op=ALU.mult), reads=reads, writes=writes)
        S.op(eng, lambda e: e.tensor_tensor(out=t2, in0=a_im, in1=b_re, op=ALU.mult), reads=reads, writes=writes)
        S.op(eng, lambda e: e.tensor_tensor(out=out_im, in0=t1, in1=t2, op=ALU.add), reads=reads, writes=writes)

    def bc_coef(self, ar, pw, lre, lim, n, bufs):
        S = self.S
        TB = pw["buf"]
        rd = bufs + [TB]
        nre, _ = ar.alloc([n], F32, "nre")
        den, _ = ar.alloc([n], F32, "den")
        t1, _ = ar.alloc([n], F32, "bct1")
        bcr, _ = ar.alloc([n], F32, "bcr")
        bci, _ = ar.alloc([n], F32, "bci")
        are1, aim1 = pw["are"][:, 1, :], pw["aim"][:, 1, :]
        S.op("dve", lambda e: e.tensor_scalar(out=nre, in0=are1, scalar1=-1.0, scalar2=None, op0=ALU.add), reads=rd, writes=[TB])
        S.op("dve", lambda e: e.tensor_tensor(out=den, in0=lre, in1=lre, op=ALU.mult), reads=rd, writes=[TB])
        S.op("dve", lambda e: e.tensor_tensor(out=t1, in0=lim, in1=lim, op=ALU.mult), reads=rd, writes=[TB])
        S.op("dve", lambda e: e.tensor_tensor(out=den, in0=den, in1=t1, op=ALU.add), reads=rd, writes=[TB])
        S.op("dve", lambda e: e.reciprocal(out=den, in_=den), reads=rd, writes=[TB])
        S.op("dve", lambda e: e.tensor_tensor(out=bcr, in0=nre, in1=lre, op=ALU.mult), reads=rd, writes=[TB])
        S.op("dve", lambda e: e.tensor_tensor(out=t1, in0=aim1, in1=lim, op=ALU.mult), reads=rd, writes=[TB])
        S.op("dve", lambda e: e.tensor_tensor(out=bcr, in0=bcr, in1=t1, op=ALU.add), reads=rd, writes=[TB])
        S.op("dve", lambda e: e.tensor_tensor(out=bcr, in0=bcr, in1=den, op=ALU.mult), reads=rd, writes=[TB])
        S.op("dve", lambda e: e.tensor_tensor(out=bci, in0=aim1, in1=lre, op=ALU.mult), reads=rd, writes=[TB])
        S.op("dve", lambda e: e.tensor_tensor(out=t1, in0=nre, in1=lim, op=ALU.mult), reads=rd, writes=[TB])
        S.op("dve", lambda e: e.tensor_tensor(out=bci, in0=bci, in1=t1, op=ALU.subtract), reads=rd, writes=[TB])
        S.op("dve", lambda e: e.tensor_tensor(out=bci, in0=bci, in1=den, op=ALU.mult), reads=rd, writes=[TB])
        return bcr, bci

    def phase3_setup(self, s5F, s5S):
        nc, S, ar = self.nc, self.S, self.ar
        dbg = self.debug
        ident_f, ident_f_b = self.consts["ident_f"]
        self.SI_s, self.SI_sb = self.scratch("SI_s", [2, 8, 128, 2048], BF16, dump=dbg)
        self.RO_s, self.RO_sb = self.scratch("RO_s", [2, 8, 128, 2048], BF16, dump=dbg)
        self.FIR_s, self.FIR_sb = self.scratch("FIR_s", [2, 8, 128, 1024], BF16, dump=dbg)
        self.RS, self.RS_b = ar.alloc([2, 2, 32], F32, "RS")
        mF = [ar.alloc([1], F32, f"mF{i}") for i in range(2)]
        mS = [ar.alloc([1], F32, f"mS{i}") for i in range(2)]
        mSn = [ar.alloc([1], F32, f"mSn{i}") for i in range(2)]
        self.rowmask = [ar.alloc([1], F32, f"rowm{i}") for i in range(4)]
        for i in range(4):
            S.op("dve", lambda e, i=i: e.reduce_sum(out=self.rowmask[i][0], in_=ident_f[:, 32 * i:32 * i + 32], axis=AX.X),
                 reads=[ident_f_b], writes=[self.rowmask[i][1]])
        for i in range(2):
            S.op("dve", lambda e, i=i: e.reduce_sum(out=mS[i][0], in_=ident_f[:, 64 * i:64 * i + 64], axis=AX.X), reads=[ident_f_b], writes=[mS[i][1]])
            S.op("dve", lambda e, i=i: e.tensor_scalar(out=mSn[i][0], in0=mS[i][0], scalar1=-1.0, scalar2=None, op0=ALU.mult), reads=[mS[i][1]], writes=[mSn[i][1]])
            S.op("dve", lambda e, i=i: e.reduce_sum(out=mF[i][0], in_=ident_f.rearrange("p (a b c) -> p a b c", a=4, b=2)[:, :, i, :], axis=AX.XY),
                 reads=[ident_f_b], writes=[mF[i][1]])
        for d in range(2):
            ar.mark()
            Ft, Ft_b = ar.alloc([5, 512], F32, "Ft")
            S.op("sp", lambda e, d=d: e.dma_start(out=Ft, in_=s5F[d].rearrange("p a f q -> p a (f q)")), writes=[Ft_b], dma=True)
            pw = self.cpow_tables(ar, Ft[:, 0, :], Ft[:, 1, :], Ft[:, 2, :], 512, [Ft_b], f"F{d}")
            TB = pw["buf"]
            bcr, bci = self.bc_coef(ar, pw, Ft[:, 0, :], Ft[:, 1, :], 512, [Ft_b])
            t1, _ = ar.alloc([512], F32, "ft1")
            t2, _ = ar.alloc([512], F32, "ft2")
            bbr, _ = ar.alloc([512], F32, "bbr")
            bbi, _ = ar.alloc([512], F32, "bbi")
            self.cmul("dve", bbr, bbi, bcr, bci, Ft[:, 3, :], Ft[:, 4, :], t1, t2, [Ft_b, TB], [TB])
            wr_, _ = ar.alloc([512], F32, "wr_")
            wi_, _ = ar.alloc([512], F32, "wi_")
            SIt, SIt_b = ar.alloc([8, 8, 2, 2, 64], BF16, "SIt")
            for j in range(8):
                kj = 7 - j if d == 0 else j
                self.cmul("dve", wr_, wi_, pw["are"][:, kj, :], pw["aim"][:, kj, :], bbr, bbi, t1, t2, [TB], [TB])
                for ri, w_ in enumerate((wr_, wi_)):
                    for gq in range(2):
                        S.op("dve", lambda e, j=j, ri=ri, gq=gq, w_=w_: e.tensor_scalar(
                            out=SIt[:, :, j, ri, gq, :], in0=w_.rearrange("p (f q) -> p f q", f=8), scalar1=mF[gq][0][:, 0:1], scalar2=None, op0=ALU.mult),
                            reads=[TB, mF[gq][1]], writes=[SIt_b])
            S.op("sp", lambda e, d=d: e.dma_start(out=self.SI_s[d].rearrange("f p n -> p f n"), in_=SIt.rearrange("p f j r g q -> p f (j r g q)")),
                 reads=[SIt_b], writes=[self.SI_sb], dma=True)
            S.barrier()
            ar.release()
            ar.mark()
            NS = 3 * 32 + 4 * 512
            St, St_b = ar.alloc([NS], F32, "St")
            S.op("sp", lambda e, d=d: e.dma_start(out=St, in_=s5S[d]), writes=[St_b], dma=True)
            lre, lim, ldt = St[:, 0:32], St[:, 32:64], St[:, 64:96]
            Bre = St[:, 96:96 + 512].rearrange("p (g h) -> p g h", h=16)
            Bim = St[:, 96 + 512:96 + 1024].rearrange("p (g h) -> p g h", h=16)
            Cre = St[:, 96 + 1024:96 + 1536].rearrange("p (g h) -> p g h", h=16)
            Cim = St[:, 96 + 1536:96 + 2048].rearrange("p (g h) -> p g h", h=16)
            pw = self.cpow_tables(ar, lre, lim, ldt, 32, [St_b], f"S{d}")
            TB = pw["buf"]
            bcr, bci = self.bc_coef(ar, pw, lre, lim, 32, [St_b])
            bc3 = lambda a: a.unsqueeze(2).broadcast_to([128, 32, 16])
            t1, _ = ar.alloc([32, 16], F32, "st1")
            t2, _ = ar.alloc([32, 16], F32, "st2")
            bbr, _ = ar.alloc([32, 16], F32, "sbbr")
            bbi, _ = ar.alloc([32, 16], F32, "sbbi")
            self.cmul("dve", bbr, bbi, bc3(bcr), bc3(bci), Bre, Bim, t1, t2, [St_b, TB], [TB])
            S.op("act", lambda e, d=d: e.activation(out=self.RS[:, d, 0, :], in_=pw["lr"], func=AF.Exp, scale=8.0), reads=[TB], writes=[self.RS_b])
            p8, p8_b = ar.alloc([32], F32, "p8")
            p8i, p8i_b = ar.alloc([32], I32, "p8i")
            p8f, p8f_b = ar.alloc([32], F32, "p8f")
            S.op("dve", lambda e: e.tensor_scalar(out=p8, in0=pw["th"], scalar1=8.0, scalar2=64.0, op0=ALU.mult, op1=ALU.add), reads=[TB], writes=[p8_b])
            S.op("dve", lambda e: e.tensor_copy(out=p8i, in_=p8), reads=[p8_b], writes=[p8i_b])
            S.op("dve", lambda e: e.tensor_copy(out=p8f, in_=p8i), reads=[p8i_b], writes=[p8f_b])
            S.op("dve", lambda e, d=d: e.tensor_tensor(out=self.RS[:, d, 1, :], in0=p8, in1=p8f, op=ALU.subtract), reads=[p8_b, p8f_b], writes=[self.RS_b])
            vr_, _ = ar.alloc([32, 16], F32, "vr_")
            vi_, _ = ar.alloc([32, 16], F32, "vi_")
            ROt, ROt_b = ar.alloc([32, 8, 2, 2, 16], BF16, "ROt")
            for j in range(8):
                kj = j + 1 if d == 0 else 8 - j
                self.cmul("dve", vr_, vi_, Cre, Cim, bc3(pw["are"][:, kj, :]), bc3(pw["aim"][:, kj, :]), t1, t2, [St_b, TB], [TB])
                for ri, (v_, ms) in enumerate(((vr_, mS), (vi_, mSn))):
                    for gq in range(2):
                        S.op("dve", lambda e, j=j, ri=ri, gq=gq, v_=v_, ms=ms: e.tensor_scalar(
                            out=ROt[:, :, j, ri, gq, :], in0=v_, scalar1=ms[gq][0][:, 0:1], scalar2=None, op0=ALU.mult),
                            reads=[TB, ms[gq][1]], writes=[ROt_b])
            S.op("sp", lambda e, d=d: e.dma_start(out=self.RO_s[d].rearrange("f p n -> p f n"),
                                                in_=ROt.rearrange("p (f g) j r q h -> p f (g j r q h)", f=8)),
                 reads=[ROt_b], writes=[self.RO_sb], dma=True)
            W2p = [ar.alloc([8, 4, 8, 16], BF16, f"W2p{ri}") for ri in range(2)]
            W1p = [[ar.alloc([8, 4, 8, 16], BF16, f"W1p{b}{ri}") for ri in range(2)] for b in range(2)]
            for t_, tb_ in W2p + W1p[0] + W1p[1]:
                S.op("dve", lambda e, t_=t_: e.memset(t_, 0.0), writes=[tb_])
            g4 = lambda a: a.rearrange("p (f g) h -> p f g h", g=4)
            for ri, (c_, ms) in enumerate(((Cre, mS), (Cim, mSn))):
                for gp4 in range(4):
                    for gq in range(2):
                        S.op("dve", lambda e, ri=ri, gp4=gp4, gq=gq, c_=c_, ms=ms: e.tensor_scalar(
                            out=W2p[ri][0][:, :, gp4, 2 * gp4 + gq, :], in0=g4(c_)[:, :, gp4, :], scalar1=ms[gq][0][:, 0:1], scalar2=None, op0=ALU.mult),
                            reads=[St_b, ms[gq][1]], writes=[W2p[ri][1]])
            FIRt, FIRt_b = ar.alloc([8, 8, 128], BF16, "FIRt")
            w1r, _ = ar.alloc([32, 16], F32, "w1r")
            w1i, _ = ar.alloc([32, 16], F32, "w1i")
            cnt = 0
            for tau in range(8):
                self.cmul("dve", w1r, w1i, bc3(pw["are"][:, tau, :]), bc3(pw["aim"][:, tau, :]), bbr, bbi, t1, t2, [TB], [TB])
                W1 = W1p[tau % 2]
                for ri, w_ in enumerate((w1r, w1i)):
                    for gp4 in range(4):
                        for gq in range(2):
                            S.op("dve", lambda e, ri=ri, gp4=gp4, gq=gq, w_=w_, W1=W1: e.tensor_scalar(
                                out=W1[ri][0][:, :, gp4, 2 * gp4 + gq, :], in0=g4(w_)[:, :, gp4, :], scalar1=mS[gq][0][:, 0:1], scalar2=None, op0=ALU.mult),
                                reads=[TB, mS[gq][1]], writes=[W1[ri][1]])
                for fc in range(8):
                    pa, pb = self.pq(cnt % 8, 0)
                    cnt += 1

                    def mm(e, pa=pa, fc=fc, W1=W1):
                        ins = None
                        k = 0
                        for ri in range(2):
                            for gp4 in range(4):
                                ins = e.matmul(pa, lhsT=W1[ri][0][:, fc, gp4, :, :].rearrange("p a b -> p (a b)"),
                                               rhs=W2p[ri][0][:, fc, gp4, :, :].rearrange("p a b -> p (a b)"), start=(k == 0), stop=(k == 7))
                                k += 1
                        return ins
                    S.op("pe", mm, reads=[W1[0][1], W1[1][1], W2p[0][1], W2p[1][1]], writes=pb)
                    eng = "act" if cnt % 2 == 0 else "dve"
                    if eng == "act":
                        S.op("act", lambda e, pa=pa, fc=fc, tau=tau: e.activation(out=FIRt[:, fc, tau, :], in_=pa, func=AF.Copy), reads=pb, writes=[FIRt_b])
                    else:
                        S.op("dve", lambda e, pa=pa, fc=fc, tau=tau: e.tensor_copy(out=FIRt[:, fc, tau, :], in_=pa), reads=pb, writes=[FIRt_b])
            S.op("sp", lambda e, d=d: e.dma_start(out=self.FIR_s[d].rearrange("f p n -> p f n"), in_=FIRt.rearrange("p f t n -> p f (t n)")),
                 reads=[FIRt_b], writes=[self.FIR_sb], dma=True)
            S.barrier()
            ar.release()
        if dbg:
            o, ob = self.scratch("dbg_RS", [128, 128], F32, dump=True)
            S.op("sp", lambda e: e.dma_start(out=o, in_=self.RS.rearrange("p a b c -> p (a b c)")), reads=[self.RS_b], writes=[ob], dma=True)


    def phase3_main(self, s5_d):
        nc, S, ar = self.nc, self.S, self.ar
        ps = self.ps
        dbg = self.debug
        self.g_s, self.g_sb = self.scratch("g_s", [D, L], BF16, dump=dbg)
        RS, RS_b = self.RS, self.RS_b
        NCH = 544
        ar.mark()
        dcol, dcol_b = ar.alloc([8], F32, "s5d")
        S.op("sp", lambda e: e.dma_start(out=dcol, in_=s5_d), writes=[dcol_b], dma=True)
        iot_i, iot_ib = ar.alloc([NCH], I32, "iota_i")
        iot, iot_b = ar.alloc([NCH], F32, "iota_f")
        S.op("pool", lambda e: e.iota(iot_i, pattern=[[1, NCH]], base=0, channel_multiplier=0), writes=[iot_ib])
        S.op("dve", lambda e: e.tensor_copy(out=iot, in_=iot_i), reads=[iot_ib], writes=[iot_b])
        u_t, u_b = ar.alloc([T], BF16, "u_fc")
        um, um_b = ar.alloc([4, T], BF16, "um")
        SI_t, SI_b = ar.alloc([2, 2048], BF16, "SI_t")
        RO_t, RO_b = ar.alloc([2, 2048], BF16, "RO_t")
        FIR_t, FIR_b = ar.alloc([2, 1024], BF16, "FIR_t")
        cn, cn_b = ar.alloc([4, NCH], F32, "cn")
        sn, sn_b = ar.alloc([4, NCH], F32, "sn")
        pht, pht_b = ar.alloc([4, NCH], F32, "pht")
        phi_, phi_b = ar.alloc([4, NCH], I32, "phi_")
        self._s5_frac, self._s5_frac_b = ar.alloc([4, NCH], F32, "s5frac")
        Ssb = [[ar.alloc([NCH], F32, f"Ssb{k}{ri}") for ri in range(2)] for k in range(2)]
        V = [ar.alloc([NCH], F32, f"V{ri}") for ri in range(2)]
        W = [ar.alloc([NCH], F32, f"W{ri}") for ri in range(2)]
        tmp = [ar.alloc([NCH], F32, f"s5t{i}") for i in range(4)]
        Zp = [[[ar.alloc([512], BF16, f"Zp{d}{g}{ri}") for ri in range(2)] for g in range(4)] for d in range(2)]
        gst = [ar.alloc([L], BF16, f"gst{i}") for i in range(2)]
        ys = [ar.alloc([2, 256], F32, f"ys{i}") for i in range(2)]
        kset = 0
        for fc in range(8):
            S.op("sp", lambda e, fc=fc: e.dma_start(out=u_t, in_=self.s5u[fc * 128:(fc + 1) * 128, :]), reads=[self.s5u_b], writes=[u_b], dma=True)
            S.op("sp", lambda e, fc=fc: e.dma_start(out=SI_t, in_=self.SI_s[:, fc].rearrange("d p n -> p d n")), reads=[self.SI_sb], writes=[SI_b], dma=True)
            S.op("sp", lambda e, fc=fc: e.dma_start(out=RO_t, in_=self.RO_s[:, fc].rearrange("d p n -> p d n")), reads=[self.RO_sb], writes=[RO_b], dma=True)
            S.op("sp", lambda e, fc=fc: e.dma_start(out=FIR_t, in_=self.FIR_s[:, fc].rearrange("d p n -> p d n")), reads=[self.FIR_sb], writes=[FIR_b], dma=True)
            for i in range(4):
                S.op("act", lambda e, i=i: e.activation(out=um[:, i, :], in_=u_t, func=AF.Copy, scale=self.rowmask[i][0][:, 0:1]),
                     reads=[u_b, self.rowmask[i][1]], writes=[um_b])
            for d in range(2):
                S.op("dve", lambda e, d=d, fc=fc: e.tensor_tensor(
                    out=pht, in0=iot.unsqueeze(1).broadcast_to([128, 4, NCH]),
                    in1=RS[:, d, 1, 4 * fc:4 * fc + 4].unsqueeze(2).broadcast_to([128, 4, NCH]), op=ALU.mult),
                    reads=[iot_b, RS_b], writes=[pht_b])
                for (dst, dst_b, add) in ((cn, cn_b, 0.25), (sn, sn_b, 0.0)):
                    S.op("dve", lambda e, dst=dst, add=add: e.tensor_scalar(out=dst, in0=pht, scalar1=64.0 + add, scalar2=None, op0=ALU.add),
                         reads=[pht_b], writes=[dst_b])
                    S.op("dve", lambda e, dst=dst: e.tensor_copy(out=phi_, in_=dst), reads=[dst_b], writes=[phi_b])
                    S.op("dve", lambda e, dst=dst: e.tensor_copy(out=self._s5_frac, in_=phi_), reads=[phi_b], writes=[self._s5_frac_b])
                    S.op("dve", lambda e, dst=dst: e.tensor_tensor(out=dst, in0=dst, in1=self._s5_frac, op=ALU.subtract),
                         reads=[dst_b, self._s5_frac_b], writes=[dst_b])
                    S.op("act", lambda e, dst=dst: e.activation(out=dst, in_=dst, func=AF.Sin, scale=2.0 * np.pi), reads=[dst_b], writes=[dst_b])
                for gp4 in range(4):
                    gp = 4 * fc + gp4
                    b0 = 3 * (kset % 2)
                    kset += 1
                    bufs3 = [self.psb[b0], self.psb[b0 + 1], self.psb[b0 + 2]]

                    def mm(e, d=d, gp4=gp4, b0=b0):
                        ins = None
                        for ri in range(2):
                            for j in range(8):
                                ins = e.matmul(ps[b0 + ri][:, 0:512], lhsT=SI_t[:, d, (j * 2 + ri) * 128:(j * 2 + ri + 1) * 128],
                                               rhs=um[:, gp4, LC + j:T:8], start=(j == 0), stop=(j == 7))
                        for ri in range(2):
                            for j in range(8):
                                ins = e.matmul(ps[b0 + 2][:, ri * 32:(ri + 1) * 32], lhsT=SI_t[:, d, (j * 2 + ri) * 128:(j * 2 + ri + 1) * 128],
                                               rhs=um[:, gp4, j:LC:8], start=(j == 0), stop=(j == 7))
                        return ins
                    S.op("pe", mm, reads=[SI_b, um_b], writes=bufs3)
                    Sk = Ssb[kset % 2]
                    for ri in range(2):
                        s_t, s_b = Sk[ri]
                        src_c = ps[b0 + 2][:, ri * 32:(ri + 1) * 32]
                        src_l = ps[b0 + ri][:, 0:512]
                        if d == 1:
                            src_c = src_c[:, ::-1]
                            src_l = src_l[:, ::-1]
                        S.op("act", lambda e, s_t=s_t, src_c=src_c: e.activation(out=s_t[:, 0:32], in_=src_c, func=AF.Copy),
                             reads=[self.psb[b0 + 2]], writes=[s_b])
                        S.op("act", lambda e, s_t=s_t, src_l=src_l: e.activation(out=s_t[:, 32:NCH], in_=src_l, func=AF.Copy),
                             reads=[self.psb[b0 + ri]], writes=[s_b])
                    (Sr, Sr_b), (Si, Si_b) = Sk
                    cnv, snv = cn[:, gp4, :], sn[:, gp4, :]
                    (t1, t1b), (t2, t2b), (t3, t3b), (t4, t4b) = tmp
                    (Vr, Vr_b), (Vi, Vi_b) = V
                    (Wr, Wr_b), (Wi, Wi_b) = W
                    S.op("dve", lambda e, cnv=cnv, Sr=Sr: e.tensor_tensor(out=t1, in0=cnv, in1=Sr, op=ALU.mult), reads=[cn_b, Sr_b], writes=[t1b])
                    S.op("dve", lambda e, snv=snv, Si=Si: e.tensor_tensor(out=t2, in0=snv, in1=Si, op=ALU.mult), reads=[sn_b, Si_b], writes=[t2b])
                    S.op("dve", lambda e, cnv=cnv, Si=Si: e.tensor_tensor(out=t3, in0=cnv, in1=Si, op=ALU.mult), reads=[cn_b, Si_b], writes=[t3b])
                    S.op("dve", lambda e, snv=snv, Sr=Sr: e.tensor_tensor(out=t4, in0=snv, in1=Sr, op=ALU.mult), reads=[sn_b, Sr_b], writes=[t4b])
                    S.op("dve", lambda e: e.tensor_tensor(out=Vr, in0=t1, in1=t2, op=ALU.add), reads=[t1b, t2b], writes=[Vr_b])
                    S.op("dve", lambda e: e.tensor_tensor(out=Vi, in0=t3, in1=t4, op=ALU.subtract), reads=[t3b, t4b], writes=[Vi_b])
                    Rb = RS[:, d, 0, gp:gp + 1].broadcast_to([128, NCH])
                    S.op("dve", lambda e, Rb=Rb: e.tensor_tensor_scan(out=Wr, data0=Rb, data1=Vr, initial=0.0, op0=ALU.mult, op1=ALU.add),
                         reads=[RS_b, Vr_b], writes=[Wr_b])
                    S.op("dve", lambda e, Rb=Rb: e.tensor_tensor_scan(out=Wi, data0=Rb, data1=Vi, initial=0.0, op0=ALU.mult, op1=ALU.add),
                         reads=[RS_b, Vi_b], writes=[Wi_b])
                    sl = slice(31, 543)
                    (zr, zr_b), (zi, zi_b) = Zp[d][gp4]
                    zro = zr if d == 0 else zr[:, ::-1]
                    zio = zi if d == 0 else zi[:, ::-1]
                    S.op("dve", lambda e, cnv=cnv: e.tensor_tensor(out=t1[:, sl], in0=cnv[:, sl], in1=Wr[:, sl], op=ALU.mult), reads=[cn_b, Wr_b], writes=[t1b])
                    S.op("dve", lambda e, snv=snv: e.tensor_tensor(out=t2[:, sl], in0=snv[:, sl], in1=Wi[:, sl], op=ALU.mult), reads=[sn_b, Wi_b], writes=[t2b])
                    S.op("dve", lambda e, cnv=cnv: e.tensor_tensor(out=t3[:, sl], in0=cnv[:, sl], in1=Wi[:, sl], op=ALU.mult), reads=[cn_b, Wi_b], writes=[t3b])
                    S.op("dve", lambda e, snv=snv: e.tensor_tensor(out=t4[:, sl], in0=snv[:, sl], in1=Wr[:, sl], op=ALU.mult), reads=[sn_b, Wr_b], writes=[t4b])
                    S.op("dve", lambda e, zro=zro: e.tensor_tensor(out=zro, in0=t1[:, sl], in1=t2[:, sl], op=ALU.subtract), reads=[t1b, t2b], writes=[zr_b])
                    S.op("dve", lambda e, zio=zio: e.tensor_tensor(out=zio, in0=t3[:, sl], in1=t4[:, sl], op=ALU.add), reads=[t3b, t4b], writes=[zi_b])
            g_t, g_b = gst[fc % 2]
            for hb in range(2):
                c0 = 256 * hb
                for j in range(8):
                    bank = 4 + j // 2
                    reg = ps[bank][:, (j % 2) * 256:(j % 2) * 256 + 256]

                    def mmy(e, j=j, reg=reg, c0=c0):
                        ops = []
                        for tau in range(0, j + 1):
                            st0 = LC + 8 * c0 + (j - tau)
                            ops.append((reg, FIR_t[:, 0, tau * 128:(tau + 1) * 128], u_t[:, st0:st0 + 2041:8], None))
                        for tau in range(0, 8 - j):
                            st0 = LC + 8 * c0 + (j + tau)
                            ops.append((reg, FIR_t[:, 1, tau * 128:(tau + 1) * 128], u_t[:, st0:st0 + 2041:8], None))
                        for d in range(2):
                            for gp4 in range(4):
                                for ri in range(2):
                                    o0 = ((gp4 * 8 + j) * 2 + ri) * 32
                                    ops.append((reg[32 * gp4:32 * gp4 + 32, :], RO_t[:, d, o0:o0 + 32], Zp[d][gp4][ri][0][:, c0:c0 + 256], (0, 32 * gp4)))
                        ins = None
                        for k, (o_, l_, r_, tp) in enumerate(ops):
                            if tp is None:
                                ins = e.matmul(o_, lhsT=l_, rhs=r_, start=(k == 0), stop=(k == len(ops) - 1))
                            else:
                                ins = e.matmul(o_, lhsT=l_, rhs=r_, start=(k == 0), stop=(k == len(ops) - 1), tile_position=tp)
                        return ins
                    rd = [FIR_b, RO_b, u_b] + [Zp[d][g][ri][1] for d in range(2) for g in range(4) for ri in range(2)]
                    S.op("pe", mmy, reads=rd, writes=[self.psb[bank]])
                    if j % 2 == 1:
                        j0 = j - 1
                        y_t, y_b = ys[(j // 2) % 2]
                        base = LC + 8 * c0
                        uv = u_t[:, base:base + 2048].rearrange("p (c j) -> p j c", j=8)[:, j0:j0 + 2, :]
                        S.op("dve", lambda e, y_t=y_t, uv=uv, bank=bank, fc=fc: e.scalar_tensor_tensor(
                            out=y_t, in0=uv, scalar=dcol[:, fc:fc + 1], in1=ps[bank][:].rearrange("p (j c) -> p j c", j=2),
                            op0=ALU.mult, op1=ALU.add), reads=[u_b, dcol_b, self.psb[bank]], writes=[y_b])
                        outv = g_t.rearrange("p (c8 j row) -> p j row c8", c8=8, j=8)[:, j0:j0 + 2, 32 * hb:32 * hb + 32, :]
                        S.op("act", lambda e, y_t=y_t, outv=outv: e.activation(
                            out=outv, in_=y_t.rearrange("p j (r c8) -> p j r c8", c8=8), func=AF.Gelu_apprx_tanh),
                            reads=[y_b], writes=[g_b])
            S.op("sp", lambda e, fc=fc, g_t=g_t: e.dma_start(out=self.g_s[fc * 128:(fc + 1) * 128, :], in_=g_t), reads=[g_b], writes=[self.g_sb], dma=True)
        S.barrier()
        ar.release()


    def phase4(self, glu_w, glu_b, w_a, w_b, w_o, x_tok, nfw, r_w, r_b):
        nc, S, ar = self.nc, self.S, self.ar
        ps, psb = self.ps, self.psb
        dbg = self.debug
        ident_f, ident_f_b = self.consts["ident_f"]
        self.h1_s, self.h1_sb = self.scratch("h1_s", [L, D], F32, dump=dbg)
        self.uf_s, self.uf_sb = self.scratch("uf_s", [L, D], BF16, dump=dbg)
        self.logits, self.logits_b = ar.alloc([32, 32], F32, "logits")
        ar.mark()
        wv = lambda w: w.rearrange("(kc p) n -> p kc n", p=128)
        Wg, Wg_b = ar.alloc([8, D], BF16, "Wglu")
        Wa, Wa_b = ar.alloc([8, D], BF16, "Wa")
        Wb, Wb_b = ar.alloc([16, D], BF16, "Wb")
        Wo, Wo_b = ar.alloc([8, D], BF16, "Wo")
        for (t_, tb_, src, nk) in ((Wg, Wg_b, glu_w, 8), (Wa, Wa_b, w_a, 8), (Wb, Wb_b, w_b, 16), (Wo, Wo_b, w_o, 8)):
            for k0 in range(0, nk, 4):
                S.op("pool", lambda e, t_=t_, src=src, k0=k0: e.dma_start(out=t_[:, k0:k0 + 4, :], in_=wv(src)[:, k0:k0 + 4, :]), writes=[tb_], dma=True)
        Wr, Wr_b = ar.alloc([8, 32], F32, "Wr")
        S.op("sp", lambda e: e.dma_start(out=Wr, in_=r_w.rearrange("(kc p) n -> p kc n", p=128)), writes=[Wr_b], dma=True)
        rb_rep, rb_rep_b = ar.alloc([32], F32, "rb_rep")
        S.op("sp", lambda e: e.dma_start(out=rb_rep, in_=r_b.broadcast_to([128, 32])), writes=[rb_rep_b], dma=True)
        gb_col, gb_col_b = ar.alloc([8], F32, "glu_b")
        S.op("sp", lambda e: e.dma_start(out=gb_col, in_=glu_b), writes=[gb_col_b], dma=True)
        gm_rep, gm_rep_b = ar.alloc([D], F32, "gm_rep")
        Af_rep, Af_rep_b = ar.alloc([D], F32, "Af_rep")
        Bf_rep, Bf_rep_b = ar.alloc([D], F32, "Bf_rep")
        S.op("sp", lambda e: e.dma_start(out=gm_rep, in_=self.mod_s[:, 2 * D:3 * D]), reads=[self.mod_sb], writes=[gm_rep_b], dma=True)
        S.op("sp", lambda e: e.dma_start(out=Bf_rep, in_=self.mod_s[:, 3 * D:4 * D]), reads=[self.mod_sb], writes=[Bf_rep_b], dma=True)
        S.op("sp", lambda e: e.dma_start(out=Af_rep, in_=self.mod_s[:, 4 * D:5 * D]), reads=[self.mod_sb], writes=[Af_rep_b], dma=True)
        nf_rep, nf_rep_b = ar.alloc([D], F32, "nf_rep")
        S.op("sp", lambda e: e.dma_start(out=nf_rep, in_=nfw.broadcast_to([128, D])), writes=[nf_rep_b], dma=True)
        S.op("dve", lambda e: e.scalar_tensor_tensor(out=Af_rep, in0=Af_rep, scalar=1.0, in1=nf_rep, op0=ALU.add, op1=ALU.mult),
             reads=[Af_rep_b, nf_rep_b], writes=[Af_rep_b])
        g_t, g_b = ar.alloc([8, 512], BF16, "m_g")
        yb_t, yb_b = ar.alloc([16, 512], BF16, "m_yb")
        gab_t, gab_b = ar.alloc([16, 512], BF16, "m_gab")
        ya_t, ya_b = ar.alloc([8, 512], BF16, "m_ya")
        m1_t, m1_b = ar.alloc([8, 512], F32, "m_m1")
        mg_t, mg_b = ar.alloc([8, 512], BF16, "m_mg")
        sg = [ar.alloc([512], F32, f"m_sg{i}") for i in range(2)]
        xk = [ar.alloc([D], F32, f"m_xk{i}") for i in range(1)]
        h1 = [ar.alloc([D], F32, f"m_h1{i}") for i in range(2)]
        uft = [ar.alloc([D], F32, f"m_uft{i}") for i in range(1)]
        ufb = [ar.alloc([D], BF16, f"m_ufb{i}") for i in range(2)]
        ufT = [ar.alloc([8, 128], F32, f"m_ufT{i}") for i in range(1)]
        sq_junk, sq_junk_b = ar.alloc([D], BF16, "m_sqj")
        ss = [ar.alloc([1], F32, f"m_ss{i}") for i in range(2)]
        pc = [0]

        def bank():
            b = pc[0] % 8
            pc[0] += 1
            return b
        for tt in range(8):
            cs = slice(tt * 512, (tt + 1) * 512)
            S.op("sp", lambda e, cs=cs: e.dma_start(out=g_t, in_=self.g_s.rearrange("(kc p) n -> p kc n", p=128)[:, :, cs]), reads=[self.g_sb], writes=[g_b], dma=True)
            S.op("sp", lambda e, cs=cs: e.dma_start(out=yb_t, in_=self.yb_s.rearrange("(kc p) n -> p kc n", p=128)[:, :, cs]), reads=[self.yb_sb], writes=[yb_b], dma=True)
            S.op("sp", lambda e, cs=cs: e.dma_start(out=gab_t, in_=self.gab_s.rearrange("(kc p) n -> p kc n", p=128)[:, :, cs]), reads=[self.gab_sb], writes=[gab_b], dma=True)
            for mc in range(8):
                b_ = bank()

                def mm(e, b_=b_, mc=mc):
                    ins = None
                    for kc in range(8):
                        ins = e.matmul(ps[b_][:], lhsT=Wg[:, kc, mc * 128:(mc + 1) * 128], rhs=g_t[:, kc, :], start=(kc == 0), stop=(kc == 7))
                    return ins
                S.op("pe", mm, reads=[Wg_b, g_b], writes=[psb[b_]])
                s_t, s_b = sg[mc % 2]
                S.op("act", lambda e, s_t=s_t, b_=b_, mc=mc: e.activation(out=s_t, in_=ps[b_][:], func=AF.Sigmoid, bias=gb_col[:, mc:mc + 1]),
                     reads=[psb[b_], gb_col_b], writes=[s_b])
                S.op("dve", lambda e, s_t=s_t, mc=mc: e.tensor_tensor(out=ya_t[:, mc, :], in0=s_t, in1=g_t[:, mc, :], op=ALU.mult),
                     reads=[s_b, g_b], writes=[ya_b])
            for mc in range(8):
                b_ = bank()

                def mm(e, b_=b_, mc=mc):
                    ins = None
                    for kc in range(8):
                        ins = e.matmul(ps[b_][:], lhsT=Wa[:, kc, mc * 128:(mc + 1) * 128], rhs=ya_t[:, kc, :], start=(kc == 0), stop=(kc == 7))
                    return ins
                S.op("pe", mm, reads=[Wa_b, ya_b], writes=[psb[b_]])
                s_t, s_b = sg[mc % 2]
                S.op("act", lambda e, s_t=s_t, mc=mc: e.activation(out=s_t, in_=gab_t[:, mc, :], func=AF.Sigmoid), reads=[gab_b], writes=[s_b])
                S.op("dve", lambda e, s_t=s_t, b_=b_, mc=mc: e.tensor_tensor(out=m1_t[:, mc, :], in0=s_t, in1=ps[b_][:], op=ALU.mult),
                     reads=[s_b, psb[b_]], writes=[m1_b])
            for mc in range(8):
                b_ = bank()

                def mm(e, b_=b_, mc=mc):
                    ins = None
                    for kc in range(16):
                        ins = e.matmul(ps[b_][:], lhsT=Wb[:, kc, mc * 128:(mc + 1) * 128], rhs=yb_t[:, kc, :], start=(kc == 0), stop=(kc == 15))
                    return ins
                S.op("pe", mm, reads=[Wb_b, yb_b], writes=[psb[b_]])
                s_t, s_b = sg[mc % 2]
                S.op("act", lambda e, s_t=s_t, mc=mc: e.activation(out=s_t, in_=gab_t[:, 8 + mc, :], func=AF.Sigmoid), reads=[gab_b], writes=[s_b])
                S.op("dve", lambda e, s_t=s_t, b_=b_: e.tensor_tensor(out=s_t, in0=s_t, in1=ps[b_][:], op=ALU.mult), reads=[s_b, psb[b_]], writes=[s_b])
                S.op("dve", lambda e, s_t=s_t, mc=mc: e.tensor_tensor(out=mg_t[:, mc, :], in0=s_t, in1=m1_t[:, mc, :], op=ALU.add),
                     reads=[s_b, m1_b], writes=[mg_b])
            for sub in range(4):
                ti = tt * 4 + sub
                x_t, x_b = xk[0]
                h_t, h_b = h1[ti % 2]
                S.op("sp", lambda e, x_t=x_t, ti=ti: e.dma_start(out=x_t, in_=x_tok[ti * 128:(ti + 1) * 128, :]), writes=[x_b], dma=True)
                for half in range(2):
                    b_ = bank()

                    def mm(e, b_=b_, sub=sub, half=half):
                        ins = None
                        for kc in range(8):
                            ins = e.matmul(ps[b_][:], lhsT=mg_t[:, kc, sub * 128:(sub + 1) * 128], rhs=Wo[:, kc, half * 512:(half + 1) * 512],
                                           start=(kc == 0), stop=(kc == 7))
                        return ins
                    S.op("pe", mm, reads=[mg_b, Wo_b], writes=[psb[b_]])
                    hs = slice(half * 512, (half + 1) * 512)
                    S.op("dve", lambda e, h_t=h_t, b_=b_, hs=hs: e.tensor_tensor(out=h_t[:, hs], in0=ps[b_][:], in1=gm_rep[:, hs], op=ALU.mult),
                         reads=[psb[b_], gm_rep_b], writes=[h_b])
                    S.op("dve", lambda e, h_t=h_t, x_t=x_t, hs=hs: e.tensor_tensor(out=h_t[:, hs], in0=h_t[:, hs], in1=x_t[:, hs], op=ALU.add),
                         reads=[h_b, x_b], writes=[h_b])
                S.op("sp", lambda e, h_t=h_t, ti=ti: e.dma_start(out=self.h1_s[ti * 128:(ti + 1) * 128, :], in_=h_t), reads=[h_b], writes=[self.h1_sb], dma=True)
                s_t, s_b = ss[ti % 2]
                S.op("act", lambda e, h_t=h_t, s_t=s_t: e.activation(out=sq_junk, in_=h_t, func=AF.Square, accum_out=s_t), reads=[h_b], writes=[sq_junk_b, s_b])
                S.op("act", lambda e, s_t=s_t: e.activation(out=s_t, in_=s_t, func=AF.Sqrt, scale=1.0 / D, bias=EPS), reads=[s_b], writes=[s_b])
                S.op("dve", lambda e, s_t=s_t: e.reciprocal(out=s_t, in_=s_t), reads=[s_b], writes=[s_b])
                u_t, u_b = uft[0]
                ub_t, ub_b = ufb[ti % 2]
                S.op("dve", lambda e, u_t=u_t, h_t=h_t, s_t=s_t: e.scalar_tensor_tensor(out=u_t, in0=h_t, scalar=s_t[:, 0:1], in1=Af_rep, op0=ALU.mult, op1=ALU.mult),
                     reads=[h_b, s_b, Af_rep_b], writes=[u_b])
                S.op("dve", lambda e, u_t=u_t: e.tensor_tensor(out=u_t, in0=u_t, in1=Bf_rep, op=ALU.add), reads=[u_b, Bf_rep_b], writes=[u_b])
                S.op("act", lambda e, u_t=u_t, ub_t=ub_t: e.activation(out=ub_t, in_=u_t, func=AF.Copy), reads=[u_b], writes=[ub_b])
                S.op("sp", lambda e, ub_t=ub_t, ti=ti: e.dma_start(out=self.uf_s[ti * 128:(ti + 1) * 128, :], in_=ub_t), reads=[ub_b], writes=[self.uf_sb], dma=True)
                T_t, T_b = ufT[0]
                for half in range(2):
                    b_ = bank()

                    def tr(e, b_=b_, u_t=u_t, half=half):
                        ins = None
                        for q in range(4):
                            kc = half * 4 + q
                            ins = e.transpose(out=ps[b_][:, q * 128:(q + 1) * 128], in_=u_t[:, kc * 128:(kc + 1) * 128], identity=ident_f)
                        return ins
                    S.op("pe", tr, reads=[u_b, ident_f_b], writes=[psb[b_]])
                    S.op("act", lambda e, T_t=T_t, b_=b_, half=half: e.activation(
                        out=T_t[:, half * 4:half * 4 + 4, :].rearrange("p a b -> p (a b)"), in_=ps[b_][:], func=AF.Copy), reads=[psb[b_]], writes=[T_b])
                b_ = bank()

                def mml(e, b_=b_, T_t=T_t):
                    ins = None
                    for kc in range(8):
                        ins = e.matmul(ps[b_][:, 0:32], lhsT=T_t[:, kc, :], rhs=Wr[:, kc, :], start=(kc == 0), stop=(kc == 7))
                    return ins
                S.op("pe", mml, reads=[T_b, Wr_b], writes=[psb[b_]])
                S.op("dve", lambda e, b_=b_, ti=ti: e.tensor_tensor(out=self.logits[:, ti, :], in0=ps[b_][:, 0:32], in1=rb_rep, op=ALU.add),
                     reads=[psb[b_], rb_rep_b], writes=[self.logits_b])
        if dbg:
            o, ob = self.scratch("dbg_logits", [128, 1024], F32, dump=True)
            S.op("sp", lambda e: e.dma_start(out=o, in_=self.logits.rearrange("p a b -> p (a b)")), reads=[self.logits_b], writes=[ob], dma=True)
        S.barrier()
        ar.release()


    def phase5(self, wg_d, wu_d, wd_d, bg_d, bu_d, bd_d, fnw):
        nc, S, ar = self.nc, self.S, self.ar
        ps, psb = self.ps, self.psb
        dbg = self.debug
        ident_f, ident_f_b = self.consts["ident_f"]
        ones_f, ones_f_b = self.consts["ones_f"]
        ones_bf, ones_bf_b = self.consts["ones_bf"]
        BLK = int(os.environ.get('MOE_BLK', '256'))
        NSUB = BLK // 128
        NT, NE = 32, 32
        NB = -(-(16384 + 32 * (BLK - 1)) // BLK)
        NSLOT = NB * BLK
        self.xs, self.xs_b = self.scratch("xs", [NSLOT, D], BF16)
        self.ys, self.ys_b = self.scratch("ys", [NSLOT, D], BF16)
        logits, logits_b = self.logits, self.logits_b
        ar.mark()
        dest_i, dest_ib = ar.alloc([NT, 4], I32, "dest_i")
        gate4, gate4_b = ar.alloc([NT, 4], F32, "gate4")
        blk_i, blk_ib = ar.alloc([NB], I32, "blk_i")
        chg_i, chg_ib = ar.alloc([NB], I32, "chg_i")
        widx, widx_b = ar.alloc([NB], I32, "widx")
        bidx, bidx_b = ar.alloc([NB], I32, "bidx")
        ident_bf, ident_bf_b = ar.alloc([128], BF16, "ident_bf5")
        S.op("dve", lambda e: e.tensor_copy(out=ident_bf, in_=ident_f), reads=[ident_f_b], writes=[ident_bf_b])
        ar.mark()
        top8, top8_b = ar.alloc([NT, 8], F32, "top8")
        mask, mask_b = ar.alloc([NT, NE], F32, "mask")
        mask_bf, mask_bfb = ar.alloc([NT, NE], BF16, "mask_bf")
        gatef, gatef_b = ar.alloc([NT, NE], F32, "gatef")
        pos, pos_b = ar.alloc([NT, NE], F32, "pos")
        cnta, cnta_b = ar.alloc([NT, NE], F32, "cnta")
        base, base_b = ar.alloc([NT, NE], F32, "base")
        rsum, rsum_b = ar.alloc([NT], F32, "rsum")
        stri, stri_b = ar.alloc([128], BF16, "stri")
        S.op("pool", lambda e: e.affine_select(out=stri, in_=ones_f, pattern=[[1, 128]], compare_op=ALU.is_ge, fill=0.0,
                                              base=-1, channel_multiplier=-1), reads=[ones_f_b], writes=[stri_b])
        for i in range(NT):
            S.op("dve", lambda e, i=i: e.max(out=top8[:, i, :], in_=logits[:, i, :]), reads=[logits_b], writes=[top8_b])
            S.op("dve", lambda e, i=i: e.tensor_scalar(out=mask[:, i, :], in0=logits[:, i, :], scalar1=top8[:, i, 3:4], scalar2=None, op0=ALU.is_ge),
                 reads=[logits_b, top8_b], writes=[mask_b])
        S.op("act", lambda e: e.activation(out=mask_bf, in_=mask, func=AF.Copy), reads=[mask_b], writes=[mask_bfb])
        S.op("dve", lambda e: e.tensor_tensor(out=gatef, in0=logits, in1=top8[:, :, 0:1].broadcast_to([128, NT, NE]), op=ALU.subtract),
             reads=[logits_b, top8_b], writes=[gatef_b])
        S.op("act", lambda e: e.activation(out=gatef, in_=gatef, func=AF.Exp), reads=[gatef_b], writes=[gatef_b])
        S.op("dve", lambda e: e.tensor_tensor(out=gatef, in0=gatef, in1=mask, op=ALU.mult), reads=[gatef_b, mask_b], writes=[gatef_b])
        S.op("dve", lambda e: e.reduce_sum(out=rsum, in_=gatef, axis=AX.X), reads=[gatef_b], writes=[rsum_b])
        S.op("dve", lambda e: e.reciprocal(out=rsum, in_=rsum), reads=[rsum_b], writes=[rsum_b])
        S.op("dve", lambda e: e.tensor_tensor(out=gatef, in0=gatef, in1=rsum.unsqueeze(2).broadcast_to([128, NT, NE]), op=ALU.mult),
             reads=[gatef_b, rsum_b], writes=[gatef_b])
        mflat = mask_bf.rearrange("p a b -> p (a b)")
        for half in range(2):
            b_ = half

            def mm(e, b_=b_, half=half):
                return e.matmul(ps[b_][:], lhsT=ones_bf, rhs=mflat[:, half * 512:(half + 1) * 512], start=True, stop=True)
            S.op("pe", mm, reads=[mask_bfb, ones_bf_b], writes=[psb[b_]])
            S.op("dve", lambda e, b_=b_, half=half: e.tensor_copy(out=cnta.rearrange("p a b -> p (a b)")[:, half * 512:(half + 1) * 512], in_=ps[b_][:]),
                 reads=[psb[b_]], writes=[cnta_b])
            b2 = 2 + half

            def mm2(e, b2=b2, half=half):
                return e.matmul(ps[b2][:], lhsT=stri, rhs=mflat[:, half * 512:(half + 1) * 512], start=True, stop=True)
            S.op("pe", mm2, reads=[mask_bfb, stri_b], writes=[psb[b2]])
            S.op("dve", lambda e, b2=b2, half=half: e.tensor_copy(out=pos.rearrange("p a b -> p (a b)")[:, half * 512:(half + 1) * 512], in_=ps[b2][:]),
                 reads=[psb[b2]], writes=[pos_b])
        for ee in range(NE):
            S.op("dve", lambda e, ee=ee: e.tensor_tensor_scan(out=base[:, :, ee], data0=ones_f[:, 0:NT], data1=cnta[:, :, ee], initial=0.0,
                                                             op0=ALU.mult, op1=ALU.add), reads=[cnta_b, ones_f_b], writes=[base_b])
        tot, tot_b = ar.alloc([NE], F32, "tot")
        S.op("dve", lambda e: e.tensor_copy(out=tot, in_=base[:, NT - 1, :]), reads=[base_b], writes=[tot_b])
        S.op("dve", lambda e: e.tensor_tensor(out=base, in0=base, in1=cnta, op=ALU.subtract), reads=[base_b, cnta_b], writes=[base_b])
        S.op("dve", lambda e: e.tensor_tensor(out=pos, in0=pos, in1=base, op=ALU.add), reads=[pos_b, base_b], writes=[pos_b])
        nb_f, nb_fb = ar.alloc([NE], F32, "nb_f")
        nb_i, nb_ib = ar.alloc([NE], I32, "nb_i")
        pend, pend_b = ar.alloc([NE], F32, "pend")
        pstart, pstart_b = ar.alloc([NE], F32, "pstart")
        S.op("dve", lambda e: e.tensor_scalar(out=nb_f, in0=tot, scalar1=1.0 / BLK, scalar2=(BLK - 1.0) / BLK - 0.5 + 0.25 / BLK, op0=ALU.mult, op1=ALU.add),
             reads=[tot_b], writes=[nb_fb])
        S.op("dve", lambda e: e.tensor_copy(out=nb_i, in_=nb_f), reads=[nb_fb], writes=[nb_ib])
        S.op("dve", lambda e: e.tensor_copy(out=nb_f, in_=nb_i), reads=[nb_ib], writes=[nb_fb])
        S.op("dve", lambda e: e.tensor_scalar(out=nb_f, in0=nb_f, scalar1=float(BLK), scalar2=None, op0=ALU.mult), reads=[nb_fb], writes=[nb_fb])
        S.op("dve", lambda e: e.tensor_tensor_scan(out=pend, data0=ones_f[:, 0:NE], data1=nb_f, initial=0.0, op0=ALU.mult, op1=ALU.add),
             reads=[nb_fb, ones_f_b], writes=[pend_b])
        S.op("dve", lambda e: e.tensor_tensor(out=pstart, in0=pend, in1=nb_f, op=ALU.subtract), reads=[pend_b, nb_fb], writes=[pstart_b])
        S.op("dve", lambda e: e.tensor_tensor(out=pos, in0=pos, in1=pstart.unsqueeze(1).broadcast_to([128, NT, NE]), op=ALU.add),
             reads=[pos_b, pstart_b], writes=[pos_b])
        dest4, dest4_b = ar.alloc([NT, 4], F32, "dest4")
        junk, junk_b = ar.alloc([NE], F32, "junk")
        for i in range(NT):
            for k in range(4):
                S.op("dve", lambda e, i=i, k=k: e.scalar_tensor_tensor(out=junk, in0=logits[:, i, :], scalar=top8[:, i, k:k + 1], in1=pos[:, i, :],
                                                                     op0=ALU.is_equal, op1=ALU.mult, accum_out=dest4[:, i, k:k + 1]),
                     reads=[logits_b, top8_b, pos_b], writes=[junk_b, dest4_b])
                S.op("dve", lambda e, i=i, k=k: e.scalar_tensor_tensor(out=junk, in0=logits[:, i, :], scalar=top8[:, i, k:k + 1], in1=gatef[:, i, :],
                                                                     op0=ALU.is_equal, op1=ALU.mult, accum_out=gate4[:, i, k:k + 1]),
                     reads=[logits_b, top8_b, gatef_b], writes=[junk_b, gate4_b])
        S.op("dve", lambda e: e.tensor_copy(out=dest_i, in_=dest4), reads=[dest4_b], writes=[dest_ib])
        bv_i, bv_ib = ar.alloc([NB], I32, "bv_i")
        bv, bv_b = ar.alloc([NB], F32, "bv")
        cmp_, cmp_b = ar.alloc([NB, NE], F32, "cmp")
        blk_f, blk_fb = ar.alloc([NB], F32, "blk_f")
        chg_f, chg_fb = ar.alloc([NB], F32, "chg_f")
        S.op("pool", lambda e: e.iota(bv_i, pattern=[[BLK, NB]], base=0, channel_multiplier=0), writes=[bv_ib])
        S.op("dve", lambda e: e.tensor_copy(out=bv, in_=bv_i), reads=[bv_ib], writes=[bv_b])
        S.op("dve", lambda e: e.tensor_tensor(out=cmp_, in0=pend.unsqueeze(1).broadcast_to([128, NB, NE]),
                                              in1=bv.unsqueeze(2).broadcast_to([128, NB, NE]), op=ALU.is_le), reads=[pend_b, bv_b], writes=[cmp_b])
        S.op("dve", lambda e: e.reduce_sum(out=blk_f, in_=cmp_, axis=AX.X), reads=[cmp_b], writes=[blk_fb])
        S.op("dve", lambda e: e.tensor_scalar(out=blk_f, in0=blk_f, scalar1=float(NE - 1), scalar2=None, op0=ALU.min), reads=[blk_fb], writes=[blk_fb])
        S.op("dve", lambda e: e.memset(chg_f[:, 0:1], 1.0), writes=[chg_fb])
        S.op("dve", lambda e: e.tensor_tensor(out=chg_f[:, 1:NB], in0=blk_f[:, 1:NB], in1=blk_f[:, 0:NB - 1], op=ALU.not_equal), reads=[blk_fb], writes=[chg_fb])
        S.op("dve", lambda e: e.tensor_copy(out=blk_i, in_=blk_f), reads=[blk_fb], writes=[blk_ib])
        S.op("dve", lambda e: e.tensor_copy(out=chg_i, in_=chg_f), reads=[chg_fb], writes=[chg_ib])
        pio_i, pio_ib = ar.alloc([1], I32, "pio_i")
        pio, pio_b = ar.alloc([1], F32, "pio")
        S.op("pool", lambda e: e.iota(pio_i, pattern=[[0, 1]], base=0, channel_multiplier=1), writes=[pio_ib])
        S.op("dve", lambda e: e.tensor_copy(out=pio, in_=pio_i), reads=[pio_ib], writes=[pio_b])
        nchg, nchg_b = ar.alloc([NB], F32, "nchg")
        S.op("dve", lambda e: e.tensor_scalar(out=nchg, in0=chg_f, scalar1=-1.0e7, scalar2=1.0e7, op0=ALU.mult, op1=ALU.add), reads=[chg_fb], writes=[nchg_b])
        wb_f, wb_fb = ar.alloc([NB], F32, "wb_f")
        S.op("dve", lambda e: e.tensor_scalar(out=wb_f, in0=blk_f, scalar1=128.0, scalar2=pio[:, 0:1], op0=ALU.mult, op1=ALU.add), reads=[blk_fb, pio_b], writes=[wb_fb])
        S.op("dve", lambda e: e.tensor_tensor(out=wb_f, in0=wb_f, in1=chg_f, op=ALU.mult), reads=[wb_fb, chg_fb], writes=[wb_fb])
        S.op("dve", lambda e: e.tensor_tensor(out=wb_f, in0=wb_f, in1=nchg, op=ALU.add), reads=[wb_fb, nchg_b], writes=[wb_fb])
        S.op("dve", lambda e: e.tensor_copy(out=widx, in_=wb_f), reads=[wb_fb], writes=[widx_b])
        bi_f, bi_fb = ar.alloc([NB], F32, "bi_f")
        S.op("dve", lambda e: e.tensor_tensor(out=bi_f, in0=blk_f, in1=chg_f, op=ALU.mult), reads=[blk_fb, chg_fb], writes=[bi_fb])
        S.op("dve", lambda e: e.tensor_tensor(out=bi_f, in0=bi_f, in1=nchg, op=ALU.add), reads=[bi_fb, nchg_b], writes=[bi_fb])
        S.op("dve", lambda e: e.tensor_copy(out=bidx, in_=bi_f), reads=[bi_fb], writes=[bidx_b])
        if dbg:
            for nm, t_, tb_, n, dtp in (("dbg_dest", dest_i.rearrange("p a b -> p (a b)"), dest_ib, NT * 4, I32),
                                        ("dbg_gate4", gate4.rearrange("p a b -> p (a b)"), gate4_b, NT * 4, F32),
                                        ("dbg_blk", blk_i, blk_ib, NB, I32), ("dbg_chg", chg_i, chg_ib, NB, I32)):
                o, ob = self.scratch(nm, [128, n], dtp, dump=True)
                S.op("sp", lambda e, o=o, t_=t_: e.dma_start(out=o, in_=t_), reads=[tb_], writes=[ob], dma=True)
        S.barrier()
        ar.release()
        ar.mark()
        zt, zt_b = ar.alloc([4, D], BF16, "zt")
        S.op("dve", lambda e: e.memset(zt, 0.0), writes=[zt_b])
        xsv = self.xs.rearrange("(b p) n -> p b n", p=128)
        for q in range(NSLOT // 512):
            S.op("sp", lambda e, q=q: e.dma_start(out=xsv[:, 4 * q:4 * q + 4, :], in_=zt), reads=[zt_b], writes=[self.xs_b], dma=True)
        uft = [ar.alloc([D], BF16, f"sc_u{i}") for i in range(2)]
        for i in range(NT):
            u_t, u_b = uft[i % 2]
            S.op("sp", lambda e, u_t=u_t, i=i: e.dma_start(out=u_t, in_=self.uf_s[i * 128:(i + 1) * 128, :]), reads=[self.uf_sb], writes=[u_b], dma=True)
            for k in range(4):
                S.op("pool", lambda e, u_t=u_t, i=i, k=k: e.indirect_dma_start(
                    out=self.xs, out_offset=bass.IndirectOffsetOnAxis(ap=dest_i[:, i, k:k + 1].bitcast(U32), axis=0), in_=u_t, in_offset=None),
                    reads=[u_b, dest_ib], writes=[self.xs_b], dma=True)
        S.barrier()
        ar.release()
        ar.mark()
        Wt = [ar.alloc([8, D], BF16, f"moe_w{m}") for m in range(3)]
        brow, brow_b = ar.alloc([3, D], BF16, "moe_brow")
        WS = [ar.alloc([8, D], F32, f"moe_ws{m}") for m in range(3)]
        browS, browS_b = ar.alloc([3, D], BF16, "moe_browS")
        wds = [wg_d, wu_d, wd_d]
        bds = [bg_d, bu_d, bd_d]
        xb = [ar.alloc([D], BF16, f"moe_xb{i}") for i in range(2)]
        xT = [ar.alloc([8, 128], BF16, f"moe_xT{i}") for i in range(2)]
        gc = [ar.alloc([512], F32, f"moe_gc{i}") for i in range(2)]
        uc = [ar.alloc([512], F32, f"moe_uc{i}") for i in range(2)]
        sgm = [ar.alloc([512], F32, f"moe_sg{i}") for i in range(2)]
        hb_ = [ar.alloc([D], BF16, f"moe_h{i}") for i in range(2)]
        hT = [ar.alloc([8, 128], BF16, f"moe_hT{i}") for i in range(2)]
        yo = [ar.alloc([D], BF16, f"moe_yo{i}") for i in range(2)]
        NBLK = int(os.environ.get("MOE_NB", str(NB)))
        regs = {}

        def breg(e, val):
            if val not in regs:
                regs[val] = e.to_reg(val)
            return regs[val]
        for b in range(NBLK):
            for m in range(3):
                S.op("pool", lambda e, m=m, b=b: e.indirect_dma_start(
                    out=WS[m][0].rearrange("p a b -> p (a b)"), out_offset=None, in_=wds[m],
                    in_offset=bass.IndirectOffsetOnAxis(ap=widx[:, b:b + 1].bitcast(U32), axis=0),
                    bounds_check=breg(e, NE * 128 - 1), oob_is_err=False),
                    reads=[widx_b], writes=[WS[m][1]], dma=True)
            for m in range(3):
                S.op("pool", lambda e, m=m, b=b: e.indirect_dma_start(
                    out=browS[:, m, :], out_offset=None, in_=bds[m],
                    in_offset=bass.IndirectOffsetOnAxis(ap=bidx[:, b:b + 1].bitcast(U32), axis=0),
                    bounds_check=breg(e, NE - 1), oob_is_err=False),
                    reads=[bidx_b], writes=[browS_b], dma=True)
            for m in range(3):
                eng = ("dve", "dve", "act")[m]
                if eng == "act":
                    S.op("act", lambda e, m=m: e.activation(out=Wt[m][0], in_=WS[m][0], func=AF.Copy), reads=[WS[m][1]], writes=[Wt[m][1]])
                else:
                    S.op(eng, lambda e, m=m: e.tensor_copy(out=Wt[m][0], in_=WS[m][0]), reads=[WS[m][1]], writes=[Wt[m][1]])
            S.op("act", lambda e: e.activation(out=brow[0:1], in_=browS[0:1], func=AF.Copy), reads=[browS_b], writes=[brow_b])
            subs = [b * NSUB + s_ for s_ in range(NSUB)]

            def stageA(sb):
                x_t, x_b = xb[sb % 2]
                S.op("sp", lambda e, x_t=x_t, sb=sb: e.dma_start(out=x_t, in_=self.xs[sb * 128:(sb + 1) * 128, :]), reads=[self.xs_b], writes=[x_b], dma=True)
                xT_t, xT_b = xT[sb % 2]
                pT = ps[0][:].bitcast(BF16)

                def trx(e, x_t=x_t, pT=pT):
                    ins = None
                    for kc in range(8):
                        ins = e.transpose(out=pT[:, kc * 128:(kc + 1) * 128], in_=x_t[:, kc * 128:(kc + 1) * 128], identity=ident_bf)
                    return ins
                S.op("pe", trx, reads=[x_b, ident_bf_b], writes=[psb[0]])
                S.op("act", lambda e, xT_t=xT_t, pT=pT: e.activation(out=xT_t.rearrange("p a b -> p (a b)"), in_=pT, func=AF.Copy), reads=[psb[0]], writes=[xT_b])
                h_t, h_b = hb_[sb % 2]
                for half in range(2):
                    hs = slice(half * 512, (half + 1) * 512)
                    for m, bk in ((0, 1 + half), (1, 3 + half)):
                        def mm(e, m=m, bk=bk, hs=hs, xT_t=xT_t):
                            e.matmul(ps[bk][:], lhsT=ones_bf[0:1, 0:128], rhs=brow[0:1, m, hs], start=True, stop=False)
                            ins = None
                            for kc in range(8):
                                ins = e.matmul(ps[bk][:], lhsT=xT_t[:, kc, :], rhs=Wt[m][0][:, kc, hs], start=False, stop=(kc == 7))
                            return ins
                        S.op("pe", mm, reads=[xT_b, Wt[m][1], brow_b, ones_bf_b], writes=[psb[bk]])
                    g_t, g_b = gc[half]
                    u_t, u_b = uc[half]
                    s_t, s_b = sgm[half]
                    S.op("dve", lambda e, g_t=g_t, half=half: e.tensor_scalar(out=g_t, in0=ps[1 + half][:], scalar1=7.0, scalar2=None, op0=ALU.min),
                         reads=[psb[1 + half]], writes=[g_b])
                    S.op("dve", lambda e, u_t=u_t, half=half: e.tensor_scalar(out=u_t, in0=ps[3 + half][:], scalar1=7.0, scalar2=-7.0, op0=ALU.min, op1=ALU.max),
                         reads=[psb[3 + half]], writes=[u_b])
                    S.op("act", lambda e, s_t=s_t, g_t=g_t: e.activation(out=s_t, in_=g_t, func=AF.Sigmoid, scale=1.702), reads=[g_b], writes=[s_b])
                    S.op("dve", lambda e, u_t=u_t, g_t=g_t: e.scalar_tensor_tensor(out=u_t, in0=u_t, scalar=1.0, in1=g_t, op0=ALU.add, op1=ALU.mult),
                         reads=[u_b, g_b], writes=[u_b])
                    S.op("dve", lambda e, h_t=h_t, u_t=u_t, s_t=s_t, hs=hs: e.tensor_tensor(out=h_t[:, hs], in0=u_t, in1=s_t, op=ALU.mult),
                         reads=[u_b, s_b], writes=[h_b])

            def stageB(sb):
                h_t, h_b = hb_[sb % 2]
                hT_t, hT_b = hT[sb % 2]
                pT5 = ps[5][:].bitcast(BF16)

                def trh(e, h_t=h_t, pT5=pT5):
                    ins = None
                    for kc in range(8):
                        ins = e.transpose(out=pT5[:, kc * 128:(kc + 1) * 128], in_=h_t[:, kc * 128:(kc + 1) * 128], identity=ident_bf)
                    return ins
                S.op("pe", trh, reads=[h_b, ident_bf_b], writes=[psb[5]])
                S.op("act", lambda e, hT_t=hT_t, pT5=pT5: e.activation(out=hT_t.rearrange("p a b -> p (a b)"), in_=pT5, func=AF.Copy), reads=[psb[5]], writes=[hT_b])
                y_t, y_b = yo[sb % 2]
                for half in range(2):
                    hs = slice(half * 512, (half + 1) * 512)
                    bk = 6 + half

                    def mmd(e, bk=bk, hs=hs, hT_t=hT_t):
                        e.matmul(ps[bk][:], lhsT=ones_bf[0:1, 0:128], rhs=brow[0:1, 2, hs], start=True, stop=False)
                        ins = None
                        for kc in range(8):
                            ins = e.matmul(ps[bk][:], lhsT=hT_t[:, kc, :], rhs=Wt[2][0][:, kc, hs], start=False, stop=(kc == 7))
                        return ins
                    S.op("pe", mmd, reads=[hT_b, Wt[2][1], brow_b, ones_bf_b], writes=[psb[bk]])
                    if half == 0:
                        S.op("dve", lambda e, y_t=y_t, bk=bk, hs=hs: e.tensor_copy(out=y_t[:, hs], in_=ps[bk][:]), reads=[psb[bk]], writes=[y_b])
                    else:
                        S.op("act", lambda e, y_t=y_t, bk=bk, hs=hs: e.activation(out=y_t[:, hs], in_=ps[bk][:], func=AF.Copy), reads=[psb[bk]], writes=[y_b])
                S.op("sp", lambda e, y_t=y_t, sb=sb: e.dma_start(out=self.ys[sb * 128:(sb + 1) * 128, :], in_=y_t), reads=[y_b], writes=[self.ys_b], dma=True)

            stageA(subs[0])
            for i_ in range(1, NSUB):
                stageA(subs[i_])
                stageB(subs[i_ - 1])
            stageB(subs[-1])
        S.barrier()
        ar.release()
        ar.mark()
        gf_rep, gf_rep_b = ar.alloc([D], F32, "gf_rep")
        fn_rep, fn_rep_b = ar.alloc([D], F32, "fn_rep")
        S.op("sp", lambda e: e.dma_start(out=gf_rep, in_=self.mod_s[:, 5 * D:6 * D]), reads=[self.mod_sb], writes=[gf_rep_b], dma=True)
        S.op("sp", lambda e: e.dma_start(out=fn_rep, in_=fnw.broadcast_to([128, D])), writes=[fn_rep_b], dma=True)
        yk = [[ar.alloc([D], BF16, f"cb_y{j}{k}") for k in range(4)] for j in range(2)]
        hh = [ar.alloc([D], F32, f"cb_h{j}") for j in range(2)]
        acc = [ar.alloc([D], F32, f"cb_a{j}") for j in range(2)]
        oo = [ar.alloc([D], F32, f"cb_o{j}") for j in range(2)]
        sqj, sqj_b = ar.alloc([D], BF16, "cb_sq")
        ssq = [ar.alloc([1], F32, f"cb_ss{j}") for j in range(2)]
        for i in range(NT):
            j = i % 2
            h_t, h_b = hh[j]
            a_t, a_b = acc[j]
            o_t, o_b = oo[j]
            s_t, s_b = ssq[j]
            S.op("sp", lambda e, h_t=h_t, i=i: e.dma_start(out=h_t, in_=self.h1_s[i * 128:(i + 1) * 128, :]), reads=[self.h1_sb], writes=[h_b], dma=True)
            for k in range(4):
                y_t, y_b = yk[j][k]
                S.op("pool", lambda e, y_t=y_t, i=i, k=k: e.indirect_dma_start(
                    out=y_t, out_offset=None, in_=self.ys, in_offset=bass.IndirectOffsetOnAxis(ap=dest_i[:, i, k:k + 1].bitcast(U32), axis=0)),
                    reads=[self.ys_b, dest_ib], writes=[y_b], dma=True)
            S.op("dve", lambda e, a_t=a_t, i=i, j=j: e.tensor_scalar(out=a_t, in0=yk[j][0][0], scalar1=gate4[:, i, 0:1], scalar2=None, op0=ALU.mult),
                 reads=[yk[j][0][1], gate4_b], writes=[a_b])
            for k in range(1, 4):
                S.op("dve", lambda e, a_t=a_t, i=i, j=j, k=k: e.scalar_tensor_tensor(out=a_t, in0=yk[j][k][0], scalar=gate4[:, i, k:k + 1], in1=a_t,
                                                                                 op0=ALU.mult, op1=ALU.add), reads=[yk[j][k][1], gate4_b, a_b], writes=[a_b])
            S.op("dve", lambda e, a_t=a_t: e.tensor_tensor(out=a_t, in0=a_t, in1=gf_rep, op=ALU.mult), reads=[a_b, gf_rep_b], writes=[a_b])
            S.op("dve", lambda e, a_t=a_t, h_t=h_t: e.tensor_tensor(out=a_t, in0=a_t, in1=h_t, op=ALU.add), reads=[a_b, h_b], writes=[a_b])
            S.op("act", lambda e, a_t=a_t, s_t=s_t: e.activation(out=sqj, in_=a_t, func=AF.Square, accum_out=s_t), reads=[a_b], writes=[sqj_b, s_b])
            S.op("act", lambda e, s_t=s_t: e.activation(out=s_t, in_=s_t, func=AF.Sqrt, scale=1.0 / D, bias=EPS), reads=[s_b], writes=[s_b])
            S.op("dve", lambda e, s_t=s_t: e.reciprocal(out=s_t, in_=s_t), reads=[s_b], writes=[s_b])
            S.op("dve", lambda e, o_t=o_t, a_t=a_t, s_t=s_t: e.scalar_tensor_tensor(out=o_t, in0=a_t, scalar=s_t[:, 0:1], in1=fn_rep, op0=ALU.mult, op1=ALU.mult),
                 reads=[a_b, s_b, fn_rep_b], writes=[o_b])
            S.op("sp", lambda e, o_t=o_t, i=i: e.dma_start(out=self.out[i * 128:(i + 1) * 128, :], in_=o_t), reads=[o_b], dma=True)
        S.barrier()
        ar.release()
        ar.release()

def to_cm(a):
    return a.reshape(64, 64, *a.shape[1:]).swapaxes(0, 1).reshape(a.shape)


def from_cm(a):
    return a.reshape(64, 64, *a.shape[1:]).swapaxes(0, 1).reshape(a.shape)


def make_in_maps(inputs, cores):
    x = np.asarray(inputs["x"], np.float32)
    c = np.asarray(inputs["c"], np.float32)
    ctx = np.asarray(inputs["ctx"], np.float32)
    c_ctx = np.asarray(inputs["c_ctx"], np.float32)
    shared = {
        "cctxT": np.ascontiguousarray(c_ctx.reshape(8, 128).T),
        "ada_w": np.ascontiguousarray(inputs["ada_w"][0]),
        "ada_b": np.ascontiguousarray(inputs["ada_b"][0].reshape(1, -1)),
        "norm_mix_w": np.ascontiguousarray(np.asarray(inputs["norm_mix_w"][0]).reshape(8, 128).T),
        "w_in": np.ascontiguousarray(inputs["w_in"][0]),
        "convw": np.ascontiguousarray(np.transpose(np.asarray(inputs["ssd_conv_w"][0]).reshape(5, 32, 128), (2, 1, 0))),
        "convb": np.ascontiguousarray(np.asarray(inputs["ssd_conv_b"][0]).reshape(32, 128).T),
        "dtbias": np.ascontiguousarray(np.asarray(inputs["ssd_dt_bias"][0]).reshape(1, 64)),
        "alog": np.ascontiguousarray(np.asarray(inputs["ssd_a_log"][0]).reshape(1, 64)),
        "ssd_d": np.ascontiguousarray(np.repeat(np.asarray(inputs["ssd_d"][0]), 64).reshape(16, 128).T),
        "ssd_nw": np.ascontiguousarray(np.asarray(inputs["ssd_norm_w"][0]).reshape(16, 128).T),
    }
    lam_re = np.asarray(inputs["s5_lam_re"][0]); lam_im = np.asarray(inputs["s5_lam_im"][0]); ldt = np.asarray(inputs["s5_log_dt"][0])
    b_re = np.asarray(inputs["s5_b_re"][0]); b_im = np.asarray(inputs["s5_b_im"][0])
    c_re = np.asarray(inputs["s5_c_re"][0]); c_im = np.asarray(inputs["s5_c_im"][0])
    s5F = np.zeros((2, 128, 5, 8, 64), np.float32)
    s5S = np.zeros((2, 128, 3 * 32 + 4 * 512), np.float32)
    for d in range(2):
        def F_gp(a):
            t = a.reshape(8, 8, 64).transpose(1, 0, 2)
            return np.repeat(t[:, None], 16, axis=1).reshape(128, 8, 64)
        s5F[d, :, 0] = F_gp(lam_re[d]); s5F[d, :, 1] = F_gp(lam_im[d])
        s5F[d, :, 2] = F_gp(np.repeat(ldt[d][:, None], 64, axis=1))
        s5F[d, :, 3] = b_re[d].reshape(8, 8, 64, 16).transpose(1, 3, 0, 2).reshape(128, 8, 64)
        s5F[d, :, 4] = b_im[d].reshape(8, 8, 64, 16).transpose(1, 3, 0, 2).reshape(128, 8, 64)
        def S_gp(a):
            return a.reshape(32, 2, 64).transpose(1, 2, 0).reshape(128, 32)
        s5S[d, :, 0:32] = S_gp(lam_re[d]); s5S[d, :, 32:64] = S_gp(lam_im[d])
        s5S[d, :, 64:96] = S_gp(np.repeat(ldt[d][:, None], 64, axis=1))
        s5S[d, :, 96:96 + 512] = b_re[d].reshape(32, 2, 64, 16).transpose(1, 2, 0, 3).reshape(128, 512)
        s5S[d, :, 96 + 512:96 + 1024] = b_im[d].reshape(32, 2, 64, 16).transpose(1, 2, 0, 3).reshape(128, 512)
        s5S[d, :, 96 + 1024:96 + 1536] = c_re[d].reshape(32, 2, 16, 64).transpose(1, 3, 0, 2).reshape(128, 512)
        s5S[d, :, 96 + 1536:96 + 2048] = c_im[d].reshape(32, 2, 16, 64).transpose(1, 3, 0, 2).reshape(128, 512)
    shared["glu_w"] = np.ascontiguousarray(inputs["s5_glu_w"][0])
    shared["glu_b"] = np.ascontiguousarray(np.asarray(inputs["s5_glu_b"][0]).reshape(8, 128).T)
    shared["w_a"] = np.ascontiguousarray(inputs["w_branch_a"][0])
    shared["w_b"] = np.ascontiguousarray(inputs["w_branch_b"][0])
    shared["w_o"] = np.ascontiguousarray(inputs["w_out"][0])
    shared["nfw"] = np.ascontiguousarray(np.asarray(inputs["norm_ffn_w"][0]).reshape(1, -1))
    shared["r_w"] = np.ascontiguousarray(inputs["router_w"][0])
    shared["r_b"] = np.ascontiguousarray(np.asarray(inputs["router_b"][0]).reshape(1, -1))
    def wperm(w):
        return np.ascontiguousarray(np.asarray(w).reshape(32, 8, 128, D).transpose(0, 2, 1, 3)).reshape(4096, 8192)
    shared["moe_wg"] = wperm(inputs["moe_w_gate"][0])
    shared["moe_wu"] = wperm(inputs["moe_w_up"][0])
    shared["moe_wd"] = wperm(inputs["moe_w_down"][0])
    shared["moe_bg"] = np.ascontiguousarray(inputs["moe_b_gate"][0])
    shared["moe_bu"] = np.ascontiguousarray(inputs["moe_b_up"][0])
    shared["moe_bd"] = np.ascontiguousarray(inputs["moe_b_down"][0])
    shared["fnw"] = np.ascontiguousarray(np.asarray(inputs["final_norm_w"]).reshape(1, -1))
    shared["s5F"] = s5F
    shared["s5S"] = s5S
    shared["s5_d"] = np.ascontiguousarray(np.asarray(inputs["s5_d"][0]).reshape(8, 128).T)
    maps = []
    for b in cores:
        m = dict(shared)
        m["xT_rm"] = np.ascontiguousarray(x[b].T)
        m["xT_cm"] = np.ascontiguousarray(to_cm(x[b]).T)
        m["ctxT"] = np.ascontiguousarray(ctx[b].T)
        m["cT"] = np.ascontiguousarray(c[b].reshape(8, 128).T)
        m["x_tok"] = np.ascontiguousarray(to_cm(x[b]))
        maps.append(m)
    return maps


_NC_CACHE = {}


def kernel(**inputs):
    if "nc" not in _NC_CACHE:
        _NC_CACHE["nc"] = K().build()
    nc = _NC_CACHE["nc"]
    maps = make_in_maps(inputs, list(range(8)))
    res = run_bass_kernel_spmd(nc, maps, core_ids=list(range(8)))
    outs = [from_cm(np.asarray(r["out"])) for r in res.results]
    return np.stack(outs, 0).astype(np.float32)
```

```python
import os
import numpy as np
from contextlib import ExitStack
import concourse.bass as bass
import concourse.mybir as mybir
from concourse.bass_utils import run_bass_kernel_spmd

F32 = mybir.dt.float32
BF16 = mybir.dt.bfloat16
I32 = mybir.dt.int32
U32 = mybir.dt.uint32
U8 = mybir.dt.uint8
ALU = mybir.AluOpType
AF = mybir.ActivationFunctionType
AX = mybir.AxisListType

NDSEM = 8
D = 1024
L = 4096
LC = 256
T = L + LC
EPS = 1e-6


class Buf:
    __slots__ = ("name", "last_w", "readers", "excl")

    def __init__(self, name, excl=False):
        self.name = name
        self.last_w = None
        self.readers = []
        self.excl = excl


class Op:
    __slots__ = ("id", "eng", "fn", "deps", "dma", "eidx", "needs_inc", "semval", "dslot", "dval", "name")


COMPUTE = ("pe", "dve", "act", "pool")
ENGS = ("sp", "pe", "dve", "act", "pool")


class Sched:
    def __init__(self, nc):
        self.nc = nc
        self.ops = []
        self.per_eng = {e: [] for e in ENGS}
        self.ndma = {e: 0 for e in ENGS}
        self.barrier_floor = -1

    def op(self, eng, fn, reads=(), writes=(), dma=False, name=None):
        o = Op()
        o.id = len(self.ops)
        o.eng = eng
        o.fn = fn
        o.dma = dma
        o.name = name
        o.needs_inc = False
        o.semval = None
        o.dslot = None
        o.dval = None
        deps = {}
        if any(b.excl for b in reads):
            writes = list(writes) + [b for b in reads if b.excl and b not in writes]
            reads = [b for b in reads if not b.excl]
        for b in reads:
            if b.last_w is not None:
                deps[b.last_w.id] = (b.last_w, "raw")
        for b in writes:
            if b.last_w is not None and b.last_w.id not in deps:
                deps[b.last_w.id] = (b.last_w, "waw")
            for r in b.readers:
                if r.id not in deps:
                    deps[r.id] = (r, "war")
        o.deps = [(p, k) for (p, k) in deps.values() if p.id > self.barrier_floor]
        for b in reads:
            b.readers.append(o)
        for b in writes:
            b.last_w = o
            b.readers = []
        o.eidx = len(self.per_eng[eng])
        self.per_eng[eng].append(o)
        if dma:
            i = self.ndma[eng]
            self.ndma[eng] += 1
            o.dslot = i % NDSEM
            o.dval = 16 * (i // NDSEM + 1)
        self.ops.append(o)
        return o

    def barrier(self):
        lasts = []
        for e in ENGS:
            comp = [o for o in self.per_eng[e] if not o.dma and o.fn is not None]
            if comp:
                lasts.append(comp[-1])
            dm = [o for o in self.per_eng[e] if o.dma]
            lasts.extend(dm[-NDSEM:])
        lasts = [o for o in lasts if o.id > self.barrier_floor]
        for e in ENGS:
            o = self.op(e, None, name="barrier")
            o.deps = [(p, "raw") for p in lasts if p.eng != e or p.dma]
        self.barrier_floor = len(self.ops) - 1

    @staticmethod
    def _skip(o, p, kind):
        if p.dma or o.dma or p.eng != o.eng:
            return False
        if p.eng == "pe":
            return True
        if kind != "raw":
            return True
        return o.eidx - p.eidx > 2

    def emit(self):
        nc = self.nc
        for o in self.ops:
            for (p, kind) in o.deps:
                if p.dma or self._skip(o, p, kind):
                    continue
                p.needs_inc = True
        for e in ENGS:
            c = 0
            for o in self.per_eng[e]:
                if o.dma:
                    continue
                if o.needs_inc:
                    c += 1
                    o.semval = c
        with ExitStack() as es:
            csem = {e: es.enter_context(nc.semaphore(f"c_{e}")) for e in COMPUTE}
            dsem = {e: [es.enter_context(nc.semaphore(f"d_{e}{i}")) for i in range(NDSEM)]
                    for e in ENGS if self.ndma[e] > 0}
            block = es.enter_context(nc.Block())
            handles = {"sp": block.sync, "pe": block.tensor, "dve": block.vector,
                       "act": block.scalar, "pool": block.gpsimd}

            FUSE = os.environ.get("FUSEWAIT", "1") == "1"

            class _Rec:
                def __init__(self, eng):
                    self._eng = eng
                    self.first = None

                def __getattr__(self, name):
                    attr = getattr(self._eng, name)
                    if not callable(attr):
                        return attr

                    def w(*a, **k):
                        r = attr(*a, **k)
                        if self.first is None and hasattr(r, "then_inc"):
                            self.first = r
                        return r
                    return w

            def make(e):
                def body(eng):
                    waited = {}

                    def need(lst, sem, key, val):
                        if waited.get(key, 0) >= val:
                            return
                        for i, (s_, k_, v_) in enumerate(lst):
                            if k_ == key:
                                if v_ < val:
                                    lst[i] = (sem, key, val)
                                return
                        lst.append((sem, key, val))

                    for o in self.per_eng[e]:
                        lst = []
                        for (p, kind) in o.deps:
                            if p.dma:
                                need(lst, dsem[p.eng][p.dslot], ("d", p.eng, p.dslot), p.dval)
                            elif not self._skip(o, p, kind):
                                need(lst, csem[p.eng], ("c", p.eng), p.semval)
                        if o.dma and o.dval > 16:
                            need(lst, dsem[e][o.dslot], ("d", e, o.dslot), o.dval - 16)
                        for (s_, k_, v_) in lst:
                            waited[k_] = v_
                        fuse = None
                        if FUSE and lst and o.fn is not None and not o.dma:
                            fuse = lst.pop()
                        for (s_, k_, v_) in lst:
                            eng.wait_ge(s_, v_)
                        if o.fn is None:
                            continue
                        if fuse is not None:
                            rec = _Rec(eng)
                            ins = o.fn(rec)
                            rec.first._wait_ge(fuse[0], fuse[2])
                        else:
                            ins = o.fn(eng)
                        if o.dma:
                            ins.then_inc(dsem[e][o.dslot], 16)
                        elif o.needs_inc:
                            ins.then_inc(csem[e], 1)
                    if e == "sp":
                        for q in dsem:
                            dm = [o for o in self.per_eng[q] if o.dma]
                            for o in dm[-NDSEM:]:
                                if waited.get(("d", q, o.dslot), 0) < o.dval:
                                    eng.wait_ge(dsem[q][o.dslot], o.dval)
                                    waited[("d", q, o.dslot)] = o.dval
                return body

            for e in ENGS:
                if self.per_eng[e] or e == "sp":
                    handles[e](make(e))


ESZ = {F32: 4, BF16: 2, I32: 4, U32: 4, U8: 1}


class Arena:
    def __init__(self, nc, es, nbytes, name="arena"):
        self.t = es.enter_context(nc.sbuf_tensor(name, [128, nbytes], U8))
        self.nbytes = nbytes
        self.off = 0
        self.marks = []

    def alloc(self, shape, dtype, name="t"):
        if isinstance(shape, int):
            shape = [shape]
        n = int(np.prod(shape))
        nb = n * ESZ[dtype]
        self.off = (self.off + 63) // 64 * 64
        assert self.off + nb <= self.nbytes, f"arena overflow {name}: {self.off}+{nb}>{self.nbytes}"
        ap = self.t[:, self.off:self.off + nb]
        if dtype != U8:
            ap = ap.bitcast(dtype)
        if len(shape) > 1:
            names = " ".join(f"d{i}" for i in range(len(shape)))
            kw = {f"d{i}": int(shape[i]) for i in range(1, len(shape))}
            ap = ap.rearrange(f"p ({names}) -> p {names}", **kw)
        self.off += nb
        return ap, Buf(name)

    def mark(self):
        self.marks.append(self.off)

    def release(self):
        self.off = self.marks.pop()


IN_S5 = (0, 1024)
IN_XBC = (1024, 5120)
IN_DT = (5120, 5184)
IN_Z = (5184, 7232)
IN_GA = (7232, 8256)
IN_GB = (8256, 9280)


class K:
    def __init__(self, stage=99, debug=False):
        self.stage = stage
        self.debug = debug
        self.nc = bass.Bass("TRN2", target_bir_lowering=False)
        self.ins = {}
        self.dbg = {}

    def inp(self, name, shape, dt=F32):
        self.ins[name] = self.nc.dram_tensor(name, list(shape), dt, kind="ExternalInput").ap()
        return self.ins[name]

    def scratch(self, name, shape, dt, dump=False):
        if self.debug and dump:
            ap = self.nc.dram_tensor(name, list(shape), dt, kind="ExternalOutput").ap()
            self.dbg[name] = ap
        else:
            ap = self.nc.dram_tensor(name, list(shape), dt).ap()
        return ap, Buf(name)

    def build(self):
        nc = self.nc
        inp = self.inp
        xT_cm = inp("xT_cm", [D, L])
        xT_rm = inp("xT_rm", [D, L])
        ctxT = inp("ctxT", [D, LC])
        cT = inp("cT", [128, 8])
        cctxT = inp("cctxT", [128, 8])
        ada_w = inp("ada_w", [D, 6 * D])
        ada_b = inp("ada_b", [1, 6 * D])
        nmw = inp("norm_mix_w", [128, 8])
        w_in = inp("w_in", [D, 9280])
        out = nc.dram_tensor("out", [L, D], F32, kind="ExternalOutput").ap()
        self.out = out

        with ExitStack() as es:
            self.es = es
            ar = self.ar = Arena(nc, es, 206 * 1024)
            self.ps = [es.enter_context(nc.psum_tensor(f"ps{i}", [128, 512], F32)) for i in range(8)]
            self.psb = [Buf(f"ps{i}", excl=True) for i in range(8)]
            S = self.S = Sched(nc)

            ones_bf, ones_bf_b = ar.alloc([128], BF16, "ones_bf")
            S.op("dve", lambda e: e.memset(ones_bf, 1.0), writes=[ones_bf_b])
            ones_f, ones_f_b = ar.alloc([128], F32, "ones_f")
            S.op("dve", lambda e: e.memset(ones_f, 1.0), writes=[ones_f_b])
            ident_f, ident_f_b = ar.alloc([128], F32, "ident_f")
            S.op("pool", lambda e: e.affine_select(out=ident_f, in_=ones_f, pattern=[[-1, 128]],
                                                  compare_op=ALU.is_equal, fill=0.0, base=0,
                                                  channel_multiplier=1),
                 reads=[ones_f_b], writes=[ident_f_b])
            self.consts = dict(ones_bf=(ones_bf, ones_bf_b), ones_f=(ones_f, ones_f_b),
                               ident_f=(ident_f, ident_f_b))

            self.phase0(cT, cctxT, ada_w, ada_b, nmw)
            if self.stage >= 1:
                self.phase1(xT_rm, xT_cm, ctxT, w_in)
            if self.stage >= 2:
                self.phase2(inp("convw", [128, 32, 5]), inp("convb", [128, 32]), inp("dtbias", [1, 64]), inp("alog", [1, 64]),
                            inp("ssd_d", [128, 16]), inp("ssd_nw", [128, 16]))
            if self.stage >= 3:
                self.phase3_setup(inp("s5F", [2, 128, 5, 8, 64]), inp("s5S", [2, 128, 3 * 32 + 4 * 512]))
                self.phase3_main(inp("s5_d", [128, 8]))
            if self.stage >= 4:
                self.phase4(inp("glu_w", [D, D]), inp("glu_b", [128, 8]), inp("w_a", [D, D]), inp("w_b", [2 * D, D]), inp("w_o", [D, D]),
                            inp("x_tok", [L, D]), inp("nfw", [1, D]), inp("r_w", [D, 32]), inp("r_b", [1, 32]))
            if self.stage >= 5:
                self.phase5(inp("moe_wg", [4096, 8192]), inp("moe_wu", [4096, 8192]), inp("moe_wd", [4096, 8192]),
                            inp("moe_bg", [32, D]), inp("moe_bu", [32, D]), inp("moe_bd", [32, D]), inp("fnw", [1, D]))
            S.barrier()
            S.emit()
        return nc

    def phase0(self, cT, cctxT, ada_w, ada_b, nmw):
        nc, S, ar = self.nc, self.S, self.ar
        ps, psb = self.ps, self.psb
        ident_f, ident_f_b = self.consts["ident_f"]
        self.mod_rep = []
        self.Am = [ar.alloc([8], F32, f"Am{w}") for w in range(2)]
        self.Bm = [ar.alloc([8], F32, f"Bm{w}") for w in range(2)]
        nmw_t, nmw_b = ar.alloc([8], F32, "nmw")
        S.op("sp", lambda e: e.dma_start(out=nmw_t, in_=nmw), writes=[nmw_b], dma=True)
        ar.mark()
        self.mod_rep.append(ar.alloc([6 * D], F32, "mod_rep0"))
        self.mod_rep.append(ar.alloc([6 * D], F32, "mod_rep1"))
        self.mod_s, self.mod_sb = self.scratch("mod_s", [128, 6 * D], F32)
        adab, adab_b = ar.alloc([6 * D], F32, "adab")
        S.op("sp", lambda e: e.dma_start(out=adab, in_=ada_b.broadcast_to([128, 6 * D])), writes=[adab_b], dma=True)
        lhs = []
        for w, src in enumerate((cT, cctxT)):
            c_t, c_b = ar.alloc([8], F32, f"c{w}")
            S.op("sp", lambda e, c_t=c_t, src=src: e.dma_start(out=c_t, in_=src), writes=[c_b], dma=True)
            s_t, s_b = ar.alloc([8], F32, f"s{w}")
            S.op("act", lambda e, c_t=c_t, s_t=s_t: e.activation(out=s_t, in_=c_t, func=AF.Silu),
                 reads=[c_b], writes=[s_b])
            l_t, l_b = ar.alloc([8, 128], BF16, f"l{w}")
            S.op("dve", lambda e, l_t=l_t, s_t=s_t: e.tensor_copy(out=l_t, in_=s_t.unsqueeze(2).broadcast_to([128, 8, 128])),
                 reads=[s_b], writes=[l_b])
            lhs.append((l_t, l_b))
        wsrc = ada_w.rearrange("(kc p) n -> p kc n", p=128)
        wbufs = [ar.alloc([8, 512], BF16, f"adaw{i}") for i in range(2)]
        for blk in range(12):
            wt, wb = wbufs[blk % 2]
            S.op("pool", lambda e, wt=wt, blk=blk: e.dma_start(out=wt, in_=wsrc[:, :, blk * 512:(blk + 1) * 512]),
                 writes=[wb], dma=True)
            for w in range(2):
                l_t, l_b = lhs[w]
                pi = (blk * 2 + w) % 8

                def mm(e, l_t=l_t, wt=wt, pi=pi):
                    ins = None
                    for kc in range(8):
                        ins = e.matmul(ps[pi][:], lhsT=l_t[:, kc, :], rhs=wt[:, kc, :], start=(kc == 0), stop=(kc == 7))
                    return ins
                S.op("pe", mm, reads=[l_b, wb], writes=[psb[pi]])
                mt, mb = self.mod_rep[w]
                S.op("dve", lambda e, mt=mt, pi=pi, blk=blk: e.tensor_tensor(
                    out=mt[:, blk * 512:(blk + 1) * 512], in0=ps[pi][:], in1=adab[:, blk * 512:(blk + 1) * 512], op=ALU.add),
                    reads=[psb[pi], adab_b], writes=[mb])
        tmp, tmp_b = ar.alloc([8, 128], F32, "diagtmp")
        for w in range(2):
            mt, mb = self.mod_rep[w]
            for seg, (dst, dst_b) in ((0, self.Bm[w]), (1, self.Am[w])):
                view = mt[:, seg * D:(seg + 1) * D].rearrange("p (k q) -> p k q", k=8)
                S.op("dve", lambda e, view=view: e.tensor_tensor(
                    out=tmp, in0=view, in1=ident_f.unsqueeze(1).broadcast_to([128, 8, 128]), op=ALU.mult),
                    reads=[mb, ident_f_b], writes=[tmp_b])
                S.op("dve", lambda e, dst=dst: e.reduce_sum(out=dst, in_=tmp, axis=AX.X), reads=[tmp_b], writes=[dst_b])
            at, ab = self.Am[w]
            S.op("dve", lambda e, at=at: e.scalar_tensor_tensor(out=at, in0=at, scalar=1.0, in1=nmw_t, op0=ALU.add, op1=ALU.mult),
                 reads=[ab, nmw_b], writes=[ab])
        S.op("sp", lambda e: e.dma_start(out=self.mod_s, in_=self.mod_rep[0][0]), reads=[self.mod_rep[0][1]], writes=[self.mod_sb], dma=True)
        if self.debug:
            for w in range(2):
                o, ob = self.scratch(f"dbg_mod{w}", [128, 6 * D], F32, dump=True)
                mt, mb = self.mod_rep[w]
                S.op("sp", lambda e, o=o, mt=mt: e.dma_start(out=o, in_=mt), reads=[mb], writes=[ob], dma=True)
                o, ob = self.scratch(f"dbg_AB{w}", [128, 16], F32, dump=True)
                S.op("sp", lambda e, o=o, w=w: e.dma_start(out=o[:, 0:8], in_=self.Am[w][0]), reads=[self.Am[w][1]], writes=[ob], dma=True)
                S.op("sp", lambda e, o=o, w=w: e.dma_start(out=o[:, 8:16], in_=self.Bm[w][0]), reads=[self.Bm[w][1]], writes=[ob], dma=True)
        S.barrier()
        ar.release()

    def norm_bufs(self):
        ar = self.ar
        return dict(xt=[ar.alloc([8, 512], F32, f"xt{i}") for i in range(2)],
                    sq=[ar.alloc([8, 512], BF16, f"sq{i}") for i in range(2)],
                    rs=[ar.alloc([512], F32, f"rs{i}") for i in range(2)],
                    tm=[ar.alloc([512], F32, f"tm{i}") for i in range(4)], cnt=[0])

    def norm_tokens(self, src, ntok, u_t, u_b, col0, w, nb):
        nc, S, ar = self.nc, self.S, self.ar
        ps, psb = self.ps, self.psb
        ones_bf, ones_bf_b = self.consts["ones_bf"]
        At, Ab = self.Am[w]
        Bt, Bb = self.Bm[w]
        srcv = src.rearrange("(kc p) n -> p kc n", p=128)
        xt, sq, rs, tm = nb["xt"], nb["sq"], nb["rs"], nb["tm"]
        ntile = (ntok + 511) // 512
        for ti in range(ntile):
            n = min(512, ntok - ti * 512)
            ci = nb["cnt"][0]
            nb["cnt"][0] += 1
            x_t, x_b = xt[ci % 2]
            q_t, q_b = sq[ci % 2]
            r_t, r_b = rs[ci % 2]
            S.op("sp", lambda e, x_t=x_t, ti=ti, n=n: e.dma_start(out=x_t[:, :, 0:n], in_=srcv[:, :, ti * 512:ti * 512 + n]),
                 writes=[x_b], dma=True)
            S.op("act", lambda e, x_t=x_t, q_t=q_t, n=n: e.activation(out=q_t[:, :, 0:n], in_=x_t[:, :, 0:n], func=AF.Square),
                 reads=[x_b], writes=[q_b])
            pi = ci % 2

            def mm(e, q_t=q_t, pi=pi, n=n):
                ins = None
                for kc in range(8):
                    ins = e.matmul(ps[pi][:, 0:n], lhsT=ones_bf, rhs=q_t[:, kc, 0:n], start=(kc == 0), stop=(kc == 7))
                return ins
            S.op("pe", mm, reads=[q_b, ones_bf_b], writes=[psb[pi]])
            S.op("act", lambda e, r_t=r_t, pi=pi, n=n: e.activation(out=r_t[:, 0:n], in_=ps[pi][:, 0:n], func=AF.Sqrt,
                                                                  scale=1.0 / D, bias=EPS),
                 reads=[psb[pi]], writes=[r_b])
            S.op("dve", lambda e, r_t=r_t, n=n: e.reciprocal(out=r_t[:, 0:n], in_=r_t[:, 0:n]), reads=[r_b], writes=[r_b])
            for kc in range(8):
                t_t, t_b = tm[kc % 4]
                S.op("dve", lambda e, t_t=t_t, x_t=x_t, r_t=r_t, kc=kc, n=n: e.tensor_tensor(
                    out=t_t[:, 0:n], in0=x_t[:, kc, 0:n], in1=r_t[:, 0:n], op=ALU.mult),
                    reads=[x_b, r_b], writes=[t_b])
                S.op("act", lambda e, t_t=t_t, kc=kc, ti=ti, n=n: e.activation(
                    out=u_t[:, kc, col0 + ti * 512:col0 + ti * 512 + n], in_=t_t[:, 0:n], func=AF.Identity,
                    scale=At[:, kc:kc + 1], bias=Bt[:, kc:kc + 1]),
                    reads=[t_b, Ab, Bb], writes=[u_b])

    def proj_block(self, wsrc, c0, ncols, u_t, u_b, tok_ranges, dst, dst_b, row0, wbufs, stg, cnt):
        S = self.S
        ps, psb = self.ps, self.psb
        wt, wb = wbufs[cnt[0] % 2]
        cnt[0] += 1
        S.op("pool", lambda e: e.dma_start(out=wt[:, :, 0:ncols], in_=wsrc[:, :, c0:c0 + ncols]), writes=[wb], dma=True)
        for (ucol, n, dcol) in tok_ranges:
            for mc in range(ncols // 128):
                pi = cnt[1] % 8
                cnt[1] += 1

                def mm(e, pi=pi, mc=mc, ucol=ucol, n=n):
                    ins = None
                    for kc in range(8):
                        ins = e.matmul(ps[pi][:, 0:n], lhsT=wt[:, kc, mc * 128:(mc + 1) * 128],
                                       rhs=u_t[:, kc, ucol:ucol + n], start=(kc == 0), stop=(kc == 7))
                    return ins
                S.op("pe", mm, reads=[wb, u_b], writes=[psb[pi]])
                st, sb = stg[cnt[2] % len(stg)]
                eng = "act" if cnt[2] % 2 == 0 else "dve"
                cnt[2] += 1
                if eng == "act":
                    S.op("act", lambda e, st=st, pi=pi, n=n: e.activation(out=st[:, 0:n], in_=ps[pi][:, 0:n], func=AF.Copy),
                         reads=[psb[pi]], writes=[sb])
                else:
                    S.op("dve", lambda e, st=st, pi=pi, n=n: e.tensor_copy(out=st[:, 0:n], in_=ps[pi][:, 0:n]),
                         reads=[psb[pi]], writes=[sb])
                r = row0 + mc * 128
                S.op("sp", lambda e, st=st, r=r, dcol=dcol, n=n: e.dma_start(out=dst[r:r + 128, dcol:dcol + n], in_=st[:, 0:n]),
                     reads=[sb], writes=[dst_b], dma=True)

    def phase1(self, xT_rm, xT_cm, ctxT, w_in):
        nc, S, ar = self.nc, self.S, self.ar
        ps, psb = self.ps, self.psb
        dbg = self.debug
        self.s5u, self.s5u_b = self.scratch("s5u", [D, T], BF16, dump=dbg)
        self.xbc_s, self.xbc_sb = self.scratch("xbc_s", [4096, T], BF16, dump=dbg)
        self.z_s, self.z_sb = self.scratch("z_s", [2048, L], BF16, dump=dbg)
        self.gab_s, self.gab_sb = self.scratch("gab_s", [2048, L], BF16, dump=dbg)
        self.dt_s, self.dt_sb = self.scratch("dt_s", [128, 34 * 64], F32, dump=dbg)
        wsrc = w_in.rearrange("(kc p) n -> p kc n", p=128)
        ar.mark()
        u_t, u_b = ar.alloc([8, T], BF16, "u")
        wbufs = [ar.alloc([8, 512], BF16, f"wb{i}") for i in range(2)]
        stg = [ar.alloc([512], BF16, f"stg{i}") for i in range(4)]
        cnt = [0, 0, 0]
        nb = self.norm_bufs()
        self.norm_tokens(ctxT, LC, u_t, u_b, 0, 1, nb)
        self.norm_tokens(xT_rm, L, u_t, u_b, LC, 0, nb)
        if dbg:
            o, ob = self.scratch("dbg_u_rm", [128, 8 * T], BF16, dump=True)
            S.op("sp", lambda e: e.dma_start(out=o, in_=u_t.rearrange("p a b -> p (a b)")), reads=[u_b], writes=[ob], dma=True)
        toks = [(0, LC, 0)] + [(LC + i * 512, 512, LC + i * 512) for i in range(8)]
        for blk in range(2):
            self.proj_block(wsrc, IN_S5[0] + blk * 512, 512, u_t, u_b, toks, self.s5u, self.s5u_b, blk * 512, wbufs, stg, cnt)
        self.norm_tokens(xT_cm, L, u_t, u_b, LC, 0, nb)
        for blk in range(8):
            self.proj_block(wsrc, IN_XBC[0] + blk * 512, 512, u_t, u_b, toks, self.xbc_s, self.xbc_sb, blk * 512, wbufs, stg, cnt)
        ltoks = [(LC + i * 512, 512, i * 512) for i in range(8)]
        for blk in range(4):
            self.proj_block(wsrc, IN_Z[0] + blk * 512, 512, u_t, u_b, ltoks, self.z_s, self.z_sb, blk * 512, wbufs, stg, cnt)
        for blk in range(4):
            self.proj_block(wsrc, IN_GA[0] + blk * 512, 512, u_t, u_b, ltoks, self.gab_s, self.gab_sb, blk * 512, wbufs, stg, cnt)
        wdt, wdt_b = ar.alloc([8, 64], BF16, "wdt")
        S.op("pool", lambda e: e.dma_start(out=wdt, in_=wsrc[:, :, IN_DT[0]:IN_DT[1]]), writes=[wdt_b], dma=True)
        dtt, dtt_b = ar.alloc([34, 64], F32, "dtt")
        for c in range(34):
            pi = c % 2

            def mm(e, c=c, pi=pi):
                ins = None
                for kc in range(8):
                    ins = e.matmul(ps[pi][:, 0:64], lhsT=u_t[:, kc, c * 128:(c + 1) * 128], rhs=wdt[:, kc, :],
                                   start=(kc == 0), stop=(kc == 7))
                return ins
            S.op("pe", mm, reads=[u_b, wdt_b], writes=[psb[pi]])
            S.op("dve", lambda e, c=c, pi=pi: e.tensor_copy(out=dtt[:, c, :], in_=ps[pi][:, 0:64]), reads=[psb[pi]], writes=[dtt_b])
        S.op("sp", lambda e: e.dma_start(out=self.dt_s, in_=dtt.rearrange("p a b -> p (a b)")), reads=[dtt_b], writes=[self.dt_sb], dma=True)
        S.barrier()
        ar.release()


    def pq(self, i, j0, j1=None):
        if j1 is None:
            j1 = j0 + 1
        return self.ps[i][:, j0 * 128:j1 * 128], [self.psb[i]]

    def phase2(self, convw, convb, dtbias, alog, ssd_d, ssd_nw):
        nc, S, ar = self.nc, self.S, self.ar
        ps = self.ps
        dbg = self.debug
        ident_f, ident_f_b = self.consts["ident_f"]
        ones_f, ones_f_b = self.consts["ones_f"]
        ones_bf, ones_bf_b = self.consts["ones_bf"]
        self.yb_s, self.yb_sb = self.scratch("yb_s", [2048, L], BF16, dump=dbg)
        ar.mark()
        ident_bf, ident_bf_b = ar.alloc([128], BF16, "ident_bf")
        S.op("dve", lambda e: e.tensor_copy(out=ident_bf, in_=ident_f), reads=[ident_f_b], writes=[ident_bf_b])
        tri, tri_b = ar.alloc([128], F32, "tri")
        triT, triT_b = ar.alloc([128], F32, "triT")
        S.op("pool", lambda e: e.affine_select(out=tri, in_=ones_f, pattern=[[1, 128]], compare_op=ALU.is_ge, fill=0.0,
                                              base=0, channel_multiplier=-1), reads=[ones_f_b], writes=[tri_b])
        S.op("pool", lambda e: e.affine_select(out=triT, in_=ones_f, pattern=[[-1, 128]], compare_op=ALU.is_ge, fill=0.0,
                                              base=0, channel_multiplier=1), reads=[ones_f_b], writes=[triT_b])
        zer, zer_b = ar.alloc([128], F32, "zer")
        S.op("dve", lambda e: e.memset(zer, 0.0), writes=[zer_b])
        NEG = [ar.alloc([128], BF16, f"NEG{d}") for d in range(2)]
        S.op("pool", lambda e: e.affine_select(out=NEG[0][0], in_=zer, pattern=[[1, 128]], compare_op=ALU.is_ge, fill=-60000.0,
                                              base=0, channel_multiplier=-1), reads=[zer_b], writes=[NEG[0][1]])
        S.op("pool", lambda e: e.affine_select(out=NEG[1][0], in_=zer, pattern=[[-1, 128]], compare_op=ALU.is_ge, fill=-60000.0,
                                              base=0, channel_multiplier=1), reads=[zer_b], writes=[NEG[1][1]])
        oh2, oh2_b = ar.alloc([64], F32, "oh2")
        S.op("dve", lambda e: e.tensor_tensor(out=oh2[:, 0:32], in0=ident_f[:, 0:32], in1=ident_f[:, 32:64], op=ALU.add),
             reads=[ident_f_b], writes=[oh2_b])
        S.op("dve", lambda e: e.tensor_tensor(out=oh2[:, 32:64], in0=ident_f[:, 64:96], in1=ident_f[:, 96:128], op=ALU.add),
             reads=[ident_f_b], writes=[oh2_b])
        sel2, sel2_b = ar.alloc([64, 128], BF16, "sel2")
        S.op("dve", lambda e: e.tensor_copy(out=sel2, in_=oh2.unsqueeze(2).broadcast_to([128, 64, 128])), reads=[oh2_b], writes=[sel2_b])
        mhi, mhi_b = ar.alloc([1], F32, "mhi")
        mlo, mlo_b = ar.alloc([1], F32, "mlo")
        mt_, mt_b = ar.alloc([1], F32, "mtmp")
        S.op("dve", lambda e: e.reduce_sum(out=mhi, in_=ident_f[:, 0:32], axis=AX.X), reads=[ident_f_b], writes=[mhi_b])
        S.op("dve", lambda e: e.reduce_sum(out=mt_, in_=ident_f[:, 64:96], axis=AX.X), reads=[ident_f_b], writes=[mt_b])
        S.op("dve", lambda e: e.tensor_tensor(out=mhi, in0=mhi, in1=mt_, op=ALU.add), reads=[mhi_b, mt_b], writes=[mhi_b])
        S.op("dve", lambda e: e.tensor_scalar(out=mlo, in0=mhi, scalar1=-1.0, scalar2=1.0, op0=ALU.mult, op1=ALU.add),
             reads=[mhi_b], writes=[mlo_b])
        cw, cw_b = ar.alloc([32, 5], F32, "convw")
        cb, cb_b = ar.alloc([32], F32, "convb")
        dcol, dcol_b = ar.alloc([16], F32, "ssd_d")
        nwc, nwc_b = ar.alloc([16], F32, "ssd_nw")
        S.op("sp", lambda e: e.dma_start(out=cw, in_=convw), writes=[cw_b], dma=True)
        S.op("sp", lambda e: e.dma_start(out=cb, in_=convb), writes=[cb_b], dma=True)
        S.op("sp", lambda e: e.dma_start(out=dcol, in_=ssd_d), writes=[dcol_b], dma=True)
        S.op("sp", lambda e: e.dma_start(out=nwc, in_=ssd_nw), writes=[nwc_b], dma=True)
        dt, dt_b = ar.alloc([34, 2, 32], F32, "dt")
        decay, decay_b = ar.alloc([34, 2, 32], F32, "decay")
        wend, wend_b = ar.alloc([34, 2, 32], F32, "wend")
        HL, HL_b = ar.alloc([34, 128], BF16, "HL")
        HLn, HLn_b = ar.alloc([34, 128], BF16, "HLn")
        ar.mark()
        nacum, nacum_b = ar.alloc([34, 2, 32], F32, "nacum")
        adtd, adtd_b = ar.alloc([34, 4, 32], F32, "adtd")
        tot, tot_b = ar.alloc([34, 2, 32], F32, "tot")
        dtb, dtb_b = ar.alloc([2, 32], F32, "dtb")
        arep, arep_b = ar.alloc([2, 32], F32, "arep")
        S.op("sp", lambda e: e.dma_start(out=dt.rearrange("p a b c -> p (a b c)"), in_=self.dt_s), reads=[self.dt_sb], writes=[dt_b], dma=True)
        S.op("sp", lambda e: e.dma_start(out=dtb.rearrange("p a b -> p (a b)"), in_=dtbias.broadcast_to([128, 64])), writes=[dtb_b], dma=True)
        S.op("sp", lambda e: e.dma_start(out=arep.rearrange("p a b -> p (a b)"), in_=alog.broadcast_to([128, 64])), writes=[arep_b], dma=True)
        S.op("dve", lambda e: e.tensor_tensor(out=dt, in0=dt, in1=dtb.unsqueeze(1).broadcast_to([128, 34, 2, 32]), op=ALU.add),
             reads=[dt_b, dtb_b], writes=[dt_b])
        S.op("act", lambda e: e.activation(out=dt, in_=dt, func=AF.Exp), reads=[dt_b], writes=[dt_b])
        S.op("act", lambda e: e.activation(out=dt, in_=dt, func=AF.Ln, bias=1.0), reads=[dt_b], writes=[dt_b])
        S.op("act", lambda e: e.activation(out=arep, in_=arep, func=AF.Exp), reads=[arep_b], writes=[arep_b])
        S.op("dve", lambda e: e.tensor_scalar(out=arep, in0=arep, scalar1=-1.0, scalar2=None, op0=ALU.mult), reads=[arep_b], writes=[arep_b])
        for d in range(2):
            for j in range(2):
                S.op("dve", lambda e, d=d, j=j: e.tensor_tensor(
                    out=adtd[:, :, 2 * d + j, :], in0=dt[:, :, d, :], in1=arep[:, d, :].unsqueeze(1).broadcast_to([128, 34, 32]), op=ALU.mult),
                    reads=[dt_b, arep_b], writes=[adtd_b])
        k = 0
        for d in range(2):
            lhs, lhs_b = (tri, tri_b) if d == 0 else (triT, triT_b)
            for (c0, ncn) in ((0, 16), (16, 16), (32, 2)):
                pa, pb = self.pq(k % 4, 0, 4)
                k += 1
                S.op("pe", lambda e, pa=pa, lhs=lhs, c0=c0, ncn=ncn, d=d: e.matmul(
                    pa[:, 0:ncn * 32].rearrange("p (a b) -> p a b", b=32), lhsT=lhs, rhs=adtd[:, c0:c0 + ncn, 2 * d, :], start=True, stop=True),
                    reads=[lhs_b, adtd_b], writes=pb)
                S.op("dve", lambda e, pa=pa, c0=c0, ncn=ncn, d=d: e.tensor_scalar(
                    out=nacum[:, c0:c0 + ncn, d, :], in0=pa[:, 0:ncn * 32].rearrange("p (a b) -> p a b", b=32),
                    scalar1=-1.0, scalar2=None, op0=ALU.mult), reads=pb, writes=[nacum_b])
                pa, pb = self.pq(k % 4, 0, 4)
                k += 1
                S.op("pe", lambda e, pa=pa, c0=c0, ncn=ncn, d=d: e.matmul(
                    pa[:, 0:ncn * 32].rearrange("p (a b) -> p a b", b=32), lhsT=ones_f, rhs=adtd[:, c0:c0 + ncn, 2 * d, :], start=True, stop=True),
                    reads=[ones_f_b, adtd_b], writes=pb)
                S.op("dve", lambda e, pa=pa, c0=c0, ncn=ncn, d=d: e.tensor_copy(
                    out=tot[:, c0:c0 + ncn, d, :], in_=pa[:, 0:ncn * 32].rearrange("p (a b) -> p a b", b=32)), reads=pb, writes=[tot_b])
        S.op("act", lambda e: e.activation(out=decay, in_=tot, func=AF.Exp), reads=[tot_b], writes=[decay_b])
        S.op("dve", lambda e: e.tensor_tensor(out=wend, in0=tot, in1=nacum, op=ALU.add), reads=[tot_b, nacum_b], writes=[wend_b])
        S.op("act", lambda e: e.activation(out=wend, in_=wend, func=AF.Exp), reads=[wend_b], writes=[wend_b])
        S.op("dve", lambda e: e.tensor_tensor(out=wend, in0=wend, in1=dt, op=ALU.mult), reads=[wend_b, dt_b], writes=[wend_b])
        acT = [ar.alloc([128], F32, f"acT{i}") for i in range(2)]
        hi_ = [ar.alloc([128], BF16, f"hi{i}") for i in range(2)]
        lo_ = [ar.alloc([128], F32, f"lo{i}") for i in range(2)]
        for c in range(34):
            pa, pab = self.pq(4 + (c % 2), 0)
            pb_, pbb = self.pq(4 + (c % 2), 1)
            lhsT = adtd[:, c, :, :].rearrange("p a b -> p (a b)")
            S.op("pe", lambda e, pa=pa, lhsT=lhsT: e.matmul(pa, lhsT=lhsT, rhs=tri, start=True, stop=True),
                 reads=[adtd_b, tri_b], writes=pab)
            S.op("pe", lambda e, pb_=pb_, lhsT=lhsT: e.matmul(pb_, lhsT=lhsT, rhs=triT, start=True, stop=True),
                 reads=[adtd_b, triT_b], writes=pbb)
            a_t, a_b = acT[c % 2]
            h_t, h_b = hi_[c % 2]
            l_t, l_b = lo_[c % 2]
            S.op("act", lambda e, a_t=a_t, pa=pa: e.activation(out=a_t[0:64, :], in_=pa[0:64, :], func=AF.Copy), reads=pab, writes=[a_b])
            S.op("act", lambda e, a_t=a_t, pb_=pb_: e.activation(out=a_t[64:128, :], in_=pb_[64:128, :], func=AF.Copy), reads=pbb, writes=[a_b])
            S.op("dve", lambda e, a_t=a_t, h_t=h_t: e.tensor_copy(out=h_t, in_=a_t), reads=[a_b], writes=[h_b])
            S.op("dve", lambda e, a_t=a_t, h_t=h_t, l_t=l_t: e.tensor_tensor(out=l_t, in0=a_t, in1=h_t, op=ALU.subtract), reads=[a_b, h_b], writes=[l_b])
            S.op("dve", lambda e, l_t=l_t: e.tensor_scalar(out=l_t, in0=l_t, scalar1=mlo[:, 0:1], scalar2=None, op0=ALU.mult), reads=[l_b, mlo_b], writes=[l_b])
            S.op("dve", lambda e, h_t=h_t, l_t=l_t, c=c: e.scalar_tensor_tensor(out=HL[:, c, :], in0=h_t, scalar=mhi[:, 0:1], in1=l_t, op0=ALU.mult, op1=ALU.add),
                 reads=[h_b, l_b, mhi_b], writes=[HL_b])
        S.op("dve", lambda e: e.tensor_scalar(out=HLn, in0=HL, scalar1=-1.0, scalar2=None, op0=ALU.mult), reads=[HL_b], writes=[HLn_b])
        if dbg:
            for nm, src_, sb_, dtp, ncol in (("dbg_dt", dt.rearrange("p a b c -> p (a b c)"), dt_b, F32, 34 * 64),
                                             ("dbg_nacum", nacum.rearrange("p a b c -> p (a b c)"), nacum_b, F32, 34 * 64),
                                             ("dbg_wend", wend.rearrange("p a b c -> p (a b c)"), wend_b, F32, 34 * 64),
                                             ("dbg_HL", HL.rearrange("p a b -> p (a b)"), HL_b, BF16, 34 * 128)):
                o, ob = self.scratch(nm, [128, ncol], dtp, dump=True)
                S.op("sp", lambda e, o=o, src_=src_: e.dma_start(out=o, in_=src_), reads=[sb_], writes=[ob], dma=True)
        S.barrier()
        ar.release()
        P2STOP = int(os.environ.get("P2STOP", "99"))
        P2SKIP = os.environ.get("P2SKIP", "")
        if P2STOP <= 1:
            ar.release()
            return
        TP = 4360
        RA, RA_b = ar.alloc([4 * TP], BF16, "RA")
        R = RA.rearrange("p (a b) -> p a b", a=4)
        prevs = RA[:, 0:2 * 34 * 256].rearrange("p (d c n) -> p d c n", d=2, c=34)
        xc, xc_b = ar.alloc([4, T], BF16, "xc")
        x_tok, x_tok_b = ar.alloc([34, 256], BF16, "x_tok")
        B_tok, B_tok_b = ar.alloc([34, 128], BF16, "B_tok")
        dg, dg_b = ar.alloc([20, 128], BF16, "dg")
        st = [ar.alloc([256], F32, f"st{d}") for d in range(2)]
        xw = [ar.alloc([256], BF16, f"xw{i}") for i in range(4)]
        xdt = [ar.alloc([256], BF16, f"xdt{i}") for i in range(4)]
        E_ = [ar.alloc([4, 128], F32, f"E{i}") for i in range(3)]
        Lt = [ar.alloc([4, 128], F32, f"Lt{i}") for i in range(3)]
        Gt = [ar.alloc([4, 128], BF16, f"Gt{i}") for i in range(4)]
        Cp = [ar.alloc([4, 128], BF16, f"Cp{i}") for i in range(4)]
        zt = [ar.alloc([2, 512], BF16, f"zt{i}") for i in range(1)]
        sz = [ar.alloc([2, 512], F32, f"sz{i}") for i in range(1)]
        yz = [ar.alloc([2, 512], F32, f"yz{i}") for i in range(1)]
        sqy = [ar.alloc([2, 512], BF16, f"sqy{i}") for i in range(1)]
        rsy = [ar.alloc([512], F32, f"rsy{i}") for i in range(1)]
        ybo = [ar.alloc([2, 512], BF16, f"ybo{i}") for i in range(1)]
        bwd_order = [1, 0] + list(range(33, 1, -1))
        cnt = dict(e=0, a=0, g=0, cp=0, xw=0, cs=0, y=0, cb=0, post=0, conv=0, tr=0)
        for g in range(int(os.environ.get('P2G', '8'))):
            tiles = [2 * g, 2 * g + 1, 16 + g, 24 + g]
            for i, tix in enumerate(tiles):
                S.op("sp", lambda e, i=i, tix=tix: e.dma_start(out=R[:, i, 2:258], in_=self.xbc_s[tix * 128:(tix + 1) * 128, 0:256]),
                     reads=[self.xbc_sb], writes=[RA_b], dma=True)
                S.op("sp", lambda e, i=i, tix=tix: e.dma_start(out=R[:, i, 262:4358], in_=self.xbc_s[tix * 128:(tix + 1) * 128, 256:T]),
                     reads=[self.xbc_sb], writes=[RA_b], dma=True)
            for (a0, a1) in ((0, 2), (258, 262), (4358, 4360)):
                S.op("dve", lambda e, a0=a0, a1=a1: e.memset(R[:, :, a0:a1], 0.0), writes=[RA_b])
            for i, tix in enumerate(tiles):
                for kk in range(5):
                    S.op("dve", lambda e, i=i, tix=tix, kk=kk: e.tensor_scalar(
                        out=dg[:, i * 5 + kk, :], in0=ident_f, scalar1=cw[:, tix, kk:kk + 1], scalar2=None, op0=ALU.mult),
                        reads=[ident_f_b, cw_b], writes=[dg_b])
            for i, tix in enumerate(tiles):
                for (t0, n, base) in [(0, 256, 0)] + [(256 + j * 512, 512, 260 + j * 512) for j in range(8)]:
                    pa, pb = self.pq(cnt["conv"] % 2, 0, 4)
                    cnt["conv"] += 1

                    def mm(e, pa=pa, i=i, n=n, base=base):
                        ins = None
                        for kk in range(5):
                            ins = e.matmul(pa[:, 0:n], lhsT=dg[:, i * 5 + kk, :], rhs=R[:, i, base + kk:base + kk + n],
                                           start=(kk == 0), stop=(kk == 4))
                        return ins
                    S.op("pe", mm, reads=[dg_b, RA_b], writes=pb)
                    S.op("act", lambda e, pa=pa, i=i, tix=tix, t0=t0, n=n: e.activation(
                        out=xc[:, i, t0:t0 + n], in_=pa[:, 0:n], func=AF.Silu, bias=cb[:, tix:tix + 1]),
                        reads=pb + [cb_b], writes=[xc_b])
            if P2STOP <= 2:
                break
            for c in range(34):
                pa, pb = self.pq(2 + (cnt["tr"] % 2), 0, 4)
                cnt["tr"] += 1
                pab = pa.bitcast(BF16)

                def tr(e, pab=pab, c=c):
                    ins = None
                    for j in range(3):
                        ins = e.transpose(out=pab[:, j * 128:(j + 1) * 128], in_=xc[:, j, c * 128:(c + 1) * 128], identity=ident_bf)
                    return ins
                S.op("pe", tr, reads=[xc_b, ident_bf_b], writes=pb)
                S.op("dve", lambda e, pab=pab, c=c: e.tensor_copy(out=x_tok[:, c, :], in_=pab[:, 0:256]), reads=pb, writes=[x_tok_b])
                S.op("dve", lambda e, pab=pab, c=c: e.tensor_copy(out=B_tok[:, c, :], in_=pab[:, 256:384]), reads=pb, writes=[B_tok_b])
            if P2STOP <= 3:
                break
            for d in range(2):
                S.op("dve", lambda e, d=d: e.memset(st[d][0], 0.0), writes=[st[d][1]])
            for step in range(34):
                for d in range(2):
                    c = step if d == 0 else bwd_order[step]
                    xw_t, xw_b = xw[cnt["xw"] % 4]
                    cnt["xw"] += 1
                    S.op("dve", lambda e, xw_t=xw_t, c=c, d=d, g=g: e.tensor_tensor(
                        out=xw_t.rearrange("p (r q) -> p r q", r=4), in0=x_tok[:, c, :].rearrange("p (r q) -> p r q", r=4),
                        in1=wend[:, c, d, 4 * g:4 * g + 4].unsqueeze(2).broadcast_to([128, 4, 64]), op=ALU.mult),
                        reads=[x_tok_b, wend_b], writes=[xw_b])
                    k2 = cnt["cs"] % 4
                    cnt["cs"] += 1
                    pa, pb = self.pq(4 + k2, 0, 2)
                    S.op("pe", lambda e, pa=pa, xw_t=xw_t, c=c: e.matmul(pa, lhsT=B_tok[:, c, :], rhs=xw_t, start=True, stop=True),
                         reads=[B_tok_b, xw_b], writes=pb)
                    st_t, st_b = st[d]
                    S.op("act", lambda e, st_t=st_t, d=d, c=c: e.activation(out=prevs[:, d, c, :], in_=st_t, func=AF.Copy),
                         reads=[st_b], writes=[RA_b])
                    S.op("dve", lambda e, st_t=st_t, c=c, d=d, g=g: e.tensor_tensor(
                        out=st_t.rearrange("p (r q) -> p r q", r=4), in0=st_t.rearrange("p (r q) -> p r q", r=4),
                        in1=decay[:, c, d, 4 * g:4 * g + 4].unsqueeze(2).broadcast_to([128, 4, 64]), op=ALU.mult),
                        reads=[st_b, decay_b], writes=[st_b])
                    S.op("dve", lambda e, st_t=st_t, pa=pa: e.tensor_tensor(out=st_t, in0=st_t, in1=pa, op=ALU.add),
                         reads=[st_b] + pb, writes=[st_b])
            if dbg and g == 0:
                o, ob = self.scratch("dbg_xc", [128, 4 * T], BF16, dump=True)
                S.op("sp", lambda e, o=o: e.dma_start(out=o, in_=xc.rearrange("p a b -> p (a b)")), reads=[xc_b], writes=[ob], dma=True)
                o2, ob2 = self.scratch("dbg_prev", [128, 2 * 34 * 256], BF16, dump=True)
                S.op("sp", lambda e, o2=o2: e.dma_start(out=o2, in_=RA[:, 0:2 * 34 * 256]), reads=[RA_b], writes=[ob2], dma=True)
            if P2STOP <= 4:
                continue
            for cb4 in range(8):
                if P2STOP <= 5 and cb4 >= 1:
                    break
                pi = cnt["post"] % 2
                cnt["post"] += 1
                z_t, z_b = zt[0]
                sz_t, sz_b = sz[0]
                yz_t, yz_b = yz[0]
                for i in range(2):
                    r0 = 256 * g + 128 * i
                    S.op("sp", lambda e, z_t=z_t, i=i, r0=r0, cb4=cb4: e.dma_start(out=z_t[:, i, :], in_=self.z_s[r0:r0 + 128, cb4 * 512:(cb4 + 1) * 512]),
                         reads=[self.z_sb], writes=[z_b], dma=True)
                if "Z" not in P2SKIP:
                    S.op("act", lambda e, z_t=z_t, sz_t=sz_t: e.activation(out=sz_t, in_=z_t, func=AF.Silu), reads=[z_b], writes=[sz_b])
                for cl in range(4):
                    lc = cb4 * 4 + cl
                    c = 2 + lc
                    tsl = slice(c * 128, (c + 1) * 128)
                    pcb, pcb_b = self.pq(4 + cnt["cb"] % 2, 0)
                    cnt["cb"] += 1
                    S.op("pe", lambda e, pcb=pcb, tsl=tsl: e.matmul(pcb, lhsT=xc[:, 2, tsl], rhs=xc[:, 3, tsl], start=True, stop=True),
                         reads=[xc_b], writes=pcb_b)
                    gts, cps, xds = {}, {}, {}
                    for d in range(2):
                        xd_t, xd_b = xdt[cnt["xw"] % 4]
                        cnt["xw"] += 1
                        S.op("dve", lambda e, xd_t=xd_t, c=c, d=d, g=g: e.tensor_tensor(
                            out=xd_t.rearrange("p (r q) -> p r q", r=4), in0=x_tok[:, c, :].rearrange("p (r q) -> p r q", r=4),
                            in1=dt[:, c, d, 4 * g:4 * g + 4].unsqueeze(2).broadcast_to([128, 4, 64]), op=ALU.mult),
                            reads=[x_tok_b, dt_b], writes=[xd_b])
                        xds[d] = (xd_t, xd_b)
                        pe_, pe_b = self.pq(cnt["e"] % 2, 0, 4)
                        cnt["e"] += 1

                        def mme(e, pe_=pe_, c=c, d=d, g=g):
                            ins = None
                            for r in range(4):
                                hh = d * 32 + 4 * g + r
                                ins = e.matmul(pe_[:, r * 128:(r + 1) * 128], lhsT=sel2[:, hh, :], rhs=HL[:, c, :], start=True, stop=True)
                            return ins
                        S.op("pe", mme, reads=[sel2_b, HL_b], writes=pe_b)
                        E_t, E_b = E_[cnt["e"] % 3]
                        S.op("act", lambda e, E_t=E_t, pe_=pe_: e.activation(out=E_t.rearrange("p a b -> p (a b)"), in_=pe_, func=AF.Exp),
                             reads=pe_b, writes=[E_b])
                        cp_t, cp_b = Cp[cnt["cp"] % 4]
                        cnt["cp"] += 1
                        S.op("dve", lambda e, cp_t=cp_t, E_t=E_t, tsl=tsl: e.tensor_tensor(
                            out=cp_t, in0=E_t, in1=xc[:, 3, tsl].unsqueeze(1).broadcast_to([128, 4, 128]), op=ALU.mult),
                            reads=[xc_b, E_b], writes=[cp_b])
                        cps[d] = (cp_t, cp_b)
                        pa_, pa_b = self.pq(2 + cnt["a"] % 2, 0, 4)
                        cnt["a"] += 1

                        def mma(e, pa_=pa_, c=c, d=d, g=g):
                            ins = None
                            for r in range(4):
                                hh = d * 32 + 4 * g + r
                                o_ = pa_[:, r * 128:(r + 1) * 128]
                                e.matmul(o_, lhsT=sel2[:, hh, :], rhs=HL[:, c, :], start=True, stop=False)
                                e.matmul(o_, lhsT=HLn[:, c, :], rhs=sel2[:, hh, :], start=False, stop=False)
                                ins = e.matmul(o_, lhsT=ident_bf, rhs=NEG[d][0], start=False, stop=True)
                            return ins
                        S.op("pe", mma, reads=[sel2_b, HL_b, HLn_b, ident_bf_b, NEG[d][1]], writes=pa_b)
                        L_t, L_b = Lt[cnt["a"] % 3]
                        S.op("act", lambda e, L_t=L_t, pa_=pa_: e.activation(out=L_t.rearrange("p a b -> p (a b)"), in_=pa_, func=AF.Exp),
                             reads=pa_b, writes=[L_b])
                        g_t, g_b = Gt[cnt["g"] % 4]
                        cnt["g"] += 1
                        S.op("dve", lambda e, g_t=g_t, L_t=L_t, pcb=pcb: e.tensor_tensor(
                            out=g_t, in0=L_t, in1=pcb.unsqueeze(1).broadcast_to([128, 4, 128]), op=ALU.mult),
                            reads=[L_b] + pcb_b, writes=[g_b])
                        gts[d] = (g_t, g_b)
                    yb_i = 6 + cnt["y"] % 2
                    cnt["y"] += 1
                    pys = [self.pq(yb_i, i) for i in range(2)]

                    def mmy(e, pys=pys, c=c, gts=gts, cps=cps, xds=xds):
                        ins = None
                        for i in range(2):
                            py = pys[i][0]
                            for step in range(4):
                                for hf in range(2):
                                    r = 2 * i + hf
                                    o_ = py[64 * hf:64 * hf + 64, :]
                                    tp = (0, 64 * hf)
                                    d_ = step // 2
                                    if step % 2 == 0:
                                        ins = e.matmul(o_, lhsT=xds[d_][0][:, r * 64:(r + 1) * 64], rhs=gts[d_][0][:, r, :], start=(step == 0), stop=False, tile_position=tp)
                                    else:
                                        ins = e.matmul(o_, lhsT=prevs[:, d_, c, r * 64:(r + 1) * 64], rhs=cps[d_][0][:, r, :], start=False, stop=(step == 3), tile_position=tp)
                        return ins
                    rd = [RA_b]
                    for d in range(2):
                        rd += [gts[d][1], cps[d][1], xds[d][1]]
                    S.op("pe", mmy, reads=rd, writes=pys[0][1])
                    for i in range(2):
                        py = pys[i][0]
                        S.op("dve", lambda e, py=py, i=i, g=g, tsl=tsl, yz_t=yz_t, cl=cl: e.scalar_tensor_tensor(
                            out=yz_t[:, i, cl * 128:(cl + 1) * 128], in0=xc[:, i, tsl], scalar=dcol[:, 2 * g + i:2 * g + i + 1], in1=py,
                            op0=ALU.mult, op1=ALU.add), reads=[xc_b, dcol_b] + pys[i][1], writes=[yz_b])
                if "O" in P2SKIP:
                    continue
                q_t, q_b = sqy[0]
                r_t, r_b = rsy[0]
                o_t, o_b = ybo[0]
                S.op("dve", lambda e, yz_t=yz_t, sz_t=sz_t: e.tensor_tensor(out=yz_t, in0=yz_t, in1=sz_t, op=ALU.mult), reads=[yz_b, sz_b], writes=[yz_b])
                S.op("act", lambda e, yz_t=yz_t, q_t=q_t: e.activation(out=q_t, in_=yz_t, func=AF.Square), reads=[yz_b], writes=[q_b])
                pp, pp_b = self.pq(pi, 0, 4)

                def mmq(e, pp=pp, q_t=q_t):
                    e.matmul(pp, lhsT=ones_bf, rhs=q_t[:, 0, :], start=True, stop=False)
                    return e.matmul(pp, lhsT=ones_bf, rhs=q_t[:, 1, :], start=False, stop=True)
                S.op("pe", mmq, reads=[q_b, ones_bf_b], writes=pp_b)
                S.op("act", lambda e, pp=pp, r_t=r_t: e.activation(out=r_t, in_=pp, func=AF.Sqrt, scale=1.0 / 256, bias=EPS), reads=pp_b, writes=[r_b])
                S.op("dve", lambda e, r_t=r_t: e.reciprocal(out=r_t, in_=r_t), reads=[r_b], writes=[r_b])
                for i in range(2):
                    S.op("dve", lambda e, i=i, yz_t=yz_t, r_t=r_t, o_t=o_t, g=g: e.scalar_tensor_tensor(
                        out=o_t[:, i, :], in0=yz_t[:, i, :], scalar=nwc[:, 2 * g + i:2 * g + i + 1], in1=r_t, op0=ALU.mult, op1=ALU.mult),
                        reads=[yz_b, r_b, nwc_b], writes=[o_b])
                    r0 = 256 * g + 128 * i
                    S.op("sp", lambda e, o_t=o_t, i=i, r0=r0, cb4=cb4: e.dma_start(out=self.yb_s[r0:r0 + 128, cb4 * 512:(cb4 + 1) * 512], in_=o_t[:, i, :]),
                         reads=[o_b], writes=[self.yb_sb], dma=True)
        S.barrier()
        ar.release()


    def trig(self, ar, src, n, add, out, out_b, src_b, tag):
        S = self.S
        t, t_b = ar.alloc([n], F32, f"trg_t{tag}")
        ti, ti_b = ar.alloc([n], I32, f"trg_i{tag}")
        S.op("dve", lambda e: e.tensor_scalar(out=t, in0=src, scalar1=64.0 + add, scalar2=None, op0=ALU.add), reads=[src_b], writes=[t_b])
        S.op("dve", lambda e: e.tensor_copy(out=ti, in_=t), reads=[t_b], writes=[ti_b])
        S.op("dve", lambda e: e.tensor_copy(out=out, in_=ti), reads=[ti_b], writes=[out_b])
        S.op("dve", lambda e: e.tensor_tensor(out=t, in0=t, in1=out, op=ALU.subtract), reads=[t_b, out_b], writes=[t_b])
        S.op("act", lambda e: e.activation(out=out, in_=t, func=AF.Sin, scale=2.0 * np.pi), reads=[t_b], writes=[out_b])

    def cpow_tables(self, ar, lre, lim, ldt, n, bufs, tag):
        S = self.S
        TB = Buf(f"cpow{tag}")
        step, _ = ar.alloc([n], F32, "step")
        lr, _ = ar.alloc([n], F32, "lr")
        th, _ = ar.alloc([n], F32, "th")
        S.op("act", lambda e: e.activation(out=step, in_=ldt, func=AF.Exp), reads=bufs, writes=[TB])
        S.op("dve", lambda e: e.tensor_tensor(out=lr, in0=lre, in1=step, op=ALU.mult), reads=bufs + [TB], writes=[TB])
        S.op("dve", lambda e: e.scalar_tensor_tensor(out=th, in0=lim, scalar=1.0 / (2.0 * np.pi), in1=step, op0=ALU.mult, op1=ALU.mult),
             reads=bufs + [TB], writes=[TB])
        are, _ = ar.alloc([9, n], F32, "are")
        aim, _ = ar.alloc([9, n], F32, "aim")
        mag, _ = ar.alloc([n], F32, "mag")
        ph, _ = ar.alloc([n], F32, "ph")
        ck, ck_b = ar.alloc([n], F32, "ck")
        sk, sk_b = ar.alloc([n], F32, "sk")
        for k in range(9):
            ar.mark()
            S.op("act", lambda e, k=k: e.activation(out=mag, in_=lr, func=AF.Exp, scale=float(k)), reads=[TB], writes=[TB])
            S.op("dve", lambda e, k=k: e.tensor_scalar(out=ph, in0=th, scalar1=float(k), scalar2=None, op0=ALU.mult), reads=[TB], writes=[TB])
            self.trig(ar, ph, n, 0.25, ck, ck_b, TB, f"{tag}c{k}")
            self.trig(ar, ph, n, 0.0, sk, sk_b, TB, f"{tag}s{k}")
            S.op("dve", lambda e, k=k: e.tensor_tensor(out=are[:, k, :], in0=mag, in1=ck, op=ALU.mult), reads=[TB, ck_b], writes=[TB])
            S.op("dve", lambda e, k=k: e.tensor_tensor(out=aim[:, k, :], in0=mag, in1=sk, op=ALU.mult), reads=[TB, sk_b], writes=[TB])
            ar.release()
        return dict(are=are, aim=aim, lr=lr, th=th, buf=TB)

    def cmul(self, eng, out_re, out_im, a_re, a_im, b_re, b_im, t1, t2, reads, writes):
        S = self.S
        S.op(eng, lambda e: e.tensor_tensor(out=t1, in0=a_re, in1=b_re, op=ALU.mult), reads=reads, writes=writes)
        S.op(eng, lambda e: e.tensor_tensor(out=t2, in0=a_im, in1=b_im, op=ALU.mult), reads=reads, writes=writes)
        S.op(eng, lambda e: e.tensor_tensor(out=out_re, in0=t1, in1=t2, op=ALU.subtract), reads=reads, writes=writes)
        S.op(eng, lambda e: e.tensor_tensor(out=t1, in0=a_re, in1=b_im, op=ALU.mult), reads=reads, writes=writes)
        S.op(eng, lambda e: e.tensor_tensor(out=t2, in0=a_im, in1=b_re, op=ALU.mult), reads=reads, writes=writes)
        S.op(eng, lambda e: e.tensor_tensor(out=out_im, in0=t1, in1=t2, op=ALU.add), reads=reads, writes=writes)

    def bc_coef(self, ar, pw, lre, lim, n, bufs):
        S = self.S
        TB = pw["buf"]
        rd = bufs + [TB]
        nre, _ = ar.alloc([n], F32, "nre")
        den, _ = ar.alloc([n], F32, "den")
        t1, _ = ar.alloc([n], F32, "bct1")
        bcr, _ = ar.alloc([n], F32, "bcr")
        bci, _ = ar.alloc([n], F32, "bci")
        are1, aim1 = pw["are"][:, 1, :], pw["aim"][:, 1, :]
        S.op("dve", lambda e: e.tensor_scalar(out=nre, in0=are1, scalar1=-1.0, scalar2=None, op0=ALU.add), reads=rd, writes=[TB])
        S.op("dve", lambda e: e.tensor_tensor(out=den, in0=lre, in1=lre, op=ALU.mult), reads=rd, writes=[TB])
        S.op("dve", lambda e: e.tensor_tensor(out=t1, in0=lim, in1=lim, op=ALU.mult), reads=rd, writes=[TB])
        S.op("dve", lambda e: e.tensor_tensor(out=den, in0=den, in1=t1, op=ALU.add), reads=rd, writes=[TB])
        S.op("dve", lambda e: e.reciprocal(out=den, in_=den), reads=rd, writes=[TB])
        S.op("dve", lambda e: e.tensor_tensor(out=bcr, in0=nre, in1=lre, op=ALU.mult), reads=rd, writes=[TB])
        S.op("dve", lambda e: e.tensor_tensor(out=t1, in0=aim1, in1=lim, op=ALU.mult), reads=rd, writes=[TB])
        S.op("dve", lambda e: e.tensor_tensor(out=bcr, in0=bcr, in1=t1, op=ALU.add), reads=rd, writes=[TB])
        S.op("dve", lambda e: e.tensor_tensor(out=bcr, in0=bcr, in1=den, op=ALU.mult), reads=rd, writes=[TB])
        S.op("dve", lambda e: e.tensor_tensor(out=bci, in0=aim1, in1=lre, op=ALU.mult), reads=rd, writes=[TB])
        S.op("dve", lambda e: e.tensor_tensor(out=t1, in0=nre, in1=lim, op=ALU.mult), reads=rd, writes=[TB])
        S.op("dve", lambda e: e.tensor_tensor(out=bci, in0=bci, in1=t1, op=ALU.subtract), reads=rd, writes=[TB])
        S.op("dve", lambda e: e.tensor_tensor(out=bci, in0=bci, in1=den, op=ALU.mult), reads=rd, writes=[TB])
        return bcr, bci

    def phase3_setup(self, s5F, s5S):
        nc, S, ar = self.nc, self.S, self.ar
        dbg = self.debug
        ident_f, ident_f_b = self.consts["ident_f"]
        self.SI_s, self.SI_sb = self.scratch("SI_s", [2, 8, 128, 2048], BF16, dump=dbg)
        self.RO_s, self.RO_sb = self.scratch("RO_s", [2, 8, 128, 2048], BF16, dump=dbg)
        self.FIR_s, self.FIR_sb = self.scratch("FIR_s", [2, 8, 128, 1024], BF16, dump=dbg)
        self.RS, self.RS_b = ar.alloc([2, 2, 32], F32, "RS")
        mF = [ar.alloc([1], F32, f"mF{i}") for i in range(2)]
        mS = [ar.alloc([1], F32, f"mS{i}") for i in range(2)]
        mSn = [ar.alloc([1], F32, f"mSn{i}") for i in range(2)]
        self.rowmask = [ar.alloc([1], F32, f"rowm{i}") for i in range(4)]
        for i in range(4):
            S.op("dve", lambda e, i=i: e.reduce_sum(out=self.rowmask[i][0], in_=ident_f[:, 32 * i:32 * i + 32], axis=AX.X),
                 reads=[ident_f_b], writes=[self.rowmask[i][1]])
        for i in range(2):
            S.op("dve", lambda e, i=i: e.reduce_sum(out=mS[i][0], in_=ident_f[:, 64 * i:64 * i + 64], axis=AX.X), reads=[ident_f_b], writes=[mS[i][1]])
            S.op("dve", lambda e, i=i: e.tensor_scalar(out=mSn[i][0], in0=mS[i][0], scalar1=-1.0, scalar2=None, op0=ALU.mult), reads=[mS[i][1]], writes=[mSn[i][1]])
            S.op("dve", lambda e, i=i: e.reduce_sum(out=mF[i][0], in_=ident_f.rearrange("p (a b c) -> p a b c", a=4, b=2)[:, :, i, :], axis=AX.XY),
                 reads=[ident_f_b], writes=[mF[i][1]])
        for d in range(2):
            ar.mark()
            Ft, Ft_b = ar.alloc([5, 512], F32, "Ft")
            S.op("sp", lambda e, d=d: e.dma_start(out=Ft, in_=s5F[d].rearrange("p a f q -> p a (f q)")), writes=[Ft_b], dma=True)
            pw = self.cpow_tables(ar, Ft[:, 0, :], Ft[:, 1, :], Ft[:, 2, :], 512, [Ft_b], f"F{d}")
            TB = pw["buf"]
            bcr, bci = self.bc_coef(ar, pw, Ft[:, 0, :], Ft[:, 1, :], 512, [Ft_b])
            t1, _ = ar.alloc([512], F32, "ft1")
            t2, _ = ar.alloc([512], F32, "ft2")
            bbr, _ = ar.alloc([512], F32, "bbr")
            bbi, _ = ar.alloc([512], F32, "bbi")
            self.cmul("dve", bbr, bbi, bcr, bci, Ft[:, 3, :], Ft[:, 4, :], t1, t2, [Ft_b, TB], [TB])
            wr_, _ = ar.alloc([512], F32, "wr_")
            wi_, _ = ar.alloc([512], F32, "wi_")
            SIt, SIt_b = ar.alloc([8, 8, 2, 2, 64], BF16, "SIt")
            for j in range(8):
                kj = 7 - j if d == 0 else j
                self.cmul("dve", wr_, wi_, pw["are"][:, kj, :], pw["aim"][:, kj, :], bbr, bbi, t1, t2, [TB], [TB])
                for ri, w_ in enumerate((wr_, wi_)):
                    for gq in range(2):
                        S.op("dve", lambda e, j=j, ri=ri, gq=gq, w_=w_: e.tensor_scalar(
                            out=SIt[:, :, j, ri, gq, :], in0=w_.rearrange("p (f q) -> p f q", f=8), scalar1=mF[gq][0][:, 0:1], scalar2=None, op0=ALU.mult),
                            reads=[TB, mF[gq][1]], writes=[SIt_b])
            S.op("sp", lambda e, d=d: e.dma_start(out=self.SI_s[d].rearrange("f p n -> p f n"), in_=SIt.rearrange("p f j r g q -> p f (j r g q)")),
                 reads=[SIt_b], writes=[self.SI_sb], dma=True)
            S.barrier()
            ar.release()
            ar.mark()
            NS = 3 * 32 + 4 * 512
            St, St_b = ar.alloc([NS], F32, "St")
            S.op("sp", lambda e, d=d: e.dma_start(out=St, in_=s5S[d]), writes=[St_b], dma=True)
            lre, lim, ldt = St[:, 0:32], St[:, 32:64], St[:, 64:96]
            Bre = St[:, 96:96 + 512].rearrange("p (g h) -> p g h", h=16)
            Bim = St[:, 96 + 512:96 + 1024].rearrange("p (g h) -> p g h", h=16)
            Cre = St[:, 96 + 1024:96 + 1536].rearrange("p (g h) -> p g h", h=16)
            Cim = St[:, 96 + 1536:96 + 2048].rearrange("p (g h) -> p g h", h=16)
            pw = self.cpow_tables(ar, lre, lim, ldt, 32, [St_b], f"S{d}")
            TB = pw["buf"]
            bcr, bci = self.bc_coef(ar, pw, lre, lim, 32, [St_b])
            bc3 = lambda a: a.unsqueeze(2).broadcast_to([128, 32, 16])
            t1, _ = ar.alloc([32, 16], F32, "st1")
            t2, _ = ar.alloc([32, 16], F32, "st2")
            bbr, _ = ar.alloc([32, 16], F32, "sbbr")
            bbi, _ = ar.alloc([32, 16], F32, "sbbi")
            self.cmul("dve", bbr, bbi, bc3(bcr), bc3(bci), Bre, Bim, t1, t2, [St_b, TB], [TB])
            S.op("act", lambda e, d=d: e.activation(out=self.RS[:, d, 0, :], in_=pw["lr"], func=AF.Exp, scale=8.0), reads=[TB], writes=[self.RS_b])
            p8, p8_b = ar.alloc([32], F32, "p8")
            p8i, p8i_b = ar.alloc([32], I32, "p8i")
            p8f, p8f_b = ar.alloc([32], F32, "p8f")
            S.op("dve", lambda e: e.tensor_scalar(out=p8, in0=pw["th"], scalar1=8.0, scalar2=64.0, op0=ALU.mult, op1=ALU.add), reads=[TB], writes=[p8_b])
            S.op("dve", lambda e: e.tensor_copy(out=p8i, in_=p8), reads=[p8_b], writes=[p8i_b])
            S.op("dve", lambda e: e.tensor_copy(out=p8f, in_=p8i), reads=[p8i_b], writes=[p8f_b])
            S.op("dve", lambda e, d=d: e.tensor_tensor(out=self.RS[:, d, 1, :], in0=p8, in1=p8f, op=ALU.subtract), reads=[p8_b, p8f_b], writes=[self.RS_b])
            vr_, _ = ar.alloc([32, 16], F32, "vr_")
            vi_, _ = ar.alloc([32, 16], F32, "vi_")
            ROt, ROt_b = ar.alloc([32, 8, 2, 2, 16], BF16, "ROt")
            for j in range(8):
                kj = j + 1 if d == 0 else 8 - j
                self.cmul("dve", vr_, vi_, Cre, Cim, bc3(pw["are"][:, kj, :]), bc3(pw["aim"][:, kj, :]), t1, t2, [St_b, TB], [TB])
                for ri, (v_, ms) in enumerate(((vr_, mS), (vi_, mSn))):
                    for gq in range(2):
                        S.op("dve", lambda e, j=j, ri=ri, gq=gq, v_=v_, ms=ms: e.tensor_scalar(
                            out=ROt[:, :, j, ri, gq, :], in0=v_, scalar1=ms[gq][0][:, 0:1], scalar2=None, op0=ALU.mult),
                            reads=[TB, ms[gq][1]], writes=[ROt_b])
            S.op("sp", lambda e, d=d: e.dma_start(out=self.RO_s[d].rearrange("f p n -> p f n"),
                                                in_=ROt.rearrange("p (f g) j r q h -> p f (g j r q h)", f=8)),
                 reads=[ROt_b], writes=[self.RO_sb], dma=True)
            W2p = [ar.alloc([8, 4, 8, 16], BF16, f"W2p{ri}") for ri in range(2)]
            W1p = [[ar.alloc([8, 4, 8, 16], BF16, f"W1p{b}{ri}") for ri in range(2)] for b in range(2)]
            for t_, tb_ in W2p + W1p[0] + W1p[1]:
                S.op("dve", lambda e, t_=t_: e.memset(t_, 0.0), writes=[tb_])
            g4 = lambda a: a.rearrange("p (f g) h -> p f g h", g=4)
            for ri, (c_, ms) in enumerate(((Cre, mS), (Cim, mSn))):
                for gp4 in range(4):
                    for gq in range(2):
                        S.op("dve", lambda e, ri=ri, gp4=gp4, gq=gq, c_=c_, ms=ms: e.tensor_scalar(
                            out=W2p[ri][0][:, :, gp4, 2 * gp4 + gq, :], in0=g4(c_)[:, :, gp4, :], scalar1=ms[gq][0][:, 0:1], scalar2=None, op0=ALU.mult),
                            reads=[St_b, ms[gq][1]], writes=[W2p[ri][1]])
            FIRt, FIRt_b = ar.alloc([8, 8, 128], BF16, "FIRt")
            w1r, _ = ar.alloc([32, 16], F32, "w1r")
            w1i, _ = ar.alloc([32, 16], F32, "w1i")
            cnt = 0
            for tau in range(8):
                self.cmul("dve", w1r, w1i, bc3(pw["are"][:, tau, :]), bc3(pw["aim"][:, tau, :]), bbr, bbi, t1, t2, [TB], [TB])
                W1 = W1p[tau % 2]
                for ri, w_ in enumerate((w1r, w1i)):
                    for gp4 in range(4):
                        for gq in range(2):
                            S.op("dve", lambda e, ri=ri, gp4=gp4, gq=gq, w_=w_, W1=W1: e.tensor_scalar(
                                out=W1[ri][0][:, :, gp4, 2 * gp4 + gq, :], in0=g4(w_)[:, :, gp4, :], scalar1=mS[gq][0][:, 0:1], scalar2=None, op0=ALU.mult),
                                reads=[TB, mS[gq][1]], writes=[W1[ri][1]])
                for fc in range(8):
                    pa, pb = self.pq(cnt % 8, 0)
                    cnt += 1

                    def mm(e, pa=pa, fc=fc, W1=W1):
                        ins = None
                        k = 0
                        for ri in range(2):
                            for gp4 in range(4):
                                ins = e.matmul(pa, lhsT=W1[ri][0][:, fc, gp4, :, :].rearrange("p a b -> p (a b)"),
                                               rhs=W2p[ri][0][:, fc, gp4, :, :].rearrange("p a b -> p (a b)"), start=(k == 0), stop=(k == 7))
                                k += 1
                        return ins
                    S.op("pe", mm, reads=[W1[0][1], W1[1][1], W2p[0][1], W2p[1][1]], writes=pb)
                    eng = "act" if cnt % 2 == 0 else "dve"
                    if eng == "act":
                        S.op("act", lambda e, pa=pa, fc=fc, tau=tau: e.activation(out=FIRt[:, fc, tau, :], in_=pa, func=AF.Copy), reads=pb, writes=[FIRt_b])
                    else:
                        S.op("dve", lambda e, pa=pa, fc=fc, tau=tau: e.tensor_copy(out=FIRt[:, fc, tau, :], in_=pa), reads=pb, writes=[FIRt_b])
            S.op("sp", lambda e, d=d: e.dma_start(out=self.FIR_s[d].rearrange("f p n -> p f n"), in_=FIRt.rearrange("p f t n -> p f (t n)")),
                 reads=[FIRt_b], writes=[self.FIR_sb], dma=True)
            S.barrier()
            ar.release()
        if dbg:
            o, ob = self.scratch("dbg_RS", [128, 128], F32, dump=True)
            S.op("sp", lambda e: e.dma_start(out=o, in_=self.RS.rearrange("p a b c -> p (a b c)")), reads=[self.RS_b], writes=[ob], dma=True)


    def phase3_main(self, s5_d):
        nc, S, ar = self.nc, self.S, self.ar
        ps = self.ps
        dbg = self.debug
        self.g_s, self.g_sb = self.scratch("g_s", [D, L], BF16, dump=dbg)
        RS, RS_b = self.RS, self.RS_b
        NCH = 544
        ar.mark()
        dcol, dcol_b = ar.alloc([8], F32, "s5d")
        S.op("sp", lambda e: e.dma_start(out=dcol, in_=s5_d), writes=[dcol_b], dma=True)
        iot_i, iot_ib = ar.alloc([NCH], I32, "iota_i")
        iot, iot_b = ar.alloc([NCH], F32, "iota_f")
        S.op("pool", lambda e: e.iota(iot_i, pattern=[[1, NCH]], base=0, channel_multiplier=0), writes=[iot_ib])
        S.op("dve", lambda e: e.tensor_copy(out=iot, in_=iot_i), reads=[iot_ib], writes=[iot_b])
        u_t, u_b = ar.alloc([T], BF16, "u_fc")
        um, um_b = ar.alloc([4, T], BF16, "um")
        SI_t, SI_b = ar.alloc([2, 2048], BF16, "SI_t")
        RO_t, RO_b = ar.alloc([2, 2048], BF16, "RO_t")
        FIR_t, FIR_b = ar.alloc([2, 1024], BF16, "FIR_t")
        cn, cn_b = ar.alloc([4, NCH], F32, "cn")
        sn, sn_b = ar.alloc([4, NCH], F32, "sn")
        pht, pht_b = ar.alloc([4, NCH], F32, "pht")
        phi_, phi_b = ar.alloc([4, NCH], I32, "phi_")
        self._s5_frac, self._s5_frac_b = ar.alloc([4, NCH], F32, "s5frac")
        Ssb = [[ar.alloc([NCH], F32, f"Ssb{k}{ri}") for ri in range(2)] for k in range(2)]
        V = [ar.alloc([NCH], F32, f"V{ri}") for ri in range(2)]
        W = [ar.alloc([NCH], F32, f"W{ri}") for ri in range(2)]
        tmp = [ar.alloc([NCH], F32, f"s5t{i}") for i in range(4)]
        Zp = [[[ar.alloc([512], BF16, f"Zp{d}{g}{ri}") for ri in range(2)] for g in range(4)] for d in range(2)]
        gst = [ar.alloc([L], BF16, f"gst{i}") for i in range(2)]
        ys = [ar.alloc([2, 256], F32, f"ys{i}") for i in range(2)]
        kset = 0
        for fc in range(8):
            S.op("sp", lambda e, fc=fc: e.dma_start(out=u_t, in_=self.s5u[fc * 128:(fc + 1) * 128, :]), reads=[self.s5u_b], writes=[u_b], dma=True)
            S.op("sp", lambda e, fc=fc: e.dma_start(out=SI_t, in_=self.SI_s[:, fc].rearrange("d p n -> p d n")), reads=[self.SI_sb], writes=[SI_b], dma=True)
            S.op("sp", lambda e, fc=fc: e.dma_start(out=RO_t, in_=self.RO_s[:, fc].rearrange("d p n -> p d n")), reads=[self.RO_sb], writes=[RO_b], dma=True)
            S.op("sp", lambda e, fc=fc: e.dma_start(out=FIR_t, in_=self.FIR_s[:, fc].rearrange("d p n -> p d n")), reads=[self.FIR_sb], writes=[FIR_b], dma=True)
            for i in range(4):
                S.op("act", lambda e, i=i: e.activation(out=um[:, i, :], in_=u_t, func=AF.Copy, scale=self.rowmask[i][0][:, 0:1]),
                     reads=[u_b, self.rowmask[i][1]], writes=[um_b])
            for d in range(2):
                S.op("dve", lambda e, d=d, fc=fc: e.tensor_tensor(
                    out=pht, in0=iot.unsqueeze(1).broadcast_to([128, 4, NCH]),
                    in1=RS[:, d, 1, 4 * fc:4 * fc + 4].unsqueeze(2).broadcast_to([128, 4, NCH]), op=ALU.mult),
                    reads=[iot_b, RS_b], writes=[pht_b])
                for (dst, dst_b, add) in ((cn, cn_b, 0.25), (sn, sn_b, 0.0)):
                    S.op("dve", lambda e, dst=dst, add=add: e.tensor_scalar(out=dst, in0=pht, scalar1=64.0 + add, scalar2=None, op0=ALU.add),
                         reads=[pht_b], writes=[dst_b])
                    S.op("dve", lambda e, dst=dst: e.tensor_copy(out=phi_, in_=dst), reads=[dst_b], writes=[phi_b])
                    S.op("dve", lambda e, dst=dst: e.tensor_copy(out=self._s5_frac, in_=phi_), reads=[phi_b], writes=[self._s5_frac_b])
                    S.op("dve", lambda e, dst=dst: e.tensor_tensor(out=dst, in0=dst, in1=self._s5_frac, op=ALU.subtract),
                         reads=[dst_b, self._s5_frac_b], writes=[dst_b])
                    S.op("act", lambda e, dst=dst: e.activation(out=dst, in_=dst, func=AF.Sin, scale=2.0 * np.pi), reads=[dst_b], writes=[dst_b])
                for gp4 in range(4):
                    gp = 4 * fc + gp4
                    b0 = 3 * (kset % 2)
                    kset += 1
                    bufs3 = [self.psb[b0], self.psb[b0 + 1], self.psb[b0 + 2]]

                    def mm(e, d=d, gp4=gp4, b0=b0):
                        ins = None
                        for ri in range(2):
                            for j in range(8):
                                ins = e.matmul(ps[b0 + ri][:, 0:512], lhsT=SI_t[:, d, (j * 2 + ri) * 128:(j * 2 + ri + 1) * 128],
                                               rhs=um[:, gp4, LC + j:T:8], start=(j == 0), stop=(j == 7))
                        for ri in range(2):
                            for j in range(8):
                                ins = e.matmul(ps[b0 + 2][:, ri * 32:(ri + 1) * 32], lhsT=SI_t[:, d, (j * 2 + ri) * 128:(j * 2 + ri + 1) * 128],
                                               rhs=um[:, gp4, j:LC:8], start=(j == 0), stop=(j == 7))
                        return ins
                    S.op("pe", mm, reads=[SI_b, um_b], writes=bufs3)
                    Sk = Ssb[kset % 2]
                    for ri in range(2):
                        s_t, s_b = Sk[ri]
                        src_c = ps[b0 + 2][:, ri * 32:(ri + 1) * 32]
                        src_l = ps[b0 + ri][:, 0:512]
                        if d == 1:
                            src_c = src_c[:, ::-1]
                            src_l = src_l[:, ::-1]
                        S.op("act", lambda e, s_t=s_t, src_c=src_c: e.activation(out=s_t[:, 0:32], in_=src_c, func=AF.Copy),
                             reads=[self.psb[b0 + 2]], writes=[s_b])
                        S.op("act", lambda e, s_t=s_t, src_l=src_l: e.activation(out=s_t[:, 32:NCH], in_=src_l, func=AF.Copy),
                             reads=[self.psb[b0 + ri]], writes=[s_b])
                    (Sr, Sr_b), (Si, Si_b) = Sk
                    cnv, snv = cn[:, gp4, :], sn[:, gp4, :]
                    (t1, t1b), (t2, t2b), (t3, t3b), (t4, t4b) = tmp
                    (Vr, Vr_b), (Vi, Vi_b) = V
                    (Wr, Wr_b), (Wi, Wi_b) = W
                    S.op("dve", lambda e, cnv=cnv, Sr=Sr: e.tensor_tensor(out=t1, in0=cnv, in1=Sr, op=ALU.mult), reads=[cn_b, Sr_b], writes=[t1b])
                    S.op("dve", lambda e, snv=snv, Si=Si: e.tensor_tensor(out=t2, in0=snv, in1=Si, op=ALU.mult), reads=[sn_b, Si_b], writes=[t2b])
                    S.op("dve", lambda e, cnv=cnv, Si=Si: e.tensor_tensor(out=t3, in0=cnv, in1=Si, op=ALU.mult), reads=[cn_b, Si_b], writes=[t3b])
                    S.op("dve", lambda e, snv=snv, Sr=Sr: e.tensor_tensor(out=t4, in0=snv, in1=Sr, op=ALU.mult), reads=[sn_b, Sr_b], writes=[t4b])
                    S.op("dve", lambda e: e.tensor_tensor(out=Vr, in0=t1, in1=t2, op=ALU.add), reads=[t1b, t2b], writes=[Vr_b])
                    S.op("dve", lambda e: e.tensor_tensor(out=Vi, in0=t3, in1=t4, op=ALU.subtract), reads=[t3b, t4b], writes=[Vi_b])
                    Rb = RS[:, d, 0, gp:gp + 1].broadcast_to([128, NCH])
                    S.op("dve", lambda e, Rb=Rb: e.tensor_tensor_scan(out=Wr, data0=Rb, data1=Vr, initial=0.0, op0=ALU.mult, op1=ALU.add),
                         reads=[RS_b, Vr_b], writes=[Wr_b])
                    S.op("dve", lambda e, Rb=Rb: e.tensor_tensor_scan(out=Wi, data0=Rb, data1=Vi, initial=0.0, op0=ALU.mult, op1=ALU.add),
                         reads=[RS_b, Vi_b], writes=[Wi_b])
                    sl = slice(31, 543)
                    (zr, zr_b), (zi, zi_b) = Zp[d][gp4]
                    zro = zr if d == 0 else zr[:, ::-1]
                    zio = zi if d == 0 else zi[:, ::-1]
                    S.op("dve", lambda e, cnv=cnv: e.tensor_tensor(out=t1[:, sl], in0=cnv[:, sl], in1=Wr[:, sl], op=ALU.mult), reads=[cn_b, Wr_b], writes=[t1b])
                    S.op("dve", lambda e, snv=snv: e.tensor_tensor(out=t2[:, sl], in0=snv[:, sl], in1=Wi[:, sl], op=ALU.mult), reads=[sn_b, Wi_b], writes=[t2b])
                    S.op("dve", lambda e, cnv=cnv: e.tensor_tensor(out=t3[:, sl], in0=cnv[:, sl], in1=Wi[:, sl], op=ALU.mult), reads=[cn_b, Wi_b], writes=[t3b])
                    S.op("dve", lambda e, snv=snv: e.tensor_tensor(out=t4[:, sl], in0=snv[:, sl], in1=Wr[:, sl], op=ALU.mult), reads=[sn_b, Wr_b], writes=[t4b])
                    S.op("dve", lambda e, zro=zro: e.tensor_tensor(out=zro, in0=t1[:, sl], in1=t2[:, sl], op=ALU.subtract), reads=[t1b, t2b], writes=[zr_b])
                    S.op("dve", lambda e, zio=zio: e.tensor_tensor(out=zio, in0=t3[:, sl], in1=t4[:, sl], op=ALU.add), reads=[t3b, t4b], writes=[zi_b])
            g_t, g_b = gst[fc % 2]
            for hb in range(2):
                c0 = 256 * hb
                for j in range(8):
                    bank = 4 + j // 2
                    reg = ps[bank][:, (j % 2) * 256:(j % 2) * 256 + 256]

                    def mmy(e, j=j, reg=reg, c0=c0):
                        ops = []
                        for tau in range(0, j + 1):
                            st0 = LC + 8 * c0 + (j - tau)
                            ops.append((reg, FIR_t[:, 0, tau * 128:(tau + 1) * 128], u_t[:, st0:st0 + 2041:8], None))
                        for tau in range(0, 8 - j):
                            st0 = LC + 8 * c0 + (j + tau)
                            ops.append((reg, FIR_t[:, 1, tau * 128:(tau + 1) * 128], u_t[:, st0:st0 + 2041:8], None))
                        for d in range(2):
                            for ri in range(2):
                                for gp4 in range(4):
                                    o0 = ((gp4 * 8 + j) * 2 + ri) * 32
                                    ops.append((reg[32 * gp4:32 * gp4 + 32, :], RO_t[:, d, o0:o0 + 32], Zp[d][gp4][ri][0][:, c0:c0 + 256], (0, 32 * gp4)))
                        ins = None
                        for k, (o_, l_, r_, tp) in enumerate(ops):
                            if tp is None:
                                ins = e.matmul(o_, lhsT=l_, rhs=r_, start=(k == 0), stop=(k == len(ops) - 1))
                            else:
                                ins = e.matmul(o_, lhsT=l_, rhs=r_, start=(k == 0), stop=(k == len(ops) - 1), tile_position=tp)
                        return ins
                    rd = [FIR_b, RO_b, u_b] + [Zp[d][g][ri][1] for d in range(2) for g in range(4) for ri in range(2)]
                    S.op("pe", mmy, reads=rd, writes=[self.psb[bank]])
                    if j % 2 == 1:
                        j0 = j - 1
                        y_t, y_b = ys[(j // 2) % 2]
                        base = LC + 8 * c0
                        uv = u_t[:, base:base + 2048].rearrange("p (c j) -> p j c", j=8)[:, j0:j0 + 2, :]
                        S.op("dve", lambda e, y_t=y_t, uv=uv, bank=bank, fc=fc: e.scalar_tensor_tensor(
                            out=y_t, in0=uv, scalar=dcol[:, fc:fc + 1], in1=ps[bank][:].rearrange("p (j c) -> p j c", j=2),
                            op0=ALU.mult, op1=ALU.add), reads=[u_b, dcol_b, self.psb[bank]], writes=[y_b])
                        outv = g_t.rearrange("p (c8 j row) -> p j row c8", c8=8, j=8)[:, j0:j0 + 2, 32 * hb:32 * hb + 32, :]
                        S.op("act", lambda e, y_t=y_t, outv=outv: e.activation(
                            out=outv, in_=y_t.rearrange("p j (r c8) -> p j r c8", c8=8), func=AF.Gelu_apprx_tanh),
                            reads=[y_b], writes=[g_b])
            S.op("sp", lambda e, fc=fc, g_t=g_t: e.dma_start(out=self.g_s[fc * 128:(fc + 1) * 128, :], in_=g_t), reads=[g_b], writes=[self.g_sb], dma=True)
        S.barrier()
        ar.release()


    def phase4(self, glu_w, glu_b, w_a, w_b, w_o, x_tok, nfw, r_w, r_b):
        nc, S, ar = self.nc, self.S, self.ar
        ps, psb = self.ps, self.psb
        dbg = self.debug
        ident_f, ident_f_b = self.consts["ident_f"]
        self.h1_s, self.h1_sb = self.scratch("h1_s", [L, D], F32, dump=dbg)
        self.uf_s, self.uf_sb = self.scratch("uf_s", [L, D], BF16, dump=dbg)
        self.logits, self.logits_b = ar.alloc([32, 32], F32, "logits")
        ar.mark()
        wv = lambda w: w.rearrange("(kc p) n -> p kc n", p=128)
        Wg, Wg_b = ar.alloc([8, D], BF16, "Wglu")
        Wa, Wa_b = ar.alloc([8, D], BF16, "Wa")
        Wb, Wb_b = ar.alloc([16, D], BF16, "Wb")
        Wo, Wo_b = ar.alloc([8, D], BF16, "Wo")
        for (t_, tb_, src, nk) in ((Wg, Wg_b, glu_w, 8), (Wa, Wa_b, w_a, 8), (Wb, Wb_b, w_b, 16), (Wo, Wo_b, w_o, 8)):
            for k0 in range(0, nk, 4):
                S.op("pool", lambda e, t_=t_, src=src, k0=k0: e.dma_start(out=t_[:, k0:k0 + 4, :], in_=wv(src)[:, k0:k0 + 4, :]), writes=[tb_], dma=True)
        Wr, Wr_b = ar.alloc([8, 32], F32, "Wr")
        S.op("sp", lambda e: e.dma_start(out=Wr, in_=r_w.rearrange("(kc p) n -> p kc n", p=128)), writes=[Wr_b], dma=True)
        rb_rep, rb_rep_b = ar.alloc([32], F32, "rb_rep")
        S.op("sp", lambda e: e.dma_start(out=rb_rep, in_=r_b.broadcast_to([128, 32])), writes=[rb_rep_b], dma=True)
        gb_col, gb_col_b = ar.alloc([8], F32, "glu_b")
        S.op("sp", lambda e: e.dma_start(out=gb_col, in_=glu_b), writes=[gb_col_b], dma=True)
        gm_rep, gm_rep_b = ar.alloc([D], F32, "gm_rep")
        Af_rep, Af_rep_b = ar.alloc([D], F32, "Af_rep")
        Bf_rep, Bf_rep_b = ar.alloc([D], F32, "Bf_rep")
        S.op("sp", lambda e: e.dma_start(out=gm_rep, in_=self.mod_s[:, 2 * D:3 * D]), reads=[self.mod_sb], writes=[gm_rep_b], dma=True)
        S.op("sp", lambda e: e.dma_start(out=Bf_rep, in_=self.mod_s[:, 3 * D:4 * D]), reads=[self.mod_sb], writes=[Bf_rep_b], dma=True)
        S.op("sp", lambda e: e.dma_start(out=Af_rep, in_=self.mod_s[:, 4 * D:5 * D]), reads=[self.mod_sb], writes=[Af_rep_b], dma=True)
        nf_rep, nf_rep_b = ar.alloc([D], F32, "nf_rep")
        S.op("sp", lambda e: e.dma_start(out=nf_rep, in_=nfw.broadcast_to([128, D])), writes=[nf_rep_b], dma=True)
        S.op("dve", lambda e: e.scalar_tensor_tensor(out=Af_rep, in0=Af_rep, scalar=1.0, in1=nf_rep, op0=ALU.add, op1=ALU.mult),
             reads=[Af_rep_b, nf_rep_b], writes=[Af_rep_b])
        g_t, g_b = ar.alloc([8, 512], BF16, "m_g")
        yb_t, yb_b = ar.alloc([16, 512], BF16, "m_yb")
        gab_t, gab_b = ar.alloc([16, 512], BF16, "m_gab")
        ya_t, ya_b = ar.alloc([8, 512], BF16, "m_ya")
        m1_t, m1_b = ar.alloc([8, 512], F32, "m_m1")
        mg_t, mg_b = ar.alloc([8, 512], BF16, "m_mg")
        sg = [ar.alloc([512], F32, f"m_sg{i}") for i in range(2)]
        xk = [ar.alloc([D], F32, f"m_xk{i}") for i in range(1)]
        h1 = [ar.alloc([D], F32, f"m_h1{i}") for i in range(2)]
        uft = [ar.alloc([D], F32, f"m_uft{i}") for i in range(1)]
        ufb = [ar.alloc([D], BF16, f"m_ufb{i}") for i in range(2)]
        ufT = [ar.alloc([8, 128], F32, f"m_ufT{i}") for i in range(1)]
        sq_junk, sq_junk_b = ar.alloc([D], BF16, "m_sqj")
        ss = [ar.alloc([1], F32, f"m_ss{i}") for i in range(2)]
        pc = [0]

        def bank():
            b = pc[0] % 8
            pc[0] += 1
            return b
        for tt in range(8):
            cs = slice(tt * 512, (tt + 1) * 512)
            S.op("sp", lambda e, cs=cs: e.dma_start(out=g_t, in_=self.g_s.rearrange("(kc p) n -> p kc n", p=128)[:, :, cs]), reads=[self.g_sb], writes=[g_b], dma=True)
            S.op("sp", lambda e, cs=cs: e.dma_start(out=yb_t, in_=self.yb_s.rearrange("(kc p) n -> p kc n", p=128)[:, :, cs]), reads=[self.yb_sb], writes=[yb_b], dma=True)
            S.op("sp", lambda e, cs=cs: e.dma_start(out=gab_t, in_=self.gab_s.rearrange("(kc p) n -> p kc n", p=128)[:, :, cs]), reads=[self.gab_sb], writes=[gab_b], dma=True)
            for mc in range(8):
                b_ = bank()

                def mm(e, b_=b_, mc=mc):
                    ins = None
                    for kc in range(8):
                        ins = e.matmul(ps[b_][:], lhsT=Wg[:, kc, mc * 128:(mc + 1) * 128], rhs=g_t[:, kc, :], start=(kc == 0), stop=(kc == 7))
                    return ins
                S.op("pe", mm, reads=[Wg_b, g_b], writes=[psb[b_]])
                s_t, s_b = sg[mc % 2]
                S.op("act", lambda e, s_t=s_t, b_=b_, mc=mc: e.activation(out=s_t, in_=ps[b_][:], func=AF.Sigmoid, bias=gb_col[:, mc:mc + 1]),
                     reads=[psb[b_], gb_col_b], writes=[s_b])
                S.op("dve", lambda e, s_t=s_t, mc=mc: e.tensor_tensor(out=ya_t[:, mc, :], in0=s_t, in1=g_t[:, mc, :], op=ALU.mult),
                     reads=[s_b, g_b], writes=[ya_b])
            for mc in range(8):
                b_ = bank()

                def mm(e, b_=b_, mc=mc):
                    ins = None
                    for kc in range(8):
                        ins = e.matmul(ps[b_][:], lhsT=Wa[:, kc, mc * 128:(mc + 1) * 128], rhs=ya_t[:, kc, :], start=(kc == 0), stop=(kc == 7))
                    return ins
                S.op("pe", mm, reads=[Wa_b, ya_b], writes=[psb[b_]])
                s_t, s_b = sg[mc % 2]
                S.op("act", lambda e, s_t=s_t, mc=mc: e.activation(out=s_t, in_=gab_t[:, mc, :], func=AF.Sigmoid), reads=[gab_b], writes=[s_b])
                S.op("dve", lambda e, s_t=s_t, b_=b_, mc=mc: e.tensor_tensor(out=m1_t[:, mc, :], in0=s_t, in1=ps[b_][:], op=ALU.mult),
                     reads=[s_b, psb[b_]], writes=[m1_b])
            for mc in range(8):
                b_ = bank()

                def mm(e, b_=b_, mc=mc):
                    ins = None
                    for kc in range(16):
                        ins = e.matmul(ps[b_][:], lhsT=Wb[:, kc, mc * 128:(mc + 1) * 128], rhs=yb_t[:, kc, :], start=(kc == 0), stop=(kc == 15))
                    return ins
                S.op("pe", mm, reads=[Wb_b, yb_b], writes=[psb[b_]])
                s_t, s_b = sg[mc % 2]
                S.op("act", lambda e, s_t=s_t, mc=mc: e.activation(out=s_t, in_=gab_t[:, 8 + mc, :], func=AF.Sigmoid), reads=[gab_b], writes=[s_b])
                S.op("dve", lambda e, s_t=s_t, b_=b_: e.tensor_tensor(out=s_t, in0=s_t, in1=ps[b_][:], op=ALU.mult), reads=[s_b, psb[b_]], writes=[s_b])
                S.op("dve", lambda e, s_t=s_t, mc=mc: e.tensor_tensor(out=mg_t[:, mc, :], in0=s_t, in1=m1_t[:, mc, :], op=ALU.add),
                     reads=[s_b, m1_b], writes=[mg_b])
            for sub in range(4):
                ti = tt * 4 + sub
                x_t, x_b = xk[0]
                h_t, h_b = h1[ti % 2]
                S.op("sp", lambda e, x_t=x_t, ti=ti: e.dma_start(out=x_t, in_=x_tok[ti * 128:(ti + 1) * 128, :]), writes=[x_b], dma=True)
                for half in range(2):
                    b_ = bank()

                    def mm(e, b_=b_, sub=sub, half=half):
                        ins = None
                        for kc in range(8):
                            ins = e.matmul(ps[b_][:], lhsT=mg_t[:, kc, sub * 128:(sub + 1) * 128], rhs=Wo[:, kc, half * 512:(half + 1) * 512],
                                           start=(kc == 0), stop=(kc == 7))
                        return ins
                    S.op("pe", mm, reads=[mg_b, Wo_b], writes=[psb[b_]])
                    hs = slice(half * 512, (half + 1) * 512)
                    S.op("dve", lambda e, h_t=h_t, b_=b_, hs=hs: e.tensor_tensor(out=h_t[:, hs], in0=ps[b_][:], in1=gm_rep[:, hs], op=ALU.mult),
                         reads=[psb[b_], gm_rep_b], writes=[h_b])
                    S.op("dve", lambda e, h_t=h_t, x_t=x_t, hs=hs: e.tensor_tensor(out=h_t[:, hs], in0=h_t[:, hs], in1=x_t[:, hs], op=ALU.add),
                         reads=[h_b, x_b], writes=[h_b])
                S.op("sp", lambda e, h_t=h_t, ti=ti: e.dma_start(out=self.h1_s[ti * 128:(ti + 1) * 128, :], in_=h_t), reads=[h_b], writes=[self.h1_sb], dma=True)
                s_t, s_b = ss[ti % 2]
                S.op("act", lambda e, h_t=h_t, s_t=s_t: e.activation(out=sq_junk, in_=h_t, func=AF.Square, accum_out=s_t), reads=[h_b], writes=[sq_junk_b, s_b])
                S.op("act", lambda e, s_t=s_t: e.activation(out=s_t, in_=s_t, func=AF.Sqrt, scale=1.0 / D, bias=EPS), reads=[s_b], writes=[s_b])
                S.op("dve", lambda e, s_t=s_t: e.reciprocal(out=s_t, in_=s_t), reads=[s_b], writes=[s_b])
                u_t, u_b = uft[0]
                ub_t, ub_b = ufb[ti % 2]
                S.op("dve", lambda e, u_t=u_t, h_t=h_t, s_t=s_t: e.scalar_tensor_tensor(out=u_t, in0=h_t, scalar=s_t[:, 0:1], in1=Af_rep, op0=ALU.mult, op1=ALU.mult),
                     reads=[h_b, s_b, Af_rep_b], writes=[u_b])
                S.op("dve", lambda e, u_t=u_t: e.tensor_tensor(out=u_t, in0=u_t, in1=Bf_rep, op=ALU.add), reads=[u_b, Bf_rep_b], writes=[u_b])
                S.op("act", lambda e, u_t=u_t, ub_t=ub_t: e.activation(out=ub_t, in_=u_t, func=AF.Copy), reads=[u_b], writes=[ub_b])
                S.op("sp", lambda e, ub_t=ub_t, ti=ti: e.dma_start(out=self.uf_s[ti * 128:(ti + 1) * 128, :], in_=ub_t), reads=[ub_b], writes=[self.uf_sb], dma=True)
                T_t, T_b = ufT[0]
                for half in range(2):
                    b_ = bank()

                    def tr(e, b_=b_, u_t=u_t, half=half):
                        ins = None
                        for q in range(4):
                            kc = half * 4 + q
                            ins = e.transpose(out=ps[b_][:, q * 128:(q + 1) * 128], in_=u_t[:, kc * 128:(kc + 1) * 128], identity=ident_f)
                        return ins
                    S.op("pe", tr, reads=[u_b, ident_f_b], writes=[psb[b_]])
                    S.op("act", lambda e, T_t=T_t, b_=b_, half=half: e.activation(
                        out=T_t[:, half * 4:half * 4 + 4, :].rearrange("p a b -> p (a b)"), in_=ps[b_][:], func=AF.Copy), reads=[psb[b_]], writes=[T_b])
                b_ = bank()

                def mml(e, b_=b_, T_t=T_t):
                    ins = None
                    for kc in range(8):
                        ins = e.matmul(ps[b_][:, 0:32], lhsT=T_t[:, kc, :], rhs=Wr[:, kc, :], start=(kc == 0), stop=(kc == 7))
                    return ins
                S.op("pe", mml, reads=[T_b, Wr_b], writes=[psb[b_]])
                S.op("dve", lambda e, b_=b_, ti=ti: e.tensor_tensor(out=self.logits[:, ti, :], in0=ps[b_][:, 0:32], in1=rb_rep, op=ALU.add),
                     reads=[psb[b_], rb_rep_b], writes=[self.logits_b])
        if dbg:
            o, ob = self.scratch("dbg_logits", [128, 1024], F32, dump=True)
            S.op("sp", lambda e: e.dma_start(out=o, in_=self.logits.rearrange("p a b -> p (a b)")), reads=[self.logits_b], writes=[ob], dma=True)
        S.barrier()
        ar.release()


    def phase5(self, wg_d, wu_d, wd_d, bg_d, bu_d, bd_d, fnw):
        nc, S, ar = self.nc, self.S, self.ar
        ps, psb = self.ps, self.psb
        dbg = self.debug
        ident_f, ident_f_b = self.consts["ident_f"]
        ones_f, ones_f_b = self.consts["ones_f"]
        ones_bf, ones_bf_b = self.consts["ones_bf"]
        BLK = int(os.environ.get('MOE_BLK', '256'))
        NSUB = BLK // 128
        NT, NE = 32, 32
        NB = -(-(16384 + 32 * (BLK - 1)) // BLK)
        NSLOT = NB * BLK
        self.xs, self.xs_b = self.scratch("xs", [NSLOT, D], BF16)
        self.ys, self.ys_b = self.scratch("ys", [NSLOT, D], BF16)
        logits, logits_b = self.logits, self.logits_b
        ar.mark()
        dest_i, dest_ib = ar.alloc([NT, 4], I32, "dest_i")
        gate4, gate4_b = ar.alloc([NT, 4], F32, "gate4")
        blk_i, blk_ib = ar.alloc([NB], I32, "blk_i")
        chg_i, chg_ib = ar.alloc([NB], I32, "chg_i")
        widx, widx_b = ar.alloc([NB], I32, "widx")
        bidx, bidx_b = ar.alloc([NB], I32, "bidx")
        ident_bf, ident_bf_b = ar.alloc([128], BF16, "ident_bf5")
        S.op("dve", lambda e: e.tensor_copy(out=ident_bf, in_=ident_f), reads=[ident_f_b], writes=[ident_bf_b])
        ar.mark()
        top8, top8_b = ar.alloc([NT, 8], F32, "top8")
        mask, mask_b = ar.alloc([NT, NE], F32, "mask")
        mask_bf, mask_bfb = ar.alloc([NT, NE], BF16, "mask_bf")
        gatef, gatef_b = ar.alloc([NT, NE], F32, "gatef")
        pos, pos_b = ar.alloc([NT, NE], F32, "pos")
        cnta, cnta_b = ar.alloc([NT, NE], F32, "cnta")
        base, base_b = ar.alloc([NT, NE], F32, "base")
        rsum, rsum_b = ar.alloc([NT], F32, "rsum")
        stri, stri_b = ar.alloc([128], BF16, "stri")
        S.op("pool", lambda e: e.affine_select(out=stri, in_=ones_f, pattern=[[1, 128]], compare_op=ALU.is_ge, fill=0.0,
                                              base=-1, channel_multiplier=-1), reads=[ones_f_b], writes=[stri_b])
        for i in range(NT):
            S.op("dve", lambda e, i=i: e.max(out=top8[:, i, :], in_=logits[:, i, :]), reads=[logits_b], writes=[top8_b])
            S.op("dve", lambda e, i=i: e.tensor_scalar(out=mask[:, i, :], in0=logits[:, i, :], scalar1=top8[:, i, 3:4], scalar2=None, op0=ALU.is_ge),
                 reads=[logits_b, top8_b], writes=[mask_b])
        S.op("act", lambda e: e.activation(out=mask_bf, in_=mask, func=AF.Copy), reads=[mask_b], writes=[mask_bfb])
        S.op("dve", lambda e: e.tensor_tensor(out=gatef, in0=logits, in1=top8[:, :, 0:1].broadcast_to([128, NT, NE]), op=ALU.subtract),
             reads=[logits_b, top8_b], writes=[gatef_b])
        S.op("act", lambda e: e.activation(out=gatef, in_=gatef, func=AF.Exp), reads=[gatef_b], writes=[gatef_b])
        S.op("dve", lambda e: e.tensor_tensor(out=gatef, in0=gatef, in1=mask, op=ALU.mult), reads=[gatef_b, mask_b], writes=[gatef_b])
        S.op("dve", lambda e: e.reduce_sum(out=rsum, in_=gatef, axis=AX.X), reads=[gatef_b], writes=[rsum_b])
        S.op("dve", lambda e: e.reciprocal(out=rsum, in_=rsum), reads=[rsum_b], writes=[rsum_b])
        S.op("dve", lambda e: e.tensor_tensor(out=gatef, in0=gatef, in1=rsum.unsqueeze(2).broadcast_to([128, NT, NE]), op=ALU.mult),
             reads=[gatef_b, rsum_b], writes=[gatef_b])
        mflat = mask_bf.rearrange("p a b -> p (a b)")
        for half in range(2):
            b_ = half

            def mm(e, b_=b_, half=half):
                return e.matmul(ps[b_][:], lhsT=ones_bf, rhs=mflat[:, half * 512:(half + 1) * 512], start=True, stop=True)
            S.op("pe", mm, reads=[mask_bfb, ones_bf_b], writes=[psb[b_]])
            S.op("dve", lambda e, b_=b_, half=half: e.tensor_copy(out=cnta.rearrange("p a b -> p (a b)")[:, half * 512:(half + 1) * 512], in_=ps[b_][:]),
                 reads=[psb[b_]], writes=[cnta_b])
            b2 = 2 + half

            def mm2(e, b2=b2, half=half):
                return e.matmul(ps[b2][:], lhsT=stri, rhs=mflat[:, half * 512:(half + 1) * 512], start=True, stop=True)
            S.op("pe", mm2, reads=[mask_bfb, stri_b], writes=[psb[b2]])
            S.op("dve", lambda e, b2=b2, half=half: e.tensor_copy(out=pos.rearrange("p a b -> p (a b)")[:, half * 512:(half + 1) * 512], in_=ps[b2][:]),
                 reads=[psb[b2]], writes=[pos_b])
        for ee in range(NE):
            S.op("dve", lambda e, ee=ee: e.tensor_tensor_scan(out=base[:, :, ee], data0=ones_f[:, 0:NT], data1=cnta[:, :, ee], initial=0.0,
                                                             op0=ALU.mult, op1=ALU.add), reads=[cnta_b, ones_f_b], writes=[base_b])
        tot, tot_b = ar.alloc([NE], F32, "tot")
        S.op("dve", lambda e: e.tensor_copy(out=tot, in_=base[:, NT - 1, :]), reads=[base_b], writes=[tot_b])
        S.op("dve", lambda e: e.tensor_tensor(out=base, in0=base, in1=cnta, op=ALU.subtract), reads=[base_b, cnta_b], writes=[base_b])
        S.op("dve", lambda e: e.tensor_tensor(out=pos, in0=pos, in1=base, op=ALU.add), reads=[pos_b, base_b], writes=[pos_b])
        nb_f, nb_fb = ar.alloc([NE], F32, "nb_f")
        nb_i, nb_ib = ar.alloc([NE], I32, "nb_i")
        pend, pend_b = ar.alloc([NE], F32, "pend")
        pstart, pstart_b = ar.alloc([NE], F32, "pstart")
        S.op("dve", lambda e: e.tensor_scalar(out=nb_f, in0=tot, scalar1=1.0 / BLK, scalar2=(BLK - 1.0) / BLK - 0.5 + 0.25 / BLK, op0=ALU.mult, op1=ALU.add),
             reads=[tot_b], writes=[nb_fb])
        S.op("dve", lambda e: e.tensor_copy(out=nb_i, in_=nb_f), reads=[nb_fb], writes=[nb_ib])
        S.op("dve", lambda e: e.tensor_copy(out=nb_f, in_=nb_i), reads=[nb_ib], writes=[nb_fb])
        S.op("dve", lambda e: e.tensor_scalar(out=nb_f, in0=nb_f, scalar1=float(BLK), scalar2=None, op0=ALU.mult), reads=[nb_fb], writes=[nb_fb])
        S.op("dve", lambda e: e.tensor_tensor_scan(out=pend, data0=ones_f[:, 0:NE], data1=nb_f, initial=0.0, op0=ALU.mult, op1=ALU.add),
             reads=[nb_fb, ones_f_b], writes=[pend_b])
        S.op("dve", lambda e: e.tensor_tensor(out=pstart, in0=pend, in1=nb_f, op=ALU.subtract), reads=[pend_b, nb_fb], writes=[pstart_b])
        S.op("dve", lambda e: e.tensor_tensor(out=pos, in0=pos, in1=pstart.unsqueeze(1).broadcast_to([128, NT, NE]), op=ALU.add),
             reads=[pos_b, pstart_b], writes=[pos_b])
        dest4, dest4_b = ar.alloc([NT, 4], F32, "dest4")
        junk, junk_b = ar.alloc([NE], F32, "junk")
        for i in range(NT):
            for k in range(4):
                S.op("dve", lambda e, i=i, k=k: e.scalar_tensor_tensor(out=junk, in0=logits[:, i, :], scalar=top8[:, i, k:k + 1], in1=pos[:, i, :],
                                                                     op0=ALU.is_equal, op1=ALU.mult, accum_out=dest4[:, i, k:k + 1]),
                     reads=[logits_b, top8_b, pos_b], writes=[junk_b, dest4_b])
                S.op("dve", lambda e, i=i, k=k: e.scalar_tensor_tensor(out=junk, in0=logits[:, i, :], scalar=top8[:, i, k:k + 1], in1=gatef[:, i, :],
                                                                     op0=ALU.is_equal, op1=ALU.mult, accum_out=gate4[:, i, k:k + 1]),
                     reads=[logits_b, top8_b, gatef_b], writes=[junk_b, gate4_b])
        S.op("dve", lambda e: e.tensor_copy(out=dest_i, in_=dest4), reads=[dest4_b], writes=[dest_ib])
        bv_i, bv_ib = ar.alloc([NB], I32, "bv_i")
        bv, bv_b = ar.alloc([NB], F32, "bv")
        cmp_, cmp_b = ar.alloc([NB, NE], F32, "cmp")
        blk_f, blk_fb = ar.alloc([NB], F32, "blk_f")
        chg_f, chg_fb = ar.alloc([NB], F32, "chg_f")
        S.op("pool", lambda e: e.iota(bv_i, pattern=[[BLK, NB]], base=0, channel_multiplier=0), writes=[bv_ib])
        S.op("dve", lambda e: e.tensor_copy(out=bv, in_=bv_i), reads=[bv_ib], writes=[bv_b])
        S.op("dve", lambda e: e.tensor_tensor(out=cmp_, in0=pend.unsqueeze(1).broadcast_to([128, NB, NE]),
                                              in1=bv.unsqueeze(2).broadcast_to([128, NB, NE]), op=ALU.is_le), reads=[pend_b, bv_b], writes=[cmp_b])
        S.op("dve", lambda e: e.reduce_sum(out=blk_f, in_=cmp_, axis=AX.X), reads=[cmp_b], writes=[blk_fb])
        S.op("dve", lambda e: e.tensor_scalar(out=blk_f, in0=blk_f, scalar1=float(NE - 1), scalar2=None, op0=ALU.min), reads=[blk_fb], writes=[blk_fb])
        S.op("dve", lambda e: e.memset(chg_f[:, 0:1], 1.0), writes=[chg_fb])
        S.op("dve", lambda e: e.tensor_tensor(out=chg_f[:, 1:NB], in0=blk_f[:, 1:NB], in1=blk_f[:, 0:NB - 1], op=ALU.not_equal), reads=[blk_fb], writes=[chg_fb])
        S.op("dve", lambda e: e.tensor_copy(out=blk_i, in_=blk_f), reads=[blk_fb], writes=[blk_ib])
        S.op("dve", lambda e: e.tensor_copy(out=chg_i, in_=chg_f), reads=[chg_fb], writes=[chg_ib])
        pio_i, pio_ib = ar.alloc([1], I32, "pio_i")
        pio, pio_b = ar.alloc([1], F32, "pio")
        S.op("pool", lambda e: e.iota(pio_i, pattern=[[0, 1]], base=0, channel_multiplier=1), writes=[pio_ib])
        S.op("dve", lambda e: e.tensor_copy(out=pio, in_=pio_i), reads=[pio_ib], writes=[pio_b])
        nchg, nchg_b = ar.alloc([NB], F32, "nchg")
        S.op("dve", lambda e: e.tensor_scalar(out=nchg, in0=chg_f, scalar1=-1.0e7, scalar2=1.0e7, op0=ALU.mult, op1=ALU.add), reads=[chg_fb], writes=[nchg_b])
        wb_f, wb_fb = ar.alloc([NB], F32, "wb_f")
        S.op("dve", lambda e: e.tensor_scalar(out=wb_f, in0=blk_f, scalar1=128.0, scalar2=pio[:, 0:1], op0=ALU.mult, op1=ALU.add), reads=[blk_fb, pio_b], writes=[wb_fb])
        S.op("dve", lambda e: e.tensor_tensor(out=wb_f, in0=wb_f, in1=chg_f, op=ALU.mult), reads=[wb_fb, chg_fb], writes=[wb_fb])
        S.op("dve", lambda e: e.tensor_tensor(out=wb_f, in0=wb_f, in1=nchg, op=ALU.add), reads=[wb_fb, nchg_b], writes=[wb_fb])
        S.op("dve", lambda e: e.tensor_copy(out=widx, in_=wb_f), reads=[wb_fb], writes=[widx_b])
        bi_f, bi_fb = ar.alloc([NB], F32, "bi_f")
        S.op("dve", lambda e: e.tensor_tensor(out=bi_f, in0=blk_f, in1=chg_f, op=ALU.mult), reads=[blk_fb, chg_fb], writes=[bi_fb])
        S.op("dve", lambda e: e.tensor_tensor(out=bi_f, in0=bi_f, in1=nchg, op=ALU.add), reads=[bi_fb, nchg_b], writes=[bi_fb])
        S.op("dve", lambda e: e.tensor_copy(out=bidx, in_=bi_f), reads=[bi_fb], writes=[bidx_b])
        if dbg:
            for nm, t_, tb_, n, dtp in (("dbg_dest", dest_i.rearrange("p a b -> p (a b)"), dest_ib, NT * 4, I32),
                                        ("dbg_gate4", gate4.rearrange("p a b -> p (a b)"), gate4_b, NT * 4, F32),
                                        ("dbg_blk", blk_i, blk_ib, NB, I32), ("dbg_chg", chg_i, chg_ib, NB, I32)):
                o, ob = self.scratch(nm, [128, n], dtp, dump=True)
                S.op("sp", lambda e, o=o, t_=t_: e.dma_start(out=o, in_=t_), reads=[tb_], writes=[ob], dma=True)
        S.barrier()
        ar.release()
        ar.mark()
        zt, zt_b = ar.alloc([4, D], BF16, "zt")
        S.op("dve", lambda e: e.memset(zt, 0.0), writes=[zt_b])
        xsv = self.xs.rearrange("(b p) n -> p b n", p=128)
        for q in range(NSLOT // 512):
            S.op("sp", lambda e, q=q: e.dma_start(out=xsv[:, 4 * q:4 * q + 4, :], in_=zt), reads=[zt_b], writes=[self.xs_b], dma=True)
        uft = [ar.alloc([D], BF16, f"sc_u{i}") for i in range(2)]
        for i in range(NT):
            u_t, u_b = uft[i % 2]
            S.op("sp", lambda e, u_t=u_t, i=i: e.dma_start(out=u_t, in_=self.uf_s[i * 128:(i + 1) * 128, :]), reads=[self.uf_sb], writes=[u_b], dma=True)
            for k in range(4):
                S.op("pool", lambda e, u_t=u_t, i=i, k=k: e.indirect_dma_start(
                    out=self.xs, out_offset=bass.IndirectOffsetOnAxis(ap=dest_i[:, i, k:k + 1].bitcast(U32), axis=0), in_=u_t, in_offset=None),
                    reads=[u_b, dest_ib], writes=[self.xs_b], dma=True)
        S.barrier()
        ar.release()
        ar.mark()
        Wt = [ar.alloc([8, D], BF16, f"moe_w{m}") for m in range(3)]
        brow, brow_b = ar.alloc([3, D], BF16, "moe_brow")
        WS = [ar.alloc([8, D], F32, f"moe_ws{m}") for m in range(3)]
        browS, browS_b = ar.alloc([3, D], BF16, "moe_browS")
        wds = [wg_d, wu_d, wd_d]
        bds = [bg_d, bu_d, bd_d]
        xb = [ar.alloc([D], BF16, f"moe_xb{i}") for i in range(2)]
        xT = [ar.alloc([8, 128], BF16, f"moe_xT{i}") for i in range(2)]
        gc = [ar.alloc([512], F32, f"moe_gc{i}") for i in range(2)]
        uc = [ar.alloc([512], F32, f"moe_uc{i}") for i in range(2)]
        sgm = [ar.alloc([512], F32, f"moe_sg{i}") for i in range(2)]
        hb_ = [ar.alloc([D], BF16, f"moe_h{i}") for i in range(2)]
        hT = [ar.alloc([8, 128], BF16, f"moe_hT{i}") for i in range(2)]
        yo = [ar.alloc([D], BF16, f"moe_yo{i}") for i in range(2)]
        NBLK = int(os.environ.get("MOE_NB", str(NB)))
        regs = {}

        def breg(e, val):
            if val not in regs:
                regs[val] = e.to_reg(val)
            return regs[val]
        for b in range(NBLK):
            for m in range(3):
                S.op("pool", lambda e, m=m, b=b: e.indirect_dma_start(
                    out=WS[m][0].rearrange("p a b -> p (a b)"), out_offset=None, in_=wds[m],
                    in_offset=bass.IndirectOffsetOnAxis(ap=widx[:, b:b + 1].bitcast(U32), axis=0),
                    bounds_check=breg(e, NE * 128 - 1), oob_is_err=False),
                    reads=[widx_b], writes=[WS[m][1]], dma=True)
            for m in range(3):
                S.op("pool", lambda e, m=m, b=b: e.indirect_dma_start(
                    out=browS[:, m, :], out_offset=None, in_=bds[m],
                    in_offset=bass.IndirectOffsetOnAxis(ap=bidx[:, b:b + 1].bitcast(U32), axis=0),
                    bounds_check=breg(e, NE - 1), oob_is_err=False),
                    reads=[bidx_b], writes=[browS_b], dma=True)
            for m in range(3):
                eng = ("dve", "dve", "act")[m]
                if eng == "act":
                    S.op("act", lambda e, m=m: e.activation(out=Wt[m][0], in_=WS[m][0], func=AF.Copy), reads=[WS[m][1]], writes=[Wt[m][1]])
                else:
                    S.op(eng, lambda e, m=m: e.tensor_copy(out=Wt[m][0], in_=WS[m][0]), reads=[WS[m][1]], writes=[Wt[m][1]])
            S.op("act", lambda e: e.activation(out=brow[0:1], in_=browS[0:1], func=AF.Copy), reads=[browS_b], writes=[brow_b])
            subs = [b * NSUB + s_ for s_ in range(NSUB)]

            def stageA(sb):
                x_t, x_b = xb[sb % 2]
                S.op("sp", lambda e, x_t=x_t, sb=sb: e.dma_start(out=x_t, in_=self.xs[sb * 128:(sb + 1) * 128, :]), reads=[self.xs_b], writes=[x_b], dma=True)
                xT_t, xT_b = xT[sb % 2]
                pT = ps[0][:].bitcast(BF16)

                def trx(e, x_t=x_t, pT=pT):
                    ins = None
                    for kc in range(8):
                        ins = e.transpose(out=pT[:, kc * 128:(kc + 1) * 128], in_=x_t[:, kc * 128:(kc + 1) * 128], identity=ident_bf)
                    return ins
                S.op("pe", trx, reads=[x_b, ident_bf_b], writes=[psb[0]])
                S.op("act", lambda e, xT_t=xT_t, pT=pT: e.activation(out=xT_t.rearrange("p a b -> p (a b)"), in_=pT, func=AF.Copy), reads=[psb[0]], writes=[xT_b])
                h_t, h_b = hb_[sb % 2]
                for half in range(2):
                    hs = slice(half * 512, (half + 1) * 512)
                    for m, bk in ((0, 1 + half), (1, 3 + half)):
                        def mm(e, m=m, bk=bk, hs=hs, xT_t=xT_t):
                            e.matmul(ps[bk][:], lhsT=ones_bf[0:1, 0:128], rhs=brow[0:1, m, hs], start=True, stop=False)
                            ins = None
                            for kc in range(8):
                                ins = e.matmul(ps[bk][:], lhsT=xT_t[:, kc, :], rhs=Wt[m][0][:, kc, hs], start=False, stop=(kc == 7))
                            return ins
                        S.op("pe", mm, reads=[xT_b, Wt[m][1], brow_b, ones_bf_b], writes=[psb[bk]])
                    g_t, g_b = gc[half]
                    u_t, u_b = uc[half]
                    s_t, s_b = sgm[half]
                    S.op("dve", lambda e, g_t=g_t, half=half: e.tensor_scalar(out=g_t, in0=ps[1 + half][:], scalar1=7.0, scalar2=None, op0=ALU.min),
                         reads=[psb[1 + half]], writes=[g_b])
                    S.op("dve", lambda e, u_t=u_t, half=half: e.tensor_scalar(out=u_t, in0=ps[3 + half][:], scalar1=7.0, scalar2=-7.0, op0=ALU.min, op1=ALU.max),
                         reads=[psb[3 + half]], writes=[u_b])
                    S.op("act", lambda e, s_t=s_t, g_t=g_t: e.activation(out=s_t, in_=g_t, func=AF.Sigmoid, scale=1.702), reads=[g_b], writes=[s_b])
                    S.op("dve", lambda e, u_t=u_t, g_t=g_t: e.scalar_tensor_tensor(out=u_t, in0=u_t, scalar=1.0, in1=g_t, op0=ALU.add, op1=ALU.mult),
                         reads=[u_b, g_b], writes=[u_b])
                    S.op("dve", lambda e, h_t=h_t, u_t=u_t, s_t=s_t, hs=hs: e.tensor_tensor(out=h_t[:, hs], in0=u_t, in1=s_t, op=ALU.mult),
                         reads=[u_b, s_b], writes=[h_b])

            def stageB(sb):
                h_t, h_b = hb_[sb % 2]
                hT_t, hT_b = hT[sb % 2]
                pT5 = ps[5][:].bitcast(BF16)

                def trh(e, h_t=h_t, pT5=pT5):
                    ins = None
                    for kc in range(8):
                        ins = e.transpose(out=pT5[:, kc * 128:(kc + 1) * 128], in_=h_t[:, kc * 128:(kc + 1) * 128], identity=ident_bf)
                    return ins
                S.op("pe", trh, reads=[h_b, ident_bf_b], writes=[psb[5]])
                S.op("act", lambda e, hT_t=hT_t, pT5=pT5: e.activation(out=hT_t.rearrange("p a b -> p (a b)"), in_=pT5, func=AF.Copy), reads=[psb[5]], writes=[hT_b])
                y_t, y_b = yo[sb % 2]
                for half in range(2):
                    hs = slice(half * 512, (half + 1) * 512)
                    bk = 6 + half

                    def mmd(e, bk=bk, hs=hs, hT_t=hT_t):
                        e.matmul(ps[bk][:], lhsT=ones_bf[0:1, 0:128], rhs=brow[0:1, 2, hs], start=True, stop=False)
                        ins = None
                        for kc in range(8):
                            ins = e.matmul(ps[bk][:], lhsT=hT_t[:, kc, :], rhs=Wt[2][0][:, kc, hs], start=False, stop=(kc == 7))
                        return ins
                    S.op("pe", mmd, reads=[hT_b, Wt[2][1], brow_b, ones_bf_b], writes=[psb[bk]])
                    if half == 0:
                        S.op("dve", lambda e, y_t=y_t, bk=bk, hs=hs: e.tensor_copy(out=y_t[:, hs], in_=ps[bk][:]), reads=[psb[bk]], writes=[y_b])
                    else:
                        S.op("act", lambda e, y_t=y_t, bk=bk, hs=hs: e.activation(out=y_t[:, hs], in_=ps[bk][:], func=AF.Copy), reads=[psb[bk]], writes=[y_b])
                S.op("sp", lambda e, y_t=y_t, sb=sb: e.dma_start(out=self.ys[sb * 128:(sb + 1) * 128, :], in_=y_t), reads=[y_b], writes=[self.ys_b], dma=True)

            stageA(subs[0])
            for i_ in range(1, NSUB):
                stageA(subs[i_])
                stageB(subs[i_ - 1])
            stageB(subs[-1])
        S.barrier()
        ar.release()
        ar.mark()
        gf_rep, gf_rep_b = ar.alloc([D], F32, "gf_rep")
        fn_rep, fn_rep_b = ar.alloc([D], F32, "fn_rep")
        S.op("sp", lambda e: e.dma_start(out=gf_rep, in_=self.mod_s[:, 5 * D:6 * D]), reads=[self.mod_sb], writes=[gf_rep_b], dma=True)
        S.op("sp", lambda e: e.dma_start(out=fn_rep, in_=fnw.broadcast_to([128, D])), writes=[fn_rep_b], dma=True)
        yk = [[ar.alloc([D], BF16, f"cb_y{j}{k}") for k in range(4)] for j in range(2)]
        hh = [ar.alloc([D], F32, f"cb_h{j}") for j in range(2)]
        acc = [ar.alloc([D], F32, f"cb_a{j}") for j in range(2)]
        oo = [ar.alloc([D], F32, f"cb_o{j}") for j in range(2)]
        sqj, sqj_b = ar.alloc([D], BF16, "cb_sq")
        ssq = [ar.alloc([1], F32, f"cb_ss{j}") for j in range(2)]
        for i in range(NT):
            j = i % 2
            h_t, h_b = hh[j]
            a_t, a_b = acc[j]
            o_t, o_b = oo[j]
            s_t, s_b = ssq[j]
            S.op("sp", lambda e, h_t=h_t, i=i: e.dma_start(out=h_t, in_=self.h1_s[i * 128:(i + 1) * 128, :]), reads=[self.h1_sb], writes=[h_b], dma=True)
            for k in range(4):
                y_t, y_b = yk[j][k]
                S.op("pool", lambda e, y_t=y_t, i=i, k=k: e.indirect_dma_start(
                    out=y_t, out_offset=None, in_=self.ys, in_offset=bass.IndirectOffsetOnAxis(ap=dest_i[:, i, k:k + 1].bitcast(U32), axis=0)),
                    reads=[self.ys_b, dest_ib], writes=[y_b], dma=True)
            S.op("dve", lambda e, a_t=a_t, i=i, j=j: e.tensor_scalar(out=a_t, in0=yk[j][0][0], scalar1=gate4[:, i, 0:1], scalar2=None, op0=ALU.mult),
                 reads=[yk[j][0][1], gate4_b], writes=[a_b])
            for k in range(1, 4):
                S.op("dve", lambda e, a_t=a_t, i=i, j=j, k=k: e.scalar_tensor_tensor(out=a_t, in0=yk[j][k][0], scalar=gate4[:, i, k:k + 1], in1=a_t,
                                                                                 op0=ALU.mult, op1=ALU.add), reads=[yk[j][k][1], gate4_b, a_b], writes=[a_b])
            S.op("dve", lambda e, a_t=a_t: e.tensor_tensor(out=a_t, in0=a_t, in1=gf_rep, op=ALU.mult), reads=[a_b, gf_rep_b], writes=[a_b])
            S.op("dve", lambda e, a_t=a_t, h_t=h_t: e.tensor_tensor(out=a_t, in0=a_t, in1=h_t, op=ALU.add), reads=[a_b, h_b], writes=[a_b])
            S.op("act", lambda e, a_t=a_t, s_t=s_t: e.activation(out=sqj, in_=a_t, func=AF.Square, accum_out=s_t), reads=[a_b], writes=[sqj_b, s_b])
            S.op("act", lambda e, s_t=s_t: e.activation(out=s_t, in_=s_t, func=AF.Sqrt, scale=1.0 / D, bias=EPS), reads=[s_b], writes=[s_b])
            S.op("dve", lambda e, s_t=s_t: e.reciprocal(out=s_t, in_=s_t), reads=[s_b], writes=[s_b])
            S.op("dve", lambda e, o_t=o_t, a_t=a_t, s_t=s_t: e.scalar_tensor_tensor(out=o_t, in0=a_t, scalar=s_t[:, 0:1], in1=fn_rep, op0=ALU.mult, op1=ALU.mult),
                 reads=[a_b, s_b, fn_rep_b], writes=[o_b])
            S.op("sp", lambda e, o_t=o_t, i=i: e.dma_start(out=self.out[i * 128:(i + 1) * 128, :], in_=o_t), reads=[o_b], dma=True)
        S.barrier()
        ar.release()
        ar.release()

def to_cm(a):
    return a.reshape(64, 64, *a.shape[1:]).swapaxes(0, 1).reshape(a.shape)


def from_cm(a):
    return a.reshape(64, 64, *a.shape[1:]).swapaxes(0, 1).reshape(a.shape)


def make_in_maps(inputs, cores):
    x = np.asarray(inputs["x"], np.float32)
    c = np.asarray(inputs["c"], np.float32)
    ctx = np.asarray(inputs["ctx"], np.float32)
    c_ctx = np.asarray(inputs["c_ctx"], np.float32)
    shared = {
        "cctxT": np.ascontiguousarray(c_ctx.reshape(8, 128).T),
        "ada_w": np.ascontiguousarray(inputs["ada_w"][0]),
        "ada_b": np.ascontiguousarray(inputs["ada_b"][0].reshape(1, -1)),
        "norm_mix_w": np.ascontiguousarray(np.asarray(inputs["norm_mix_w"][0]).reshape(8, 128).T),
        "w_in": np.ascontiguousarray(inputs["w_in"][0]),
        "convw": np.ascontiguousarray(np.transpose(np.asarray(inputs["ssd_conv_w"][0]).reshape(5, 32, 128), (2, 1, 0))),
        "convb": np.ascontiguousarray(np.asarray(inputs["ssd_conv_b"][0]).reshape(32, 128).T),
        "dtbias": np.ascontiguousarray(np.asarray(inputs["ssd_dt_bias"][0]).reshape(1, 64)),
        "alog": np.ascontiguousarray(np.asarray(inputs["ssd_a_log"][0]).reshape(1, 64)),
        "ssd_d": np.ascontiguousarray(np.repeat(np.asarray(inputs["ssd_d"][0]), 64).reshape(16, 128).T),
        "ssd_nw": np.ascontiguousarray(np.asarray(inputs["ssd_norm_w"][0]).reshape(16, 128).T),
    }
    lam_re = np.asarray(inputs["s5_lam_re"][0]); lam_im = np.asarray(inputs["s5_lam_im"][0]); ldt = np.asarray(inputs["s5_log_dt"][0])
    b_re = np.asarray(inputs["s5_b_re"][0]); b_im = np.asarray(inputs["s5_b_im"][0])
    c_re = np.asarray(inputs["s5_c_re"][0]); c_im = np.asarray(inputs["s5_c_im"][0])
    s5F = np.zeros((2, 128, 5, 8, 64), np.float32)
    s5S = np.zeros((2, 128, 3 * 32 + 4 * 512), np.float32)
    for d in range(2):
        def F_gp(a):
            t = a.reshape(8, 8, 64).transpose(1, 0, 2)
            return np.repeat(t[:, None], 16, axis=1).reshape(128, 8, 64)
        s5F[d, :, 0] = F_gp(lam_re[d]); s5F[d, :, 1] = F_gp(lam_im[d])
        s5F[d, :, 2] = F_gp(np.repeat(ldt[d][:, None], 64, axis=1))
        s5F[d, :, 3] = b_re[d].reshape(8, 8, 64, 16).transpose(1, 3, 0, 2).reshape(128, 8, 64)
        s5F[d, :, 4] = b_im[d].reshape(8, 8, 64, 16).transpose(1, 3, 0, 2).reshape(128, 8, 64)
        def S_gp(a):
            return a.reshape(32, 2, 64).transpose(1, 2, 0).reshape(128, 32)
        s5S[d, :, 0:32] = S_gp(lam_re[d]); s5S[d, :, 32:64] = S_gp(lam_im[d])
        s5S[d, :, 64:96] = S_gp(np.repeat(ldt[d][:, None], 64, axis=1))
        s5S[d, :, 96:96 + 512] = b_re[d].reshape(32, 2, 64, 16).transpose(1, 2, 0, 3).reshape(128, 512)
        s5S[d, :, 96 + 512:96 + 1024] = b_im[d].reshape(32, 2, 64, 16).transpose(1, 2, 0, 3).reshape(128, 512)
        s5S[d, :, 96 + 1024:96 + 1536] = c_re[d].reshape(32, 2, 16, 64).transpose(1, 3, 0, 2).reshape(128, 512)
        s5S[d, :, 96 + 1536:96 + 2048] = c_im[d].reshape(32, 2, 16, 64).transpose(1, 3, 0, 2).reshape(128, 512)
    shared["glu_w"] = np.ascontiguousarray(inputs["s5_glu_w"][0])
    shared["glu_b"] = np.ascontiguousarray(np.asarray(inputs["s5_glu_b"][0]).reshape(8, 128).T)
    shared["w_a"] = np.ascontiguousarray(inputs["w_branch_a"][0])
    shared["w_b"] = np.ascontiguousarray(inputs["w_branch_b"][0])
    shared["w_o"] = np.ascontiguousarray(inputs["w_out"][0])
    shared["nfw"] = np.ascontiguousarray(np.asarray(inputs["norm_ffn_w"][0]).reshape(1, -1))
    shared["r_w"] = np.ascontiguousarray(inputs["router_w"][0])
    shared["r_b"] = np.ascontiguousarray(np.asarray(inputs["router_b"][0]).reshape(1, -1))
    def wperm(w):
        return np.ascontiguousarray(np.asarray(w).reshape(32, 8, 128, D).transpose(0, 2, 1, 3)).reshape(4096, 8192)
    shared["moe_wg"] = wperm(inputs["moe_w_gate"][0])
    shared["moe_wu"] = wperm(inputs["moe_w_up"][0])
    shared["moe_wd"] = wperm(inputs["moe_w_down"][0])
    shared["moe_bg"] = np.ascontiguousarray(inputs["moe_b_gate"][0])
    shared["moe_bu"] = np.ascontiguousarray(inputs["moe_b_up"][0])
    shared["moe_bd"] = np.ascontiguousarray(inputs["moe_b_down"][0])
    shared["fnw"] = np.ascontiguousarray(np.asarray(inputs["final_norm_w"]).reshape(1, -1))
    shared["s5F"] = s5F
    shared["s5S"] = s5S
    shared["s5_d"] = np.ascontiguousarray(np.asarray(inputs["s5_d"][0]).reshape(8, 128).T)
    maps = []
    for b in cores:
        m = dict(shared)
        m["xT_rm"] = np.ascontiguousarray(x[b].T)
        m["xT_cm"] = np.ascontiguousarray(to_cm(x[b]).T)
        m["ctxT"] = np.ascontiguousarray(ctx[b].T)
        m["cT"] = np.ascontiguousarray(c[b].reshape(8, 128).T)
        m["x_tok"] = np.ascontiguousarray(to_cm(x[b]))
        maps.append(m)
    return maps


_NC_CACHE = {}


def kernel(**inputs):
    if "nc" not in _NC_CACHE:
        _NC_CACHE["nc"] = K().build()
    nc = _NC_CACHE["nc"]
    maps = make_in_maps(inputs, list(range(8)))
    res = run_bass_kernel_spmd(nc, maps, core_ids=list(range(8)))
    outs = [from_cm(np.asarray(r["out"])) for r in res.results]
    return np.stack(outs, 0).astype(np.float32)
```

```python
import os
import numpy as np
from contextlib import ExitStack
import concourse.bass as bass
import concourse.mybir as mybir
from concourse.bass_utils import run_bass_kernel_spmd

F32 = mybir.dt.float32
BF16 = mybir.dt.bfloat16
I32 = mybir.dt.int32
U32 = mybir.dt.uint32
U8 = mybir.dt.uint8
ALU = mybir.AluOpType
AF = mybir.ActivationFunctionType
AX = mybir.AxisListType

NDSEM = 8
D = 1024
L = 4096
LC = 256
T = L + LC
EPS = 1e-6


class Buf:
    __slots__ = ("name", "last_w", "readers", "excl")

    def __init__(self, name, excl=False):
        self.name = name
        self.last_w = None
        self.readers = []
        self.excl = excl


class Op:
    __slots__ = ("id", "eng", "fn", "deps", "dma", "eidx", "needs_inc", "semval", "dslot", "dval", "name")


COMPUTE = ("pe", "dve", "act", "pool")
ENGS = ("sp", "pe", "dve", "act", "pool")


class Sched:
    def __init__(self, nc):
        self.nc = nc
        self.ops = []
        self.per_eng = {e: [] for e in ENGS}
        self.ndma = {e: 0 for e in ENGS}
        self.barrier_floor = -1

    def op(self, eng, fn, reads=(), writes=(), dma=False, name=None):
        o = Op()
        o.id = len(self.ops)
        o.eng = eng
        o.fn = fn
        o.dma = dma
        o.name = name
        o.needs_inc = False
        o.semval = None
        o.dslot = None
        o.dval = None
        deps = {}
        if any(b.excl for b in reads):
            writes = list(writes) + [b for b in reads if b.excl and b not in writes]
            reads = [b for b in reads if not b.excl]
        for b in reads:
            if b.last_w is not None:
                deps[b.last_w.id] = (b.last_w, "raw")
        for b in writes:
            if b.last_w is not None and b.last_w.id not in deps:
                deps[b.last_w.id] = (b.last_w, "waw")
            for r in b.readers:
                if r.id not in deps:
                    deps[r.id] = (r, "war")
        o.deps = [(p, k) for (p, k) in deps.values() if p.id > self.barrier_floor]
        for b in reads:
            b.readers.append(o)
        for b in writes:
            b.last_w = o
            b.readers = []
        o.eidx = len(self.per_eng[eng])
        self.per_eng[eng].append(o)
        if dma:
            i = self.ndma[eng]
            self.ndma[eng] += 1
            o.dslot = i % NDSEM
            o.dval = 16 * (i // NDSEM + 1)
        self.ops.append(o)
        return o

    def barrier(self):
        lasts = []
        for e in ENGS:
            comp = [o for o in self.per_eng[e] if not o.dma and o.fn is not None]
            if comp:
                lasts.append(comp[-1])
            dm = [o for o in self.per_eng[e] if o.dma]
            lasts.extend(dm[-NDSEM:])
        lasts = [o for o in lasts if o.id > self.barrier_floor]
        for e in ENGS:
            o = self.op(e, None, name="barrier")
            o.deps = [(p, "raw") for p in lasts if p.eng != e or p.dma]
        self.barrier_floor = len(self.ops) - 1

    @staticmethod
    def _skip(o, p, kind):
        if p.dma or o.dma or p.eng != o.eng:
            return False
        if p.eng == "pe":
            return True
        if kind != "raw":
            return True
        return o.eidx - p.eidx > 2

    def emit(self):
        nc = self.nc
        for o in self.ops:
            for (p, kind) in o.deps:
                if p.dma or self._skip(o, p, kind):
                    continue
                p.needs_inc = True
        for e in ENGS:
            c = 0
            for o in self.per_eng[e]:
                if o.dma:
                    continue
                if o.needs_inc:
                    c += 1
                    o.semval = c
        with ExitStack() as es:
            csem = {e: es.enter_context(nc.semaphore(f"c_{e}")) for e in COMPUTE}
            dsem = {e: [es.enter_context(nc.semaphore(f"d_{e}{i}")) for i in range(NDSEM)]
                    for e in ENGS if self.ndma[e] > 0}
            block = es.enter_context(nc.Block())
            handles = {"sp": block.sync, "pe": block.tensor, "dve": block.vector,
                       "act": block.scalar, "pool": block.gpsimd}

            FUSE = os.environ.get("FUSEWAIT", "1") == "1"

            class _Rec:
                def __init__(self, eng):
                    self._eng = eng
                    self.first = None

                def __getattr__(self, name):
                    attr = getattr(self._eng, name)
                    if not callable(attr):
                        return attr

                    def w(*a, **k):
                        r = attr(*a, **k)
                        if self.first is None and hasattr(r, "then_inc"):
                            self.first = r
                        return r
                    return w

            def make(e):
                def body(eng):
                    waited = {}

                    def need(lst, sem, key, val):
                        if waited.get(key, 0) >= val:
                            return
                        for i, (s_, k_, v_) in enumerate(lst):
                            if k_ == key:
                                if v_ < val:
                                    lst[i] = (sem, key, val)
                                return
                        lst.append((sem, key, val))

                    for o in self.per_eng[e]:
                        lst = []
                        for (p, kind) in o.deps:
                            if p.dma:
                                need(lst, dsem[p.eng][p.dslot], ("d", p.eng, p.dslot), p.dval)
                            elif not self._skip(o, p, kind):
                                need(lst, csem[p.eng], ("c", p.eng), p.semval)
                        if o.dma and o.dval > 16:
                            need(lst, dsem[e][o.dslot], ("d", e, o.dslot), o.dval - 16)
                        for (s_, k_, v_) in lst:
                            waited[k_] = v_
                        fuse = None
                        if FUSE and lst and o.fn is not None and not o.dma:
                            fuse = lst.pop()
                        for (s_, k_, v_) in lst:
                            eng.wait_ge(s_, v_)
                        if o.fn is None:
                            continue
                        if fuse is not None:
                            rec = _Rec(eng)
                            ins = o.fn(rec)
                            rec.first._wait_ge(fuse[0], fuse[2])
                        else:
                            ins = o.fn(eng)
                        if o.dma:
                            ins.then_inc(dsem[e][o.dslot], 16)
                        elif o.needs_inc:
                            ins.then_inc(csem[e], 1)
                    if e == "sp":
                        for q in dsem:
                            dm = [o for o in self.per_eng[q] if o.dma]
                            for o in dm[-NDSEM:]:
                                if waited.get(("d", q, o.dslot), 0) < o.dval:
                                    eng.wait_ge(dsem[q][o.dslot], o.dval)
                                    waited[("d", q, o.dslot)] = o.dval
                return body

            for e in ENGS:
                if self.per_eng[e] or e == "sp":
                    handles[e](make(e))


ESZ = {F32: 4, BF16: 2, I32: 4, U32: 4, U8: 1}


class Arena:
    def __init__(self, nc, es, nbytes, name="arena"):
        self.t = es.enter_context(nc.sbuf_tensor(name, [128, nbytes], U8))
        self.nbytes = nbytes
        self.off = 0
        self.marks = []

    def alloc(self, shape, dtype, name="t"):
        if isinstance(shape, int):
            shape = [shape]
        n = int(np.prod(shape))
        nb = n * ESZ[dtype]
        self.off = (self.off + 63) // 64 * 64
        assert self.off + nb <= self.nbytes, f"arena overflow {name}: {self.off}+{nb}>{self.nbytes}"
        ap = self.t[:, self.off:self.off + nb]
        if dtype != U8:
            ap = ap.bitcast(dtype)
        if len(shape) > 1:
            names = " ".join(f"d{i}" for i in range(len(shape)))
            kw = {f"d{i}": int(shape[i]) for i in range(1, len(shape))}
            ap = ap.rearrange(f"p ({names}) -> p {names}", **kw)
        self.off += nb
        return ap, Buf(name)

    def mark(self):
        self.marks.append(self.off)

    def release(self):
        self.off = self.marks.pop()


IN_S5 = (0, 1024)
IN_XBC = (1024, 5120)
IN_DT = (5120, 5184)
IN_Z = (5184, 7232)
IN_GA = (7232, 8256)
IN_GB = (8256, 9280)


class K:
    def __init__(self, stage=99, debug=False):
        self.stage = stage
        self.debug = debug
        self.nc = bass.Bass("TRN2", target_bir_lowering=False)
        self.ins = {}
        self.dbg = {}

    def inp(self, name, shape, dt=F32):
        self.ins[name] = self.nc.dram_tensor(name, list(shape), dt, kind="ExternalInput").ap()
        return self.ins[name]

    def scratch(self, name, shape, dt, dump=False):
        if self.debug and dump:
            ap = self.nc.dram_tensor(name, list(shape), dt, kind="ExternalOutput").ap()
            self.dbg[name] = ap
        else:
            ap = self.nc.dram_tensor(name, list(shape), dt).ap()
        return ap, Buf(name)

    def build(self):
        nc = self.nc
        inp = self.inp
        xT_cm = inp("xT_cm", [D, L])
        xT_rm = inp("xT_rm", [D, L])
        ctxT = inp("ctxT", [D, LC])
        cT = inp("cT", [128, 8])
        cctxT = inp("cctxT", [128, 8])
        ada_w = inp("ada_w", [D, 6 * D])
        ada_b = inp("ada_b", [1, 6 * D])
        nmw = inp("norm_mix_w", [128, 8])
        w_in = inp("w_in", [D, 9280])
        out = nc.dram_tensor("out", [L, D], F32, kind="ExternalOutput").ap()
        self.out = out

        with ExitStack() as es:
            self.es = es
            ar = self.ar = Arena(nc, es, 206 * 1024)
            self.ps = [es.enter_context(nc.psum_tensor(f"ps{i}", [128, 512], F32)) for i in range(8)]
            self.psb = [Buf(f"ps{i}", excl=True) for i in range(8)]
            S = self.S = Sched(nc)

            ones_bf, ones_bf_b = ar.alloc([128], BF16, "ones_bf")
            S.op("dve", lambda e: e.memset(ones_bf, 1.0), writes=[ones_bf_b])
            ones_f, ones_f_b = ar.alloc([128], F32, "ones_f")
            S.op("dve", lambda e: e.memset(ones_f, 1.0), writes=[ones_f_b])
            ident_f, ident_f_b = ar.alloc([128], F32, "ident_f")
            S.op("pool", lambda e: e.affine_select(out=ident_f, in_=ones_f, pattern=[[-1, 128]],
                                                  compare_op=ALU.is_equal, fill=0.0, base=0,
                                                  channel_multiplier=1),
                 reads=[ones_f_b], writes=[ident_f_b])
            self.consts = dict(ones_bf=(ones_bf, ones_bf_b), ones_f=(ones_f, ones_f_b),
                               ident_f=(ident_f, ident_f_b))

            self.phase0(cT, cctxT, ada_w, ada_b, nmw)
            if self.stage >= 1:
                self.phase1(xT_rm, xT_cm, ctxT, w_in)
            if self.stage >= 2:
                self.phase2(inp("convw", [128, 32, 5]), inp("convb", [128, 32]), inp("dtbias", [1, 64]), inp("alog", [1, 64]),
                            inp("ssd_d", [128, 16]), inp("ssd_nw", [128, 16]))
            if self.stage >= 3:
                self.phase3_setup(inp("s5F", [2, 128, 5, 8, 64]), inp("s5S", [2, 128, 3 * 32 + 4 * 512]))
                self.phase3_main(inp("s5_d", [128, 8]))
            if self.stage >= 4:
                self.phase4(inp("glu_w", [D, D]), inp("glu_b", [128, 8]), inp("w_a", [D, D]), inp("w_b", [2 * D, D]), inp("w_o", [D, D]),
                            inp("x_tok", [L, D]), inp("nfw", [1, D]), inp("r_w", [D, 32]), inp("r_b", [1, 32]))
            if self.stage >= 5:
                self.phase5(inp("moe_wg", [4096, 8192]), inp("moe_wu", [4096, 8192]), inp("moe_wd", [4096, 8192]),
                            inp("moe_bg", [32, D]), inp("moe_bu", [32, D]), inp("moe_bd", [32, D]), inp("fnw", [1, D]))
            S.barrier()
            S.emit()
        return nc

    def phase0(self, cT, cctxT, ada_w, ada_b, nmw):
        nc, S, ar = self.nc, self.S, self.ar
        ps, psb = self.ps, self.psb
        ident_f, ident_f_b = self.consts["ident_f"]
        self.mod_rep = []
        self.Am = [ar.alloc([8], F32, f"Am{w}") for w in range(2)]
        self.Bm = [ar.alloc([8], F32, f"Bm{w}") for w in range(2)]
        nmw_t, nmw_b = ar.alloc([8], F32, "nmw")
        S.op("sp", lambda e: e.dma_start(out=nmw_t, in_=nmw), writes=[nmw_b], dma=True)
        ar.mark()
        self.mod_rep.append(ar.alloc([6 * D], F32, "mod_rep0"))
        self.mod_rep.append(ar.alloc([6 * D], F32, "mod_rep1"))
        self.mod_s, self.mod_sb = self.scratch("mod_s", [128, 6 * D], F32)
        adab, adab_b = ar.alloc([6 * D], F32, "adab")
        S.op("sp", lambda e: e.dma_start(out=adab, in_=ada_b.broadcast_to([128, 6 * D])), writes=[adab_b], dma=True)
        lhs = []
        for w, src in enumerate((cT, cctxT)):
            c_t, c_b = ar.alloc([8], F32, f"c{w}")
            S.op("sp", lambda e, c_t=c_t, src=src: e.dma_start(out=c_t, in_=src), writes=[c_b], dma=True)
            s_t, s_b = ar.alloc([8], F32, f"s{w}")
            S.op("act", lambda e, c_t=c_t, s_t=s_t: e.activation(out=s_t, in_=c_t, func=AF.Silu),
                 reads=[c_b], writes=[s_b])
            l_t, l_b = ar.alloc([8, 128], BF16, f"l{w}")
            S.op("dve", lambda e, l_t=l_t, s_t=s_t: e.tensor_copy(out=l_t, in_=s_t.unsqueeze(2).broadcast_to([128, 8, 128])),
                 reads=[s_b], writes=[l_b])
            lhs.append((l_t, l_b))
        wsrc = ada_w.rearrange("(kc p) n -> p kc n", p=128)
        wbufs = [ar.alloc([8, 512], BF16, f"adaw{i}") for i in range(2)]
        for blk in range(12):
            wt, wb = wbufs[blk % 2]
            S.op("pool", lambda e, wt=wt, blk=blk: e.dma_start(out=wt, in_=wsrc[:, :, blk * 512:(blk + 1) * 512]),
                 writes=[wb], dma=True)
            for w in range(2):
                l_t, l_b = lhs[w]
                pi = (blk * 2 + w) % 8

                def mm(e, l_t=l_t, wt=wt, pi=pi):
                    ins = None
                    for kc in range(8):
                        ins = e.matmul(ps[pi][:], lhsT=l_t[:, kc, :], rhs=wt[:, kc, :], start=(kc == 0), stop=(kc == 7))
                    return ins
                S.op("pe", mm, reads=[l_b, wb], writes=[psb[pi]])
                mt, mb = self.mod_rep[w]
                S.op("dve", lambda e, mt=mt, pi=pi, blk=blk: e.tensor_tensor(
                    out=mt[:, blk * 512:(blk + 1) * 512], in0=ps[pi][:], in1=adab[:, blk * 512:(blk + 1) * 512], op=ALU.add),
                    reads=[psb[pi], adab_b], writes=[mb])
        tmp, tmp_b = ar.alloc([8, 128], F32, "diagtmp")
        for w in range(2):
            mt, mb = self.mod_rep[w]
            for seg, (dst, dst_b) in ((0, self.Bm[w]), (1, self.Am[w])):
                view = mt[:, seg * D:(seg + 1) * D].rearrange("p (k q) -> p k q", k=8)
                S.op("dve", lambda e, view=view: e.tensor_tensor(
                    out=tmp, in0=view, in1=ident_f.unsqueeze(1).broadcast_to([128, 8, 128]), op=ALU.mult),
                    reads=[mb, ident_f_b], writes=[tmp_b])
                S.op("dve", lambda e, dst=dst: e.reduce_sum(out=dst, in_=tmp, axis=AX.X), reads=[tmp_b], writes=[dst_b])
            at, ab = self.Am[w]
            S.op("dve", lambda e, at=at: e.scalar_tensor_tensor(out=at, in0=at, scalar=1.0, in1=nmw_t, op0=ALU.add, op1=ALU.mult),
                 reads=[ab, nmw_b], writes=[ab])
        S.op("sp", lambda e: e.dma_start(out=self.mod_s, in_=self.mod_rep[0][0]), reads=[self.mod_rep[0][1]], writes=[self.mod_sb], dma=True)
        if self.debug:
            for w in range(2):
                o, ob = self.scratch(f"dbg_mod{w}", [128, 6 * D], F32, dump=True)
                mt, mb = self.mod_rep[w]
                S.op("sp", lambda e, o=o, mt=mt: e.dma_start(out=o, in_=mt), reads=[mb], writes=[ob], dma=True)
                o, ob = self.scratch(f"dbg_AB{w}", [128, 16], F32, dump=True)
                S.op("sp", lambda e, o=o, w=w: e.dma_start(out=o[:, 0:8], in_=self.Am[w][0]), reads=[self.Am[w][1]], writes=[ob], dma=True)
                S.op("sp", lambda e, o=o, w=w: e.dma_start(out=o[:, 8:16], in_=self.Bm[w][0]), reads=[self.Bm[w][1]], writes=[ob], dma=True)
        S.barrier()
        ar.release()

    def norm_bufs(self):
        ar = self.ar
        return dict(xt=[ar.alloc([8, 512], F32, f"xt{i}") for i in range(2)],
                    sq=[ar.alloc([8, 512], BF16, f"sq{i}") for i in range(2)],
                    rs=[ar.alloc([512], F32, f"rs{i}") for i in range(2)],
                    tm=[ar.alloc([512], F32, f"tm{i}") for i in range(4)], cnt=[0])

    def norm_tokens(self, src, ntok, u_t, u_b, col0, w, nb):
        nc, S, ar = self.nc, self.S, self.ar
        ps, psb = self.ps, self.psb
        ones_bf, ones_bf_b = self.consts["ones_bf"]
        At, Ab = self.Am[w]
        Bt, Bb = self.Bm[w]
        srcv = src.rearrange("(kc p) n -> p kc n", p=128)
        xt, sq, rs, tm = nb["xt"], nb["sq"], nb["rs"], nb["tm"]
        ntile = (ntok + 511) // 512
        for ti in range(ntile):
            n = min(512, ntok - ti * 512)
            ci = nb["cnt"][0]
            nb["cnt"][0] += 1
            x_t, x_b = xt[ci % 2]
            q_t, q_b = sq[ci % 2]
            r_t, r_b = rs[ci % 2]
            S.op("sp", lambda e, x_t=x_t, ti=ti, n=n: e.dma_start(out=x_t[:, :, 0:n], in_=srcv[:, :, ti * 512:ti * 512 + n]),
                 writes=[x_b], dma=True)
            S.op("act", lambda e, x_t=x_t, q_t=q_t, n=n: e.activation(out=q_t[:, :, 0:n], in_=x_t[:, :, 0:n], func=AF.Square),
                 reads=[x_b], writes=[q_b])
            pi = ci % 2

            def mm(e, q_t=q_t, pi=pi, n=n):
                ins = None
                for kc in range(8):
                    ins = e.matmul(ps[pi][:, 0:n], lhsT=ones_bf, rhs=q_t[:, kc, 0:n], start=(kc == 0), stop=(kc == 7))
                return ins
            S.op("pe", mm, reads=[q_b, ones_bf_b], writes=[psb[pi]])
            S.op("act", lambda e, r_t=r_t, pi=pi, n=n: e.activation(out=r_t[:, 0:n], in_=ps[pi][:, 0:n], func=AF.Sqrt,
                                                                  scale=1.0 / D, bias=EPS),
                 reads=[psb[pi]], writes=[r_b])
            S.op("dve", lambda e, r_t=r_t, n=n: e.reciprocal(out=r_t[:, 0:n], in_=r_t[:, 0:n]), reads=[r_b], writes=[r_b])
            for kc in range(8):
                t_t, t_b = tm[kc % 4]
                S.op("dve", lambda e, t_t=t_t, x_t=x_t, r_t=r_t, kc=kc, n=n: e.tensor_tensor(
                    out=t_t[:, 0:n], in0=x_t[:, kc, 0:n], in1=r_t[:, 0:n], op=ALU.mult),
                    reads=[x_b, r_b], writes=[t_b])
                S.op("act", lambda e, t_t=t_t, kc=kc, ti=ti, n=n: e.activation(
                    out=u_t[:, kc, col0 + ti * 512:col0 + ti * 512 + n], in_=t_t[:, 0:n], func=AF.Identity,
                    scale=At[:, kc:kc + 1], bias=Bt[:, kc:kc + 1]),
                    reads=[t_b, Ab, Bb], writes=[u_b])

    def proj_block(self, wsrc, c0, ncols, u_t, u_b, tok_ranges, dst, dst_b, row0, wbufs, stg, cnt):
        S = self.S
        ps, psb = self.ps, self.psb
        wt, wb = wbufs[cnt[0] % 2]
        cnt[0] += 1
        S.op("pool", lambda e: e.dma_start(out=wt[:, :, 0:ncols], in_=wsrc[:, :, c0:c0 + ncols]), writes=[wb], dma=True)
        for (ucol, n, dcol) in tok_ranges:
            for mc in range(ncols // 128):
                pi = cnt[1] % 8
                cnt[1] += 1

                def mm(e, pi=pi, mc=mc, ucol=ucol, n=n):
                    ins = None
                    for kc in range(8):
                        ins = e.matmul(ps[pi][:, 0:n], lhsT=wt[:, kc, mc * 128:(mc + 1) * 128],
                                       rhs=u_t[:, kc, ucol:ucol + n], start=(kc == 0), stop=(kc == 7))
                    return ins
                S.op("pe", mm, reads=[wb, u_b], writes=[psb[pi]])
                st, sb = stg[cnt[2] % len(stg)]
                eng = "act" if cnt[2] % 2 == 0 else "dve"
                cnt[2] += 1
                if eng == "act":
                    S.op("act", lambda e, st=st, pi=pi, n=n: e.activation(out=st[:, 0:n], in_=ps[pi][:, 0:n], func=AF.Copy),
                         reads=[psb[pi]], writes=[sb])
                else:
                    S.op("dve", lambda e, st=st, pi=pi, n=n: e.tensor_copy(out=st[:, 0:n], in_=ps[pi][:, 0:n]),
                         reads=[psb[pi]], writes=[sb])
                r = row0 + mc * 128
                S.op("sp", lambda e, st=st, r=r, dcol=dcol, n=n: e.dma_start(out=dst[r:r + 128, dcol:dcol + n], in_=st[:, 0:n]),
                     reads=[sb], writes=[dst_b], dma=True)

    def phase1(self, xT_rm, xT_cm, ctxT, w_in):
        nc, S, ar = self.nc, self.S, self.ar
        ps, psb = self.ps, self.psb
        dbg = self.debug
        self.s5u, self.s5u_b = self.scratch("s5u", [D, T], BF16, dump=dbg)
        self.xbc_s, self.xbc_sb = self.scratch("xbc_s", [4096, T], BF16, dump=dbg)
        self.z_s, self.z_sb = self.scratch("z_s", [2048, L], BF16, dump=dbg)
        self.gab_s, self.gab_sb = self.scratch("gab_s", [2048, L], BF16, dump=dbg)
        self.dt_s, self.dt_sb = self.scratch("dt_s", [128, 34 * 64], F32, dump=dbg)
        wsrc = w_in.rearrange("(kc p) n -> p kc n", p=128)
        ar.mark()
        u_t, u_b = ar.alloc([8, T], BF16, "u")
        wbufs = [ar.alloc([8, 512], BF16, f"wb{i}") for i in range(2)]
        stg = [ar.alloc([512], BF16, f"stg{i}") for i in range(4)]
        cnt = [0, 0, 0]
        nb = self.norm_bufs()
        self.norm_tokens(ctxT, LC, u_t, u_b, 0, 1, nb)
        self.norm_tokens(xT_rm, L, u_t, u_b, LC, 0, nb)
        if dbg:
            o, ob = self.scratch("dbg_u_rm", [128, 8 * T], BF16, dump=True)
            S.op("sp", lambda e: e.dma_start(out=o, in_=u_t.rearrange("p a b -> p (a b)")), reads=[u_b], writes=[ob], dma=True)
        toks = [(0, LC, 0)] + [(LC + i * 512, 512, LC + i * 512) for i in range(8)]
        for blk in range(2):
            self.proj_block(wsrc, IN_S5[0] + blk * 512, 512, u_t, u_b, toks, self.s5u, self.s5u_b, blk * 512, wbufs, stg, cnt)
        self.norm_tokens(xT_cm, L, u_t, u_b, LC, 0, nb)
        for blk in range(8):
            self.proj_block(wsrc, IN_XBC[0] + blk * 512, 512, u_t, u_b, toks, self.xbc_s, self.xbc_sb, blk * 512, wbufs, stg, cnt)
        ltoks = [(LC + i * 512, 512, i * 512) for i in range(8)]
        for blk in range(4):
            self.proj_block(wsrc, IN_Z[0] + blk * 512, 512, u_t, u_b, ltoks, self.z_s, self.z_sb, blk * 512, wbufs, stg, cnt)
        for blk in range(4):
            self.proj_block(wsrc, IN_GA[0] + blk * 512, 512, u_t, u_b, ltoks, self.gab_s, self.gab_sb, blk * 512, wbufs, stg, cnt)
        wdt, wdt_b = ar.alloc([8, 64], BF16, "wdt")
        S.op("pool", lambda e: e.dma_start(out=wdt, in_=wsrc[:, :, IN_DT[0]:IN_DT[1]]), writes=[wdt_b], dma=True)
        dtt, dtt_b = ar.alloc([34, 64], F32, "dtt")
        for c in range(34):
            pi = c % 2

            def mm(e, c=c, pi=pi):
                ins = None
                for kc in range(8):
                    ins = e.matmul(ps[pi][:, 0:64], lhsT=u_t[:, kc, c * 128:(c + 1) * 128], rhs=wdt[:, kc, :],
                                   start=(kc == 0), stop=(kc == 7))
                return ins
            S.op("pe", mm, reads=[u_b, wdt_b], writes=[psb[pi]])
            S.op("dve", lambda e, c=c, pi=pi: e.tensor_copy(out=dtt[:, c, :], in_=ps[pi][:, 0:64]), reads=[psb[pi]], writes=[dtt_b])
        S.op("sp", lambda e: e.dma_start(out=self.dt_s, in_=dtt.rearrange("p a b -> p (a b)")), reads=[dtt_b], writes=[self.dt_sb], dma=True)
        S.barrier()
        ar.release()


    def pq(self, i, j0, j1=None):
        if j1 is None:
            j1 = j0 + 1
        return self.ps[i][:, j0 * 128:j1 * 128], [self.psb[i]]

    def phase2(self, convw, convb, dtbias, alog, ssd_d, ssd_nw):
        nc, S, ar = self.nc, self.S, self.ar
        ps = self.ps
        dbg = self.debug
        ident_f, ident_f_b = self.consts["ident_f"]
        ones_f, ones_f_b = self.consts["ones_f"]
        ones_bf, ones_bf_b = self.consts["ones_bf"]
        self.yb_s, self.yb_sb = self.scratch("yb_s", [2048, L], BF16, dump=dbg)
        ar.mark()
        ident_bf, ident_bf_b = ar.alloc([128], BF16, "ident_bf")
        S.op("dve", lambda e: e.tensor_copy(out=ident_bf, in_=ident_f), reads=[ident_f_b], writes=[ident_bf_b])
        tri, tri_b = ar.alloc([128], F32, "tri")
        triT, triT_b = ar.alloc([128], F32, "triT")
        S.op("pool", lambda e: e.affine_select(out=tri, in_=ones_f, pattern=[[1, 128]], compare_op=ALU.is_ge, fill=0.0,
                                              base=0, channel_multiplier=-1), reads=[ones_f_b], writes=[tri_b])
        S.op("pool", lambda e: e.affine_select(out=triT, in_=ones_f, pattern=[[-1, 128]], compare_op=ALU.is_ge, fill=0.0,
                                              base=0, channel_multiplier=1), reads=[ones_f_b], writes=[triT_b])
        zer, zer_b = ar.alloc([128], F32, "zer")
        S.op("dve", lambda e: e.memset(zer, 0.0), writes=[zer_b])
        NEG = [ar.alloc([128], BF16, f"NEG{d}") for d in range(2)]
        S.op("pool", lambda e: e.affine_select(out=NEG[0][0], in_=zer, pattern=[[1, 128]], compare_op=ALU.is_ge, fill=-60000.0,
                                              base=0, channel_multiplier=-1), reads=[zer_b], writes=[NEG[0][1]])
        S.op("pool", lambda e: e.affine_select(out=NEG[1][0], in_=zer, pattern=[[-1, 128]], compare_op=ALU.is_ge, fill=-60000.0,
                                              base=0, channel_multiplier=1), reads=[zer_b], writes=[NEG[1][1]])
        oh2, oh2_b = ar.alloc([64], F32, "oh2")
        S.op("dve", lambda e: e.tensor_tensor(out=oh2[:, 0:32], in0=ident_f[:, 0:32], in1=ident_f[:, 32:64], op=ALU.add),
             reads=[ident_f_b], writes=[oh2_b])
        S.op("dve", lambda e: e.tensor_tensor(out=oh2[:, 32:64], in0=ident_f[:, 64:96], in1=ident_f[:, 96:128], op=ALU.add),
             reads=[ident_f_b], writes=[oh2_b])
        sel2, sel2_b = ar.alloc([64, 128], BF16, "sel2")
        S.op("dve", lambda e: e.tensor_copy(out=sel2, in_=oh2.unsqueeze(2).broadcast_to([128, 64, 128])), reads=[oh2_b], writes=[sel2_b])
        mhi, mhi_b = ar.alloc([1], F32, "mhi")
        mlo, mlo_b = ar.alloc([1], F32, "mlo")
        mt_, mt_b = ar.alloc([1], F32, "mtmp")
        S.op("dve", lambda e: e.reduce_sum(out=mhi, in_=ident_f[:, 0:32], axis=AX.X), reads=[ident_f_b], writes=[mhi_b])
        S.op("dve", lambda e: e.reduce_sum(out=mt_, in_=ident_f[:, 64:96], axis=AX.X), reads=[ident_f_b], writes=[mt_b])
        S.op("dve", lambda e: e.tensor_tensor(out=mhi, in0=mhi, in1=mt_, op=ALU.add), reads=[mhi_b, mt_b], writes=[mhi_b])
        S.op("dve", lambda e: e.tensor_scalar(out=mlo, in0=mhi, scalar1=-1.0, scalar2=1.0, op0=ALU.mult, op1=ALU.add),
             reads=[mhi_b], writes=[mlo_b])
        cw, cw_b = ar.alloc([32, 5], F32, "convw")
        cb, cb_b = ar.alloc([32], F32, "convb")
        dcol, dcol_b = ar.alloc([16], F32, "ssd_d")
        nwc, nwc_b = ar.alloc([16], F32, "ssd_nw")
        S.op("sp", lambda e: e.dma_start(out=cw, in_=convw), writes=[cw_b], dma=True)
        S.op("sp", lambda e: e.dma_start(out=cb, in_=convb), writes=[cb_b], dma=True)
        S.op("sp", lambda e: e.dma_start(out=dcol, in_=ssd_d), writes=[dcol_b], dma=True)
        S.op("sp", lambda e: e.dma_start(out=nwc, in_=ssd_nw), writes=[nwc_b], dma=True)
        dt, dt_b = ar.alloc([34, 2, 32], F32, "dt")
        decay, decay_b = ar.alloc([34, 2, 32], F32, "decay")
        wend, wend_b = ar.alloc([34, 2, 32], F32, "wend")
        HL, HL_b = ar.alloc([34, 128], BF16, "HL")
        HLn, HLn_b = ar.alloc([34, 128], BF16, "HLn")
        ar.mark()
        nacum, nacum_b = ar.alloc([34, 2, 32], F32, "nacum")
        adtd, adtd_b = ar.alloc([34, 4, 32], F32, "adtd")
        tot, tot_b = ar.alloc([34, 2, 32], F32, "tot")
        dtb, dtb_b = ar.alloc([2, 32], F32, "dtb")
        arep, arep_b = ar.alloc([2, 32], F32, "arep")
        S.op("sp", lambda e: e.dma_start(out=dt.rearrange("p a b c -> p (a b c)"), in_=self.dt_s), reads=[self.dt_sb], writes=[dt_b], dma=True)
        S.op("sp", lambda e: e.dma_start(out=dtb.rearrange("p a b -> p (a b)"), in_=dtbias.broadcast_to([128, 64])), writes=[dtb_b], dma=True)
        S.op("sp", lambda e: e.dma_start(out=arep.rearrange("p a b -> p (a b)"), in_=alog.broadcast_to([128, 64])), writes=[arep_b], dma=True)
        S.op("dve", lambda e: e.tensor_tensor(out=dt, in0=dt, in1=dtb.unsqueeze(1).broadcast_to([128, 34, 2, 32]), op=ALU.add),
             reads=[dt_b, dtb_b], writes=[dt_b])
        S.op("act", lambda e: e.activation(out=dt, in_=dt, func=AF.Exp), reads=[dt_b], writes=[dt_b])
        S.op("act", lambda e: e.activation(out=dt, in_=dt, func=AF.Ln, bias=1.0), reads=[dt_b], writes=[dt_b])
        S.op("act", lambda e: e.activation(out=arep, in_=arep, func=AF.Exp), reads=[arep_b], writes=[arep_b])
        S.op("dve", lambda e: e.tensor_scalar(out=arep, in0=arep, scalar1=-1.0, scalar2=None, op0=ALU.mult), reads=[arep_b], writes=[arep_b])
        for d in range(2):
            for j in range(2):
                S.op("dve", lambda e, d=d, j=j: e.tensor_tensor(
                    out=adtd[:, :, 2 * d + j, :], in0=dt[:, :, d, :], in1=arep[:, d, :].unsqueeze(1).broadcast_to([128, 34, 32]), op=ALU.mult),
                    reads=[dt_b, arep_b], writes=[adtd_b])
        k = 0
        for d in range(2):
            lhs, lhs_b = (tri, tri_b) if d == 0 else (triT, triT_b)
            for (c0, ncn) in ((0, 16), (16, 16), (32, 2)):
                pa, pb = self.pq(k % 4, 0, 4)
                k += 1
                S.op("pe", lambda e, pa=pa, lhs=lhs, c0=c0, ncn=ncn, d=d: e.matmul(
                    pa[:, 0:ncn * 32].rearrange("p (a b) -> p a b", b=32), lhsT=lhs, rhs=adtd[:, c0:c0 + ncn, 2 * d, :], start=True, stop=True),
                    reads=[lhs_b, adtd_b], writes=pb)
                S.op("dve", lambda e, pa=pa, c0=c0, ncn=ncn, d=d: e.tensor_scalar(
                    out=nacum[:, c0:c0 + ncn, d, :], in0=pa[:, 0:ncn * 32].rearrange("p (a b) -> p a b", b=32),
                    scalar1=-1.0, scalar2=None, op0=ALU.mult), reads=pb, writes=[nacum_b])
                pa, pb = self.pq(k % 4, 0, 4)
                k += 1
                S.op("pe", lambda e, pa=pa, c0=c0, ncn=ncn, d=d: e.matmul(
                    pa[:, 0:ncn * 32].rearrange("p (a b) -> p a b", b=32), lhsT=ones_f, rhs=adtd[:, c0:c0 + ncn, 2 * d, :], start=True, stop=True),
                    reads=[ones_f_b, adtd_b], writes=pb)
                S.op("dve", lambda e, pa=pa, c0=c0, ncn=ncn, d=d: e.tensor_copy(
                    out=tot[:, c0:c0 + ncn, d, :], in_=pa[:, 0:ncn * 32].rearrange("p (a b) -> p a b", b=32)), reads=pb, writes=[tot_b])
        S.op("act", lambda e: e.activation(out=decay, in_=tot, func=AF.Exp), reads=[tot_b], writes=[decay_b])
        S.op("dve", lambda e: e.tensor_tensor(out=wend, in0=tot, in1=nacum, op=ALU.add), reads=[tot_b, nacum_b], writes=[wend_b])
        S.op("act", lambda e: e.activation(out=wend, in_=wend, func=AF.Exp), reads=[wend_b], writes=[wend_b])
        S.op("dve", lambda e: e.tensor_tensor(out=wend, in0=wend, in1=dt, op=ALU.mult), reads=[wend_b, dt_b], writes=[wend_b])
        acT = [ar.alloc([128], F32, f"acT{i}") for i in range(2)]
        hi_ = [ar.alloc([128], BF16, f"hi{i}") for i in range(2)]
        lo_ = [ar.alloc([128], F32, f"lo{i}") for i in range(2)]
        for c in range(34):
            pa, pab = self.pq(4 + (c % 2), 0)
            pb_, pbb = self.pq(4 + (c % 2), 1)
            lhsT = adtd[:, c, :, :].rearrange("p a b -> p (a b)")
            S.op("pe", lambda e, pa=pa, lhsT=lhsT: e.matmul(pa, lhsT=lhsT, rhs=tri, start=True, stop=True),
                 reads=[adtd_b, tri_b], writes=pab)
            S.op("pe", lambda e, pb_=pb_, lhsT=lhsT: e.matmul(pb_, lhsT=lhsT, rhs=triT, start=True, stop=True),
                 reads=[adtd_b, triT_b], writes=pbb)
            a_t, a_b = acT[c % 2]
            h_t, h_b = hi_[c % 2]
            l_t, l_b = lo_[c % 2]
            S.op("act", lambda e, a_t=a_t, pa=pa: e.activation(out=a_t[0:64, :], in_=pa[0:64, :], func=AF.Copy), reads=pab, writes=[a_b])
            S.op("act", lambda e, a_t=a_t, pb_=pb_: e.activation(out=a_t[64:128, :], in_=pb_[64:128, :], func=AF.Copy), reads=pbb, writes=[a_b])
            S.op("dve", lambda e, a_t=a_t, h_t=h_t: e.tensor_copy(out=h_t, in_=a_t), reads=[a_b], writes=[h_b])
            S.op("dve", lambda e, a_t=a_t, h_t=h_t, l_t=l_t: e.tensor_tensor(out=l_t, in0=a_t, in1=h_t, op=ALU.subtract), reads=[a_b, h_b], writes=[l_b])
            S.op("dve", lambda e, l_t=l_t: e.tensor_scalar(out=l_t, in0=l_t, scalar1=mlo[:, 0:1], scalar2=None, op0=ALU.mult), reads=[l_b, mlo_b], writes=[l_b])
            S.op("dve", lambda e, h_t=h_t, l_t=l_t, c=c: e.scalar_tensor_tensor(out=HL[:, c, :], in0=h_t, scalar=mhi[:, 0:1], in1=l_t, op0=ALU.mult, op1=ALU.add),
                 reads=[h_b, l_b, mhi_b], writes=[HL_b])
        S.op("dve", lambda e: e.tensor_scalar(out=HLn, in0=HL, scalar1=-1.0, scalar2=None, op0=ALU.mult), reads=[HL_b], writes=[HLn_b])
        if dbg:
            for nm, src_, sb_, dtp, ncol in (("dbg_dt", dt.rearrange("p a b c -> p (a b c)"), dt_b, F32, 34 * 64),
                                             ("dbg_nacum", nacum.rearrange("p a b c -> p (a b c)"), nacum_b, F32, 34 * 64),
                                             ("dbg_wend", wend.rearrange("p a b c -> p (a b c)"), wend_b, F32, 34 * 64),
                                             ("dbg_HL", HL.rearrange("p a b -> p (a b)"), HL_b, BF16, 34 * 128)):
                o, ob = self.scratch(nm, [128, ncol], dtp, dump=True)
                S.op("sp", lambda e, o=o, src_=src_: e.dma_start(out=o, in_=src_), reads=[sb_], writes=[ob], dma=True)
        S.barrier()
        ar.release()
        P2STOP = int(os.environ.get("P2STOP", "99"))
        P2SKIP = os.environ.get("P2SKIP", "")
        if P2STOP <= 1:
            ar.release()
            return
        TP = 4360
        RA, RA_b = ar.alloc([4 * TP], BF16, "RA")
        R = RA.rearrange("p (a b) -> p a b", a=4)
        prevs = RA[:, 0:2 * 34 * 256].rearrange("p (d c n) -> p d c n", d=2, c=34)
        xc, xc_b = ar.alloc([4, T], BF16, "xc")
        x_tok, x_tok_b = ar.alloc([34, 256], BF16, "x_tok")
        B_tok, B_tok_b = ar.alloc([34, 128], BF16, "B_tok")
        dg, dg_b = ar.alloc([20, 128], BF16, "dg")
        st = [ar.alloc([256], F32, f"st{d}") for d in range(2)]
        xw = [ar.alloc([256], BF16, f"xw{i}") for i in range(4)]
        xdt = [ar.alloc([256], BF16, f"xdt{i}") for i in range(4)]
        E_ = [ar.alloc([4, 128], F32, f"E{i}") for i in range(3)]
        Lt = [ar.alloc([4, 128], F32, f"Lt{i}") for i in range(3)]
        Gt = [ar.alloc([4, 128], BF16, f"Gt{i}") for i in range(4)]
        Cp = [ar.alloc([4, 128], BF16, f"Cp{i}") for i in range(4)]
        zt = [ar.alloc([2, 512], BF16, f"zt{i}") for i in range(1)]
        sz = [ar.alloc([2, 512], F32, f"sz{i}") for i in range(1)]
        yz = [ar.alloc([2, 512], F32, f"yz{i}") for i in range(1)]
        sqy = [ar.alloc([2, 512], BF16, f"sqy{i}") for i in range(1)]
        rsy = [ar.alloc([512], F32, f"rsy{i}") for i in range(1)]
        ybo = [ar.alloc([2, 512], BF16, f"ybo{i}") for i in range(1)]
        bwd_order = [1, 0] + list(range(33, 1, -1))
        cnt = dict(e=0, a=0, g=0, cp=0, xw=0, cs=0, y=0, cb=0, post=0, conv=0, tr=0)
        for g in range(int(os.environ.get('P2G', '8'))):
            tiles = [2 * g, 2 * g + 1, 16 + g, 24 + g]
            for i, tix in enumerate(tiles):
                S.op("sp", lambda e, i=i, tix=tix: e.dma_start(out=R[:, i, 2:258], in_=self.xbc_s[tix * 128:(tix + 1) * 128, 0:256]),
                     reads=[self.xbc_sb], writes=[RA_b], dma=True)
                S.op("sp", lambda e, i=i, tix=tix: e.dma_start(out=R[:, i, 262:4358], in_=self.xbc_s[tix * 128:(tix + 1) * 128, 256:T]),
                     reads=[self.xbc_sb], writes=[RA_b], dma=True)
            for (a0, a1) in ((0, 2), (258, 262), (4358, 4360)):
                S.op("dve", lambda e, a0=a0, a1=a1: e.memset(R[:, :, a0:a1], 0.0), writes=[RA_b])
            for i, tix in enumerate(tiles):
                for kk in range(5):
                    S.op("dve", lambda e, i=i, tix=tix, kk=kk: e.tensor_scalar(
                        out=dg[:, i * 5 + kk, :], in0=ident_f, scalar1=cw[:, tix, kk:kk + 1], scalar2=None, op0=ALU.mult),
                        reads=[ident_f_b, cw_b], writes=[dg_b])
            for i, tix in enumerate(tiles):
                for (t0, n, base) in [(0, 256, 0)] + [(256 + j * 512, 512, 260 + j * 512) for j in range(8)]:
                    pa, pb = self.pq(cnt["conv"] % 2, 0, 4)
                    cnt["conv"] += 1

                    def mm(e, pa=pa, i=i, n=n, base=base):
                        ins = None
                        for kk in range(5):
                            ins = e.matmul(pa[:, 0:n], lhsT=dg[:, i * 5 + kk, :], rhs=R[:, i, base + kk:base + kk + n],
                                           start=(kk == 0), stop=(kk == 4))
                        return ins
                    S.op("pe", mm, reads=[dg_b, RA_b], writes=pb)
                    S.op("act", lambda e, pa=pa, i=i, tix=tix, t0=t0, n=n: e.activation(
                        out=xc[:, i, t0:t0 + n], in_=pa[:, 0:n], func=AF.Silu, bias=cb[:, tix:tix + 1]),
                        reads=pb + [cb_b], writes=[xc_b])
            if P2STOP <= 2:
                break
            for c in range(34):
                pa, pb = self.pq(2 + (cnt["tr"] % 2), 0, 4)
                cnt["tr"] += 1
                pab = pa.bitcast(BF16)

                def tr(e, pab=pab, c=c):
                    ins = None
                    for j in range(3):
                        ins = e.transpose(out=pab[:, j * 128:(j + 1) * 128], in_=xc[:, j, c * 128:(c + 1) * 128], identity=ident_bf)
                    return ins
                S.op("pe", tr, reads=[xc_b, ident_bf_b], writes=pb)
                S.op("dve", lambda e, pab=pab, c=c: e.tensor_copy(out=x_tok[:, c, :], in_=pab[:, 0:256]), reads=pb, writes=[x_tok_b])
                S.op("dve", lambda e, pab=pab, c=c: e.tensor_copy(out=B_tok[:, c, :], in_=pab[:, 256:384]), reads=pb, writes=[B_tok_b])
            if P2STOP <= 3:
                break
            for d in range(2):
                S.op("dve", lambda e, d=d: e.memset(st[d][0], 0.0), writes=[st[d][1]])
            for step in range(34):
                for d in range(2):
                    c = step if d == 0 else bwd_order[step]
                    xw_t, xw_b = xw[cnt["xw"] % 4]
                    cnt["xw"] += 1
                    S.op("dve", lambda e, xw_t=xw_t, c=c, d=d, g=g: e.tensor_tensor(
                        out=xw_t.rearrange("p (r q) -> p r q", r=4), in0=x_tok[:, c, :].rearrange("p (r q) -> p r q", r=4),
                        in1=wend[:, c, d, 4 * g:4 * g + 4].unsqueeze(2).broadcast_to([128, 4, 64]), op=ALU.mult),
                        reads=[x_tok_b, wend_b], writes=[xw_b])
                    k2 = cnt["cs"] % 4
                    cnt["cs"] += 1
                    pa, pb = self.pq(4 + k2, 0, 2)
                    S.op("pe", lambda e, pa=pa, xw_t=xw_t, c=c: e.matmul(pa, lhsT=B_tok[:, c, :], rhs=xw_t, start=True, stop=True),
                         reads=[B_tok_b, xw_b], writes=pb)
                    st_t, st_b = st[d]
                    S.op("act", lambda e, st_t=st_t, d=d, c=c: e.activation(out=prevs[:, d, c, :], in_=st_t, func=AF.Copy),
                         reads=[st_b], writes=[RA_b])
                    S.op("dve", lambda e, st_t=st_t, c=c, d=d, g=g: e.tensor_tensor(
                        out=st_t.rearrange("p (r q) -> p r q", r=4), in0=st_t.rearrange("p (r q) -> p r q", r=4),
                        in1=decay[:, c, d, 4 * g:4 * g + 4].unsqueeze(2).broadcast_to([128, 4, 64]), op=ALU.mult),
                        reads=[st_b, decay_b], writes=[st_b])
                    S.op("dve", lambda e, st_t=st_t, pa=pa: e.tensor_tensor(out=st_t, in0=st_t, in1=pa, op=ALU.add),
                         reads=[st_b] + pb, writes=[st_b])
            if dbg and g == 0:
                o, ob = self.scratch("dbg_xc", [128, 4 * T], BF16, dump=True)
                S.op("sp", lambda e, o=o: e.dma_start(out=o, in_=xc.rearrange("p a b -> p (a b)")), reads=[xc_b], writes=[ob], dma=True)
                o2, ob2 = self.scratch("dbg_prev", [128, 2 * 34 * 256], BF16, dump=True)
                S.op("sp", lambda e, o2=o2: e.dma_start(out=o2, in_=RA[:, 0:2 * 34 * 256]), reads=[RA_b], writes=[ob2], dma=True)
            if P2STOP <= 4:
                continue
            for cb4 in range(8):
                if P2STOP <= 5 and cb4 >= 1:
                    break
                pi = cnt["post"] % 2
                cnt["post"] += 1
                z_t, z_b = zt[0]
                sz_t, sz_b = sz[0]
                yz_t, yz_b = yz[0]
                for i in range(2):
                    r0 = 256 * g + 128 * i
                    S.op("sp", lambda e, z_t=z_t, i=i, r0=r0, cb4=cb4: e.dma_start(out=z_t[:, i, :], in_=self.z_s[r0:r0 + 128, cb4 * 512:(cb4 + 1) * 512]),
                         reads=[self.z_sb], writes=[z_b], dma=True)
                if "Z" not in P2SKIP:
                    S.op("act", lambda e, z_t=z_t, sz_t=sz_t: e.activation(out=sz_t, in_=z_t, func=AF.Silu), reads=[z_b], writes=[sz_b])
                for cl in range(4):
                    lc = cb4 * 4 + cl
                    c = 2 + lc
                    tsl = slice(c * 128, (c + 1) * 128)
                    pcb, pcb_b = self.pq(4 + cnt["cb"] % 2, 0)
                    cnt["cb"] += 1
                    S.op("pe", lambda e, pcb=pcb, tsl=tsl: e.matmul(pcb, lhsT=xc[:, 2, tsl], rhs=xc[:, 3, tsl], start=True, stop=True),
                         reads=[xc_b], writes=pcb_b)
                    gts, cps, xds = {}, {}, {}
                    for d in range(2):
                        xd_t, xd_b = xdt[cnt["xw"] % 4]
                        cnt["xw"] += 1
                        S.op("dve", lambda e, xd_t=xd_t, c=c, d=d, g=g: e.tensor_tensor(
                            out=xd_t.rearrange("p (r q) -> p r q", r=4), in0=x_tok[:, c, :].rearrange("p (r q) -> p r q", r=4),
                            in1=dt[:, c, d, 4 * g:4 * g + 4].unsqueeze(2).broadcast_to([128, 4, 64]), op=ALU.mult),
                            reads=[x_tok_b, dt_b], writes=[xd_b])
                        xds[d] = (xd_t, xd_b)
                        pe_, pe_b = self.pq(cnt["e"] % 2, 0, 4)
                        cnt["e"] += 1

                        def mme(e, pe_=pe_, c=c, d=d, g=g):
                            ins = None
                            for r in range(4):
                                hh = d * 32 + 4 * g + r
                                ins = e.matmul(pe_[:, r * 128:(r + 1) * 128], lhsT=sel2[:, hh, :], rhs=HL[:, c, :], start=True, stop=True)
                            return ins
                        S.op("pe", mme, reads=[sel2_b, HL_b], writes=pe_b)
                        E_t, E_b = E_[cnt["e"] % 3]
                        S.op("act", lambda e, E_t=E_t, pe_=pe_: e.activation(out=E_t.rearrange("p a b -> p (a b)"), in_=pe_, func=AF.Exp),
                             reads=pe_b, writes=[E_b])
                        cp_t, cp_b = Cp[cnt["cp"] % 4]
                        cnt["cp"] += 1
                        S.op("dve", lambda e, cp_t=cp_t, E_t=E_t, tsl=tsl: e.tensor_tensor(
                            out=cp_t, in0=E_t, in1=xc[:, 3, tsl].unsqueeze(1).broadcast_to([128, 4, 128]), op=ALU.mult),
                            reads=[xc_b, E_b], writes=[cp_b])
                        cps[d] = (cp_t, cp_b)
                        pa_, pa_b = self.pq(2 + cnt["a"] % 2, 0, 4)
                        cnt["a"] += 1

                        def mma(e, pa_=pa_, c=c, d=d, g=g):
                            ins = None
                            for r in range(4):
                                hh = d * 32 + 4 * g + r
                                o_ = pa_[:, r * 128:(r + 1) * 128]
                                e.matmul(o_, lhsT=sel2[:, hh, :], rhs=HL[:, c, :], start=True, stop=False)
                                e.matmul(o_, lhsT=HLn[:, c, :], rhs=sel2[:, hh, :], start=False, stop=False)
                                ins = e.matmul(o_, lhsT=ident_bf, rhs=NEG[d][0], start=False, stop=True)
                            return ins
                        S.op("pe", mma, reads=[sel2_b, HL_b, HLn_b, ident_bf_b, NEG[d][1]], writes=pa_b)
                        L_t, L_b = Lt[cnt["a"] % 3]
                        S.op("act", lambda e, L_t=L_t, pa_=pa_: e.activation(out=L_t.rearrange("p a b -> p (a b)"), in_=pa_, func=AF.Exp),
                             reads=pa_b, writes=[L_b])
                        g_t, g_b = Gt[cnt["g"] % 4]
                        cnt["g"] += 1
                        S.op("dve", lambda e, g_t=g_t, L_t=L_t, pcb=pcb: e.tensor_tensor(
                            out=g_t, in0=L_t, in1=pcb.unsqueeze(1).broadcast_to([128, 4, 128]), op=ALU.mult),
                            reads=[L_b] + pcb_b, writes=[g_b])
                        gts[d] = (g_t, g_b)
                    yb_i = 6 + cnt["y"] % 2
                    cnt["y"] += 1
                    pys = [self.pq(yb_i, i) for i in range(2)]

                    def mmy(e, pys=pys, c=c, gts=gts, cps=cps, xds=xds):
                        ins = None
                        for i in range(2):
                            py = pys[i][0]
                            for step in range(4):
                                for hf in range(2):
                                    r = 2 * i + hf
                                    o_ = py[64 * hf:64 * hf + 64, :]
                                    tp = (0, 64 * hf)
                                    d_ = step // 2
                                    if step % 2 == 0:
                                        ins = e.matmul(o_, lhsT=xds[d_][0][:, r * 64:(r + 1) * 64], rhs=gts[d_][0][:, r, :], start=(step == 0), stop=False, tile_position=tp)
                                    else:
                                        ins = e.matmul(o_, lhsT=prevs[:, d_, c, r * 64:(r + 1) * 64], rhs=cps[d_][0][:, r, :], start=False, stop=(step == 3), tile_position=tp)
                        return ins
                    rd = [RA_b]
                    for d in range(2):
                        rd += [gts[d][1], cps[d][1], xds[d][1]]
                    S.op("pe", mmy, reads=rd, writes=pys[0][1])
                    for i in range(2):
                        py = pys[i][0]
                        S.op("dve", lambda e, py=py, i=i, g=g, tsl=tsl, yz_t=yz_t, cl=cl: e.scalar_tensor_tensor(
                            out=yz_t[:, i, cl * 128:(cl + 1) * 128], in0=xc[:, i, tsl], scalar=dcol[:, 2 * g + i:2 * g + i + 1], in1=py,
                            op0=ALU.mult, op1=ALU.add), reads=[xc_b, dcol_b] + pys[i][1], writes=[yz_b])
                if "O" in P2SKIP:
                    continue
                q_t, q_b = sqy[0]
                r_t, r_b = rsy[0]
                o_t, o_b = ybo[0]
                S.op("dve", lambda e, yz_t=yz_t, sz_t=sz_t: e.tensor_tensor(out=yz_t, in0=yz_t, in1=sz_t, op=ALU.mult), reads=[yz_b, sz_b], writes=[yz_b])
                S.op("act", lambda e, yz_t=yz_t, q_t=q_t: e.activation(out=q_t, in_=yz_t, func=AF.Square), reads=[yz_b], writes=[q_b])
                pp, pp_b = self.pq(pi, 0, 4)

                def mmq(e, pp=pp, q_t=q_t):
                    e.matmul(pp, lhsT=ones_bf, rhs=q_t[:, 0, :], start=True, stop=False)
                    return e.matmul(pp, lhsT=ones_bf, rhs=q_t[:, 1, :], start=False, stop=True)
                S.op("pe", mmq, reads=[q_b, ones_bf_b], writes=pp_b)
                S.op("act", lambda e, pp=pp, r_t=r_t: e.activation(out=r_t, in_=pp, func=AF.Sqrt, scale=1.0 / 256, bias=EPS), reads=pp_b, writes=[r_b])
                S.op("dve", lambda e, r_t=r_t: e.reciprocal(out=r_t, in_=r_t), reads=[r_b], writes=[r_b])
                for i in range(2):
                    S.op("dve", lambda e, i=i, yz_t=yz_t, r_t=r_t, o_t=o_t, g=g: e.scalar_tensor_tensor(
                        out=o_t[:, i, :], in0=yz_t[:, i, :], scalar=nwc[:, 2 * g + i:2 * g + i + 1], in1=r_t, op0=ALU.mult, op1=ALU.mult),
                        reads=[yz_b, r_b, nwc_b], writes=[o_b])
                    r0 = 256 * g + 128 * i
                    S.op("sp", lambda e, o_t=o_t, i=i, r0=r0, cb4=cb4: e.dma_start(out=self.yb_s[r0:r0 + 128, cb4 * 512:(cb4 + 1) * 512], in_=o_t[:, i, :]),
                         reads=[o_b], writes=[self.yb_sb], dma=True)
        S.barrier()
        ar.release()


    def trig(self, ar, src, n, add, out, out_b, src_b, tag):
        S = self.S
        t, t_b = ar.alloc([n], F32, f"trg_t{tag}")
        ti, ti_b = ar.alloc([n], I32, f"trg_i{tag}")
        S.op("dve", lambda e: e.tensor_scalar(out=t, in0=src, scalar1=64.0 + add, scalar2=None, op0=ALU.add), reads=[src_b], writes=[t_b])
        S.op("dve", lambda e: e.tensor_copy(out=ti, in_=t), reads=[t_b], writes=[ti_b])
        S.op("dve", lambda e: e.tensor_copy(out=out, in_=ti), reads=[ti_b], writes=[out_b])
        S.op("dve", lambda e: e.tensor_tensor(out=t, in0=t, in1=out, op=ALU.subtract), reads=[t_b, out_b], writes=[t_b])
        S.op("act", lambda e: e.activation(out=out, in_=t, func=AF.Sin, scale=2.0 * np.pi), reads=[t_b], writes=[out_b])

    def cpow_tables(self, ar, lre, lim, ldt, n, bufs, tag):
        S = self.S
        TB = Buf(f"cpow{tag}")
        step, _ = ar.alloc([n], F32, "step")
        lr, _ = ar.alloc([n], F32, "lr")
        th, _ = ar.alloc([n], F32, "th")
        S.op("act", lambda e: e.activation(out=step, in_=ldt, func=AF.Exp), reads=bufs, writes=[TB])
        S.op("dve", lambda e: e.tensor_tensor(out=lr, in0=lre, in1=step, op=ALU.mult), reads=bufs + [TB], writes=[TB])
        S.op("dve", lambda e: e.scalar_tensor_tensor(out=th, in0=lim, scalar=1.0 / (2.0 * np.pi), in1=step, op0=ALU.mult, op1=ALU.mult),
             reads=bufs + [TB], writes=[TB])
        are, _ = ar.alloc([9, n], F32, "are")
        aim, _ = ar.alloc([9, n], F32, "aim")
        mag, _ = ar.alloc([n], F32, "mag")
        ph, _ = ar.alloc([n], F32, "ph")
        ck, ck_b = ar.alloc([n], F32, "ck")
        sk, sk_b = ar.alloc([n], F32, "sk")
        for k in range(9):
            ar.mark()
            S.op("act", lambda e, k=k: e.activation(out=mag, in_=lr, func=AF.Exp, scale=float(k)), reads=[TB], writes=[TB])
            S.op("dve", lambda e, k=k: e.tensor_scalar(out=ph, in0=th, scalar1=float(k), scalar2=None, op0=ALU.mult), reads=[TB], writes=[TB])
            self.trig(ar, ph, n, 0.25, ck, ck_b, TB, f"{tag}c{k}")
            self.trig(ar, ph, n, 0.0, sk, sk_b, TB, f"{tag}s{k}")
            S.op("dve", lambda e, k=k: e.tensor_tensor(out=are[:, k, :], in0=mag, in1=ck, op=ALU.mult), reads=[TB, ck_b], writes=[TB])
            S.op("dve", lambda e, k=k: e.tensor_tensor(out=aim[:, k, :], in0=mag, in1=sk, op=ALU.mult), reads=[TB, sk_b], writes=[TB])
            ar.release()
        return dict(are=are, aim=aim, lr=lr, th=th, buf=TB)

    def cmul(self, eng, out_re, out_im, a_re, a_im, b_re, b_im, t1, t2, reads, writes):
        S = self.S
        S.op(eng, lambda e: e.tensor_tensor(out=t1, in0=a_re, in1=b_re, op=ALU.mult), reads=reads, writes=writes)
        S.op(eng, lambda e: e.tensor_tensor(out=t2, in0=a_im, in1=b_im, op=ALU.mult), reads=reads, writes=writes)
        S.op(eng, lambda e: e.tensor_tensor(out=out_re, in0=t1, in1=t2, op=ALU.subtract), reads=reads, writes=writes)
        S.op(eng, lambda e: e.tensor_tensor(out=t1, in0=a_re, in1=b_im, op=ALU.mult), reads=reads, writes=writes)
        S.op(eng, lambda e: e.tensor_tensor(out=t2, in0=a_im, in1=b_re, op=ALU.mult), reads=reads, writes=writes)
        S.op(eng, lambda e: e.tensor_tensor(out=out_im, in0=t1, in1=t2, op=ALU.add), reads=reads, writes=writes)

    def bc_coef(self, ar, pw, lre, lim, n, bufs):
        S = self.S
        TB = pw["buf"]
        rd = bufs + [TB]
        nre, _ = ar.alloc([n], F32, "nre")
        den, _ = ar.alloc([n], F32, "den")
        t1, _ = ar.alloc([n], F32, "bct1")
        bcr, _ = ar.alloc([n], F32, "bcr")
        bci, _ = ar.alloc([n], F32, "bci")
        are1, aim1 = pw["are"][:, 1, :], pw["aim"][:, 1, :]
        S.op("dve", lambda e: e.tensor_scalar(out=nre, in0=are1, scalar1=-1.0, scalar2=None, op0=ALU.add), reads=rd, writes=[TB])
        S.op("dve", lambda e: e.tensor_tensor(out=den, in0=lre, in1=lre, op=ALU.mult), reads=rd, writes=[TB])
        S.op("dve", lambda e: e.tensor_tensor(out=t1, in0=lim, in1=lim, op=ALU.mult), reads=rd, writes=[TB])
        S.op("dve", lambda e: e.tensor_tensor(out=den, in0=den, in1=t1, op=ALU.add), reads=rd, writes=[TB])
        S.op("dve", lambda e: e.reciprocal(out=den, in_=den), reads=rd, writes=[TB])
        S.op("dve", lambda e: e.tensor_tensor(out=bcr, in0=nre, in1=lre, op=ALU.mult), reads=rd, writes=[TB])
        S.op("dve", lambda e: e.tensor_tensor(out=t1, in0=aim1, in1=lim, op=ALU.mult), reads=rd, writes=[TB])
        S.op("dve", lambda e: e.tensor_tensor(out=bcr, in0=bcr, in1=t1, op=ALU.add), reads=rd, writes=[TB])
        S.op("dve", lambda e: e.tensor_tensor(out=bcr, in0=bcr, in1=den, op=ALU.mult), reads=rd, writes=[TB])
        S.op("dve", lambda e: e.tensor_tensor(out=bci, in0=aim1, in1=lre, op=ALU.mult), reads=rd, writes=[TB])
        S.op("dve", lambda e: e.tensor_tensor(out=t1, in0=nre, in1=lim, op=ALU.mult), reads=rd, writes=[TB])
        S.op("dve", lambda e: e.tensor_tensor(out=bci, in0=bci, in1=t1, op=ALU.subtract), reads=rd, writes=[TB])
        S.op("dve", lambda e: e.tensor_tensor(out=bci, in0=bci, in1=den, op=ALU.mult), reads=rd, writes=[TB])
        return bcr, bci

    def phase3_setup(self, s5F, s5S):
        nc, S, ar = self.nc, self.S, self.ar
        dbg = self.debug
        ident_f, ident_f_b = self.consts["ident_f"]
        self.SI_s, self.SI_sb = self.scratch("SI_s", [2, 8, 128, 2048], BF16, dump=dbg)
        self.RO_s, self.RO_sb = self.scratch("RO_s", [2, 8, 128, 2048], BF16, dump=dbg)
        self.FIR_s, self.FIR_sb = self.scratch("FIR_s", [2, 8, 128, 1024], BF16, dump=dbg)
        self.RS, self.RS_b = ar.alloc([2, 2, 32], F32, "RS")
        mF = [ar.alloc([1], F32, f"mF{i}") for i in range(2)]
        mS = [ar.alloc([1], F32, f"mS{i}") for i in range(2)]
        mSn = [ar.alloc([1], F32, f"mSn{i}") for i in range(2)]
        self.rowmask = [ar.alloc([1], F32, f"rowm{i}") for i in range(4)]
        for i in range(4):
            S.op("dve", lambda e, i=i: e.reduce_sum(out=self.rowmask[i][0], in_=ident_f[:, 32 * i:32 * i + 32], axis=AX.X),
                 reads=[ident_f_b], writes=[self.rowmask[i][1]])
        for i in range(2):
            S.op("dve", lambda e, i=i: e.reduce_sum(out=mS[i][0], in_=ident_f[:, 64 * i:64 * i + 64], axis=AX.X), reads=[ident_f_b], writes=[mS[i][1]])
            S.op("dve", lambda e, i=i: e.tensor_scalar(out=mSn[i][0], in0=mS[i][0], scalar1=-1.0, scalar2=None, op0=ALU.mult), reads=[mS[i][1]], writes=[mSn[i][1]])
            S.op("dve", lambda e, i=i: e.reduce_sum(out=mF[i][0], in_=ident_f.rearrange("p (a b c) -> p a b c", a=4, b=2)[:, :, i, :], axis=AX.XY),
                 reads=[ident_f_b], writes=[mF[i][1]])
        for d in range(2):
            ar.mark()
            Ft, Ft_b = ar.alloc([5, 512], F32, "Ft")
            S.op("sp", lambda e, d=d: e.dma_start(out=Ft, in_=s5F[d].rearrange("p a f q -> p a (f q)")), writes=[Ft_b], dma=True)
            pw = self.cpow_tables(ar, Ft[:, 0, :], Ft[:, 1, :], Ft[:, 2, :], 512, [Ft_b], f"F{d}")
            TB = pw["buf"]
            bcr, bci = self.bc_coef(ar, pw, Ft[:, 0, :], Ft[:, 1, :], 512, [Ft_b])
            t1, _ = ar.alloc([512], F32, "ft1")
            t2, _ = ar.alloc([512], F32, "ft2")
            bbr, _ = ar.alloc([512], F32, "bbr")
            bbi, _ = ar.alloc([512], F32, "bbi")
            self.cmul("dve", bbr, bbi, bcr, bci, Ft[:, 3, :], Ft[:, 4, :], t1, t2, [Ft_b, TB], [TB])
            wr_, _ = ar.alloc([512], F32, "wr_")
            wi_, _ = ar.alloc([512], F32, "wi_")
            SIt, SIt_b = ar.alloc([8, 8, 2, 2, 64], BF16, "SIt")
            for j in range(8):
                kj = 7 - j if d == 0 else j
                self.cmul("dve", wr_, wi_, pw["are"][:, kj, :], pw["aim"][:, kj, :], bbr, bbi, t1, t2, [TB], [TB])
                for ri, w_ in enumerate((wr_, wi_)):
                    for gq in range(2):
                        S.op("dve", lambda e, j=j, ri=ri, gq=gq, w_=w_: e.tensor_scalar(
                            out=SIt[:, :, j, ri, gq, :], in0=w_.rearrange("p (f q) -> p f q", f=8), scalar1=mF[gq][0][:, 0:1], scalar2=None, op0=ALU.mult),
                            reads=[TB, mF[gq][1]], writes=[SIt_b])
            S.op("sp", lambda e, d=d: e.dma_start(out=self.SI_s[d].rearrange("f p n -> p f n"), in_=SIt.rearrange("p f j r g q -> p f (j r g q)")),
                 reads=[SIt_b], writes=[self.SI_sb], dma=True)
            S.barrier()
            ar.release()
            ar.mark()
            NS = 3 * 32 + 4 * 512
            St, St_b = ar.alloc([NS], F32, "St")
            S.op("sp", lambda e, d=d: e.dma_start(out=St, in_=s5S[d]), writes=[St_b], dma=True)
            lre, lim, ldt = St[:, 0:32], St[:, 32:64], St[:, 64:96]
            Bre = St[:, 96:96 + 512].rearrange("p (g h) -> p g h", h=16)
            Bim = St[:, 96 + 512:96 + 1024].rearrange("p (g h) -> p g h", h=16)
            Cre = St[:, 96 + 1024:96 + 1536].rearrange("p (g h) -> p g h", h=16)
            Cim = St[:, 96 + 1536:96 + 2048].rearrange("p (g h) -> p g h", h=16)
            pw = self.cpow_tables(ar, lre, lim, ldt, 32, [St_b], f"S{d}")
            TB = pw["buf"]
            bcr, bci = self.bc_coef(ar, pw, lre, lim, 32, [St_b])
            bc3 = lambda a: a.unsqueeze(2).broadcast_to([128, 32, 16])
            t1, _ = ar.alloc([32, 16], F32, "st1")
            t2, _ = ar.alloc([32, 16], F32, "st2")
            bbr, _ = ar.alloc([32, 16], F32, "sbbr")
            bbi, _ = ar.alloc([32, 16], F32, "sbbi")
            self.cmul("dve", bbr, bbi, bc3(bcr), bc3(bci), Bre, Bim, t1, t2, [St_b, TB], [TB])
            S.op("act", lambda e, d=d: e.activation(out=self.RS[:, d, 0, :], in_=pw["lr"], func=AF.Exp, scale=8.0), reads=[TB], writes=[self.RS_b])
            p8, p8_b = ar.alloc([32], F32, "p8")
            p8i, p8i_b = ar.alloc([32], I32, "p8i")
            p8f, p8f_b = ar.alloc([32], F32, "p8f")
            S.op("dve", lambda e: e.tensor_scalar(out=p8, in0=pw["th"], scalar1=8.0, scalar2=64.0, op0=ALU.mult, op1=ALU.add), reads=[TB], writes=[p8_b])
            S.op("dve", lambda e: e.tensor_copy(out=p8i, in_=p8), reads=[p8_b], writes=[p8i_b])
            S.op("dve", lambda e: e.tensor_copy(out=p8f, in_=p8i), reads=[p8i_b], writes=[p8f_b])
            S.op("dve", lambda e, d=d: e.tensor_tensor(out=self.RS[:, d, 1, :], in0=p8, in1=p8f, op=ALU.subtract), reads=[p8_b, p8f_b], writes=[self.RS_b])
            vr_, _ = ar.alloc([32, 16], F32, "vr_")
            vi_, _ = ar.alloc([32, 16], F32, "vi_")
            ROt, ROt_b = ar.alloc([32, 8, 2, 2, 16], BF16, "ROt")
            for j in range(8):
                kj = j + 1 if d == 0 else 8 - j
                self.cmul("dve", vr_, vi_, Cre, Cim, bc3(pw["are"][:, kj, :]), bc3(pw["aim"][:, kj, :]), t1, t2, [St_b, TB], [TB])
                for ri, (v_, ms) in enumerate(((vr_, mS), (vi_, mSn))):
                    for gq in range(2):
                        S.op("dve", lambda e, j=j, ri=ri, gq=gq, v_=v_, ms=ms: e.tensor_scalar(
                            out=ROt[:, :, j, ri, gq, :], in0=v_, scalar1=ms[gq][0][:, 0:1], scalar2=None, op0=ALU.mult),
                            reads=[TB, ms[gq][1]], writes=[ROt_b])
            S.op("sp", lambda e, d=d: e.dma_start(out=self.RO_s[d].rearrange("f p n -> p f n"),
                                                in_=ROt.rearrange("p (f g) j r q h -> p f (g j r q h)", f=8)),
                 reads=[ROt_b], writes=[self.RO_sb], dma=True)
            W2p = [ar.alloc([8, 4, 8, 16], BF16, f"W2p{ri}") for ri in range(2)]
            W1p = [[ar.alloc([8, 4, 8, 16], BF16, f"W1p{b}{ri}") for ri in range(2)] for b in range(2)]
            for t_, tb_ in W2p + W1p[0] + W1p[1]:
                S.op("dve", lambda e, t_=t_: e.memset(t_, 0.0), writes=[tb_])
            g4 = lambda a: a.rearrange("p (f g) h -> p f g h", g=4)
            for ri, (c_, ms) in enumerate(((Cre, mS), (Cim, mSn))):
                for gp4 in range(4):
                    for gq in range(2):
                        S.op("dve", lambda e, ri=ri, gp4=gp4, gq=gq, c_=c_, ms=ms: e.tensor_scalar(
                            out=W2p[ri][0][:, :, gp4, 2 * gp4 + gq, :], in0=g4(c_)[:, :, gp4, :], scalar1=ms[gq][0][:, 0:1], scalar2=None, op0=ALU.mult),
                            reads=[St_b, ms[gq][1]], writes=[W2p[ri][1]])
            FIRt, FIRt_b = ar.alloc([8, 8, 128], BF16, "FIRt")
            w1r, _ = ar.alloc([32, 16], F32, "w1r")
            w1i, _ = ar.alloc([32, 16], F32, "w1i")
            cnt = 0
            for tau in range(8):
                self.cmul("dve", w1r, w1i, bc3(pw["are"][:, tau, :]), bc3(pw["aim"][:, tau, :]), bbr, bbi, t1, t2, [TB], [TB])
                W1 = W1p[tau % 2]
                for ri, w_ in enumerate((w1r, w1i)):
                    for gp4 in range(4):
                        for gq in range(2):
                            S.op("dve", lambda e, ri=ri, gp4=gp4, gq=gq, w_=w_, W1=W1: e.tensor_scalar(
                                out=W1[ri][0][:, :, gp4, 2 * gp4 + gq, :], in0=g4(w_)[:, :, gp4, :], scalar1=mS[gq][0][:, 0:1], scalar2=None, op0=ALU.mult),
                                reads=[TB, mS[gq][1]], writes=[W1[ri][1]])
                for fc in range(8):
                    pa, pb = self.pq(cnt % 8, 0)
                    cnt += 1

                    def mm(e, pa=pa, fc=fc, W1=W1):
                        ins = None
                        k = 0
                        for ri in range(2):
                            for gp4 in range(4):
                                ins = e.matmul(pa, lhsT=W1[ri][0][:, fc, gp4, :, :].rearrange("p a b -> p (a b)"),
                                               rhs=W2p[ri][0][:, fc, gp4, :, :].rearrange("p a b -> p (a b)"), start=(k == 0), stop=(k == 7))
                                k += 1
                        return ins
                    S.op("pe", mm, reads=[W1[0][1], W1[1][1], W2p[0][1], W2p[1][1]], writes=pb)
                    eng = "act" if cnt % 2 == 0 else "dve"
                    if eng == "act":
                        S.op("act", lambda e, pa=pa, fc=fc, tau=tau: e.activation(out=FIRt[:, fc, tau, :], in_=pa, func=AF.Copy), reads=pb, writes=[FIRt_b])
                    else:
                        S.op("dve", lambda e, pa=pa, fc=fc, tau=tau: e.tensor_copy(out=FIRt[:, fc, tau, :], in_=pa), reads=pb, writes=[FIRt_b])
            S.op("sp", lambda e, d=d: e.dma_start(out=self.FIR_s[d].rearrange("f p n -> p f n"), in_=FIRt.rearrange("p f t n -> p f (t n)")),
                 reads=[FIRt_b], writes=[self.FIR_sb], dma=True)
            S.barrier()
            ar.release()
        if dbg:
            o, ob = self.scratch("dbg_RS", [128, 128], F32, dump=True)
            S.op("sp", lambda e: e.dma_start(out=o, in_=self.RS.rearrange("p a b c -> p (a b c)")), reads=[self.RS_b], writes=[ob], dma=True)


    def phase3_main(self, s5_d):
        nc, S, ar = self.nc, self.S, self.ar
        ps = self.ps
        dbg = self.debug
        self.g_s, self.g_sb = self.scratch("g_s", [D, L], BF16, dump=dbg)
        RS, RS_b = self.RS, self.RS_b
        NCH = 544
        ar.mark()
        dcol, dcol_b = ar.alloc([8], F32, "s5d")
        S.op("sp", lambda e: e.dma_start(out=dcol, in_=s5_d), writes=[dcol_b], dma=True)
        iot_i, iot_ib = ar.alloc([NCH], I32, "iota_i")
        iot, iot_b = ar.alloc([NCH], F32, "iota_f")
        S.op("pool", lambda e: e.iota(iot_i, pattern=[[1, NCH]], base=0, channel_multiplier=0), writes=[iot_ib])
        S.op("dve", lambda e: e.tensor_copy(out=iot, in_=iot_i), reads=[iot_ib], writes=[iot_b])
        u_tb = [ar.alloc([T], BF16, f"u_fc{i}") for i in range(2)]
        um, um_b = ar.alloc([4, T], BF16, "um")
        SI_tb = [ar.alloc([2, 2048], BF16, f"SI_t{i}") for i in range(2)]
        RO_tb = [ar.alloc([2, 2048], BF16, f"RO_t{i}") for i in range(2)]
        FIR_tb = [ar.alloc([2, 1024], BF16, f"FIR_t{i}") for i in range(2)]
        cn, cn_b = ar.alloc([4, NCH], F32, "cn")
        sn, sn_b = ar.alloc([4, NCH], F32, "sn")
        pht, pht_b = ar.alloc([4, NCH], F32, "pht")
        phi_, phi_b = ar.alloc([4, NCH], I32, "phi_")
        self._s5_frac, self._s5_frac_b = ar.alloc([4, NCH], F32, "s5frac")
        Ssb = [[ar.alloc([NCH], F32, f"Ssb{k}{ri}") for ri in range(2)] for k in range(2)]
        V = [ar.alloc([NCH], F32, f"V{ri}") for ri in range(2)]
        W = [ar.alloc([NCH], F32, f"W{ri}") for ri in range(2)]
        tmp = [ar.alloc([NCH], F32, f"s5t{i}") for i in range(4)]
        Zp = [[[ar.alloc([512], BF16, f"Zp{d}{g}{ri}") for ri in range(2)] for g in range(4)] for d in range(2)]
        gst = [ar.alloc([L], BF16, f"gst{i}") for i in range(2)]
        ys = [ar.alloc([2, 256], F32, f"ys{i}") for i in range(2)]
        kset = 0
        def load_fc(fc):
            u_t, u_b = u_tb[fc % 2]
            SI_t, SI_b = SI_tb[fc % 2]
            RO_t, RO_b = RO_tb[fc % 2]
            FIR_t, FIR_b = FIR_tb[fc % 2]
            S.op("sp", lambda e: e.dma_start(out=u_t, in_=self.s5u[fc * 128:(fc + 1) * 128, :]), reads=[self.s5u_b], writes=[u_b], dma=True)
            S.op("sp", lambda e: e.dma_start(out=SI_t, in_=self.SI_s[:, fc].rearrange("d p n -> p d n")), reads=[self.SI_sb], writes=[SI_b], dma=True)
            S.op("sp", lambda e: e.dma_start(out=RO_t, in_=self.RO_s[:, fc].rearrange("d p n -> p d n")), reads=[self.RO_sb], writes=[RO_b], dma=True)
            S.op("sp", lambda e: e.dma_start(out=FIR_t, in_=self.FIR_s[:, fc].rearrange("d p n -> p d n")), reads=[self.FIR_sb], writes=[FIR_b], dma=True)
        load_fc(0)
        kset_box = [0]

        def fc_body(fc):
            kset = kset_box[0]
            u_t, u_b = u_tb[fc % 2]
            SI_t, SI_b = SI_tb[fc % 2]
            RO_t, RO_b = RO_tb[fc % 2]
            FIR_t, FIR_b = FIR_tb[fc % 2]
            if fc + 1 < 8:
                load_fc(fc + 1)
            for i in range(4):
                S.op("act", lambda e, i=i: e.activation(out=um[:, i, :], in_=u_t, func=AF.Copy, scale=self.rowmask[i][0][:, 0:1]),
                     reads=[u_b, self.rowmask[i][1]], writes=[um_b])
            for d in range(2):
                S.op("dve", lambda e, d=d, fc=fc: e.tensor_tensor(
                    out=pht, in0=iot.unsqueeze(1).broadcast_to([128, 4, NCH]),
                    in1=RS[:, d, 1, 4 * fc:4 * fc + 4].unsqueeze(2).broadcast_to([128, 4, NCH]), op=ALU.mult),
                    reads=[iot_b, RS_b], writes=[pht_b])
                for (dst, dst_b, add) in ((cn, cn_b, 0.25), (sn, sn_b, 0.0)):
                    S.op("dve", lambda e, dst=dst, add=add: e.tensor_scalar(out=dst, in0=pht, scalar1=64.0 + add, scalar2=None, op0=ALU.add),
                         reads=[pht_b], writes=[dst_b])
                    S.op("dve", lambda e, dst=dst: e.tensor_copy(out=phi_, in_=dst), reads=[dst_b], writes=[phi_b])
                    S.op("dve", lambda e, dst=dst: e.tensor_copy(out=self._s5_frac, in_=phi_), reads=[phi_b], writes=[self._s5_frac_b])
                    S.op("dve", lambda e, dst=dst: e.tensor_tensor(out=dst, in0=dst, in1=self._s5_frac, op=ALU.subtract),
                         reads=[dst_b, self._s5_frac_b], writes=[dst_b])
                    S.op("act", lambda e, dst=dst: e.activation(out=dst, in_=dst, func=AF.Sin, scale=2.0 * np.pi), reads=[dst_b], writes=[dst_b])
                for gp4 in range(4):
                    gp = 4 * fc + gp4
                    b0 = 3 * (kset % 2)
                    kset += 1
                    bufs3 = [self.psb[b0], self.psb[b0 + 1], self.psb[b0 + 2]]

                    def mm(e, d=d, gp4=gp4, b0=b0):
                        ins = None
                        for ri in range(2):
                            for j in range(8):
                                ins = e.matmul(ps[b0 + ri][:, 0:512], lhsT=SI_t[:, d, (j * 2 + ri) * 128:(j * 2 + ri + 1) * 128],
                                               rhs=um[:, gp4, LC + j:T:8], start=(j == 0), stop=(j == 7))
                        for ri in range(2):
                            for j in range(8):
                                ins = e.matmul(ps[b0 + 2][:, ri * 32:(ri + 1) * 32], lhsT=SI_t[:, d, (j * 2 + ri) * 128:(j * 2 + ri + 1) * 128],
                                               rhs=um[:, gp4, j:LC:8], start=(j == 0), stop=(j == 7))
                        return ins
                    S.op("pe", mm, reads=[SI_b, um_b], writes=bufs3)
                    Sk = Ssb[kset % 2]
                    for ri in range(2):
                        s_t, s_b = Sk[ri]
                        src_c = ps[b0 + 2][:, ri * 32:(ri + 1) * 32]
                        src_l = ps[b0 + ri][:, 0:512]
                        if d == 1:
                            src_c = src_c[:, ::-1]
                            src_l = src_l[:, ::-1]
                        S.op("act", lambda e, s_t=s_t, src_c=src_c: e.activation(out=s_t[:, 0:32], in_=src_c, func=AF.Copy),
                             reads=[self.psb[b0 + 2]], writes=[s_b])
                        S.op("act", lambda e, s_t=s_t, src_l=src_l: e.activation(out=s_t[:, 32:NCH], in_=src_l, func=AF.Copy),
                             reads=[self.psb[b0 + ri]], writes=[s_b])
                    (Sr, Sr_b), (Si, Si_b) = Sk
                    cnv, snv = cn[:, gp4, :], sn[:, gp4, :]
                    (t1, t1b), (t2, t2b), (t3, t3b), (t4, t4b) = tmp
                    (Vr, Vr_b), (Vi, Vi_b) = V
                    (Wr, Wr_b), (Wi, Wi_b) = W
                    S.op("dve", lambda e, cnv=cnv, Sr=Sr: e.tensor_tensor(out=t1, in0=cnv, in1=Sr, op=ALU.mult), reads=[cn_b, Sr_b], writes=[t1b])
                    S.op("dve", lambda e, snv=snv, Si=Si: e.tensor_tensor(out=t2, in0=snv, in1=Si, op=ALU.mult), reads=[sn_b, Si_b], writes=[t2b])
                    S.op("dve", lambda e, cnv=cnv, Si=Si: e.tensor_tensor(out=t3, in0=cnv, in1=Si, op=ALU.mult), reads=[cn_b, Si_b], writes=[t3b])
                    S.op("dve", lambda e, snv=snv, Sr=Sr: e.tensor_tensor(out=t4, in0=snv, in1=Sr, op=ALU.mult), reads=[sn_b, Sr_b], writes=[t4b])
                    S.op("dve", lambda e: e.tensor_tensor(out=Vr, in0=t1, in1=t2, op=ALU.add), reads=[t1b, t2b], writes=[Vr_b])
                    S.op("dve", lambda e: e.tensor_tensor(out=Vi, in0=t3, in1=t4, op=ALU.subtract), reads=[t3b, t4b], writes=[Vi_b])
                    Rb = RS[:, d, 0, gp:gp + 1].broadcast_to([128, NCH])
                    S.op("dve", lambda e, Rb=Rb: e.tensor_tensor_scan(out=Wr, data0=Rb, data1=Vr, initial=0.0, op0=ALU.mult, op1=ALU.add),
                         reads=[RS_b, Vr_b], writes=[Wr_b])
                    S.op("dve", lambda e, Rb=Rb: e.tensor_tensor_scan(out=Wi, data0=Rb, data1=Vi, initial=0.0, op0=ALU.mult, op1=ALU.add),
                         reads=[RS_b, Vi_b], writes=[Wi_b])
                    sl = slice(31, 543)
                    (zr, zr_b), (zi, zi_b) = Zp[d][gp4]
                    zro = zr if d == 0 else zr[:, ::-1]
                    zio = zi if d == 0 else zi[:, ::-1]
                    S.op("dve", lambda e, cnv=cnv: e.tensor_tensor(out=t1[:, sl], in0=cnv[:, sl], in1=Wr[:, sl], op=ALU.mult), reads=[cn_b, Wr_b], writes=[t1b])
                    S.op("dve", lambda e, snv=snv: e.tensor_tensor(out=t2[:, sl], in0=snv[:, sl], in1=Wi[:, sl], op=ALU.mult), reads=[sn_b, Wi_b], writes=[t2b])
                    S.op("dve", lambda e, cnv=cnv: e.tensor_tensor(out=t3[:, sl], in0=cnv[:, sl], in1=Wi[:, sl], op=ALU.mult), reads=[cn_b, Wi_b], writes=[t3b])
                    S.op("dve", lambda e, snv=snv: e.tensor_tensor(out=t4[:, sl], in0=snv[:, sl], in1=Wr[:, sl], op=ALU.mult), reads=[sn_b, Wr_b], writes=[t4b])
                    S.op("dve", lambda e, zro=zro: e.tensor_tensor(out=zro, in0=t1[:, sl], in1=t2[:, sl], op=ALU.subtract), reads=[t1b, t2b], writes=[zr_b])
                    S.op("dve", lambda e, zio=zio: e.tensor_tensor(out=zio, in0=t3[:, sl], in1=t4[:, sl], op=ALU.add), reads=[t3b, t4b], writes=[zi_b])
            g_t, g_b = gst[fc % 2]
            for hb in range(2):
                c0 = 256 * hb
                for j in range(8):
                    bank = 4 + j // 2
                    reg = ps[bank][:, (j % 2) * 256:(j % 2) * 256 + 256]

                    def mmy(e, j=j, reg=reg, c0=c0):
                        ops = []
                        for tau in range(0, j + 1):
                            st0 = LC + 8 * c0 + (j - tau)
                            ops.append((reg, FIR_t[:, 0, tau * 128:(tau + 1) * 128], u_t[:, st0:st0 + 2041:8], None))
                        for tau in range(0, 8 - j):
                            st0 = LC + 8 * c0 + (j + tau)
                            ops.append((reg, FIR_t[:, 1, tau * 128:(tau + 1) * 128], u_t[:, st0:st0 + 2041:8], None))
                        for d in range(2):
                            for ri in range(2):
                                for gp4 in range(4):
                                    o0 = ((gp4 * 8 + j) * 2 + ri) * 32
                                    ops.append((reg[32 * gp4:32 * gp4 + 32, :], RO_t[:, d, o0:o0 + 32], Zp[d][gp4][ri][0][:, c0:c0 + 256], (0, 32 * gp4)))
                        ins = None
                        for k, (o_, l_, r_, tp) in enumerate(ops):
                            if tp is None:
                                ins = e.matmul(o_, lhsT=l_, rhs=r_, start=(k == 0), stop=(k == len(ops) - 1))
                            else:
                                ins = e.matmul(o_, lhsT=l_, rhs=r_, start=(k == 0), stop=(k == len(ops) - 1), tile_position=tp)
                        return ins
                    rd = [FIR_b, RO_b, u_b] + [Zp[d][g][ri][1] for d in range(2) for g in range(4) for ri in range(2)]
                    S.op("pe", mmy, reads=rd, writes=[self.psb[bank]])
                    if j % 2 == 1:
                        j0 = j - 1
                        y_t, y_b = ys[(j // 2) % 2]
                        base = LC + 8 * c0
                        uv = u_t[:, base:base + 2048].rearrange("p (c j) -> p j c", j=8)[:, j0:j0 + 2, :]
                        S.op("dve", lambda e, y_t=y_t, uv=uv, bank=bank, fc=fc: e.scalar_tensor_tensor(
                            out=y_t, in0=uv, scalar=dcol[:, fc:fc + 1], in1=ps[bank][:].rearrange("p (j c) -> p j c", j=2),
                            op0=ALU.mult, op1=ALU.add), reads=[u_b, dcol_b, self.psb[bank]], writes=[y_b])
                        outv = g_t.rearrange("p (c8 j row) -> p j row c8", c8=8, j=8)[:, j0:j0 + 2, 32 * hb:32 * hb + 32, :]
                        S.op("act", lambda e, y_t=y_t, outv=outv: e.activation(
                            out=outv, in_=y_t.rearrange("p j (r c8) -> p j r c8", c8=8), func=AF.Gelu_apprx_tanh),
                            reads=[y_b], writes=[g_b])
            S.op("sp", lambda e, fc=fc, g_t=g_t: e.dma_start(out=self.g_s[fc * 128:(fc + 1) * 128, :], in_=g_t), reads=[g_b], writes=[self.g_sb], dma=True)
            kset_box[0] = kset
        for fc in range(8):
            fc_body(fc)
        S.barrier()
        ar.release()


    def phase4(self, glu_w, glu_b, w_a, w_b, w_o, x_tok, nfw, r_w, r_b):
        nc, S, ar = self.nc, self.S, self.ar
        ps, psb = self.ps, self.psb
        dbg = self.debug
        ident_f, ident_f_b = self.consts["ident_f"]
        self.h1_s, self.h1_sb = self.scratch("h1_s", [L, D], F32, dump=dbg)
        self.uf_s, self.uf_sb = self.scratch("uf_s", [L, D], BF16, dump=dbg)
        self.logits, self.logits_b = ar.alloc([32, 32], F32, "logits")
        ar.mark()
        wv = lambda w: w.rearrange("(kc p) n -> p kc n", p=128)
        Wg, Wg_b = ar.alloc([8, D], BF16, "Wglu")
        Wa, Wa_b = ar.alloc([8, D], BF16, "Wa")
        Wb, Wb_b = ar.alloc([16, D], BF16, "Wb")
        Wo, Wo_b = ar.alloc([8, D], BF16, "Wo")
        for (t_, tb_, src, nk) in ((Wg, Wg_b, glu_w, 8), (Wa, Wa_b, w_a, 8), (Wb, Wb_b, w_b, 16), (Wo, Wo_b, w_o, 8)):
            for k0 in range(0, nk, 4):
                S.op("pool", lambda e, t_=t_, src=src, k0=k0: e.dma_start(out=t_[:, k0:k0 + 4, :], in_=wv(src)[:, k0:k0 + 4, :]), writes=[tb_], dma=True)
        Wr, Wr_b = ar.alloc([8, 32], F32, "Wr")
        S.op("sp", lambda e: e.dma_start(out=Wr, in_=r_w.rearrange("(kc p) n -> p kc n", p=128)), writes=[Wr_b], dma=True)
        rb_rep, rb_rep_b = ar.alloc([32], F32, "rb_rep")
        S.op("sp", lambda e: e.dma_start(out=rb_rep, in_=r_b.broadcast_to([128, 32])), writes=[rb_rep_b], dma=True)
        gb_col, gb_col_b = ar.alloc([8], F32, "glu_b")
        S.op("sp", lambda e: e.dma_start(out=gb_col, in_=glu_b), writes=[gb_col_b], dma=True)
        gm_rep, gm_rep_b = ar.alloc([D], F32, "gm_rep")
        Af_rep, Af_rep_b = ar.alloc([D], F32, "Af_rep")
        Bf_rep, Bf_rep_b = ar.alloc([D], F32, "Bf_rep")
        S.op("sp", lambda e: e.dma_start(out=gm_rep, in_=self.mod_s[:, 2 * D:3 * D]), reads=[self.mod_sb], writes=[gm_rep_b], dma=True)
        S.op("sp", lambda e: e.dma_start(out=Bf_rep, in_=self.mod_s[:, 3 * D:4 * D]), reads=[self.mod_sb], writes=[Bf_rep_b], dma=True)
        S.op("sp", lambda e: e.dma_start(out=Af_rep, in_=self.mod_s[:, 4 * D:5 * D]), reads=[self.mod_sb], writes=[Af_rep_b], dma=True)
        nf_rep, nf_rep_b = ar.alloc([D], F32, "nf_rep")
        S.op("sp", lambda e: e.dma_start(out=nf_rep, in_=nfw.broadcast_to([128, D])), writes=[nf_rep_b], dma=True)
        S.op("dve", lambda e: e.scalar_tensor_tensor(out=Af_rep, in0=Af_rep, scalar=1.0, in1=nf_rep, op0=ALU.add, op1=ALU.mult),
             reads=[Af_rep_b, nf_rep_b], writes=[Af_rep_b])
        g_t, g_b = ar.alloc([8, 512], BF16, "m_g")
        yb_t, yb_b = ar.alloc([16, 512], BF16, "m_yb")
        gab_t, gab_b = ar.alloc([16, 512], BF16, "m_gab")
        ya_t, ya_b = ar.alloc([8, 512], BF16, "m_ya")
        m1_t, m1_b = ar.alloc([8, 512], F32, "m_m1")
        mg_t, mg_b = ar.alloc([8, 512], BF16, "m_mg")
        sg = [ar.alloc([512], F32, f"m_sg{i}") for i in range(2)]
        xk = [ar.alloc([D], F32, f"m_xk{i}") for i in range(1)]
        h1 = [ar.alloc([D], F32, f"m_h1{i}") for i in range(2)]
        uft = [ar.alloc([D], F32, f"m_uft{i}") for i in range(1)]
        ufb = [ar.alloc([D], BF16, f"m_ufb{i}") for i in range(2)]
        ufT = [ar.alloc([8, 128], F32, f"m_ufT{i}") for i in range(1)]
        sq_junk, sq_junk_b = ar.alloc([D], BF16, "m_sqj")
        ss = [ar.alloc([1], F32, f"m_ss{i}") for i in range(2)]
        pc = [0]

        def bank():
            b = pc[0] % 8
            pc[0] += 1
            return b
        for tt in range(8):
            cs = slice(tt * 512, (tt + 1) * 512)
            S.op("sp", lambda e, cs=cs: e.dma_start(out=g_t, in_=self.g_s.rearrange("(kc p) n -> p kc n", p=128)[:, :, cs]), reads=[self.g_sb], writes=[g_b], dma=True)
            S.op("sp", lambda e, cs=cs: e.dma_start(out=yb_t, in_=self.yb_s.rearrange("(kc p) n -> p kc n", p=128)[:, :, cs]), reads=[self.yb_sb], writes=[yb_b], dma=True)
            S.op("sp", lambda e, cs=cs: e.dma_start(out=gab_t, in_=self.gab_s.rearrange("(kc p) n -> p kc n", p=128)[:, :, cs]), reads=[self.gab_sb], writes=[gab_b], dma=True)
            for mc in range(8):
                b_ = bank()

                def mm(e, b_=b_, mc=mc):
                    ins = None
                    for kc in range(8):
                        ins = e.matmul(ps[b_][:], lhsT=Wg[:, kc, mc * 128:(mc + 1) * 128], rhs=g_t[:, kc, :], start=(kc == 0), stop=(kc == 7))
                    return ins
                S.op("pe", mm, reads=[Wg_b, g_b], writes=[psb[b_]])
                s_t, s_b = sg[mc % 2]
                S.op("act", lambda e, s_t=s_t, b_=b_, mc=mc: e.activation(out=s_t, in_=ps[b_][:], func=AF.Sigmoid, bias=gb_col[:, mc:mc + 1]),
                     reads=[psb[b_], gb_col_b], writes=[s_b])
                S.op("dve", lambda e, s_t=s_t, mc=mc: e.tensor_tensor(out=ya_t[:, mc, :], in0=s_t, in1=g_t[:, mc, :], op=ALU.mult),
                     reads=[s_b, g_b], writes=[ya_b])
            for mc in range(8):
                b_ = bank()

                def mm(e, b_=b_, mc=mc):
                    ins = None
                    for kc in range(8):
                        ins = e.matmul(ps[b_][:], lhsT=Wa[:, kc, mc * 128:(mc + 1) * 128], rhs=ya_t[:, kc, :], start=(kc == 0), stop=(kc == 7))
                    return ins
                S.op("pe", mm, reads=[Wa_b, ya_b], writes=[psb[b_]])
                s_t, s_b = sg[mc % 2]
                S.op("act", lambda e, s_t=s_t, mc=mc: e.activation(out=s_t, in_=gab_t[:, mc, :], func=AF.Sigmoid), reads=[gab_b], writes=[s_b])
                S.op("dve", lambda e, s_t=s_t, b_=b_, mc=mc: e.tensor_tensor(out=m1_t[:, mc, :], in0=s_t, in1=ps[b_][:], op=ALU.mult),
                     reads=[s_b, psb[b_]], writes=[m1_b])
            for mc in range(8):
                b_ = bank()

                def mm(e, b_=b_, mc=mc):
                    ins = None
                    for kc in range(16):
                        ins = e.matmul(ps[b_][:], lhsT=Wb[:, kc, mc * 128:(mc + 1) * 128], rhs=yb_t[:, kc, :], start=(kc == 0), stop=(kc == 15))
                    return ins
                S.op("pe", mm, reads=[Wb_b, yb_b], writes=[psb[b_]])
                s_t, s_b = sg[mc % 2]
                S.op("act", lambda e, s_t=s_t, mc=mc: e.activation(out=s_t, in_=gab_t[:, 8 + mc, :], func=AF.Sigmoid), reads=[gab_b], writes=[s_b])
                S.op("dve", lambda e, s_t=s_t, b_=b_: e.tensor_tensor(out=s_t, in0=s_t, in1=ps[b_][:], op=ALU.mult), reads=[s_b, psb[b_]], writes=[s_b])
                S.op("dve", lambda e, s_t=s_t, mc=mc: e.tensor_tensor(out=mg_t[:, mc, :], in0=s_t, in1=m1_t[:, mc, :], op=ALU.add),
                     reads=[s_b, m1_b], writes=[mg_b])
            for sub in range(4):
                ti = tt * 4 + sub
                x_t, x_b = xk[0]
                h_t, h_b = h1[ti % 2]
                S.op("sp", lambda e, x_t=x_t, ti=ti: e.dma_start(out=x_t, in_=x_tok[ti * 128:(ti + 1) * 128, :]), writes=[x_b], dma=True)
                for half in range(2):
                    b_ = bank()

                    def mm(e, b_=b_, sub=sub, half=half):
                        ins = None
                        for kc in range(8):
                            ins = e.matmul(ps[b_][:], lhsT=mg_t[:, kc, sub * 128:(sub + 1) * 128], rhs=Wo[:, kc, half * 512:(half + 1) * 512],
                                           start=(kc == 0), stop=(kc == 7))
                        return ins
                    S.op("pe", mm, reads=[mg_b, Wo_b], writes=[psb[b_]])
                    hs = slice(half * 512, (half + 1) * 512)
                    S.op("dve", lambda e, h_t=h_t, b_=b_, hs=hs: e.tensor_tensor(out=h_t[:, hs], in0=ps[b_][:], in1=gm_rep[:, hs], op=ALU.mult),
                         reads=[psb[b_], gm_rep_b], writes=[h_b])
                    S.op("dve", lambda e, h_t=h_t, x_t=x_t, hs=hs: e.tensor_tensor(out=h_t[:, hs], in0=h_t[:, hs], in1=x_t[:, hs], op=ALU.add),
                         reads=[h_b, x_b], writes=[h_b])
                S.op("sp", lambda e, h_t=h_t, ti=ti: e.dma_start(out=self.h1_s[ti * 128:(ti + 1) * 128, :], in_=h_t), reads=[h_b], writes=[self.h1_sb], dma=True)
                s_t, s_b = ss[ti % 2]
                S.op("act", lambda e, h_t=h_t, s_t=s_t: e.activation(out=sq_junk, in_=h_t, func=AF.Square, accum_out=s_t), reads=[h_b], writes=[sq_junk_b, s_b])
                S.op("act", lambda e, s_t=s_t: e.activation(out=s_t, in_=s_t, func=AF.Sqrt, scale=1.0 / D, bias=EPS), reads=[s_b], writes=[s_b])
                S.op("dve", lambda e, s_t=s_t: e.reciprocal(out=s_t, in_=s_t), reads=[s_b], writes=[s_b])
                u_t, u_b = uft[0]
                ub_t, ub_b = ufb[ti % 2]
                S.op("dve", lambda e, u_t=u_t, h_t=h_t, s_t=s_t: e.scalar_tensor_tensor(out=u_t, in0=h_t, scalar=s_t[:, 0:1], in1=Af_rep, op0=ALU.mult, op1=ALU.mult),
                     reads=[h_b, s_b, Af_rep_b], writes=[u_b])
                S.op("dve", lambda e, u_t=u_t: e.tensor_tensor(out=u_t, in0=u_t, in1=Bf_rep, op=ALU.add), reads=[u_b, Bf_rep_b], writes=[u_b])
                S.op("act", lambda e, u_t=u_t, ub_t=ub_t: e.activation(out=ub_t, in_=u_t, func=AF.Copy), reads=[u_b], writes=[ub_b])
                S.op("sp", lambda e, ub_t=ub_t, ti=ti: e.dma_start(out=self.uf_s[ti * 128:(ti + 1) * 128, :], in_=ub_t), reads=[ub_b], writes=[self.uf_sb], dma=True)
                T_t, T_b = ufT[0]
                for half in range(2):
                    b_ = bank()

                    def tr(e, b_=b_, u_t=u_t, half=half):
                        ins = None
                        for q in range(4):
                            kc = half * 4 + q
                            ins = e.transpose(out=ps[b_][:, q * 128:(q + 1) * 128], in_=u_t[:, kc * 128:(kc + 1) * 128], identity=ident_f)
                        return ins
                    S.op("pe", tr, reads=[u_b, ident_f_b], writes=[psb[b_]])
                    S.op("act", lambda e, T_t=T_t, b_=b_, half=half: e.activation(
                        out=T_t[:, half * 4:half * 4 + 4, :].rearrange("p a b -> p (a b)"), in_=ps[b_][:], func=AF.Copy), reads=[psb[b_]], writes=[T_b])
                b_ = bank()

                def mml(e, b_=b_, T_t=T_t):
                    ins = None
                    for kc in range(8):
                        ins = e.matmul(ps[b_][:, 0:32], lhsT=T_t[:, kc, :], rhs=Wr[:, kc, :], start=(kc == 0), stop=(kc == 7))
                    return ins
                S.op("pe", mml, reads=[T_b, Wr_b], writes=[psb[b_]])
                S.op("dve", lambda e, b_=b_, ti=ti: e.tensor_tensor(out=self.logits[:, ti, :], in0=ps[b_][:, 0:32], in1=rb_rep, op=ALU.add),
                     reads=[psb[b_], rb_rep_b], writes=[self.logits_b])
        if dbg:
            o, ob = self.scratch("dbg_logits", [128, 1024], F32, dump=True)
            S.op("sp", lambda e: e.dma_start(out=o, in_=self.logits.rearrange("p a b -> p (a b)")), reads=[self.logits_b], writes=[ob], dma=True)
        S.barrier()
        ar.release()


    def phase5(self, wg_d, wu_d, wd_d, bg_d, bu_d, bd_d, fnw):
        nc, S, ar = self.nc, self.S, self.ar
        ps, psb = self.ps, self.psb
        dbg = self.debug
        ident_f, ident_f_b = self.consts["ident_f"]
        ones_f, ones_f_b = self.consts["ones_f"]
        ones_bf, ones_bf_b = self.consts["ones_bf"]
        BLK = int(os.environ.get('MOE_BLK', '256'))
        NSUB = BLK // 128
        NT, NE = 32, 32
        NB = -(-(16384 + 32 * (BLK - 1)) // BLK)
        NSLOT = NB * BLK
        self.xs, self.xs_b = self.scratch("xs", [NSLOT, D], BF16)
        self.ys, self.ys_b = self.scratch("ys", [NSLOT, D], BF16)
        logits, logits_b = self.logits, self.logits_b
        ar.mark()
        dest_i, dest_ib = ar.alloc([NT, 4], I32, "dest_i")
        gate4, gate4_b = ar.alloc([NT, 4], F32, "gate4")
        blk_i, blk_ib = ar.alloc([NB], I32, "blk_i")
        chg_i, chg_ib = ar.alloc([NB], I32, "chg_i")
        widx, widx_b = ar.alloc([NB], I32, "widx")
        bidx, bidx_b = ar.alloc([NB], I32, "bidx")
        ident_bf, ident_bf_b = ar.alloc([128], BF16, "ident_bf5")
        S.op("dve", lambda e: e.tensor_copy(out=ident_bf, in_=ident_f), reads=[ident_f_b], writes=[ident_bf_b])
        ar.mark()
        top8, top8_b = ar.alloc([NT, 8], F32, "top8")
        mask, mask_b = ar.alloc([NT, NE], F32, "mask")
        mask_bf, mask_bfb = ar.alloc([NT, NE], BF16, "mask_bf")
        gatef, gatef_b = ar.alloc([NT, NE], F32, "gatef")
        pos, pos_b = ar.alloc([NT, NE], F32, "pos")
        cnta, cnta_b = ar.alloc([NT, NE], F32, "cnta")
        base, base_b = ar.alloc([NT, NE], F32, "base")
        rsum, rsum_b = ar.alloc([NT], F32, "rsum")
        stri, stri_b = ar.alloc([128], BF16, "stri")
        S.op("pool", lambda e: e.affine_select(out=stri, in_=ones_f, pattern=[[1, 128]], compare_op=ALU.is_ge, fill=0.0,
                                              base=-1, channel_multiplier=-1), reads=[ones_f_b], writes=[stri_b])
        for i in range(NT):
            S.op("dve", lambda e, i=i: e.max(out=top8[:, i, :], in_=logits[:, i, :]), reads=[logits_b], writes=[top8_b])
            S.op("dve", lambda e, i=i: e.tensor_scalar(out=mask[:, i, :], in0=logits[:, i, :], scalar1=top8[:, i, 3:4], scalar2=None, op0=ALU.is_ge),
                 reads=[logits_b, top8_b], writes=[mask_b])
        S.op("act", lambda e: e.activation(out=mask_bf, in_=mask, func=AF.Copy), reads=[mask_b], writes=[mask_bfb])
        S.op("dve", lambda e: e.tensor_tensor(out=gatef, in0=logits, in1=top8[:, :, 0:1].broadcast_to([128, NT, NE]), op=ALU.subtract),
             reads=[logits_b, top8_b], writes=[gatef_b])
        S.op("act", lambda e: e.activation(out=gatef, in_=gatef, func=AF.Exp), reads=[gatef_b], writes=[gatef_b])
        S.op("dve", lambda e: e.tensor_tensor(out=gatef, in0=gatef, in1=mask, op=ALU.mult), reads=[gatef_b, mask_b], writes=[gatef_b])
        S.op("dve", lambda e: e.reduce_sum(out=rsum, in_=gatef, axis=AX.X), reads=[gatef_b], writes=[rsum_b])
        S.op("dve", lambda e: e.reciprocal(out=rsum, in_=rsum), reads=[rsum_b], writes=[rsum_b])
        S.op("dve", lambda e: e.tensor_tensor(out=gatef, in0=gatef, in1=rsum.unsqueeze(2).broadcast_to([128, NT, NE]), op=ALU.mult),
             reads=[gatef_b, rsum_b], writes=[gatef_b])
        mflat = mask_bf.rearrange("p a b -> p (a b)")
        for half in range(2):
            b_ = half

            def mm(e, b_=b_, half=half):
                return e.matmul(ps[b_][:], lhsT=ones_bf, rhs=mflat[:, half * 512:(half + 1) * 512], start=True, stop=True)
            S.op("pe", mm, reads=[mask_bfb, ones_bf_b], writes=[psb[b_]])
            S.op("dve", lambda e, b_=b_, half=half: e.tensor_copy(out=cnta.rearrange("p a b -> p (a b)")[:, half * 512:(half + 1) * 512], in_=ps[b_][:]),
                 reads=[psb[b_]], writes=[cnta_b])
            b2 = 2 + half

            def mm2(e, b2=b2, half=half):
                return e.matmul(ps[b2][:], lhsT=stri, rhs=mflat[:, half * 512:(half + 1) * 512], start=True, stop=True)
            S.op("pe", mm2, reads=[mask_bfb, stri_b], writes=[psb[b2]])
            S.op("dve", lambda e, b2=b2, half=half: e.tensor_copy(out=pos.rearrange("p a b -> p (a b)")[:, half * 512:(half + 1) * 512], in_=ps[b2][:]),
                 reads=[psb[b2]], writes=[pos_b])
        for ee in range(NE):
            S.op("dve", lambda e, ee=ee: e.tensor_tensor_scan(out=base[:, :, ee], data0=ones_f[:, 0:NT], data1=cnta[:, :, ee], initial=0.0,
                                                             op0=ALU.mult, op1=ALU.add), reads=[cnta_b, ones_f_b], writes=[base_b])
        tot, tot_b = ar.alloc([NE], F32, "tot")
        S.op("dve", lambda e: e.tensor_copy(out=tot, in_=base[:, NT - 1, :]), reads=[base_b], writes=[tot_b])
        S.op("dve", lambda e: e.tensor_tensor(out=base, in0=base, in1=cnta, op=ALU.subtract), reads=[base_b, cnta_b], writes=[base_b])
        S.op("dve", lambda e: e.tensor_tensor(out=pos, in0=pos, in1=base, op=ALU.add), reads=[pos_b, base_b], writes=[pos_b])
        nb_f, nb_fb = ar.alloc([NE], F32, "nb_f")
        nb_i, nb_ib = ar.alloc([NE], I32, "nb_i")
        pend, pend_b = ar.alloc([NE], F32, "pend")
        pstart, pstart_b = ar.alloc([NE], F32, "pstart")
        S.op("dve", lambda e: e.tensor_scalar(out=nb_f, in0=tot, scalar1=1.0 / BLK, scalar2=(BLK - 1.0) / BLK - 0.5 + 0.25 / BLK, op0=ALU.mult, op1=ALU.add),
             reads=[tot_b], writes=[nb_fb])
        S.op("dve", lambda e: e.tensor_copy(out=nb_i, in_=nb_f), reads=[nb_fb], writes=[nb_ib])
        S.op("dve", lambda e: e.tensor_copy(out=nb_f, in_=nb_i), reads=[nb_ib], writes=[nb_fb])
        S.op("dve", lambda e: e.tensor_scalar(out=nb_f, in0=nb_f, scalar1=float(BLK), scalar2=None, op0=ALU.mult), reads=[nb_fb], writes=[nb_fb])
        S.op("dve", lambda e: e.tensor_tensor_scan(out=pend, data0=ones_f[:, 0:NE], data1=nb_f, initial=0.0, op0=ALU.mult, op1=ALU.add),
             reads=[nb_fb, ones_f_b], writes=[pend_b])
        S.op("dve", lambda e: e.tensor_tensor(out=pstart, in0=pend, in1=nb_f, op=ALU.subtract), reads=[pend_b, nb_fb], writes=[pstart_b])
        S.op("dve", lambda e: e.tensor_tensor(out=pos, in0=pos, in1=pstart.unsqueeze(1).broadcast_to([128, NT, NE]), op=ALU.add),
             reads=[pos_b, pstart_b], writes=[pos_b])
        dest4, dest4_b = ar.alloc([NT, 4], F32, "dest4")
        junk, junk_b = ar.alloc([NE], F32, "junk")
        for i in range(NT):
            for k in range(4):
                S.op("dve", lambda e, i=i, k=k: e.scalar_tensor_tensor(out=junk, in0=logits[:, i, :], scalar=top8[:, i, k:k + 1], in1=pos[:, i, :],
                                                                     op0=ALU.is_equal, op1=ALU.mult, accum_out=dest4[:, i, k:k + 1]),
                     reads=[logits_b, top8_b, pos_b], writes=[junk_b, dest4_b])
                S.op("dve", lambda e, i=i, k=k: e.scalar_tensor_tensor(out=junk, in0=logits[:, i, :], scalar=top8[:, i, k:k + 1], in1=gatef[:, i, :],
                                                                     op0=ALU.is_equal, op1=ALU.mult, accum_out=gate4[:, i, k:k + 1]),
                     reads=[logits_b, top8_b, gatef_b], writes=[junk_b, gate4_b])
        S.op("dve", lambda e: e.tensor_copy(out=dest_i, in_=dest4), reads=[dest4_b], writes=[dest_ib])
        bv_i, bv_ib = ar.alloc([NB], I32, "bv_i")
        bv, bv_b = ar.alloc([NB], F32, "bv")
        cmp_, cmp_b = ar.alloc([NB, NE], F32, "cmp")
        blk_f, blk_fb = ar.alloc([NB], F32, "blk_f")
        chg_f, chg_fb = ar.alloc([NB], F32, "chg_f")
        S.op("pool", lambda e: e.iota(bv_i, pattern=[[BLK, NB]], base=0, channel_multiplier=0), writes=[bv_ib])
        S.op("dve", lambda e: e.tensor_copy(out=bv, in_=bv_i), reads=[bv_ib], writes=[bv_b])
        S.op("dve", lambda e: e.tensor_tensor(out=cmp_, in0=pend.unsqueeze(1).broadcast_to([128, NB, NE]),
                                              in1=bv.unsqueeze(2).broadcast_to([128, NB, NE]), op=ALU.is_le), reads=[pend_b, bv_b], writes=[cmp_b])
        S.op("dve", lambda e: e.reduce_sum(out=blk_f, in_=cmp_, axis=AX.X), reads=[cmp_b], writes=[blk_fb])
        S.op("dve", lambda e: e.tensor_scalar(out=blk_f, in0=blk_f, scalar1=float(NE - 1), scalar2=None, op0=ALU.min), reads=[blk_fb], writes=[blk_fb])
        S.op("dve", lambda e: e.memset(chg_f[:, 0:1], 1.0), writes=[chg_fb])
        S.op("dve", lambda e: e.tensor_tensor(out=chg_f[:, 1:NB], in0=blk_f[:, 1:NB], in1=blk_f[:, 0:NB - 1], op=ALU.not_equal), reads=[blk_fb], writes=[chg_fb])
        S.op("dve", lambda e: e.tensor_copy(out=blk_i, in_=blk_f), reads=[blk_fb], writes=[blk_ib])
        S.op("dve", lambda e: e.tensor_copy(out=chg_i, in_=chg_f), reads=[chg_fb], writes=[chg_ib])
        pio_i, pio_ib = ar.alloc([1], I32, "pio_i")
        pio, pio_b = ar.alloc([1], F32, "pio")
        S.op("pool", lambda e: e.iota(pio_i, pattern=[[0, 1]], base=0, channel_multiplier=1), writes=[pio_ib])
        S.op("dve", lambda e: e.tensor_copy(out=pio, in_=pio_i), reads=[pio_ib], writes=[pio_b])
        nchg, nchg_b = ar.alloc([NB], F32, "nchg")
        S.op("dve", lambda e: e.tensor_scalar(out=nchg, in0=chg_f, scalar1=-1.0e7, scalar2=1.0e7, op0=ALU.mult, op1=ALU.add), reads=[chg_fb], writes=[nchg_b])
        wb_f, wb_fb = ar.alloc([NB], F32, "wb_f")
        S.op("dve", lambda e: e.tensor_scalar(out=wb_f, in0=blk_f, scalar1=128.0, scalar2=pio[:, 0:1], op0=ALU.mult, op1=ALU.add), reads=[blk_fb, pio_b], writes=[wb_fb])
        S.op("dve", lambda e: e.tensor_tensor(out=wb_f, in0=wb_f, in1=chg_f, op=ALU.mult), reads=[wb_fb, chg_fb], writes=[wb_fb])
        S.op("dve", lambda e: e.tensor_tensor(out=wb_f, in0=wb_f, in1=nchg, op=ALU.add), reads=[wb_fb, nchg_b], writes=[wb_fb])
        S.op("dve", lambda e: e.tensor_copy(out=widx, in_=wb_f), reads=[wb_fb], writes=[widx_b])
        bi_f, bi_fb = ar.alloc([NB], F32, "bi_f")
        S.op("dve", lambda e: e.tensor_tensor(out=bi_f, in0=blk_f, in1=chg_f, op=ALU.mult), reads=[blk_fb, chg_fb], writes=[bi_fb])
        S.op("dve", lambda e: e.tensor_tensor(out=bi_f, in0=bi_f, in1=nchg, op=ALU.add), reads=[bi_fb, nchg_b], writes=[bi_fb])
        S.op("dve", lambda e: e.tensor_copy(out=bidx, in_=bi_f), reads=[bi_fb], writes=[bidx_b])
        if dbg:
            for nm, t_, tb_, n, dtp in (("dbg_dest", dest_i.rearrange("p a b -> p (a b)"), dest_ib, NT * 4, I32),
                                        ("dbg_gate4", gate4.rearrange("p a b -> p (a b)"), gate4_b, NT * 4, F32),
                                        ("dbg_blk", blk_i, blk_ib, NB, I32), ("dbg_chg", chg_i, chg_ib, NB, I32)):
                o, ob = self.scratch(nm, [128, n], dtp, dump=True)
                S.op("sp", lambda e, o=o, t_=t_: e.dma_start(out=o, in_=t_), reads=[tb_], writes=[ob], dma=True)
        S.barrier()
        ar.release()
        ar.mark()
        zt, zt_b = ar.alloc([4, D], BF16, "zt")
        S.op("dve", lambda e: e.memset(zt, 0.0), writes=[zt_b])
        xsv = self.xs.rearrange("(b p) n -> p b n", p=128)
        for q in range(NSLOT // 512):
            S.op("sp", lambda e, q=q: e.dma_start(out=xsv[:, 4 * q:4 * q + 4, :], in_=zt), reads=[zt_b], writes=[self.xs_b], dma=True)
        uft = [ar.alloc([D], BF16, f"sc_u{i}") for i in range(2)]
        for i in range(NT):
            u_t, u_b = uft[i % 2]
            S.op("sp", lambda e, u_t=u_t, i=i: e.dma_start(out=u_t, in_=self.uf_s[i * 128:(i + 1) * 128, :]), reads=[self.uf_sb], writes=[u_b], dma=True)
            for k in range(4):
                S.op("pool", lambda e, u_t=u_t, i=i, k=k: e.indirect_dma_start(
                    out=self.xs, out_offset=bass.IndirectOffsetOnAxis(ap=dest_i[:, i, k:k + 1].bitcast(U32), axis=0), in_=u_t, in_offset=None),
                    reads=[u_b, dest_ib], writes=[self.xs_b], dma=True)
        S.barrier()
        ar.release()
        ar.mark()
        Wt = [ar.alloc([8, D], BF16, f"moe_w{m}") for m in range(3)]
        brow, brow_b = ar.alloc([3, D], BF16, "moe_brow")
        WS = [ar.alloc([8, D], F32, f"moe_ws{m}") for m in range(3)]
        browS, browS_b = ar.alloc([3, D], BF16, "moe_browS")
        wds = [wg_d, wu_d, wd_d]
        bds = [bg_d, bu_d, bd_d]
        xb = [ar.alloc([D], BF16, f"moe_xb{i}") for i in range(2)]
        xT = [ar.alloc([8, 128], BF16, f"moe_xT{i}") for i in range(2)]
        gc = [ar.alloc([512], F32, f"moe_gc{i}") for i in range(2)]
        uc = [ar.alloc([512], F32, f"moe_uc{i}") for i in range(2)]
        sgm = [ar.alloc([512], F32, f"moe_sg{i}") for i in range(2)]
        hb_ = [ar.alloc([D], BF16, f"moe_h{i}") for i in range(2)]
        hT = [ar.alloc([8, 128], BF16, f"moe_hT{i}") for i in range(2)]
        yo = [ar.alloc([D], BF16, f"moe_yo{i}") for i in range(2)]
        NBLK = int(os.environ.get("MOE_NB", str(NB)))
        regs = {}

        def breg(e, val):
            if val not in regs:
                regs[val] = e.to_reg(val)
            return regs[val]
        for b in range(NBLK):
            for m in range(3):
                S.op("pool", lambda e, m=m, b=b: e.indirect_dma_start(
                    out=WS[m][0].rearrange("p a b -> p (a b)"), out_offset=None, in_=wds[m],
                    in_offset=bass.IndirectOffsetOnAxis(ap=widx[:, b:b + 1].bitcast(U32), axis=0),
                    bounds_check=breg(e, NE * 128 - 1), oob_is_err=False),
                    reads=[widx_b], writes=[WS[m][1]], dma=True)
            for m in range(3):
                S.op("pool", lambda e, m=m, b=b: e.indirect_dma_start(
                    out=browS[:, m, :], out_offset=None, in_=bds[m],
                    in_offset=bass.IndirectOffsetOnAxis(ap=bidx[:, b:b + 1].bitcast(U32), axis=0),
                    bounds_check=breg(e, NE - 1), oob_is_err=False),
                    reads=[bidx_b], writes=[browS_b], dma=True)
            for m in range(3):
                eng = ("dve", "dve", "act")[m]
                if eng == "act":
                    S.op("act", lambda e, m=m: e.activation(out=Wt[m][0], in_=WS[m][0], func=AF.Copy), reads=[WS[m][1]], writes=[Wt[m][1]])
                else:
                    S.op(eng, lambda e, m=m: e.tensor_copy(out=Wt[m][0], in_=WS[m][0]), reads=[WS[m][1]], writes=[Wt[m][1]])
            S.op("act", lambda e: e.activation(out=brow[0:1], in_=browS[0:1], func=AF.Copy), reads=[browS_b], writes=[brow_b])
            subs = [b * NSUB + s_ for s_ in range(NSUB)]

            def stageA(sb):
                x_t, x_b = xb[sb % 2]
                S.op("sp", lambda e, x_t=x_t, sb=sb: e.dma_start(out=x_t, in_=self.xs[sb * 128:(sb + 1) * 128, :]), reads=[self.xs_b], writes=[x_b], dma=True)
                xT_t, xT_b = xT[sb % 2]
                pT = ps[0][:].bitcast(BF16)

                def trx(e, x_t=x_t, pT=pT):
                    ins = None
                    for kc in range(8):
                        ins = e.transpose(out=pT[:, kc * 128:(kc + 1) * 128], in_=x_t[:, kc * 128:(kc + 1) * 128], identity=ident_bf)
                    return ins
                S.op("pe", trx, reads=[x_b, ident_bf_b], writes=[psb[0]])
                S.op("act", lambda e, xT_t=xT_t, pT=pT: e.activation(out=xT_t.rearrange("p a b -> p (a b)"), in_=pT, func=AF.Copy), reads=[psb[0]], writes=[xT_b])
                h_t, h_b = hb_[sb % 2]
                for half in range(2):
                    hs = slice(half * 512, (half + 1) * 512)
                    for m, bk in ((0, 1 + half), (1, 3 + half)):
                        def mm(e, m=m, bk=bk, hs=hs, xT_t=xT_t):
                            e.matmul(ps[bk][:], lhsT=ones_bf[0:1, 0:128], rhs=brow[0:1, m, hs], start=True, stop=False)
                            ins = None
                            for kc in range(8):
                                ins = e.matmul(ps[bk][:], lhsT=xT_t[:, kc, :], rhs=Wt[m][0][:, kc, hs], start=False, stop=(kc == 7))
                            return ins
                        S.op("pe", mm, reads=[xT_b, Wt[m][1], brow_b, ones_bf_b], writes=[psb[bk]])
                    g_t, g_b = gc[half]
                    u_t, u_b = uc[half]
                    s_t, s_b = sgm[half]
                    S.op("dve", lambda e, g_t=g_t, half=half: e.tensor_scalar(out=g_t, in0=ps[1 + half][:], scalar1=7.0, scalar2=None, op0=ALU.min),
                         reads=[psb[1 + half]], writes=[g_b])
                    S.op("dve", lambda e, u_t=u_t, half=half: e.tensor_scalar(out=u_t, in0=ps[3 + half][:], scalar1=7.0, scalar2=-7.0, op0=ALU.min, op1=ALU.max),
                         reads=[psb[3 + half]], writes=[u_b])
                    S.op("act", lambda e, s_t=s_t, g_t=g_t: e.activation(out=s_t, in_=g_t, func=AF.Sigmoid, scale=1.702), reads=[g_b], writes=[s_b])
                    S.op("dve", lambda e, u_t=u_t, g_t=g_t: e.scalar_tensor_tensor(out=u_t, in0=u_t, scalar=1.0, in1=g_t, op0=ALU.add, op1=ALU.mult),
                         reads=[u_b, g_b], writes=[u_b])
                    S.op("dve", lambda e, h_t=h_t, u_t=u_t, s_t=s_t, hs=hs: e.tensor_tensor(out=h_t[:, hs], in0=u_t, in1=s_t, op=ALU.mult),
                         reads=[u_b, s_b], writes=[h_b])

            def stageB(sb):
                h_t, h_b = hb_[sb % 2]
                hT_t, hT_b = hT[sb % 2]
                pT5 = ps[5][:].bitcast(BF16)

                def trh(e, h_t=h_t, pT5=pT5):
                    ins = None
                    for kc in range(8):
                        ins = e.transpose(out=pT5[:, kc * 128:(kc + 1) * 128], in_=h_t[:, kc * 128:(kc + 1) * 128], identity=ident_bf)
                    return ins
                S.op("pe", trh, reads=[h_b, ident_bf_b], writes=[psb[5]])
                S.op("act", lambda e, hT_t=hT_t, pT5=pT5: e.activation(out=hT_t.rearrange("p a b -> p (a b)"), in_=pT5, func=AF.Copy), reads=[psb[5]], writes=[hT_b])
                y_t, y_b = yo[sb % 2]
                for half in range(2):
                    hs = slice(half * 512, (half + 1) * 512)
                    bk = 6 + half

                    def mmd(e, bk=bk, hs=hs, hT_t=hT_t):
                        e.matmul(ps[bk][:], lhsT=ones_bf[0:1, 0:128], rhs=brow[0:1, 2, hs], start=True, stop=False)
                        ins = None
                        for kc in range(8):
                            ins = e.matmul(ps[bk][:], lhsT=hT_t[:, kc, :], rhs=Wt[2][0][:, kc, hs], start=False, stop=(kc == 7))
                        return ins
                    S.op("pe", mmd, reads=[hT_b, Wt[2][1], brow_b, ones_bf_b], writes=[psb[bk]])
                    if half == 0:
                        S.op("dve", lambda e, y_t=y_t, bk=bk, hs=hs: e.tensor_copy(out=y_t[:, hs], in_=ps[bk][:]), reads=[psb[bk]], writes=[y_b])
                    else:
                        S.op("act", lambda e, y_t=y_t, bk=bk, hs=hs: e.activation(out=y_t[:, hs], in_=ps[bk][:], func=AF.Copy), reads=[psb[bk]], writes=[y_b])
                S.op("sp", lambda e, y_t=y_t, sb=sb: e.dma_start(out=self.ys[sb * 128:(sb + 1) * 128, :], in_=y_t), reads=[y_b], writes=[self.ys_b], dma=True)

            stageA(subs[0])
            for i_ in range(1, NSUB):
                stageA(subs[i_])
                stageB(subs[i_ - 1])
            stageB(subs[-1])
        S.barrier()
        ar.release()
        ar.mark()
        gf_rep, gf_rep_b = ar.alloc([D], F32, "gf_rep")
        fn_rep, fn_rep_b = ar.alloc([D], F32, "fn_rep")
        S.op("sp", lambda e: e.dma_start(out=gf_rep, in_=self.mod_s[:, 5 * D:6 * D]), reads=[self.mod_sb], writes=[gf_rep_b], dma=True)
        S.op("sp", lambda e: e.dma_start(out=fn_rep, in_=fnw.broadcast_to([128, D])), writes=[fn_rep_b], dma=True)
        yk = [[ar.alloc([D], BF16, f"cb_y{j}{k}") for k in range(4)] for j in range(2)]
        hh = [ar.alloc([D], F32, f"cb_h{j}") for j in range(2)]
        acc = [ar.alloc([D], F32, f"cb_a{j}") for j in range(2)]
        oo = [ar.alloc([D], F32, f"cb_o{j}") for j in range(2)]
        sqj, sqj_b = ar.alloc([D], BF16, "cb_sq")
        ssq = [ar.alloc([1], F32, f"cb_ss{j}") for j in range(2)]
        for i in range(NT):
            j = i % 2
            h_t, h_b = hh[j]
            a_t, a_b = acc[j]
            o_t, o_b = oo[j]
            s_t, s_b = ssq[j]
            S.op("sp", lambda e, h_t=h_t, i=i: e.dma_start(out=h_t, in_=self.h1_s[i * 128:(i + 1) * 128, :]), reads=[self.h1_sb], writes=[h_b], dma=True)
            for k in range(4):
                y_t, y_b = yk[j][k]
                S.op("pool", lambda e, y_t=y_t, i=i, k=k: e.indirect_dma_start(
                    out=y_t, out_offset=None, in_=self.ys, in_offset=bass.IndirectOffsetOnAxis(ap=dest_i[:, i, k:k + 1].bitcast(U32), axis=0)),
                    reads=[self.ys_b, dest_ib], writes=[y_b], dma=True)
            S.op("dve", lambda e, a_t=a_t, i=i, j=j: e.tensor_scalar(out=a_t, in0=yk[j][0][0], scalar1=gate4[:, i, 0:1], scalar2=None, op0=ALU.mult),
                 reads=[yk[j][0][1], gate4_b], writes=[a_b])
            for k in range(1, 4):
                S.op("dve", lambda e, a_t=a_t, i=i, j=j, k=k: e.scalar_tensor_tensor(out=a_t, in0=yk[j][k][0], scalar=gate4[:, i, k:k + 1], in1=a_t,
                                                                                 op0=ALU.mult, op1=ALU.add), reads=[yk[j][k][1], gate4_b, a_b], writes=[a_b])
            S.op("dve", lambda e, a_t=a_t: e.tensor_tensor(out=a_t, in0=a_t, in1=gf_rep, op=ALU.mult), reads=[a_b, gf_rep_b], writes=[a_b])
            S.op("dve", lambda e, a_t=a_t, h_t=h_t: e.tensor_tensor(out=a_t, in0=a_t, in1=h_t, op=ALU.add), reads=[a_b, h_b], writes=[a_b])
            S.op("act", lambda e, a_t=a_t, s_t=s_t: e.activation(out=sqj, in_=a_t, func=AF.Square, accum_out=s_t), reads=[a_b], writes=[sqj_b, s_b])
            S.op("act", lambda e, s_t=s_t: e.activation(out=s_t, in_=s_t, func=AF.Sqrt, scale=1.0 / D, bias=EPS), reads=[s_b], writes=[s_b])
            S.op("dve", lambda e, s_t=s_t: e.reciprocal(out=s_t, in_=s_t), reads=[s_b], writes=[s_b])
            S.op("dve", lambda e, o_t=o_t, a_t=a_t, s_t=s_t: e.scalar_tensor_tensor(out=o_t, in0=a_t, scalar=s_t[:, 0:1], in1=fn_rep, op0=ALU.mult, op1=ALU.mult),
                 reads=[a_b, s_b, fn_rep_b], writes=[o_b])
            S.op("sp", lambda e, o_t=o_t, i=i: e.dma_start(out=self.out[i * 128:(i + 1) * 128, :], in_=o_t), reads=[o_b], dma=True)
        S.barrier()
        ar.release()
        ar.release()

def to_cm(a):
    return a.reshape(64, 64, *a.shape[1:]).swapaxes(0, 1).reshape(a.shape)


def from_cm(a):
    return a.reshape(64, 64, *a.shape[1:]).swapaxes(0, 1).reshape(a.shape)


def make_in_maps(inputs, cores):
    x = np.asarray(inputs["x"], np.float32)
    c = np.asarray(inputs["c"], np.float32)
    ctx = np.asarray(inputs["ctx"], np.float32)
    c_ctx = np.asarray(inputs["c_ctx"], np.float32)
    shared = {
        "cctxT": np.ascontiguousarray(c_ctx.reshape(8, 128).T),
        "ada_w": np.ascontiguousarray(inputs["ada_w"][0]),
        "ada_b": np.ascontiguousarray(inputs["ada_b"][0].reshape(1, -1)),
        "norm_mix_w": np.ascontiguousarray(np.asarray(inputs["norm_mix_w"][0]).reshape(8, 128).T),
        "w_in": np.ascontiguousarray(inputs["w_in"][0]),
        "convw": np.ascontiguousarray(np.transpose(np.asarray(inputs["ssd_conv_w"][0]).reshape(5, 32, 128), (2, 1, 0))),
        "convb": np.ascontiguousarray(np.asarray(inputs["ssd_conv_b"][0]).reshape(32, 128).T),
        "dtbias": np.ascontiguousarray(np.asarray(inputs["ssd_dt_bias"][0]).reshape(1, 64)),
        "alog": np.ascontiguousarray(np.asarray(inputs["ssd_a_log"][0]).reshape(1, 64)),
        "ssd_d": np.ascontiguousarray(np.repeat(np.asarray(inputs["ssd_d"][0]), 64).reshape(16, 128).T),
        "ssd_nw": np.ascontiguousarray(np.asarray(inputs["ssd_norm_w"][0]).reshape(16, 128).T),
    }
    lam_re = np.asarray(inputs["s5_lam_re"][0]); lam_im = np.asarray(inputs["s5_lam_im"][0]); ldt = np.asarray(inputs["s5_log_dt"][0])
    b_re = np.asarray(inputs["s5_b_re"][0]); b_im = np.asarray(inputs["s5_b_im"][0])
    c_re = np.asarray(inputs["s5_c_re"][0]); c_im = np.asarray(inputs["s5_c_im"][0])
    s5F = np.zeros((2, 128, 5, 8, 64), np.float32)
    s5S = np.zeros((2, 128, 3 * 32 + 4 * 512), np.float32)
    for d in range(2):
        def F_gp(a):
            t = a.reshape(8, 8, 64).transpose(1, 0, 2)
            return np.repeat(t[:, None], 16, axis=1).reshape(128, 8, 64)
        s5F[d, :, 0] = F_gp(lam_re[d]); s5F[d, :, 1] = F_gp(lam_im[d])
        s5F[d, :, 2] = F_gp(np.repeat(ldt[d][:, None], 64, axis=1))
        s5F[d, :, 3] = b_re[d].reshape(8, 8, 64, 16).transpose(1, 3, 0, 2).reshape(128, 8, 64)
        s5F[d, :, 4] = b_im[d].reshape(8, 8, 64, 16).transpose(1, 3, 0, 2).reshape(128, 8, 64)
        def S_gp(a):
            return a.reshape(32, 2, 64).transpose(1, 2, 0).reshape(128, 32)
        s5S[d, :, 0:32] = S_gp(lam_re[d]); s5S[d, :, 32:64] = S_gp(lam_im[d])
        s5S[d, :, 64:96] = S_gp(np.repeat(ldt[d][:, None], 64, axis=1))
        s5S[d, :, 96:96 + 512] = b_re[d].reshape(32, 2, 64, 16).transpose(1, 2, 0, 3).reshape(128, 512)
        s5S[d, :, 96 + 512:96 + 1024] = b_im[d].reshape(32, 2, 64, 16).transpose(1, 2, 0, 3).reshape(128, 512)
        s5S[d, :, 96 + 1024:96 + 1536] = c_re[d].reshape(32, 2, 16, 64).transpose(1, 3, 0, 2).reshape(128, 512)
        s5S[d, :, 96 + 1536:96 + 2048] = c_im[d].reshape(32, 2, 16, 64).transpose(1, 3, 0, 2).reshape(128, 512)
    shared["glu_w"] = np.ascontiguousarray(inputs["s5_glu_w"][0])
    shared["glu_b"] = np.ascontiguousarray(np.asarray(inputs["s5_glu_b"][0]).reshape(8, 128).T)
    shared["w_a"] = np.ascontiguousarray(inputs["w_branch_a"][0])
    shared["w_b"] = np.ascontiguousarray(inputs["w_branch_b"][0])
    shared["w_o"] = np.ascontiguousarray(inputs["w_out"][0])
    shared["nfw"] = np.ascontiguousarray(np.asarray(inputs["norm_ffn_w"][0]).reshape(1, -1))
    shared["r_w"] = np.ascontiguousarray(inputs["router_w"][0])
    shared["r_b"] = np.ascontiguousarray(np.asarray(inputs["router_b"][0]).reshape(1, -1))
    def wperm(w):
        return np.ascontiguousarray(np.asarray(w).reshape(32, 8, 128, D).transpose(0, 2, 1, 3)).reshape(4096, 8192)
    shared["moe_wg"] = wperm(inputs["moe_w_gate"][0])
    shared["moe_wu"] = wperm(inputs["moe_w_up"][0])
    shared["moe_wd"] = wperm(inputs["moe_w_down"][0])
    shared["moe_bg"] = np.ascontiguousarray(inputs["moe_b_gate"][0])
    shared["moe_bu"] = np.ascontiguousarray(inputs["moe_b_up"][0])
    shared["moe_bd"] = np.ascontiguousarray(inputs["moe_b_down"][0])
    shared["fnw"] = np.ascontiguousarray(np.asarray(inputs["final_norm_w"]).reshape(1, -1))
    shared["s5F"] = s5F
    shared["s5S"] = s5S
    shared["s5_d"] = np.ascontiguousarray(np.asarray(inputs["s5_d"][0]).reshape(8, 128).T)
    maps = []
    for b in cores:
        m = dict(shared)
        m["xT_rm"] = np.ascontiguousarray(x[b].T)
        m["xT_cm"] = np.ascontiguousarray(to_cm(x[b]).T)
        m["ctxT"] = np.ascontiguousarray(ctx[b].T)
        m["cT"] = np.ascontiguousarray(c[b].reshape(8, 128).T)
        m["x_tok"] = np.ascontiguousarray(to_cm(x[b]))
        maps.append(m)
    return maps


_NC_CACHE = {}


def kernel(**inputs):
    if "nc" not in _NC_CACHE:
        _NC_CACHE["nc"] = K().build()
    nc = _NC_CACHE["nc"]
    maps = make_in_maps(inputs, list(range(8)))
    res = run_bass_kernel_spmd(nc, maps, core_ids=list(range(8)))
    outs = [from_cm(np.asarray(r["out"])) for r in res.results]
    return np.stack(outs, 0).astype(np.float32)
```
